# Optimizing a Trainium2 kernel written in Bass

```python
import math
import numpy as np
import jax, jax.numpy as jnp
from jax import lax


D_MODEL = 2048
BATCH = 8
SEQ = 4096
DEPTH = 4

RWKV_HEAD_DIM = 64
RWKV_WIDTH = D_MODEL // 2
RWKV_HEADS = RWKV_WIDTH // RWKV_HEAD_DIM
DIFF_QK_DIM = 64
DIFF_V_DIM = 2 * DIFF_QK_DIM
DIFF_WIDTH = D_MODEL - RWKV_WIDTH
DIFF_HEADS = DIFF_WIDTH // DIFF_V_DIM
DECAY_LORA = 64
AAA_LORA = 64
MV_LORA = 32
GATE_LORA = 160
RWKV_COLS = 3 * RWKV_WIDTH + DECAY_LORA + AAA_LORA + GATE_LORA
DIFF_COLS = 2 * DIFF_HEADS * 2 * DIFF_QK_DIM + DIFF_WIDTH
IN_COLS = RWKV_COLS + DIFF_COLS
D_FF = 5632
CONV_WIDTH = 3
ROPE_THETA = 500000.0
ROPE_DIM = DIFF_QK_DIM // 4
Q_BLOCK = 128
NORM_EPS = 1e-6
GN_EPS = 64e-5
SUBLN_EPS = 1e-5

kernel_name = 'hybrid_rwkv7_diffattn_sandwich'


def rms_norm(x, g, eps=NORM_EPS):
    xf = x.astype(jnp.float32)
    y = xf * lax.rsqrt(jnp.mean(xf * xf, axis=-1, keepdims=True) + eps)
    return (y * g.astype(jnp.float32)).astype(x.dtype)


def token_shift(p, mu):
    prev = jnp.pad(p, ((0, 0), (1, 0), (0, 0)))[:, :-1]
    return p + mu * (prev - p)


def rwkv7_scan(r, decay, k, v, kk, a):
    B, S, H, N = r.shape

    def step(state, inp):
        r_t, w_t, k_t, v_t, kk_t, a_t = inp
        sa = jnp.einsum('bhij,bhj->bhi', state, -kk_t)
        state = (state * w_t[:, :, None, :]
                 + sa[..., None] * (kk_t * a_t)[:, :, None, :]
                 + v_t[..., None] * k_t[:, :, None, :])
        o_t = jnp.einsum('bhij,bhj->bhi', state, r_t)
        return state, o_t

    xs = tuple(jnp.moveaxis(t, 1, 0) for t in (r, decay, k, v, kk, a))
    state0 = jnp.zeros((B, H, N, N), jnp.float32)
    _, out = lax.scan(step, state0, xs)
    return jnp.moveaxis(out, 0, 1)


def rwkv7_group(p_main, p_mv, mu_main, mu_mv, w0, w2, a0, a2, g2, k_k, k_a, r_k,
                gn_w, gn_b, v0, v2, v_first):
    B, S, _ = p_main.shape
    f32 = jnp.float32
    p_main = token_shift(p_main.astype(f32), mu_main)
    sizes = (RWKV_WIDTH, DECAY_LORA, RWKV_WIDTH, RWKV_WIDTH, AAA_LORA, GATE_LORA)
    r, wl, k, v, al, gl = jnp.split(p_main, np.cumsum(sizes)[:-1].tolist(), axis=-1)
    w_log = -jax.nn.softplus(-(w0 + jnp.tanh(wl) @ w2)) - 0.5
    decay = jnp.exp(-jnp.exp(w_log.astype(f32)))
    a = jax.nn.sigmoid(a0 + al @ a2)
    g = jax.nn.sigmoid(gl) @ g2
    if v_first is None:
        v_first = v
    else:
        vl = token_shift(p_mv.astype(f32), mu_mv)
        v = v + (v_first - v) * jax.nn.sigmoid(v0 + vl @ v2)
    heads = lambda t: t.reshape(B, S, RWKV_HEADS, RWKV_HEAD_DIM).astype(f32)
    kk = heads(k * k_k)
    kk = kk / jnp.maximum(jnp.sqrt(jnp.sum(kk * kk, axis=-1, keepdims=True)), 1e-12)
    k = k * (1.0 + (a - 1.0) * k_a)
    r_h, k_h, v_h = heads(r), heads(k), heads(v)
    o = rwkv7_scan(r_h, heads(decay), k_h, v_h, kk, heads(a))
    mean = jnp.mean(o, axis=-1, keepdims=True)
    var = jnp.mean(jnp.square(o - mean), axis=-1, keepdims=True)
    o = ((o - mean) * lax.rsqrt(var + GN_EPS)).reshape(B, S, RWKV_WIDTH) * gn_w + gn_b
    bonus = jnp.sum(r_h * k_h * r_k, axis=-1, keepdims=True) * v_h
    o = (o + bonus.reshape(B, S, RWKV_WIDTH)) * g
    return o, v_first


def rotary_partial(t, cos, sin):
    half = ROPE_DIM // 2
    c = cos[:, :, None, None, :]
    s = sin[:, :, None, None, :]
    t1 = t[..., :half]
    t2 = t[..., half:ROPE_DIM]
    return jnp.concatenate([t1 * c - t2 * s, t2 * c + t1 * s, t[..., ROPE_DIM:]], axis=-1)


def diff_attention_group(q, k, v, cos, sin, lam_q1, lam_k1, lam_q2, lam_k2, subln_w, lambda_init):
    B, S, _ = q.shape
    f32 = jnp.float32
    q = rotary_partial(q.reshape(B, S, DIFF_HEADS, 2, DIFF_QK_DIM).astype(f32), cos, sin)
    k = rotary_partial(k.reshape(B, S, DIFF_HEADS, 2, DIFF_QK_DIM).astype(f32), cos, sin)
    v = jnp.transpose(v.reshape(B, S, DIFF_HEADS, DIFF_V_DIM).astype(f32), (0, 2, 1, 3))
    lam = (jnp.exp(jnp.sum(lam_q1.astype(f32) * lam_k1.astype(f32)))
           - jnp.exp(jnp.sum(lam_q2.astype(f32) * lam_k2.astype(f32))) + lambda_init)
    q = jnp.transpose(q, (0, 2, 3, 1, 4)) * (DIFF_QK_DIM ** -0.5)
    k = jnp.transpose(k, (0, 2, 3, 1, 4))
    key_pos = jnp.arange(S)

    def block(i):
        start = i * Q_BLOCK
        qb = lax.dynamic_slice_in_dim(q, start, Q_BLOCK, axis=3)
        s = jnp.einsum('bhmqd,bhmkd->bhmqk', qb, k)
        q_pos = start + jnp.arange(Q_BLOCK)
        mask = key_pos[None, :] <= q_pos[:, None]
        p = jax.nn.softmax(jnp.where(mask, s, -jnp.inf), axis=-1)
        attn = p[:, :, 0] - lam * p[:, :, 1]
        return jnp.einsum('bhqk,bhkd->bhqd', attn, v)

    o = lax.map(block, jnp.arange(S // Q_BLOCK))
    o = jnp.transpose(o, (1, 0, 3, 2, 4)).reshape(B, S, DIFF_HEADS, DIFF_V_DIM)
    o = o * lax.rsqrt(jnp.mean(o * o, axis=-1, keepdims=True) + SUBLN_EPS) * subln_w
    o = o * (1.0 - lambda_init)
    return o.reshape(B, S, DIFF_WIDTH)


def conv_glu_ffn(h, w_up, conv_w, conv_b, w_down):
    S = h.shape[1]
    gate, up = jnp.split(h @ w_up, 2, axis=-1)
    gp = jnp.pad(gate, ((0, 0), (CONV_WIDTH - 1, 0), (0, 0)))
    gate = sum(gp[:, j:j + S] * conv_w[j] for j in range(CONV_WIDTH)) + conv_b
    return (jax.nn.gelu(gate, approximate=True) * up) @ w_down


def setup_inputs(seed: int = 0) -> dict:
    key = jax.random.key(seed)
    keys = jax.random.split(key, 40)
    counter = [0]

    def nxt():
        counter[0] += 1
        return keys[counter[0] - 1]

    f32 = jnp.float32
    nrm = lambda shape, scale: jax.random.normal(nxt(), shape, f32) * scale
    unif = lambda shape, lo, hi: jax.random.uniform(nxt(), shape, f32, lo, hi)
    L, Lv = DEPTH, DEPTH - 1
    x = nrm((BATCH, SEQ, D_MODEL), 1.0)
    positions = (jax.random.randint(nxt(), (BATCH, 1), 0, 4096, jnp.int32)
                 + jnp.arange(SEQ, dtype=jnp.int32)[None, :])
    gain = lambda: 1.0 + nrm((L, D_MODEL), 0.05)
    conv_center = jnp.zeros((CONV_WIDTH,), f32).at[-1].set(1.0)
    return {
        'x': x,
        'positions': positions,
        'pre_mix_norm': gain(),
        'post_mix_norm': gain(),
        'pre_ffn_norm': gain(),
        'post_ffn_norm': gain(),
        'w_in': nrm((L, D_MODEL, IN_COLS), D_MODEL ** -0.5),
        'w_mv_down': nrm((Lv, D_MODEL, MV_LORA), D_MODEL ** -0.5),
        'shift_mu': unif((L, RWKV_COLS), 0.0, 1.0),
        'shift_mu_mv': unif((Lv, MV_LORA), 0.0, 1.0),
        'w0': unif((L, RWKV_WIDTH), -6.0, 1.0),
        'w2': nrm((L, DECAY_LORA, RWKV_WIDTH), 0.5 * DECAY_LORA ** -0.5),
        'a0': nrm((L, RWKV_WIDTH), 0.1),
        'a2': nrm((L, AAA_LORA, RWKV_WIDTH), AAA_LORA ** -0.5),
        'g2': nrm((L, GATE_LORA, RWKV_WIDTH), GATE_LORA ** -0.5),
        'k_k': 0.85 + nrm((L, RWKV_WIDTH), 0.05),
        'k_a': 1.0 + nrm((L, RWKV_WIDTH), 0.05),
        'r_k': nrm((L, RWKV_HEADS, RWKV_HEAD_DIM), 0.1),
        'gn_w': 1.0 + nrm((L, RWKV_WIDTH), 0.05),
        'gn_b': nrm((L, RWKV_WIDTH), 0.02),
        'v0': 1.0 + nrm((Lv, RWKV_WIDTH), 0.1),
        'v2': nrm((Lv, MV_LORA, RWKV_WIDTH), MV_LORA ** -0.5),
        'lam_q1': nrm((L, DIFF_QK_DIM), 0.1),
        'lam_k1': nrm((L, DIFF_QK_DIM), 0.1),
        'lam_q2': nrm((L, DIFF_QK_DIM), 0.1),
        'lam_k2': nrm((L, DIFF_QK_DIM), 0.1),
        'subln_w': 1.0 + nrm((L, DIFF_V_DIM), 0.05),
        'w_out': nrm((L, D_MODEL, D_MODEL), D_MODEL ** -0.5),
        'w_up': nrm((L, D_MODEL, 2 * D_FF), D_MODEL ** -0.5),
        'conv_w': nrm((L, CONV_WIDTH, D_FF), 0.2) + conv_center[None, :, None],
        'conv_b': nrm((L, D_FF), 0.02),
        'w_down': nrm((L, D_FF, D_MODEL), D_FF ** -0.5),
    }


def reference(x, positions, pre_mix_norm, post_mix_norm, pre_ffn_norm, post_ffn_norm,
              w_in, w_mv_down, shift_mu, shift_mu_mv, w0, w2, a0, a2, g2, k_k, k_a, r_k,
              gn_w, gn_b, v0, v2, lam_q1, lam_k1, lam_q2, lam_k2, subln_w, w_out,
              w_up, conv_w, conv_b, w_down):
    f32 = jnp.float32
    inv_freq = ROPE_THETA ** (-jnp.arange(0, ROPE_DIM, 2, dtype=f32) / ROPE_DIM)
    ang = positions.astype(f32)[..., None] * inv_freq
    cos, sin = jnp.cos(ang), jnp.sin(ang)
    v_first = None
    for l in range(DEPTH):
        h = rms_norm(x, pre_mix_norm[l])
        if l == 0:
            p = h @ w_in[0]
            p_mv, mu_mv, v0_l, v2_l = None, None, None, None
        else:
            p = h @ jnp.concatenate([w_in[l], w_mv_down[l - 1]], axis=-1)
            p_mv, mu_mv, v0_l, v2_l = p[..., IN_COLS:], shift_mu_mv[l - 1], v0[l - 1], v2[l - 1]
        p_rwkv = p[..., :RWKV_COLS]
        q_d, k_d, v_d = jnp.split(p[..., RWKV_COLS:IN_COLS],
                                  [DIFF_HEADS * 2 * DIFF_QK_DIM, 2 * DIFF_HEADS * 2 * DIFF_QK_DIM], axis=-1)
        o_rwkv, v_first = rwkv7_group(p_rwkv, p_mv, shift_mu[l], mu_mv, w0[l], w2[l], a0[l], a2[l],
                                      g2[l], k_k[l], k_a[l], r_k[l], gn_w[l], gn_b[l], v0_l, v2_l, v_first)
        lambda_init = 0.8 - 0.6 * math.exp(-0.3 * l)
        o_diff = diff_attention_group(q_d, k_d, v_d, cos, sin, lam_q1[l], lam_k1[l], lam_q2[l],
                                      lam_k2[l], subln_w[l], lambda_init)
        mix = jnp.concatenate([o_rwkv, o_diff], axis=-1).astype(x.dtype) @ w_out[l]
        x = x + rms_norm(mix, post_mix_norm[l])
        h = rms_norm(x, pre_ffn_norm[l])
        ff = conv_glu_ffn(h, w_up[l], conv_w[l], conv_b[l], w_down[l])
        x = x + rms_norm(ff, post_ffn_norm[l])
    return x
```

```python
import math
from contextlib import ExitStack, contextmanager
import numpy as np
import concourse.bass as bass
import concourse.mybir as mybir
from concourse.bass_utils import run_bass_kernel_spmd

F32 = mybir.dt.float32
BF16 = mybir.dt.bfloat16
I32 = mybir.dt.int32
AF = mybir.ActivationFunctionType
ALU = mybir.AluOpType

D = 2048
KC = 16
FF = 5632
FC = 44
RW = 1024
RCOLS = 3360
INC = 6432
C0 = math.exp(-0.5)
NORM_EPS = 1e-6
GN_EPS = 64e-5
SUBLN_EPS = 1e-5

CV = {}
_o = 0
for _n, _w in (("g_pre", 16), ("g_pm", 16), ("g_pf", 16), ("g_ff", 16), ("mu_r", 8), ("mu_k", 8), ("mu_v", 8),
               ("mu_wa", 1), ("mu_g1", 1), ("mu_g2", 1), ("w0", 8), ("a0", 8), ("k_k", 8), ("k_a", 8), ("r_k", 8),
               ("gn_w", 8), ("gn_b", 8), ("v0", 8), ("subln", 1), ("cw0", 44), ("cw1", 44), ("cw2", 44), ("cb", 44),
               ("lq1", 64), ("lk1", 64), ("lq2", 64), ("lk2", 64)):
    CV[_n] = _o
    _o += _w
NCV = _o
DV = {}
_o = 0
for _n, _w in (("om_r", 8), ("om_k", 8), ("om_v", 8), ("om_wa", 1), ("om_g1", 1), ("om_g2", 1), ("omka", 8), ("lam", 1), ("nlam", 1), ("sub2", 1)):
    DV[_n] = _o
    _o += _w
NDV = _o

CN = {"ident": 0, "bones": 128, "ones": 256, "perm": 384, "m2": 512, "msl": 768, "invf": 896, "sign": 897, "dmask": 898, "scanm": 2946, "hm": 3458}
NCNP = 898
NCN = 3458 + 7 * 256


def make_consts():
    c = np.zeros((128, NCN), np.float32)
    p = np.arange(128)
    c[:, 0:128] = np.eye(128)
    c[:, 128:256] = (p[:, None] // 64 == p[None, :] // 64)
    c[:, 256:384] = 1.0
    part = p.copy()
    for b in (0, 64):
        for i in range(8):
            part[b + i] = b + i + 8
            part[b + 8 + i] = b + i
    perm = np.zeros((128, 128), np.float32)
    for m in range(128):
        if part[m] != m:
            perm[part[m], m] = 1.0
    c[:, 384:512] = perm
    c[:, 512:640] = (p[:, None] < p[None, :])
    c[:, 640:768] = (p[:, None] <= p[None, :])
    c[:, 768:896] = (p[None, :] < p[:, None])
    q = np.arange(512)
    for j in range(4):
        c[:, 898 + j * 512:898 + (j + 1) * 512] = ((j * 128 + p)[:, None] <= q[None, :])
    invf = np.zeros(128, np.float64)
    sign = np.zeros(128, np.float32)
    fr = 500000.0 ** (-np.arange(0, 16, 2, dtype=np.float32) / 16)
    for b in (0, 64):
        for i in range(8):
            invf[b + i] = fr[i]
            invf[b + 8 + i] = fr[i]
            sign[b + i] = -1.0
            sign[b + 8 + i] = 1.0
    c[:, 896] = invf.astype(np.float32)
    c[:, 897] = sign
    sm = np.ones(512, np.float32)
    sm[::128] = 0.0
    c[:, 2946:2946 + 512] = sm[None, :]
    for k in range(7):
        t = p[:, None]
        s_ = p[None, :]
        mk = ((t >> (k + 1)) == (s_ >> (k + 1))) & (((t >> k) & 1) == 1) & (((s_ >> k) & 1) == 0)
        c[:, 3458 + k * 256:3458 + k * 256 + 128] = mk
        c[:, 3458 + k * 256 + 128:3458 + (k + 1) * 256] = mk.T
    return c


class Buf:
    __slots__ = ("w", "r")

    def __init__(self):
        self.w = {}
        self.r = {}


class Eng:
    def __init__(self, name, h, sem):
        self.name, self.h, self.sem, self.cnt, self.seen = name, h, sem, 0, {}


class KB:
    def __init__(self, nc, es, n_epochs=1):
        self.nc = nc
        self.E = {}
        self.sems = {}
        self.keyeng = {}
        self.dead = set()
        self.pool_sems = {}
        hs = (("pe", nc.tensor), ("act", nc.scalar), ("dve", nc.vector), ("pool", nc.gpsimd), ("sp", nc.sync))
        for name, h in hs:
            self.pool_sems[name] = [es.enter_context(nc.semaphore("s_%s%d" % (name, i))) for i in range(n_epochs if name != "sp" else 1)]
            e = Eng(name, h, self.pool_sems[name][0])
            e.key = name + "#0"
            e.epoch = 0
            self.E[name] = e
            self.sems[e.key] = (e.sem, 1)
            self.keyeng[e.key] = e
        self.slots = {}
        for q, n in (("sp", 10),):
            sl = []
            for i in range(n):
                key = "d%s%d" % (q, i)
                sem = es.enter_context(nc.semaphore("s_" + key))
                self.sems[key] = (sem, 16)
                sl.append([key, 0])
            self.slots[q] = [sl, 0]

    def new_epoch(self):
        self.barrier()
        for name in ("pe", "act", "dve", "pool"):
            e = self.E[name]
            if e.epoch + 1 >= len(self.pool_sems[name]):
                continue
            self.dead.add(e.key)
            e.epoch += 1
            e.sem = self.pool_sems[name][e.epoch]
            e.cnt = 0
            e.key = "%s#%d" % (name, e.epoch)
            self.sems[e.key] = (e.sem, 1)
            self.keyeng[e.key] = e

    def _need(self, e, key, n, raw):
        if key in self.dead:
            return
        if key == e.key and (not raw) and e.name == "pe":
            return
        if e.seen.get(key, 0) >= n:
            return
        sem, unit = self.sems[key]
        if key in self.keyeng and key != e.key:
            assert n <= self.keyeng[key].cnt, ("pending ticket", key, n)
        e.h.wait_ge(sem, n * unit)
        e.seen[key] = n

    def _deps(self, e, reads, writes):
        for b in reads:
            for k, n in b.w.items():
                self._need(e, k, n, True)
        for b in writes:
            for k, n in b.w.items():
                self._need(e, k, n, False)
            for k, n in b.r.items():
                self._need(e, k, n, False)

    def op(self, eng, fn, reads=(), writes=(), inc=True):
        e = self.E[eng]
        self._deps(e, reads, writes)
        ins = fn(e.h)
        if inc:
            ins.then_inc(e.sem, 1)
            e.cnt += 1
            t = e.cnt
        else:
            t = e.cnt + 1
        for b in writes:
            b.w = {e.key: t}
            b.r = {}
        for b in reads:
            b.r[e.key] = t
        return ins

    def dma(self, q, out, in_, reads=(), writes=()):
        e = self.E[q]
        self._deps(e, reads, writes)
        sl, idx = self.slots[q]
        key, cnt = sl[idx]
        self.slots[q][1] = (idx + 1) % len(sl)
        if cnt > 0:
            self._need(e, key, cnt, True)
        sem, unit = self.sems[key]
        e.h.dma_start(out=out, in_=in_).then_inc(sem, 16)
        sl[idx][1] = cnt + 1
        for b in writes:
            b.w = {key: cnt + 1}
            b.r = {}
        for b in reads:
            b.r[key] = cnt + 1

    def barrier(self):
        for en in ("pe", "act", "dve", "pool", "sp"):
            e = self.E[en]
            for k2 in ("pe", "act", "dve", "pool"):
                o = self.E[k2]
                if k2 != en and o.cnt > 0:
                    self._need(e, o.key, o.cnt, True)
            for q in self.slots:
                for key, cnt in self.slots[q][0]:
                    if cnt > 0:
                        self._need(e, key, cnt, True)

    def finish(self):
        e = self.E["sp"]
        for q in self.slots:
            for key, cnt in self.slots[q][0]:
                if cnt > 0:
                    self._need(e, key, cnt, True)


_UID = [0]


class Ring:
    def __init__(self, es, nc, name, n, shape, dt, psum=False):
        self.t = []
        for i in range(n):
            _UID[0] += 1
            t = es.enter_context((nc.psum_tensor if psum else nc.sbuf_tensor)("rg_%s_%d" % (name, _UID[0]), shape, dt))
            self.t.append((t, Buf()))
        self.i = 0

    def next(self):
        r = self.t[self.i]
        self.i = (self.i + 1) % len(self.t)
        return r


def build(S, NL, dbg=False, parts=('mix', 'ffn')):
    nc = bass.Bass("TRN2", target_bir_lowering=False)
    NTB = S // 512
    NT = S // 128

    def din(name, shape, dt=F32):
        return nc.dram_tensor(name, shape, dt, kind="ExternalInput").ap()

    x_in = din("x", [S, D])
    pos_in = din("pos", [1, S], I32)
    cv_in = din("cv", [128, NL * NCV])
    cn_in = din("cn", [128, NCN])
    w_in = din("w_in", [NL, D, INC])
    w_mv = din("w_mv", [max(NL - 1, 1), D, 32])
    w2_in = din("w2", [NL, 64, RW])
    a2_in = din("a2", [NL, 64, RW])
    g2_in = din("g2", [NL, 160, RW])
    v2_in = din("v2", [max(NL - 1, 1), 32, RW])
    w_out = din("w_out", [NL, D, D])
    w_up = din("w_up", [NL, D, 2 * FF])
    w_down = din("w_down", [NL, FF, D])
    y_out = nc.dram_tensor("y", [S, D], F32, kind="ExternalOutput").ap()

    kindS = "ExternalOutput" if dbg else "Internal"

    def dsc(name, shape, dt):
        return nc.dram_tensor(name, shape, dt, kind=kindS).ap()

    xT = dsc("xT", [D, S], F32)
    yT = dsc("yT", [D, S], F32)
    rT = dsc("rT", [RW, S], BF16)
    kT = dsc("kT", [RW, S], BF16)
    vT = dsc("vT", [RW, S], BF16)
    vfT = dsc("vfT", [RW, S], BF16)
    waT = dsc("waT", [128, S], BF16)
    g1T = dsc("g1T", [128, S], BF16)
    g2T = dsc("g2T", [64, S], BF16)
    qT = dsc("qT", [RW, S], BF16)
    kdT = dsc("kdT", [RW, S], BF16)
    vd = dsc("vd", [S, RW], BF16)
    mixT = dsc("mixT", [D, S], BF16)
    actT = dsc("actT", [FF, S], BF16)
    rotC = dsc("rotC", [128, S], F32)
    rotS = dsc("rotS", [128, S], F32)
    DB = {}

    def db(name, i=0):
        k = (name, i)
        if k not in DB:
            DB[k] = Buf()
        return DB[k]

    with ExitStack() as es0:
        kb = KB(nc, es0, n_epochs=NL + 1)
        op, dma = kb.op, kb.dma

        @contextmanager
        def scope():
            with ExitStack() as e_:
                yield e_
                kb.barrier()

        def sb(es, name, shape, dt=F32):
            _UID[0] += 1
            return es.enter_context(nc.sbuf_tensor("sb_%s_%d" % (name, _UID[0]), shape, dt))

        cn = sb(es0, "cn", [128, NCNP]); cn_b = Buf()
        cv = sb(es0, "cv", [128, NL * NCV]); cv_b = Buf()
        dv = sb(es0, "dv", [128, NL * NDV]); dv_b = Buf()
        identb = sb(es0, "identb", [128, 128], BF16)
        bonesb = sb(es0, "bonesb", [128, 128], BF16)
        onesb = sb(es0, "onesb", [128, 128], BF16)
        permb = sb(es0, "permb", [128, 128], BF16)
        cb_b = Buf()
        PS = [es0.enter_context(nc.psum_tensor("ps%d" % i, [128, 512], F32)) for i in range(7)]
        PSB = [Buf() for _ in range(7)]
        PSH = es0.enter_context(nc.psum_tensor("psh", [128, 1024], BF16)); PSH_b = [Buf()] * 4

        dma("sp", cn[:], cn_in[:, 0:NCNP], writes=[cn_b])
        dma("sp", cv[:], cv_in[:, :], writes=[cv_b])
        for t, o in ((identb, CN["ident"]), (bonesb, CN["bones"]), (onesb, CN["ones"]), (permb, CN["perm"])):
            op("dve", lambda h, t=t, o=o: h.tensor_copy(out=t[:], in_=cn[:, o:o + 128]), reads=[cn_b], writes=[cb_b])

        def cvc(l, name, i=0, n=1, p0=0, p1=128):
            o = l * NCV + CV[name] + i
            return cv[p0:p1, o:o + n]

        def dvc(l, name, i=0, n=1, p0=0, p1=128):
            o = l * NDV + DV[name] + i
            return dv[p0:p1, o:o + n]

        for l in range(NL):
            for a, b_, n in (("om_r", "mu_r", 8), ("om_k", "mu_k", 8), ("om_v", "mu_v", 8), ("om_wa", "mu_wa", 1),
                             ("om_g1", "mu_g1", 1), ("om_g2", "mu_g2", 1), ("omka", "k_a", 8)):
                op("dve", lambda h, l=l, a=a, b_=b_, n=n: h.tensor_scalar(out=dvc(l, a, 0, n), in0=cvc(l, b_, 0, n), scalar1=-1.0, scalar2=1.0,
                                                                        op0=ALU.mult, op1=ALU.add), reads=[cv_b], writes=[dv_b])
        with scope() as es:
            tmp = sb(es, "lamtmp", [128, 64]); tb_ = Buf()
            acc = sb(es, "lamacc", [128, 4]); ab_ = Buf()
            for l in range(NL):
                li = 0.8 - 0.6 * math.exp(-0.3 * l)
                for j, (qa, ka) in enumerate((("lq1", "lk1"), ("lq2", "lk2"))):
                    op("dve", lambda h, l=l, qa=qa, ka=ka: h.tensor_tensor(out=tmp[:], in0=cvc(l, qa, 0, 64), in1=cvc(l, ka, 0, 64), op=ALU.mult),
                       reads=[cv_b], writes=[tb_])
                    op("dve", lambda h, j=j: h.reduce_sum(out=acc[:, j:j + 1], in_=tmp[:], axis=mybir.AxisListType.X), reads=[tb_], writes=[ab_])
                op("act", lambda h: h.activation(out=acc[:, 2:4], in_=acc[:, 0:2], func=AF.Exp), reads=[ab_], writes=[ab_])
                op("dve", lambda h, l=l: h.tensor_tensor(out=dvc(l, "lam"), in0=acc[:, 2:3], in1=acc[:, 3:4], op=ALU.subtract), reads=[ab_, dv_b], writes=[dv_b])
                op("dve", lambda h, l=l, li=li: h.tensor_scalar(out=dvc(l, "nlam"), in0=dvc(l, "lam"), scalar1=float(li), scalar2=-1.0, op0=ALU.add, op1=ALU.mult),
                   reads=[dv_b], writes=[dv_b])
                op("dve", lambda h, l=l, li=li: h.tensor_scalar(out=dvc(l, "sub2"), in0=cvc(l, "subln"), scalar1=float(1.0 - li), scalar2=None, op0=ALU.mult),
                   reads=[cv_b, dv_b], writes=[dv_b])

        with scope() as es:
          if 'norot' not in parts:
              pi_ = sb(es, "posi", [128, 512], I32); pib = Buf()
              pf = sb(es, "posf", [128, 512]); pfb = Buf()
              t1 = sb(es, "rt1", [128, 512]); t1b = Buf()
              t2 = sb(es, "rt2", [128, 512]); t2b = Buf()
              ki = sb(es, "rki", [128, 512], I32); kib = Buf()
              ro = Ring(es, nc, "rto", 2, [128, 512], F32)
              TWO_PI = float(2 * np.pi)
              for tb in range(NTB):
                  ts = slice(tb * 512, (tb + 1) * 512)
                  dma("sp", pi_[:], pos_in[0:1, ts].partition_broadcast(128), writes=[pib])
                  op("dve", lambda h: h.tensor_copy(out=pf[:], in_=pi_[:]), reads=[pib], writes=[pfb])
                  op("dve", lambda h: h.tensor_scalar(out=pf[:], in0=pf[:], scalar1=cn[:, CN["invf"]:CN["invf"] + 1], scalar2=None, op0=ALU.mult),
                     reads=[pfb, cn_b], writes=[pfb])
                  for which, off, dst in (("c", float(np.pi / 2), rotC), ("s", 0.0, rotS)):
                      op("dve", lambda h, off=off: h.tensor_scalar(out=t1[:], in0=pf[:], scalar1=off, scalar2=None, op0=ALU.add), reads=[pfb], writes=[t1b])
                      op("dve", lambda h: h.tensor_scalar(out=t2[:], in0=t1[:], scalar1=float(1 / (2 * np.pi)), scalar2=None, op0=ALU.mult), reads=[t1b], writes=[t2b])
                      op("dve", lambda h: h.tensor_copy(out=ki[:], in_=t2[:]), reads=[t2b], writes=[kib])
                      op("dve", lambda h: h.tensor_copy(out=t2[:], in_=ki[:]), reads=[kib], writes=[t2b])
                      op("dve", lambda h: h.scalar_tensor_tensor(out=t1[:], in0=t2[:], scalar=-TWO_PI, in1=t1[:], op0=ALU.mult, op1=ALU.add), reads=[t2b, t1b], writes=[t1b])
                      op("dve", lambda h: h.tensor_scalar(out=t2[:], in0=t1[:], scalar1=float(np.pi), scalar2=-TWO_PI, op0=ALU.is_gt, op1=ALU.mult), reads=[t1b], writes=[t2b])
                      op("dve", lambda h: h.tensor_tensor(out=t1[:], in0=t1[:], in1=t2[:], op=ALU.add), reads=[t1b, t2b], writes=[t1b])
                      op("dve", lambda h: h.tensor_scalar(out=t2[:], in0=t1[:], scalar1=float(-np.pi), scalar2=TWO_PI, op0=ALU.is_lt, op1=ALU.mult), reads=[t1b], writes=[t2b])
                      op("dve", lambda h: h.tensor_tensor(out=t1[:], in0=t1[:], in1=t2[:], op=ALU.add), reads=[t1b, t2b], writes=[t1b])
                      o_, ob_ = ro.next()
                      op("act", lambda h, o_=o_: h.activation(out=o_[:], in_=t1[:], func=AF.Sin), reads=[t1b], writes=[ob_])
                      if which == "s":
                          op("dve", lambda h, o_=o_: h.tensor_scalar(out=o_[:], in0=o_[:], scalar1=cn[:, CN["sign"]:CN["sign"] + 1], scalar2=None, op0=ALU.mult),
                             reads=[ob_, cn_b], writes=[ob_])
                      dma("sp", dst[:, ts], o_[:], reads=[ob_], writes=[db("rot")])

        with scope() as es:
            xr = Ring(es, nc, "xin", 2, [128, D], F32)
            xo = Ring(es, nc, "xto", 2, [128, KC, 128], F32)
            for tt in range(NT):
                xi, xib = xr.next()
                dma("sp", xi[:], x_in[tt * 128:(tt + 1) * 128, :], writes=[xib])
                o_, ob_ = xo.next()
                for kc in range(KC):
                    pi = kc % 4
                    op("pe", lambda h, kc=kc, pi=pi: h.transpose(out=PS[pi][:, 0:128], in_=xi[:, kc * 128:(kc + 1) * 128], identity=cn[:, 0:128]),
                       reads=[xib, cn_b], writes=[PSB[pi]])
                    op("act" if kc % 2 else "dve", lambda h, kc=kc, pi=pi: (h.copy if kc % 2 else h.tensor_copy)(out=o_[:, kc, :], in_=PS[pi][:, 0:128]),
                       reads=[PSB[pi]], writes=[ob_])
                dma("sp", xT[:, tt * 128:(tt + 1) * 128].rearrange("(kc p) t -> p kc t", p=128), o_[:], reads=[ob_], writes=[db("xT", kc) for kc in range(KC)])

        def norm_to_hT(es, l, gname, hT, hTb):
            xs = Ring(es, nc, "nxs", 16, [128, 512], F32)
            sq = Ring(es, nc, "nsq", 2, [128, 512], BF16)
            rs = sb(es, "nrs", [128, 512]); rsb = Buf()
            for tb in range(NTB):
                ts = slice(tb * 512, (tb + 1) * 512)
                tl = []
                for kc in range(KC):
                    x_, xb_ = xs.next()
                    dma("sp", x_[:], xT[kc * 128:(kc + 1) * 128, ts], reads=[db("xT", kc)], writes=[xb_])
                    s_, sb_ = sq.next()
                    op("act", lambda h, x_=x_, s_=s_: h.activation(out=s_[:], in_=x_[:], func=AF.Square), reads=[xb_], writes=[sb_])
                    op("pe", lambda h, s_=s_, kc=kc: h.matmul(PS[6][:], lhsT=onesb[:], rhs=s_[:], start=(kc == 0), stop=(kc == KC - 1)),
                       reads=[sb_, cb_b], writes=[PSB[6]])
                    tl.append((x_, xb_))
                op("dve", lambda h: h.tensor_scalar(out=rs[:], in0=PS[6][:], scalar1=1.0 / D, scalar2=NORM_EPS, op0=ALU.mult, op1=ALU.add), reads=[PSB[6]], writes=[rsb])
                op("act", lambda h: h.activation(out=rs[:], in_=rs[:], func=AF.Sqrt), reads=[rsb], writes=[rsb])
                op("dve", lambda h: h.reciprocal(out=rs[:], in_=rs[:]), reads=[rsb], writes=[rsb])
                for kc in range(KC):
                    x_, xb_ = tl[kc]
                    op("dve", lambda h, x_=x_, kc=kc: h.scalar_tensor_tensor(out=hT[:, kc, ts], in0=x_[:], scalar=cvc(l, gname, kc), in1=rs[:],
                                                                                                   op0=ALU.mult, op1=ALU.mult),
                       reads=[xb_, rsb, cv_b], writes=[hTb[tb]])

        def proj_fm(es, hT, hTb, tasks, nper=1):
            st = Ring(es, nc, "wst", 2 if nper == 1 else 3, [128, KC, 128], F32)
            wb = Ring(es, nc, "wbf", 2 if nper == 1 else 4, [128, KC, 128], BF16)
            psr = [0]
            for ti in range(0, len(tasks), nper):
                grp = tasks[ti:ti + nper]
                wts = []
                for segs, M, epi in grp:
                    s_, sb_ = st.next()
                    o = 0
                    for ap, n in segs:
                        dma("sp", s_[:, :, o:o + n], ap.rearrange("(kc p) n -> p kc n", p=128), writes=[sb_])
                        o += n
                    w_, wb_ = wb.next()
                    op("pool", lambda h, s_=s_, w_=w_, M=M: h.tensor_copy(out=w_[:, :, 0:M], in_=s_[:, :, 0:M]), reads=[sb_], writes=[wb_])
                    wts.append((w_, wb_, M))
                for tb in range(NTB):
                    ts = slice(tb * 512, (tb + 1) * 512)
                    pss = []
                    for w_, wb_, M in wts:
                        pi = psr[0] % 4
                        psr[0] += 1
                        for kc in range(KC):
                            op("pe", lambda h, w_=w_, M=M, kc=kc, pi=pi: h.matmul(PS[pi][0:M, :], lhsT=w_[:, kc, 0:M], rhs=hT[:, kc, ts], start=(kc == 0), stop=(kc == KC - 1)),
                               reads=[wb_, hTb[tb]], writes=[PSB[pi]], inc=(kc == KC - 1))
                        pss.append(pi)
                    grp[0][2](tb, pss)

        def mk_rings(es):
            return {"t": Ring(es, nc, "ept", 2, [128, 512], F32), "of": Ring(es, nc, "epof", 2, [128, 512], F32),
                    "ob": Ring(es, nc, "epob", 3, [128, 512], BF16), "car": sb(es, "epcar", [128, 64]), "ncar": [0]}

        def shift_epi(R, name, M, mu_ap, om_ap, dests, act=None):
            tr = R["t"]
            orr = R["of"] if act else R["ob"]
            obr = R["ob"] if act else None
            ci = R["ncar"][0]
            R["ncar"][0] += 1
            carry = R["car"][:, ci:ci + 1]; cb = Buf()

            def epi(tb, pss):
                pi = pss[0]
                ts = slice(tb * 512, (tb + 1) * 512)
                t_, tb_ = tr.next()
                o_, ob_ = orr.next()
                op("act", lambda h: h.activation(out=t_[0:M, :], in_=PS[pi][0:M, :], func=AF.Identity, scale=om_ap), reads=[PSB[pi], dv_b], writes=[tb_])
                op("dve", lambda h: h.scalar_tensor_tensor(out=o_[0:M, 1:512], in0=PS[pi][0:M, 0:511], scalar=mu_ap, in1=t_[0:M, 1:512], op0=ALU.mult, op1=ALU.add),
                   reads=[PSB[pi], tb_, cv_b], writes=[ob_])
                if tb == 0:
                    op("dve", lambda h: h.tensor_copy(out=o_[0:M, 0:1], in_=t_[0:M, 0:1]), reads=[tb_], writes=[ob_])
                else:
                    op("dve", lambda h: h.scalar_tensor_tensor(out=o_[0:M, 0:1], in0=carry[0:M, :], scalar=mu_ap, in1=t_[0:M, 0:1], op0=ALU.mult, op1=ALU.add),
                       reads=[cb, tb_, cv_b], writes=[ob_])
                op("act", lambda h: h.copy(out=carry[0:M, :], in_=PS[pi][0:M, 511:512]), reads=[PSB[pi]], writes=[cb])
                if act:
                    f_, fb_ = obr.next()
                    for p0, p1, fn in act:
                        if fn is None:
                            op("dve", lambda h, p0=p0, p1=p1: h.tensor_copy(out=f_[p0:p1, :], in_=o_[p0:p1, :]), reads=[ob_], writes=[fb_])
                        else:
                            op("act", lambda h, p0=p0, p1=p1, fn=fn: h.activation(out=f_[p0:p1, :], in_=o_[p0:p1, :], func=fn), reads=[ob_], writes=[fb_])
                    o_, ob_ = f_, fb_
                for dap, dbuf in dests:
                    dma("sp", dap[:, ts], o_[0:M, :], reads=[ob_], writes=[dbuf])
            return epi

        def plain_epi(R, name, dest, dbuf, flip):
            orr = R["ob"]

            def epi(tb, pss):
                pi = pss[0]
                o_, ob_ = orr.next()
                if (tb + flip) % 2:
                    op("act", lambda h: h.copy(out=o_[:], in_=PS[pi][:]), reads=[PSB[pi]], writes=[ob_])
                else:
                    op("dve", lambda h: h.tensor_copy(out=o_[:], in_=PS[pi][:]), reads=[PSB[pi]], writes=[ob_])
                dma("sp", dest[:, tb * 512:(tb + 1) * 512], o_[:], reads=[ob_], writes=[dbuf])
            return epi

        def proj_res(es, l, src, srcname, KCn, W, gname):
            TBD = min(1024, S)
            NH = TBD // 512
            srct = sb(es, "prs", [128, KCn, TBD], BF16); srcb = Buf()
            KH = KCn // 4
            st = Ring(es, nc, "prst", 2, [128, KH, 128], F32)
            wbr = Ring(es, nc, "prwb", 2, [128, KCn, 128], BF16)
            yr = Ring(es, nc, "pry", 2, [128, TBD], F32)
            sqr = Ring(es, nc, "prsq", 2, [128, 512], BF16)
            rst = sb(es, "prrs", [128, TBD]); rsb = Buf()
            xr = Ring(es, nc, "prx", 2, [128, TBD], F32)
            for tbd in range(S // TBD):
                ts = slice(tbd * TBD, (tbd + 1) * TBD)
                for kc in range(KCn):
                    dma("sp", srct[:, kc, :], src[kc * 128:(kc + 1) * 128, ts], reads=[db(srcname, kc)], writes=[srcb])
                for c in range(KC):
                    w_, wb_ = wbr.next()
                    for hf in range(4):
                        s_, sb_ = st.next()
                        dma("sp", s_[:], W[hf * KH * 128:(hf + 1) * KH * 128, c * 128:(c + 1) * 128].rearrange("(kc p) n -> p kc n", p=128), writes=[sb_])
                        op("pool", lambda h, s_=s_, w_=w_, hf=hf: h.tensor_copy(out=w_[:, hf * KH:(hf + 1) * KH, :], in_=s_[:]), reads=[sb_], writes=[wb_])
                    y_, yb_ = yr.next()
                    for hh in range(NH):
                        pi = (c * NH + hh) % 4
                        for kc in range(KCn):
                            op("pe", lambda h, w_=w_, kc=kc, pi=pi, hh=hh: h.matmul(PS[pi][:], lhsT=w_[:, kc, :], rhs=srct[:, kc, hh * 512:(hh + 1) * 512],
                                                                                   start=(kc == 0), stop=(kc == KCn - 1)),
                               reads=[wb_, srcb], writes=[PSB[pi]], inc=(kc == KCn - 1))
                        op("dve", lambda h, y_=y_, pi=pi, hh=hh: h.tensor_copy(out=y_[:, hh * 512:(hh + 1) * 512], in_=PS[pi][:]), reads=[PSB[pi]], writes=[yb_])
                        q_, qb_ = sqr.next()
                        op("act", lambda h, q_=q_, y_=y_, hh=hh: h.activation(out=q_[:], in_=y_[:, hh * 512:(hh + 1) * 512], func=AF.Square), reads=[yb_], writes=[qb_])
                        op("pe", lambda h, q_=q_, hh=hh, c=c: h.matmul(PS[4 + hh][:], lhsT=onesb[:], rhs=q_[:], start=(c == 0), stop=(c == KC - 1)),
                           reads=[qb_, cb_b], writes=[PSB[4 + hh]])
                    dma("sp", yT[c * 128:(c + 1) * 128, ts], y_[:], reads=[yb_], writes=[db("yT", c)])
                for hh in range(NH):
                    hs = slice(hh * 512, (hh + 1) * 512)
                    op("dve", lambda h, hh=hh, hs=hs: h.tensor_scalar(out=rst[:, hs], in0=PS[4 + hh][:], scalar1=1.0 / D, scalar2=NORM_EPS, op0=ALU.mult, op1=ALU.add),
                       reads=[PSB[4 + hh]], writes=[rsb])
                op("act", lambda h: h.activation(out=rst[:], in_=rst[:], func=AF.Sqrt), reads=[rsb], writes=[rsb])
                op("dve", lambda h: h.reciprocal(out=rst[:], in_=rst[:]), reads=[rsb], writes=[rsb])
                for c in range(KC):
                    y_, yb_ = yr.next()
                    x_, xb_ = xr.next()
                    dma("sp", y_[:], yT[c * 128:(c + 1) * 128, ts], reads=[db("yT", c)], writes=[yb_])
                    dma("sp", x_[:], xT[c * 128:(c + 1) * 128, ts], reads=[db("xT", c)], writes=[xb_])
                    op("dve", lambda h, y_=y_, c=c: h.scalar_tensor_tensor(out=y_[:], in0=y_[:], scalar=cvc(l, gname, c), in1=rst[:], op0=ALU.mult, op1=ALU.mult),
                       reads=[yb_, rsb, cv_b], writes=[yb_])
                    op("dve", lambda h, y_=y_, x_=x_: h.tensor_tensor(out=x_[:], in0=x_[:], in1=y_[:], op=ALU.add), reads=[yb_, xb_], writes=[xb_])
                    dma("sp", xT[c * 128:(c + 1) * 128, ts], x_[:], reads=[xb_], writes=[db("xT", c)])


        def attn_phase(es, l):
            C = sb(es, "arC", [128, S]); Cb = Buf()
            Sg = sb(es, "arS", [128, S]); Sgb = Buf()
            dma("sp", C[:], rotC[:, :], reads=[db("rot")], writes=[Cb])
            dma("sp", Sg[:], rotS[:, :], reads=[db("rot")], writes=[Sgb])
            dmf = sb(es, "admf", [128, 2048]); dmfb = Buf()
            dm = sb(es, "adm", [128, 4, 512], BF16); dmb = Buf()
            dma("sp", dmf[:], cn_in[:, CN["dmask"]:CN["dmask"] + 2048], writes=[dmfb])
            op("pool", lambda h: h.tensor_copy(out=dm[:], in_=dmf[:].rearrange("p (a b) -> p a b", a=4)), reads=[dmfb], writes=[dmb])
            raws = Ring(es, nc, "araw", 2, [128, 512], BF16)
            t1r = Ring(es, nc, "at1", 2, [128, 512], F32)
            t2r = Ring(es, nc, "at2", 2, [128, 512], F32)
            qk = Ring(es, nc, "aqk", 4, [128, S], BF16)
            Vr = Ring(es, nc, "aV", 2, [128, NT, 128], BF16)
            Er = Ring(es, nc, "aE", 4, [128, 512], BF16)
            wk = Ring(es, nc, "awk", 6, [128, 512], F32)
            sqr = Ring(es, nc, "asq", 2, [128, 512], BF16)
            outr = Ring(es, nc, "aout", 2, [128, 512], BF16)
            for hd in range(8):
                rot = []
                for src, nm in ((qT, "qT"), (kdT, "kdT")):
                    d_, db_ = qk.next()
                    for tb in range(NTB):
                        ts = slice(tb * 512, (tb + 1) * 512)
                        r_, rb_ = raws.next()
                        dma("sp", r_[:], src[hd * 128:(hd + 1) * 128, ts], reads=[db(nm, hd)], writes=[rb_])
                        op("pe", lambda h, r_=r_: h.matmul(PS[0][:], lhsT=permb[:], rhs=r_[:], start=True, stop=True), reads=[rb_, cb_b], writes=[PSB[0]])
                        a_, ab_ = t1r.next()
                        b_, bb_ = t2r.next()
                        op("pool", lambda h, r_=r_, a_=a_, ts=ts: h.tensor_tensor(out=a_[:], in0=r_[:], in1=C[:, ts], op=ALU.mult), reads=[rb_, Cb], writes=[ab_])
                        op("dve", lambda h, b_=b_, ts=ts: h.tensor_tensor(out=b_[:], in0=PS[0][:], in1=Sg[:, ts], op=ALU.mult), reads=[PSB[0], Sgb], writes=[bb_])
                        op("dve", lambda h, a_=a_, b_=b_, d_=d_, ts=ts: h.tensor_tensor(out=d_[:, ts], in0=a_[:], in1=b_[:], op=ALU.add), reads=[ab_, bb_], writes=[db_])
                    rot.append((d_, db_))
                (q_, qb_), (k_, kb_) = rot
                V_, Vb_ = Vr.next()
                dma("sp", V_[:], vd[:, hd * 128:(hd + 1) * 128].rearrange("(tt p) c -> p tt c", p=128), reads=[db("vd", hd)], writes=[Vb_])
                for qb in range(NTB):
                    qs = slice(qb * 512, (qb + 1) * 512)
                    nk = 4 * qb + 4
                    cnt = 0
                    for kt in range(nk):
                        for m in range(2):
                            pi = cnt % 3
                            cnt += 1
                            op("pe", lambda h, m=m, kt=kt, pi=pi: h.matmul(PS[pi][:], lhsT=k_[m * 64:(m + 1) * 64, kt * 128:(kt + 1) * 128], rhs=q_[m * 64:(m + 1) * 64, qs],
                                                                          start=True, stop=True), reads=[kb_, qb_], writes=[PSB[pi]])
                            e_, eb_ = Er.next()
                            op("act", lambda h, e_=e_, pi=pi: h.activation(out=e_[:], in_=PS[pi][:], func=AF.Exp, scale=0.125), reads=[PSB[pi]], writes=[eb_])
                            if kt >= 4 * qb:
                                op("pool", lambda h, e_=e_, j=kt - 4 * qb: h.tensor_tensor(out=e_[:], in0=e_[:], in1=dm[:, j, :], op=ALU.mult), reads=[eb_, dmb], writes=[eb_])
                            op("pe", lambda h, e_=e_, kt=kt, m=m: h.matmul(PS[3 + m][:], lhsT=V_[:, kt, :], rhs=e_[:], start=(kt == 0), stop=(kt == nk - 1)),
                               reads=[Vb_, eb_], writes=[PSB[3 + m]], inc=False)
                            op("pe", lambda h, e_=e_, kt=kt, m=m: h.matmul(PS[5 + m][:], lhsT=onesb[:], rhs=e_[:], start=(kt == 0), stop=(kt == nk - 1)),
                               reads=[cb_b, eb_], writes=[PSB[5 + m]])
                    w = [wk.next() for _ in range(6)]
                    for m in range(2):
                        op("dve", lambda h, m=m: h.reciprocal(out=w[m][0][:], in_=PS[5 + m][:]), reads=[PSB[5 + m]], writes=[w[m][1]])
                        op("dve", lambda h, m=m: h.tensor_tensor(out=w[2 + m][0][:], in0=PS[3 + m][:], in1=w[m][0][:], op=ALU.mult), reads=[PSB[3 + m], w[m][1]], writes=[w[2 + m][1]])
                    op("dve", lambda h: h.scalar_tensor_tensor(out=w[4][0][:], in0=w[3][0][:], scalar=dvc(l, "nlam"), in1=w[2][0][:], op0=ALU.mult, op1=ALU.add),
                       reads=[w[3][1], w[2][1], dv_b], writes=[w[4][1]])
                    s_, sb_ = sqr.next()
                    op("act", lambda h, s_=s_: h.activation(out=s_[:], in_=w[4][0][:], func=AF.Square), reads=[w[4][1]], writes=[sb_])
                    op("pe", lambda h, s_=s_: h.matmul(PS[0][:], lhsT=onesb[:], rhs=s_[:], start=True, stop=True), reads=[sb_, cb_b], writes=[PSB[0]])
                    op("dve", lambda h: h.tensor_scalar(out=w[5][0][:], in0=PS[0][:], scalar1=1.0 / 128, scalar2=SUBLN_EPS, op0=ALU.mult, op1=ALU.add), reads=[PSB[0]], writes=[w[5][1]])
                    op("act", lambda h: h.activation(out=w[5][0][:], in_=w[5][0][:], func=AF.Sqrt), reads=[w[5][1]], writes=[w[5][1]])
                    op("dve", lambda h: h.reciprocal(out=w[5][0][:], in_=w[5][0][:]), reads=[w[5][1]], writes=[w[5][1]])
                    o_, ob_ = outr.next()
                    op("dve", lambda h, o_=o_: h.scalar_tensor_tensor(out=o_[:], in0=w[4][0][:], scalar=dvc(l, "sub2"), in1=w[5][0][:], op0=ALU.mult, op1=ALU.mult),
                       reads=[w[4][1], w[5][1], dv_b], writes=[ob_])
                    dma("sp", mixT[1024 + hd * 128:1024 + (hd + 1) * 128, qs], o_[:], reads=[ob_], writes=[db("mixT", 8 + hd)])

        def rwkv_phase(es, l):
            f32t = lambda nm, sh=[128, 512]: (sb(es, nm, sh), Buf())
            bft = lambda nm, sh=[128, 512]: (sb(es, nm, sh, BF16), Buf())
            scanm, scb = f32t("scanm")
            dma("sp", scanm[:], cn_in[:, CN["scanm"]:CN["scanm"] + 512], writes=[scb])
            hm, hmb = f32t("hm", [128, 7, 256])
            dma("sp", hm[:], cn_in[:, CN["hm"]:CN["hm"] + 7 * 256].rearrange("p (k c) -> p k c", k=7), writes=[hmb])
            DG = [bft("DG%d" % i, [128, 256]) for i in range(2)]; ZZ, ZZb = bft("ZZ", [128, 256]); tW, tWb = bft("tW", [128, 256])
            m2b, m2bb = bft("m2b", [128, 256]); mslb, mslbb = bft("mslb", [128, 128])
            op("pool", lambda h: h.tensor_copy(out=m2b[:], in_=cn[:, CN["m2"]:CN["m2"] + 256]), reads=[cn_b], writes=[m2bb])
            op("pool", lambda h: h.tensor_copy(out=mslb[:], in_=cn[:, CN["msl"]:CN["msl"] + 128]), reads=[cn_b], writes=[mslbb])
            wst, wstb = f32t("lst", [128, 1024])
            wa2b, wa2bb = bft("wa2b", [128, 1024]); g2ab, g2abb = bft("g2ab", [128, 1024]); g2bv, g2bvb = bft("g2bv", [64, 1024])
            dma("sp", wst[0:64, :], w2_in[l], writes=[wstb]); dma("sp", wst[64:128, :], a2_in[l], writes=[wstb])
            op("pool", lambda h: h.tensor_copy(out=wa2b[:], in_=wst[:]), reads=[wstb], writes=[wa2bb])
            dma("sp", wst[:], g2_in[l][0:128, :], writes=[wstb])
            op("pool", lambda h: h.tensor_copy(out=g2ab[:], in_=wst[:]), reads=[wstb], writes=[g2abb])
            dma("sp", wst[0:32, :], g2_in[l][128:160, :], writes=[wstb])
            if l > 0:
                dma("sp", wst[32:64, :], v2_in[l - 1], writes=[wstb])
            op("pool", lambda h: h.tensor_copy(out=g2bv[0:64 if l > 0 else 32, :], in_=wst[0:64 if l > 0 else 32, :]), reads=[wstb], writes=[g2bvb])
            Hf, Hfb = f32t("Hf", [128, 8, 64]); Hb, Hbb = bft("Hb", [128, 8, 64]); HG, HGb = f32t("HG", [128, 64])
            op("pool", lambda h: h.memset(Hf[:], 0.0), writes=[Hfb]); op("pool", lambda h: h.memset(Hb[:], 0.0), writes=[Hbb])
            wa_t, wab = bft("wa_t"); g1_t, g1b = bft("g1_t"); g2_t, g2b_ = bft("g2_t", [64, 512])
            r_t, rb = bft("r_t"); k_t, kb_ = bft("k_t"); v_t, vb = bft("v_t"); vf_t, vfb = bft("vf_t")
            sg, sgb = f32t("sg"); cs, csb = f32t("cs"); ex, exb = f32t("ex"); eG, eGb = f32t("eG"); eGi, eGib = f32t("eGi"); eGx, eGxb = f32t("eGx")
            a_t, ab = f32t("a_t"); gt, gtb = f32t("gt"); v2t, v2b = f32t("v2t"); kk, kkb = f32t("kk"); sqb_, sqbb = bft("sqb"); rn, rnb = f32t("rn")
            k2, k2b = f32t("k2"); tmp, tmpb = f32t("tmp"); AR, ARb = bft("AR", [128, 2, 512]); BhT, BhTb = bft("BhT"); KhT, KhTb = bft("KhT"); vbf, vbfb = bft("vbf")
            rkb, rkbb = bft("rkb"); bon, bonb = f32t("bon"); OT, OTb = f32t("OT"); ob16, ob16b = bft("ob16"); mean, meanb = f32t("mean"); var, varb = f32t("var")
            Bh = [bft("Bh%d" % i, [128, 128]) for i in range(4)]; Kh = [bft("Kh%d" % i, [128, 128]) for i in range(4)]; Vt = [bft("Vt%d" % i, [128, 128]) for i in range(4)]
            T1, T1b = bft("T1", [128, 256]); T2, T2b = bft("T2", [128, 256]); T3, T3b = bft("T3", [128, 128])
            W1, W1b = bft("W1", [128, 128]); W2, W2b = bft("W2", [128, 256])
            PP = [bft("PP%d" % i, [128, 256]) for i in range(2)]; NTt = [bft("NT%d" % i, [128, 128]) for i in range(2)]
            Xb, Xbb = bft("Xb", [128, 64]); Ub, Ubb = bft("Ub", [128, 64]); mo, mob = bft("mo")
            tt_ = lambda e, o, a, b, opn, rd, wr: op(e, lambda h: h.tensor_tensor(out=o, in0=a, in1=b, op=opn), reads=rd, writes=wr)
            trc = [0]
            G2R = 64 if l > 0 else 32
            for tb in range(NTB):
                ts = slice(tb * 512, (tb + 1) * 512)
                dma("sp", wa_t[:], waT[:, ts], reads=[db("waT")], writes=[wab]); dma("sp", g1_t[:], g1T[:, ts], reads=[db("g1T")], writes=[g1b])
                dma("sp", g2_t[0:G2R, :], g2T[0:G2R, ts], reads=[db("g2T")], writes=[g2b_])
                for c in range(8):
                    cs_ = slice(c * 128, (c + 1) * 128)
                    dma("sp", r_t[:], rT[cs_, ts], reads=[db("rT", c)], writes=[rb]); dma("sp", k_t[:], kT[cs_, ts], reads=[db("kT", c)], writes=[kb_])
                    dma("sp", v_t[:], vT[cs_, ts], reads=[db("vT", c)], writes=[vb])
                    op("pe", lambda h: h.matmul(PS[0][:], lhsT=wa2b[0:64, cs_], rhs=wa_t[0:64, :], start=True, stop=True), reads=[wa2bb, wab], writes=[PSB[0]])
                    op("act", lambda h: h.activation(out=sg[:], in_=PS[0][:], func=AF.Sigmoid, bias=cvc(l, "w0", c)), reads=[PSB[0], cv_b], writes=[sgb])
                    op("dve", lambda h: h.tensor_tensor_scan(out=cs[:], data0=scanm[:], data1=sg[:], initial=0.0, op0=ALU.mult, op1=ALU.add), reads=[scb, sgb], writes=[csb])
                    tt_("dve", ex[:], cs[:], sg[:], ALU.subtract, [csb, sgb], [exb])
                    op("act", lambda h: h.activation(out=eG[:], in_=cs[:], func=AF.Exp, scale=-C0), reads=[csb], writes=[eGb])
                    op("act", lambda h: h.activation(out=eGi[:], in_=cs[:], func=AF.Exp, scale=C0), reads=[csb], writes=[eGib])
                    op("act", lambda h: h.activation(out=eGx[:], in_=ex[:], func=AF.Exp, scale=-C0), reads=[exb], writes=[eGxb])
                    op("pe", lambda h: h.matmul(PS[0][:], lhsT=wa2b[64:128, cs_], rhs=wa_t[64:128, :], start=True, stop=True), reads=[wa2bb, wab], writes=[PSB[0]])
                    op("act", lambda h: h.activation(out=a_t[:], in_=PS[0][:], func=AF.Sigmoid, bias=cvc(l, "a0", c)), reads=[PSB[0], cv_b], writes=[ab])
                    op("pe", lambda h: h.matmul(PS[0][:], lhsT=g2ab[:, cs_], rhs=g1_t[:], start=True, stop=False), reads=[g2abb, g1b], writes=[PSB[0]], inc=False)
                    op("pe", lambda h: h.matmul(PS[0][:], lhsT=g2bv[0:32, cs_], rhs=g2_t[0:32, :], start=False, stop=True), reads=[g2bvb, g2b_], writes=[PSB[0]])
                    op("act", lambda h: h.copy(out=gt[:], in_=PS[0][:]), reads=[PSB[0]], writes=[gtb])
                    if l > 0:
                        dma("sp", vf_t[:], vfT[cs_, ts], reads=[db("vfT", c)], writes=[vfb])
                        op("pe", lambda h: h.matmul(PS[0][:], lhsT=g2bv[32:64, cs_], rhs=g2_t[32:64, :], start=True, stop=True), reads=[g2bvb, g2b_], writes=[PSB[0]])
                        op("act", lambda h: h.activation(out=tmp[:], in_=PS[0][:], func=AF.Sigmoid, bias=cvc(l, "v0", c)), reads=[PSB[0], cv_b], writes=[tmpb])
                        tt_("dve", v2t[:], vf_t[:], v_t[:], ALU.subtract, [vfb, vb], [v2b])
                        tt_("dve", v2t[:], v2t[:], tmp[:], ALU.mult, [v2b, tmpb], [v2b])
                        tt_("dve", v2t[:], v2t[:], v_t[:], ALU.add, [v2b, vb], [v2b])
                    else:
                        op("dve", lambda h: h.tensor_copy(out=v2t[:], in_=v_t[:]), reads=[vb], writes=[v2b])
                    op("pool", lambda h: h.tensor_copy(out=vbf[:], in_=v2t[:]), reads=[v2b], writes=[vbfb])
                    op("dve", lambda h: h.tensor_scalar(out=kk[:], in0=k_t[:], scalar1=cvc(l, "k_k", c), scalar2=None, op0=ALU.mult), reads=[kb_, cv_b], writes=[kkb])
                    tt_("pool", sqb_[:], kk[:], kk[:], ALU.mult, [kkb], [sqbb])
                    op("pe", lambda h: h.matmul(PS[0][:], lhsT=bonesb[:], rhs=sqb_[:], start=True, stop=True), reads=[cb_b, sqbb], writes=[PSB[0]])
                    op("act", lambda h: h.activation(out=rn[:], in_=PS[0][:], func=AF.Sqrt, bias=1e-20), reads=[PSB[0]], writes=[rnb])
                    op("dve", lambda h: h.reciprocal(out=rn[:], in_=rn[:]), reads=[rnb], writes=[rnb])
                    tt_("dve", kk[:], kk[:], rn[:], ALU.mult, [kkb, rnb], [kkb])
                    op("dve", lambda h: h.tensor_scalar(out=tmp[:], in0=a_t[:], scalar1=cvc(l, "k_a", c), scalar2=dvc(l, "omka", c), op0=ALU.mult, op1=ALU.add),
                       reads=[ab, cv_b, dv_b], writes=[tmpb])
                    tt_("dve", k2[:], k_t[:], tmp[:], ALU.mult, [kb_, tmpb], [k2b])
                    op("dve", lambda h: h.scalar_tensor_tensor(out=AR[:, 0, :], in0=kk[:], scalar=-1.0, in1=eGx[:], op0=ALU.mult, op1=ALU.mult), reads=[kkb, eGxb], writes=[ARb])
                    tt_("pool", AR[:, 1, :], r_t[:], eG[:], ALU.mult, [rb, eGb], [ARb])
                    tt_("dve", tmp[:], kk[:], a_t[:], ALU.mult, [kkb, ab], [tmpb])
                    tt_("dve", BhT[:], tmp[:], eGi[:], ALU.mult, [tmpb, eGib], [BhTb])
                    tt_("pool", KhT[:], k2[:], eGi[:], ALU.mult, [k2b, eGib], [KhTb])
                    op("dve", lambda h: h.scalar_tensor_tensor(out=rkb[:], in0=r_t[:], scalar=cvc(l, "r_k", c), in1=k2[:], op0=ALU.mult, op1=ALU.mult), reads=[rb, k2b, cv_b], writes=[rkbb])
                    op("pe", lambda h: h.matmul(PS[0][:], lhsT=bonesb[:], rhs=rkb[:], start=True, stop=True), reads=[cb_b, rkbb], writes=[PSB[0]])
                    tt_("dve", bon[:], PS[0][:], v2t[:], ALU.mult, [PSB[0], v2b], [bonb])
                    for n in range(4):
                        tsl = slice(n * 128, (n + 1) * 128)
                        for src, srcb, dst in ((BhT, BhTb, Bh[n]), (KhT, KhTb, Kh[n]), (vbf, vbfb, Vt[n])):
                            ri = trc[0] % 4
                            trc[0] += 1
                            op("pe", lambda h, src=src, ri=ri: h.transpose(out=PSH[:, ri * 128:(ri + 1) * 128], in_=src[:, tsl], identity=identb[:]), reads=[srcb, cb_b], writes=[PSH_b[ri]])
                            op("act" if ri % 2 else "dve", lambda h, dst=dst, ri=ri: (h.copy if ri % 2 else h.tensor_copy)(out=dst[0][:], in_=PSH[:, ri * 128:(ri + 1) * 128]),
                               reads=[PSH_b[ri]], writes=[dst[1]])
                    for n in range(4):
                        tsl = slice(n * 128, (n + 1) * 128)
                        for hh in range(2):
                            pb = 64 * hh
                            P_ = slice(pb, pb + 64)
                            mm = lambda out, lhsT, rhs, rd, wr, st=True, sp=True, inc=True: op("pe", lambda h: h.matmul(out, lhsT=lhsT, rhs=rhs, start=st, stop=sp), reads=rd, writes=wr, inc=inc)
                            mm(PS[1][:, 0:256], BhT[P_, tsl], AR[P_, :, tsl], [BhTb, ARb], [PSB[1]])
                            mm(PS[2][:, 0:256], KhT[P_, tsl], AR[P_, :, tsl], [KhTb, ARb], [PSB[2]])
                            mm(PS[3][:, 0:128], AR[P_, 0, tsl], BhT[P_, tsl], [ARb, BhTb], [PSB[3]])
                            op("act", lambda h: h.copy(out=T1[:], in_=PS[1][:, 0:256]), reads=[PSB[1]], writes=[T1b])
                            op("dve", lambda h: h.tensor_copy(out=T2[:], in_=PS[2][:, 0:256]), reads=[PSB[2]], writes=[T2b])
                            op("act", lambda h: h.copy(out=T3[:], in_=PS[3][:, 0:128]), reads=[PSB[3]], writes=[T3b])
                            p0, p0b = PP[0]
                            tt_("pool", p0[:, 0:128], T3[:], mslb[:], ALU.mult, [T3b, mslbb], [p0b])
                            tt_("pool", p0[:, 128:256], T1[:, 0:128], m2b[:, 0:128], ALU.mult, [T1b, m2bb], [p0b])
                            tt_("pool", W1[:], T1[:, 128:256], m2b[:, 128:256], ALU.mult, [T1b, m2bb], [W1b])
                            tt_("pool", W2[:], T2[:], m2b[:], ALU.mult, [T2b, m2bb], [W2b])
                            d0, d0b = DG[0]
                            tt_("pool", d0[:, 0:128], T3[:], hm[:, 0, 0:128], ALU.mult, [T3b, hmb], [d0b])
                            tt_("pool", d0[:, 128:256], T1[:, 0:128], hm[:, 0, 128:256], ALU.mult, [T1b, hmb], [d0b])
                            tt_("pool", d0[:, 0:128], d0[:, 0:128], identb[:], ALU.add, [d0b, cb_b], [d0b])
                            tt_("pool", d0[:, 128:256], d0[:, 128:256], identb[:], ALU.add, [d0b, cb_b], [d0b])
                            for k in range(1, 7):
                                dc, dcb = DG[(k - 1) % 2]
                                dn, dnb = DG[k % 2]
                                mm(PS[1][:, 0:128], p0[:, 128:256], dc[:, 0:128], [p0b, dcb], [PSB[1]], inc=False)
                                mm(PS[1][:, 128:256], p0[:, 0:128], dc[:, 128:256], [p0b, dcb], [PSB[1]])
                                op("act" if k % 2 else "dve", lambda h, k=k: (h.copy if k % 2 else h.tensor_copy)(out=ZZ[:], in_=PS[1][:, 0:256]), reads=[PSB[1]], writes=[ZZb])
                                mm(PS[4][:, 0:128], dc[:, 128:256], ZZ[:, 0:128], [dcb, ZZb], [PSB[4]], inc=False)
                                mm(PS[4][:, 128:256], dc[:, 0:128], ZZ[:, 128:256], [dcb, ZZb], [PSB[4]])
                                tt_("dve", tW[:], PS[4][:, 0:256], hm[:, k, :], ALU.mult, [PSB[4], hmb], [tWb])
                                tt_("pool", dn[:], dc[:], tW[:], ALU.add, [dcb, tWb], [dnb])
                            ntf, ntfb = DG[0][0][:, 128:256], DG[0][1]
                            vt, vtb = Vt[n]
                            mm(PS[5][:, 0:64], AR[P_, 0, tsl], Hb[P_, c, :], [ARb, Hbb], [PSB[5]], True, False, False)
                            mm(PS[5][:, 0:64], W2[:, 0:128], vt[:, P_], [W2b, vtb], [PSB[5]], False, True)
                            op("act", lambda h: h.copy(out=Xb[:], in_=PS[5][:, 0:64]), reads=[PSB[5]], writes=[Xbb])
                            mm(PS[5][:, 64:128], ntf, Xb[:], [ntfb, Xbb], [PSB[5]])
                            op("dve", lambda h: h.tensor_copy(out=Ub[:], in_=PS[5][:, 64:128]), reads=[PSB[5]], writes=[Ubb])
                            mm(PS[6][P_, 0:128], Hb[P_, c, :], AR[P_, 1, tsl], [Hbb, ARb], [PSB[6]], True, False, False)
                            mm(PS[6][P_, 0:128], Ub[:], W1[:], [Ubb, W1b], [PSB[6]], False, False, False)
                            mm(PS[6][P_, 0:128], vt[:, P_], W2[:, 128:256], [vtb, W2b], [PSB[6]], False, True)
                            op("act", lambda h: h.copy(out=OT[P_, tsl], in_=PS[6][P_, 0:128]), reads=[PSB[6]], writes=[OTb])
                            mm(PS[3][P_, 128:192], Bh[n][0][:, P_], Ub[:], [Bh[n][1], Ubb], [PSB[3]], True, False, False)
                            mm(PS[3][P_, 128:192], Kh[n][0][:, P_], vt[:, P_], [Kh[n][1], vtb], [PSB[3]], False, True)
                            gcol = eG[P_, n * 128 + 127:n * 128 + 128]
                            op("dve", lambda h: h.tensor_scalar(out=HG[P_, :], in0=Hf[P_, c, :], scalar1=gcol, scalar2=None, op0=ALU.mult), reads=[Hfb, eGb], writes=[HGb])
                            op("dve", lambda h: h.scalar_tensor_tensor(out=Hf[P_, c, :], in0=PS[3][P_, 128:192], scalar=gcol, in1=HG[P_, :], op0=ALU.mult, op1=ALU.add),
                               reads=[PSB[3], eGb, HGb], writes=[Hfb])
                            op("act", lambda h: h.copy(out=Hb[P_, c, :], in_=Hf[P_, c, :]), reads=[Hfb], writes=[Hbb])
                    op("pool", lambda h: h.tensor_copy(out=ob16[:], in_=OT[:]), reads=[OTb], writes=[ob16b])
                    op("pe", lambda h: h.matmul(PS[0][:], lhsT=bonesb[:], rhs=ob16[:], start=True, stop=True), reads=[cb_b, ob16b], writes=[PSB[0]])
                    op("dve", lambda h: h.tensor_scalar(out=mean[:], in0=PS[0][:], scalar1=1.0 / 64, scalar2=None, op0=ALU.mult), reads=[PSB[0]], writes=[meanb])
                    tt_("dve", OT[:], OT[:], mean[:], ALU.subtract, [OTb, meanb], [OTb])
                    op("act", lambda h: h.activation(out=sqb_[:], in_=OT[:], func=AF.Square), reads=[OTb], writes=[sqbb])
                    op("pe", lambda h: h.matmul(PS[0][:], lhsT=bonesb[:], rhs=sqb_[:], start=True, stop=True), reads=[cb_b, sqbb], writes=[PSB[0]])
                    op("act", lambda h: h.activation(out=var[:], in_=PS[0][:], func=AF.Sqrt, scale=1.0 / 64, bias=GN_EPS), reads=[PSB[0]], writes=[varb])
                    op("dve", lambda h: h.reciprocal(out=var[:], in_=var[:]), reads=[varb], writes=[varb])
                    tt_("dve", OT[:], OT[:], var[:], ALU.mult, [OTb, varb], [OTb])
                    op("act", lambda h: h.activation(out=OT[:], in_=OT[:], func=AF.Identity, scale=cvc(l, "gn_w", c), bias=cvc(l, "gn_b", c)), reads=[OTb, cv_b], writes=[OTb])
                    tt_("dve", OT[:], OT[:], bon[:], ALU.add, [OTb, bonb], [OTb])
                    tt_("dve", mo[:], OT[:], gt[:], ALU.mult, [OTb, gtb], [mob])
                    dma("sp", mixT[cs_, ts], mo[:], reads=[mob], writes=[db("mixT", c)])

        def lockstep(gens):
            gens = list(gens)
            while gens:
                nxt = []
                for g in gens:
                    try:
                        next(g)
                        nxt.append(g)
                    except StopIteration:
                        pass
                gens = nxt

        for l in range(NL if 'nolayers' not in parts else 0):
            kb.new_epoch()
            if 'mix' in parts:
                with scope() as es:
                    hT = sb(es, "hT", [128, KC, S], BF16)
                    hTb = [Buf() for _ in range(NTB)]
                    with scope() as es2:
                        norm_to_hT(es2, l, "g_pre", hT, hTb)
                    with scope() as es2:
                        W = w_in[l]
                        R = mk_rings(es2)
                        tasks = []
                        for c in range(8):
                            tasks.append(([(W[:, c * 128:(c + 1) * 128], 128)], 128,
                                          shift_epi(R, "er%d" % (c % 2), 128, cvc(l, "mu_r", c), dvc(l, "om_r", c), [(rT[c * 128:(c + 1) * 128], db("rT", c))])))
                        for c in range(8):
                            tasks.append(([(W[:, 1088 + c * 128:1088 + (c + 1) * 128], 128)], 128,
                                          shift_epi(R, "ek%d" % (c % 2), 128, cvc(l, "mu_k", c), dvc(l, "om_k", c), [(kT[c * 128:(c + 1) * 128], db("kT", c))])))
                        for c in range(8):
                            dsts = [(vT[c * 128:(c + 1) * 128], db("vT", c))]
                            if l == 0:
                                dsts.append((vfT[c * 128:(c + 1) * 128], db("vfT", c)))
                            tasks.append(([(W[:, 2112 + c * 128:2112 + (c + 1) * 128], 128)], 128,
                                          shift_epi(R, "ev%d" % (c % 2), 128, cvc(l, "mu_v", c), dvc(l, "om_v", c), dsts)))
                        tasks.append(([(W[:, 1024:1088], 64), (W[:, 3136:3200], 64)], 128,
                                      shift_epi(R, "ewa", 128, cvc(l, "mu_wa"), dvc(l, "om_wa"), [(waT, db("waT"))], act=[(0, 64, AF.Tanh), (64, 128, None)])))
                        tasks.append(([(W[:, 3200:3328], 128)], 128,
                                      shift_epi(R, "eg1", 128, cvc(l, "mu_g1"), dvc(l, "om_g1"), [(g1T, db("g1T"))], act=[(0, 128, AF.Sigmoid)])))
                        if l == 0:
                            tasks.append(([(W[:, 3328:3360], 32)], 32,
                                          shift_epi(R, "eg2", 32, cvc(l, "mu_g2", 0, 1, 0, 32), dvc(l, "om_g2", 0, 1, 0, 32), [(g2T[0:32], db("g2T"))], act=[(0, 32, AF.Sigmoid)])))
                        else:
                            tasks.append(([(W[:, 3328:3360], 32), (w_mv[l - 1], 32)], 64,
                                          shift_epi(R, "eg2", 64, cvc(l, "mu_g2", 0, 1, 0, 64), dvc(l, "om_g2", 0, 1, 0, 64), [(g2T, db("g2T"))],
                                                    act=[(0, 32, AF.Sigmoid), (32, 64, None)])))
                        for c in range(8):
                            tasks.append(([(W[:, 3360 + c * 128:3360 + (c + 1) * 128], 128)], 128, plain_epi(R, "eq%d" % (c % 2), qT[c * 128:(c + 1) * 128], db("qT", c), 0)))
                        for c in range(8):
                            tasks.append(([(W[:, 4384 + c * 128:4384 + (c + 1) * 128], 128)], 128, plain_epi(R, "ekd%d" % (c % 2), kdT[c * 128:(c + 1) * 128], db("kdT", c), 1)))
                        proj_fm(es2, hT, hTb, tasks)
                    with scope() as es2:
                        st = Ring(es2, nc, "vst", 2, [128, KC, 128], F32)
                        wbr = Ring(es2, nc, "vwb", 2, [128, KC, 128], BF16)
                        orr = Ring(es2, nc, "vo", 2, [128, 128], BF16)
                        for c in range(8):
                            s_, sb_ = st.next()
                            dma("sp", s_[:], w_in[l][:, 5408 + c * 128:5408 + (c + 1) * 128].rearrange("(kc p) n -> p kc n", p=128), writes=[sb_])
                            w_, wb_ = wbr.next()
                            op("pool", lambda h, s_=s_, w_=w_: h.tensor_copy(out=w_[:], in_=s_[:]), reads=[sb_], writes=[wb_])
                            for tt in range(NT):
                                pi = tt % 4
                                for kc in range(KC):
                                    op("pe", lambda h, w_=w_, kc=kc, pi=pi, tt=tt: h.matmul(PS[pi][:, 0:128], lhsT=hT[:, kc, tt * 128:(tt + 1) * 128], rhs=w_[:, kc, :],
                                                                                           start=(kc == 0), stop=(kc == KC - 1)),
                                       reads=[wb_, hTb[tt // 4]], writes=[PSB[pi]], inc=(kc == KC - 1))
                                o_, ob_ = orr.next()
                                op("act" if tt % 2 else "dve", lambda h, o_=o_, pi=pi, tt=tt: (h.copy if tt % 2 else h.tensor_copy)(out=o_[:], in_=PS[pi][:, 0:128]),
                                   reads=[PSB[pi]], writes=[ob_])
                                dma("sp", vd[tt * 128:(tt + 1) * 128, c * 128:(c + 1) * 128], o_[:], reads=[ob_], writes=[db("vd", c)])

                with scope() as es:
                    rwkv_phase(es, l)
                with scope() as es:
                    attn_phase(es, l)
                with scope() as es:
                    proj_res(es, l, mixT, "mixT", KC, w_out[l], "g_pm")
            with scope() as es:
                hT = sb(es, "hT2", [128, KC, S], BF16)
                hTb = [Buf() for _ in range(NTB)]
                with scope() as es2:
                    norm_to_hT(es2, l, "g_pf", hT, hTb)
                with scope() as es2:
                    tr = Ring(es2, nc, "ft", 2, [128, 512], F32)
                    gr = Ring(es2, nc, "fg", 2, [128, 512], F32)
                    orr = Ring(es2, nc, "fo", 2, [128, 512], BF16)
                    carr = [(sb(es2, "fc%d" % i, [128, 2]), Buf()) for i in range(2)]

                    def ffn_epi(c):
                        car, carb = carr[c % 2]

                        def epi(tb, pss):
                            pg, pu = pss
                            ts = slice(tb * 512, (tb + 1) * 512)
                            t_, tb_ = tr.next()
                            g_, gb_ = gr.next()
                            o_, ob_ = orr.next()
                            op("act", lambda h: h.activation(out=t_[:], in_=PS[pg][:], func=AF.Identity, scale=cvc(l, "cw2", c), bias=cvc(l, "cb", c)),
                               reads=[PSB[pg], cv_b], writes=[tb_])
                            op("dve", lambda h: h.scalar_tensor_tensor(out=t_[:, 1:512], in0=PS[pg][:, 0:511], scalar=cvc(l, "cw1", c), in1=t_[:, 1:512], op0=ALU.mult, op1=ALU.add),
                               reads=[PSB[pg], tb_, cv_b], writes=[tb_])
                            op("dve", lambda h: h.scalar_tensor_tensor(out=t_[:, 2:512], in0=PS[pg][:, 0:510], scalar=cvc(l, "cw0", c), in1=t_[:, 2:512], op0=ALU.mult, op1=ALU.add),
                               reads=[PSB[pg], tb_, cv_b], writes=[tb_])
                            if tb > 0:
                                op("dve", lambda h: h.scalar_tensor_tensor(out=t_[:, 0:2], in0=car[:, 0:2], scalar=cvc(l, "cw0", c), in1=t_[:, 0:2], op0=ALU.mult, op1=ALU.add),
                                   reads=[carb, tb_, cv_b], writes=[tb_])
                                op("dve", lambda h: h.scalar_tensor_tensor(out=t_[:, 0:1], in0=car[:, 1:2], scalar=cvc(l, "cw1", c), in1=t_[:, 0:1], op0=ALU.mult, op1=ALU.add),
                                   reads=[carb, tb_, cv_b], writes=[tb_])
                            op("act", lambda h: h.copy(out=car[:, 0:2], in_=PS[pg][:, 510:512]), reads=[PSB[pg]], writes=[carb])
                            op("act", lambda h: h.activation(out=g_[:], in_=t_[:], func=AF.Gelu_apprx_tanh), reads=[tb_], writes=[gb_])
                            op("dve", lambda h: h.tensor_tensor(out=o_[:], in0=PS[pu][:], in1=g_[:], op=ALU.mult), reads=[PSB[pu], gb_], writes=[ob_])
                            dma("sp", actT[c * 128:(c + 1) * 128, ts], o_[:], reads=[ob_], writes=[db("actT", c)])
                        return epi
                    tasks = []
                    for c in range(FC):
                        e = ffn_epi(c)
                        tasks.append(([(w_up[l][:, c * 128:(c + 1) * 128], 128)], 128, e))
                        tasks.append(([(w_up[l][:, FF + c * 128:FF + (c + 1) * 128], 128)], 128, e))
                    if 'noproj' not in parts:
                        proj_fm(es2, hT, hTb, tasks, nper=2)
            with scope() as es:
                if 'nores' not in parts:
                    proj_res(es, l, actT, "actT", FC, w_down[l], "g_ff")

        with scope() as es:
            xr = Ring(es, nc, "oxi", 2, [128, KC, 128], F32)
            xo = Ring(es, nc, "oxo", 2, [128, D], F32)
            for tt in range(NT):
                xi, xib = xr.next()
                dma("sp", xi[:], xT[:, tt * 128:(tt + 1) * 128].rearrange("(kc p) t -> p kc t", p=128), reads=[db("xT", kc) for kc in range(KC)], writes=[xib])
                o_, ob_ = xo.next()
                for kc in range(KC):
                    pi = kc % 4
                    op("pe", lambda h, kc=kc, pi=pi: h.transpose(out=PS[pi][:, 0:128], in_=xi[:, kc, :], identity=cn[:, 0:128]), reads=[xib, cn_b], writes=[PSB[pi]])
                    op("act" if kc % 2 else "dve", lambda h, kc=kc, pi=pi: (h.copy if kc % 2 else h.tensor_copy)(out=o_[:, kc * 128:(kc + 1) * 128], in_=PS[pi][:, 0:128]),
                       reads=[PSB[pi]], writes=[ob_])
                dma("sp", y_out[tt * 128:(tt + 1) * 128, :], o_[:], reads=[ob_], writes=[db("yout")])
        kb.finish()
    return nc


def pack_inputs(inp, S, NL):
    f = np.float32
    cv = np.zeros((128, NL, NCV), f)

    def put(l, name, vec):
        vec = np.asarray(vec, f).reshape(-1)
        n = (vec.size + 127) // 128
        pad = np.zeros(n * 128, f)
        pad[:vec.size] = vec
        cv[:, l, CV[name]:CV[name] + n] = pad.reshape(n, 128).T
    for l in range(NL):
        put(l, "g_pre", inp["pre_mix_norm"][l]); put(l, "g_pm", inp["post_mix_norm"][l])
        put(l, "g_pf", inp["pre_ffn_norm"][l]); put(l, "g_ff", inp["post_ffn_norm"][l])
        mu = np.asarray(inp["shift_mu"][l], f)
        put(l, "mu_r", mu[0:1024]); put(l, "mu_k", mu[1088:2112]); put(l, "mu_v", mu[2112:3136])
        put(l, "mu_wa", np.concatenate([mu[1024:1088], mu[3136:3200]]))
        put(l, "mu_g1", mu[3200:3328])
        g2 = np.zeros(128, f)
        g2[0:32] = mu[3328:3360]
        if l > 0:
            g2[32:64] = np.asarray(inp["shift_mu_mv"][l - 1], f)
        put(l, "mu_g2", g2)
        for nm in ("w0", "a0", "k_k", "k_a", "r_k", "gn_w", "gn_b"):
            put(l, nm, inp[nm][l])
        if l > 0:
            put(l, "v0", inp["v0"][l - 1])
        put(l, "subln", inp["subln_w"][l])
        cw = np.asarray(inp["conv_w"][l], f)
        put(l, "cw0", cw[0]); put(l, "cw1", cw[1]); put(l, "cw2", cw[2]); put(l, "cb", inp["conv_b"][l])
        for nm, k in (("lq1", "lam_q1"), ("lk1", "lam_k1"), ("lq2", "lam_q2"), ("lk2", "lam_k2")):
            cv[:, l, CV[nm]:CV[nm] + 64] = np.asarray(inp[k][l], f)[None, :]
    shared = {
        "cv": np.ascontiguousarray(cv.reshape(128, NL * NCV)),
        "cn": make_consts(),
        "w_in": np.ascontiguousarray(inp["w_in"][:NL], dtype=f),
        "w_mv": np.ascontiguousarray(inp["w_mv_down"][:max(NL - 1, 1)], dtype=f),
        "w2": np.ascontiguousarray(inp["w2"][:NL], dtype=f), "a2": np.ascontiguousarray(inp["a2"][:NL], dtype=f),
        "g2": np.ascontiguousarray(inp["g2"][:NL], dtype=f), "v2": np.ascontiguousarray(inp["v2"][:max(NL - 1, 1)], dtype=f),
        "w_out": np.ascontiguousarray(inp["w_out"][:NL], dtype=f), "w_up": np.ascontiguousarray(inp["w_up"][:NL], dtype=f),
        "w_down": np.ascontiguousarray(inp["w_down"][:NL], dtype=f),
    }
    return shared


def kernel(**inp):
    x = np.asarray(inp["x"], np.float32)
    B, S, _ = x.shape
    NL = 4
    nc = build(S, NL)
    shared = pack_inputs(inp, S, NL)
    pos = np.asarray(inp["positions"], np.int32)
    in_maps = []
    for b in range(B):
        m = dict(shared)
        m["x"] = np.ascontiguousarray(x[b])
        m["pos"] = np.ascontiguousarray(pos[b:b + 1])
        in_maps.append(m)
    res = run_bass_kernel_spmd(nc, in_maps, core_ids=list(range(B)))
    return np.stack([np.asarray(r["y"], np.float32) for r in res.results], 0)
```

```python
import math
from contextlib import ExitStack, contextmanager
import numpy as np
import concourse.bass as bass
import concourse.mybir as mybir
from concourse.bass_utils import run_bass_kernel_spmd

F32 = mybir.dt.float32
BF16 = mybir.dt.bfloat16
I32 = mybir.dt.int32
AF = mybir.ActivationFunctionType
ALU = mybir.AluOpType

D = 2048
KC = 16
FF = 5632
FC = 44
RW = 1024
RCOLS = 3360
INC = 6432
C0 = math.exp(-0.5)
NORM_EPS = 1e-6
GN_EPS = 64e-5
SUBLN_EPS = 1e-5

CV = {}
_o = 0
for _n, _w in (("g_pre", 16), ("g_pm", 16), ("g_pf", 16), ("g_ff", 16), ("mu_r", 8), ("mu_k", 8), ("mu_v", 8),
               ("mu_wa", 1), ("mu_g1", 1), ("mu_g2", 1), ("w0", 8), ("a0", 8), ("k_k", 8), ("k_a", 8), ("r_k", 8),
               ("gn_w", 8), ("gn_b", 8), ("v0", 8), ("subln", 1), ("cw0", 44), ("cw1", 44), ("cw2", 44), ("cb", 44),
               ("lq1", 64), ("lk1", 64), ("lq2", 64), ("lk2", 64)):
    CV[_n] = _o
    _o += _w
NCV = _o
DV = {}
_o = 0
for _n, _w in (("om_r", 8), ("om_k", 8), ("om_v", 8), ("om_wa", 1), ("om_g1", 1), ("om_g2", 1), ("omka", 8), ("lam", 1), ("nlam", 1), ("sub2", 1)):
    DV[_n] = _o
    _o += _w
NDV = _o

CN = {"ident": 0, "bones": 128, "ones": 256, "perm": 384, "m2": 512, "msl": 768, "invf": 896, "sign": 897, "dmask": 898, "scanm": 2946, "hm": 3458}
NCNP = 898
NCN = 3458 + 7 * 256


def make_consts():
    c = np.zeros((128, NCN), np.float32)
    p = np.arange(128)
    c[:, 0:128] = np.eye(128)
    c[:, 128:256] = (p[:, None] // 64 == p[None, :] // 64)
    c[:, 256:384] = 1.0
    part = p.copy()
    for b in (0, 64):
        for i in range(8):
            part[b + i] = b + i + 8
            part[b + 8 + i] = b + i
    perm = np.zeros((128, 128), np.float32)
    for m in range(128):
        if part[m] != m:
            perm[part[m], m] = 1.0
    c[:, 384:512] = perm
    c[:, 512:640] = (p[:, None] < p[None, :])
    c[:, 640:768] = (p[:, None] <= p[None, :])
    c[:, 768:896] = (p[None, :] < p[:, None])
    q = np.arange(512)
    for j in range(4):
        c[:, 898 + j * 512:898 + (j + 1) * 512] = ((j * 128 + p)[:, None] <= q[None, :])
    invf = np.zeros(128, np.float64)
    sign = np.zeros(128, np.float32)
    fr = 500000.0 ** (-np.arange(0, 16, 2, dtype=np.float32) / 16)
    for b in (0, 64):
        for i in range(8):
            invf[b + i] = fr[i]
            invf[b + 8 + i] = fr[i]
            sign[b + i] = -1.0
            sign[b + 8 + i] = 1.0
    c[:, 896] = invf.astype(np.float32)
    c[:, 897] = sign
    sm = np.ones(512, np.float32)
    sm[::128] = 0.0
    c[:, 2946:2946 + 512] = sm[None, :]
    for k in range(7):
        t = p[:, None]
        s_ = p[None, :]
        mk = ((t >> (k + 1)) == (s_ >> (k + 1))) & (((t >> k) & 1) == 1) & (((s_ >> k) & 1) == 0)
        c[:, 3458 + k * 256:3458 + k * 256 + 128] = mk
        c[:, 3458 + k * 256 + 128:3458 + (k + 1) * 256] = mk.T
    return c


class Buf:
    __slots__ = ("w", "r")

    def __init__(self):
        self.w = {}
        self.r = {}


class Eng:
    def __init__(self, name, h, sem):
        self.name, self.h, self.sem, self.cnt, self.seen = name, h, sem, 0, {}


class KB:
    def __init__(self, nc, es, n_epochs=1):
        self.nc = nc
        self.E = {}
        self.sems = {}
        self.keyeng = {}
        self.dead = set()
        self.pool_sems = {}
        hs = (("pe", nc.tensor), ("act", nc.scalar), ("dve", nc.vector), ("pool", nc.gpsimd), ("sp", nc.sync))
        for name, h in hs:
            self.pool_sems[name] = [es.enter_context(nc.semaphore("s_%s%d" % (name, i))) for i in range(n_epochs if name != "sp" else 1)]
            e = Eng(name, h, self.pool_sems[name][0])
            e.key = name + "#0"
            e.epoch = 0
            self.E[name] = e
            self.sems[e.key] = (e.sem, 1)
            self.keyeng[e.key] = e
        self.slots = {}
        for q, n in (("sp", 10),):
            sl = []
            for i in range(n):
                key = "d%s%d" % (q, i)
                sem = es.enter_context(nc.semaphore("s_" + key))
                self.sems[key] = (sem, 16)
                sl.append([key, 0])
            self.slots[q] = [sl, 0]

    def new_epoch(self):
        self.barrier()
        for name in ("pe", "act", "dve", "pool"):
            e = self.E[name]
            if e.epoch + 1 >= len(self.pool_sems[name]):
                continue
            self.dead.add(e.key)
            e.epoch += 1
            e.sem = self.pool_sems[name][e.epoch]
            e.cnt = 0
            e.key = "%s#%d" % (name, e.epoch)
            self.sems[e.key] = (e.sem, 1)
            self.keyeng[e.key] = e

    def _need(self, e, key, n, raw):
        if key in self.dead:
            return
        if key == e.key and (not raw) and e.name == "pe":
            return
        if e.seen.get(key, 0) >= n:
            return
        sem, unit = self.sems[key]
        if key in self.keyeng and key != e.key:
            assert n <= self.keyeng[key].cnt, ("pending ticket", key, n)
        e.h.wait_ge(sem, n * unit)
        e.seen[key] = n

    def _deps(self, e, reads, writes):
        for b in reads:
            for k, n in b.w.items():
                self._need(e, k, n, True)
        for b in writes:
            for k, n in b.w.items():
                self._need(e, k, n, False)
            for k, n in b.r.items():
                self._need(e, k, n, False)

    def op(self, eng, fn, reads=(), writes=(), inc=True):
        e = self.E[eng]
        self._deps(e, reads, writes)
        ins = fn(e.h)
        if inc:
            ins.then_inc(e.sem, 1)
            e.cnt += 1
            t = e.cnt
        else:
            t = e.cnt + 1
        for b in writes:
            b.w = {e.key: t}
            b.r = {}
        for b in reads:
            b.r[e.key] = t
        return ins

    def dma(self, q, out, in_, reads=(), writes=()):
        e = self.E[q]
        self._deps(e, reads, writes)
        sl, idx = self.slots[q]
        key, cnt = sl[idx]
        self.slots[q][1] = (idx + 1) % len(sl)
        if cnt > 0:
            self._need(e, key, cnt, True)
        sem, unit = self.sems[key]
        e.h.dma_start(out=out, in_=in_).then_inc(sem, 16)
        sl[idx][1] = cnt + 1
        for b in writes:
            b.w = {key: cnt + 1}
            b.r = {}
        for b in reads:
            b.r[key] = cnt + 1

    def barrier(self):
        for en in ("pe", "act", "dve", "pool", "sp"):
            e = self.E[en]
            for k2 in ("pe", "act", "dve", "pool"):
                o = self.E[k2]
                if k2 != en and o.cnt > 0:
                    self._need(e, o.key, o.cnt, True)
            for q in self.slots:
                for key, cnt in self.slots[q][0]:
                    if cnt > 0:
                        self._need(e, key, cnt, True)

    def finish(self):
        e = self.E["sp"]
        for q in self.slots:
            for key, cnt in self.slots[q][0]:
                if cnt > 0:
                    self._need(e, key, cnt, True)


_UID = [0]


class Ring:
    def __init__(self, es, nc, name, n, shape, dt, psum=False):
        self.t = []
        for i in range(n):
            _UID[0] += 1
            t = es.enter_context((nc.psum_tensor if psum else nc.sbuf_tensor)("rg_%s_%d" % (name, _UID[0]), shape, dt))
            self.t.append((t, Buf()))
        self.i = 0

    def next(self):
        r = self.t[self.i]
        self.i = (self.i + 1) % len(self.t)
        return r


def build(S, NL, dbg=False, parts=('mix', 'ffn')):
    nc = bass.Bass("TRN2", target_bir_lowering=False)
    NTB = S // 512
    NT = S // 128

    def din(name, shape, dt=F32):
        return nc.dram_tensor(name, shape, dt, kind="ExternalInput").ap()

    x_in = din("x", [S, D])
    pos_in = din("pos", [1, S], I32)
    cv_in = din("cv", [128, NL * NCV])
    cn_in = din("cn", [128, NCN])
    w_in = din("w_in", [NL, D, INC])
    w_mv = din("w_mv", [max(NL - 1, 1), D, 32])
    w2_in = din("w2", [NL, 64, RW])
    a2_in = din("a2", [NL, 64, RW])
    g2_in = din("g2", [NL, 160, RW])
    v2_in = din("v2", [max(NL - 1, 1), 32, RW])
    w_out = din("w_out", [NL, D, D])
    w_up = din("w_up", [NL, D, 2 * FF])
    w_down = din("w_down", [NL, FF, D])
    y_out = nc.dram_tensor("y", [S, D], F32, kind="ExternalOutput").ap()

    kindS = "ExternalOutput" if dbg else "Internal"

    def dsc(name, shape, dt):
        return nc.dram_tensor(name, shape, dt, kind=kindS).ap()

    xT = dsc("xT", [D, S], F32)
    yT = dsc("yT", [D, S], F32)
    rT = dsc("rT", [RW, S], BF16)
    kT = dsc("kT", [RW, S], BF16)
    vT = dsc("vT", [RW, S], BF16)
    vfT = dsc("vfT", [RW, S], BF16)
    waT = dsc("waT", [128, S], BF16)
    g1T = dsc("g1T", [128, S], BF16)
    g2T = dsc("g2T", [64, S], BF16)
    qT = dsc("qT", [RW, S], BF16)
    kdT = dsc("kdT", [RW, S], BF16)
    vd = dsc("vd", [S, RW], BF16)
    mixT = dsc("mixT", [D, S], BF16)
    actT = dsc("actT", [FF, S], BF16)
    rotC = dsc("rotC", [128, S], F32)
    rotS = dsc("rotS", [128, S], F32)
    DB = {}

    def db(name, i=0):
        k = (name, i)
        if k not in DB:
            DB[k] = Buf()
        return DB[k]

    with ExitStack() as es0:
        kb = KB(nc, es0, n_epochs=NL + 1)
        op, dma = kb.op, kb.dma

        @contextmanager
        def scope():
            with ExitStack() as e_:
                yield e_
                kb.barrier()

        def sb(es, name, shape, dt=F32):
            _UID[0] += 1
            return es.enter_context(nc.sbuf_tensor("sb_%s_%d" % (name, _UID[0]), shape, dt))

        cn = sb(es0, "cn", [128, NCNP]); cn_b = Buf()
        cv = sb(es0, "cv", [128, NL * NCV]); cv_b = Buf()
        dv = sb(es0, "dv", [128, NL * NDV]); dv_b = Buf()
        identb = sb(es0, "identb", [128, 128], BF16)
        bonesb = sb(es0, "bonesb", [128, 128], BF16)
        onesb = sb(es0, "onesb", [128, 128], BF16)
        permb = sb(es0, "permb", [128, 128], BF16)
        cb_b = Buf()
        PS = [es0.enter_context(nc.psum_tensor("ps%d" % i, [128, 512], F32)) for i in range(7)]
        PSB = [Buf() for _ in range(7)]
        PSH = es0.enter_context(nc.psum_tensor("psh", [128, 1024], BF16)); PSH_b = [Buf()] * 4

        dma("sp", cn[:], cn_in[:, 0:NCNP], writes=[cn_b])
        dma("sp", cv[:], cv_in[:, :], writes=[cv_b])
        for t, o in ((identb, CN["ident"]), (bonesb, CN["bones"]), (onesb, CN["ones"]), (permb, CN["perm"])):
            op("dve", lambda h, t=t, o=o: h.tensor_copy(out=t[:], in_=cn[:, o:o + 128]), reads=[cn_b], writes=[cb_b])

        def cvc(l, name, i=0, n=1, p0=0, p1=128):
            o = l * NCV + CV[name] + i
            return cv[p0:p1, o:o + n]

        def dvc(l, name, i=0, n=1, p0=0, p1=128):
            o = l * NDV + DV[name] + i
            return dv[p0:p1, o:o + n]

        for l in range(NL):
            for a, b_, n in (("om_r", "mu_r", 8), ("om_k", "mu_k", 8), ("om_v", "mu_v", 8), ("om_wa", "mu_wa", 1),
                             ("om_g1", "mu_g1", 1), ("om_g2", "mu_g2", 1), ("omka", "k_a", 8)):
                op("dve", lambda h, l=l, a=a, b_=b_, n=n: h.tensor_scalar(out=dvc(l, a, 0, n), in0=cvc(l, b_, 0, n), scalar1=-1.0, scalar2=1.0,
                                                                        op0=ALU.mult, op1=ALU.add), reads=[cv_b], writes=[dv_b])
        with scope() as es:
            tmp = sb(es, "lamtmp", [128, 64]); tb_ = Buf()
            acc = sb(es, "lamacc", [128, 4]); ab_ = Buf()
            for l in range(NL):
                li = 0.8 - 0.6 * math.exp(-0.3 * l)
                for j, (qa, ka) in enumerate((("lq1", "lk1"), ("lq2", "lk2"))):
                    op("dve", lambda h, l=l, qa=qa, ka=ka: h.tensor_tensor(out=tmp[:], in0=cvc(l, qa, 0, 64), in1=cvc(l, ka, 0, 64), op=ALU.mult),
                       reads=[cv_b], writes=[tb_])
                    op("dve", lambda h, j=j: h.reduce_sum(out=acc[:, j:j + 1], in_=tmp[:], axis=mybir.AxisListType.X), reads=[tb_], writes=[ab_])
                op("act", lambda h: h.activation(out=acc[:, 2:4], in_=acc[:, 0:2], func=AF.Exp), reads=[ab_], writes=[ab_])
                op("dve", lambda h, l=l: h.tensor_tensor(out=dvc(l, "lam"), in0=acc[:, 2:3], in1=acc[:, 3:4], op=ALU.subtract), reads=[ab_, dv_b], writes=[dv_b])
                op("dve", lambda h, l=l, li=li: h.tensor_scalar(out=dvc(l, "nlam"), in0=dvc(l, "lam"), scalar1=float(li), scalar2=-1.0, op0=ALU.add, op1=ALU.mult),
                   reads=[dv_b], writes=[dv_b])
                op("dve", lambda h, l=l, li=li: h.tensor_scalar(out=dvc(l, "sub2"), in0=cvc(l, "subln"), scalar1=float(1.0 - li), scalar2=None, op0=ALU.mult),
                   reads=[cv_b, dv_b], writes=[dv_b])

        with scope() as es:
          if 'norot' not in parts:
              pi_ = sb(es, "posi", [128, 512], I32); pib = Buf()
              pf = sb(es, "posf", [128, 512]); pfb = Buf()
              t1 = sb(es, "rt1", [128, 512]); t1b = Buf()
              t2 = sb(es, "rt2", [128, 512]); t2b = Buf()
              ki = sb(es, "rki", [128, 512], I32); kib = Buf()
              ro = Ring(es, nc, "rto", 2, [128, 512], F32)
              TWO_PI = float(2 * np.pi)
              for tb in range(NTB):
                  ts = slice(tb * 512, (tb + 1) * 512)
                  dma("sp", pi_[:], pos_in[0:1, ts].partition_broadcast(128), writes=[pib])
                  op("dve", lambda h: h.tensor_copy(out=pf[:], in_=pi_[:]), reads=[pib], writes=[pfb])
                  op("dve", lambda h: h.tensor_scalar(out=pf[:], in0=pf[:], scalar1=cn[:, CN["invf"]:CN["invf"] + 1], scalar2=None, op0=ALU.mult),
                     reads=[pfb, cn_b], writes=[pfb])
                  for which, off, dst in (("c", float(np.pi / 2), rotC), ("s", 0.0, rotS)):
                      op("dve", lambda h, off=off: h.tensor_scalar(out=t1[:], in0=pf[:], scalar1=off, scalar2=None, op0=ALU.add), reads=[pfb], writes=[t1b])
                      op("dve", lambda h: h.tensor_scalar(out=t2[:], in0=t1[:], scalar1=float(1 / (2 * np.pi)), scalar2=None, op0=ALU.mult), reads=[t1b], writes=[t2b])
                      op("dve", lambda h: h.tensor_copy(out=ki[:], in_=t2[:]), reads=[t2b], writes=[kib])
                      op("dve", lambda h: h.tensor_copy(out=t2[:], in_=ki[:]), reads=[kib], writes=[t2b])
                      op("dve", lambda h: h.scalar_tensor_tensor(out=t1[:], in0=t2[:], scalar=-TWO_PI, in1=t1[:], op0=ALU.mult, op1=ALU.add), reads=[t2b, t1b], writes=[t1b])
                      op("dve", lambda h: h.tensor_scalar(out=t2[:], in0=t1[:], scalar1=float(np.pi), scalar2=-TWO_PI, op0=ALU.is_gt, op1=ALU.mult), reads=[t1b], writes=[t2b])
                      op("dve", lambda h: h.tensor_tensor(out=t1[:], in0=t1[:], in1=t2[:], op=ALU.add), reads=[t1b, t2b], writes=[t1b])
                      op("dve", lambda h: h.tensor_scalar(out=t2[:], in0=t1[:], scalar1=float(-np.pi), scalar2=TWO_PI, op0=ALU.is_lt, op1=ALU.mult), reads=[t1b], writes=[t2b])
                      op("dve", lambda h: h.tensor_tensor(out=t1[:], in0=t1[:], in1=t2[:], op=ALU.add), reads=[t1b, t2b], writes=[t1b])
                      o_, ob_ = ro.next()
                      op("act", lambda h, o_=o_: h.activation(out=o_[:], in_=t1[:], func=AF.Sin), reads=[t1b], writes=[ob_])
                      if which == "s":
                          op("dve", lambda h, o_=o_: h.tensor_scalar(out=o_[:], in0=o_[:], scalar1=cn[:, CN["sign"]:CN["sign"] + 1], scalar2=None, op0=ALU.mult),
                             reads=[ob_, cn_b], writes=[ob_])
                      dma("sp", dst[:, ts], o_[:], reads=[ob_], writes=[db("rot")])

        with scope() as es:
            xr = Ring(es, nc, "xin", 2, [128, D], F32)
            xo = Ring(es, nc, "xto", 2, [128, KC, 128], F32)
            for tt in range(NT):
                xi, xib = xr.next()
                dma("sp", xi[:], x_in[tt * 128:(tt + 1) * 128, :], writes=[xib])
                o_, ob_ = xo.next()
                for kc in range(KC):
                    pi = kc % 4
                    op("pe", lambda h, kc=kc, pi=pi: h.transpose(out=PS[pi][:, 0:128], in_=xi[:, kc * 128:(kc + 1) * 128], identity=cn[:, 0:128]),
                       reads=[xib, cn_b], writes=[PSB[pi]])
                    op("act" if kc % 2 else "dve", lambda h, kc=kc, pi=pi: (h.copy if kc % 2 else h.tensor_copy)(out=o_[:, kc, :], in_=PS[pi][:, 0:128]),
                       reads=[PSB[pi]], writes=[ob_])
                dma("sp", xT[:, tt * 128:(tt + 1) * 128].rearrange("(kc p) t -> p kc t", p=128), o_[:], reads=[ob_], writes=[db("xT", kc) for kc in range(KC)])

        def norm_to_hT(es, l, gname, hT, hTb):
            xs = Ring(es, nc, "nxs", 16, [128, 512], F32)
            sq = Ring(es, nc, "nsq", 2, [128, 512], BF16)
            rs = sb(es, "nrs", [128, 512]); rsb = Buf()
            for tb in range(NTB):
                ts = slice(tb * 512, (tb + 1) * 512)
                tl = []
                for kc in range(KC):
                    x_, xb_ = xs.next()
                    dma("sp", x_[:], xT[kc * 128:(kc + 1) * 128, ts], reads=[db("xT", kc)], writes=[xb_])
                    s_, sb_ = sq.next()
                    op("act", lambda h, x_=x_, s_=s_: h.activation(out=s_[:], in_=x_[:], func=AF.Square), reads=[xb_], writes=[sb_])
                    op("pe", lambda h, s_=s_, kc=kc: h.matmul(PS[6][:], lhsT=onesb[:], rhs=s_[:], start=(kc == 0), stop=(kc == KC - 1)),
                       reads=[sb_, cb_b], writes=[PSB[6]])
                    tl.append((x_, xb_))
                op("dve", lambda h: h.tensor_scalar(out=rs[:], in0=PS[6][:], scalar1=1.0 / D, scalar2=NORM_EPS, op0=ALU.mult, op1=ALU.add), reads=[PSB[6]], writes=[rsb])
                op("act", lambda h: h.activation(out=rs[:], in_=rs[:], func=AF.Sqrt), reads=[rsb], writes=[rsb])
                op("dve", lambda h: h.reciprocal(out=rs[:], in_=rs[:]), reads=[rsb], writes=[rsb])
                for kc in range(KC):
                    x_, xb_ = tl[kc]
                    op("dve", lambda h, x_=x_, kc=kc: h.scalar_tensor_tensor(out=hT[:, kc, ts], in0=x_[:], scalar=cvc(l, gname, kc), in1=rs[:],
                                                                                                   op0=ALU.mult, op1=ALU.mult),
                       reads=[xb_, rsb, cv_b], writes=[hTb[tb]])

        def proj_fm(es, hT, hTb, tasks, nper=1):
            st = Ring(es, nc, "wst", 2 if nper == 1 else 3, [128, KC, 128], F32)
            wb = Ring(es, nc, "wbf", 3 if nper == 1 else 4, [128, KC, 128], BF16)
            psr = [0]
            groups = [tasks[ti:ti + nper] for ti in range(0, len(tasks), nper)]

            def load(grp):
                wts = []
                for segs, M, epi in grp:
                    s_, sb_ = st.next()
                    o = 0
                    for ap, n in segs:
                        dma("sp", s_[:, :, o:o + n], ap.rearrange("(kc p) n -> p kc n", p=128), writes=[sb_])
                        o += n
                    w_, wb_ = wb.next()
                    op("pool", lambda h, s_=s_, w_=w_, M=M: h.tensor_copy(out=w_[:, :, 0:M], in_=s_[:, :, 0:M]), reads=[sb_], writes=[wb_])
                    wts.append((w_, wb_, M))
                return wts
            nxt = load(groups[0])
            for gi, grp in enumerate(groups):
                wts = nxt
                if gi + 1 < len(groups):
                    nxt = load(groups[gi + 1])
                for tb in range(NTB):
                    ts = slice(tb * 512, (tb + 1) * 512)
                    pss = []
                    for w_, wb_, M in wts:
                        pi = psr[0] % 4
                        psr[0] += 1
                        for kc in range(KC):
                            op("pe", lambda h, w_=w_, M=M, kc=kc, pi=pi: h.matmul(PS[pi][0:M, :], lhsT=w_[:, kc, 0:M], rhs=hT[:, kc, ts], start=(kc == 0), stop=(kc == KC - 1)),
                               reads=[wb_, hTb[tb]], writes=[PSB[pi]], inc=(kc == KC - 1))
                        pss.append(pi)
                    grp[0][2](tb, pss)

        def mk_rings(es):
            return {"t": Ring(es, nc, "ept", 2, [128, 512], F32), "of": Ring(es, nc, "epof", 2, [128, 512], F32),
                    "ob": Ring(es, nc, "epob", 3, [128, 512], BF16), "car": sb(es, "epcar", [128, 64]), "ncar": [0]}

        def shift_epi(R, name, M, mu_ap, om_ap, dests, act=None):
            tr = R["t"]
            orr = R["of"] if act else R["ob"]
            obr = R["ob"] if act else None
            ci = R["ncar"][0]
            R["ncar"][0] += 1
            carry = R["car"][:, ci:ci + 1]; cb = Buf()

            def epi(tb, pss):
                pi = pss[0]
                ts = slice(tb * 512, (tb + 1) * 512)
                t_, tb_ = tr.next()
                o_, ob_ = orr.next()
                op("act", lambda h: h.activation(out=t_[0:M, :], in_=PS[pi][0:M, :], func=AF.Identity, scale=om_ap), reads=[PSB[pi], dv_b], writes=[tb_])
                op("dve", lambda h: h.scalar_tensor_tensor(out=o_[0:M, 1:512], in0=PS[pi][0:M, 0:511], scalar=mu_ap, in1=t_[0:M, 1:512], op0=ALU.mult, op1=ALU.add),
                   reads=[PSB[pi], tb_, cv_b], writes=[ob_])
                if tb == 0:
                    op("dve", lambda h: h.tensor_copy(out=o_[0:M, 0:1], in_=t_[0:M, 0:1]), reads=[tb_], writes=[ob_])
                else:
                    op("dve", lambda h: h.scalar_tensor_tensor(out=o_[0:M, 0:1], in0=carry[0:M, :], scalar=mu_ap, in1=t_[0:M, 0:1], op0=ALU.mult, op1=ALU.add),
                       reads=[cb, tb_, cv_b], writes=[ob_])
                op("act", lambda h: h.copy(out=carry[0:M, :], in_=PS[pi][0:M, 511:512]), reads=[PSB[pi]], writes=[cb])
                if act:
                    f_, fb_ = obr.next()
                    for p0, p1, fn in act:
                        if fn is None:
                            op("dve", lambda h, p0=p0, p1=p1: h.tensor_copy(out=f_[p0:p1, :], in_=o_[p0:p1, :]), reads=[ob_], writes=[fb_])
                        else:
                            op("act", lambda h, p0=p0, p1=p1, fn=fn: h.activation(out=f_[p0:p1, :], in_=o_[p0:p1, :], func=fn), reads=[ob_], writes=[fb_])
                    o_, ob_ = f_, fb_
                for dap, dbuf in dests:
                    dma("sp", dap[:, ts], o_[0:M, :], reads=[ob_], writes=[dbuf])
            return epi

        def plain_epi(R, name, dest, dbuf, flip):
            orr = R["ob"]

            def epi(tb, pss):
                pi = pss[0]
                o_, ob_ = orr.next()
                if (tb + flip) % 2:
                    op("act", lambda h: h.copy(out=o_[:], in_=PS[pi][:]), reads=[PSB[pi]], writes=[ob_])
                else:
                    op("dve", lambda h: h.tensor_copy(out=o_[:], in_=PS[pi][:]), reads=[PSB[pi]], writes=[ob_])
                dma("sp", dest[:, tb * 512:(tb + 1) * 512], o_[:], reads=[ob_], writes=[dbuf])
            return epi

        def proj_res(es, l, src, srcname, KCn, W, gname):
            TBD = min(1024, S)
            NH = TBD // 512
            srct = sb(es, "prs", [128, KCn, TBD], BF16); srcb = Buf()
            KH = KCn // 4
            st = Ring(es, nc, "prst", 4, [128, KH, 128], F32)
            wbr = Ring(es, nc, "prwb", 3, [128, KCn, 128], BF16)
            yr = Ring(es, nc, "pry", 3, [128, TBD], F32)
            sqr = Ring(es, nc, "prsq", 2, [128, 512], BF16)
            rst = sb(es, "prrs", [128, TBD]); rsb = Buf()
            xr = Ring(es, nc, "prx", 3, [128, TBD], F32)
            for tbd in range(S // TBD):
                ts = slice(tbd * TBD, (tbd + 1) * TBD)
                for kc in range(KCn):
                    dma("sp", srct[:, kc, :], src[kc * 128:(kc + 1) * 128, ts], reads=[db(srcname, kc)], writes=[srcb])
                def loadw(c):
                    w_, wb_ = wbr.next()
                    for hf in range(4):
                        s_, sb_ = st.next()
                        dma("sp", s_[:], W[hf * KH * 128:(hf + 1) * KH * 128, c * 128:(c + 1) * 128].rearrange("(kc p) n -> p kc n", p=128), writes=[sb_])
                        op("pool", lambda h, s_=s_, w_=w_, hf=hf: h.tensor_copy(out=w_[:, hf * KH:(hf + 1) * KH, :], in_=s_[:]), reads=[sb_], writes=[wb_])
                    return w_, wb_
                nxtw = loadw(0)
                for c in range(KC):
                    w_, wb_ = nxtw
                    if c + 1 < KC:
                        nxtw = loadw(c + 1)
                    y_, yb_ = yr.next()
                    for hh in range(NH):
                        pi = (c * NH + hh) % 4
                        for kc in range(KCn):
                            op("pe", lambda h, w_=w_, kc=kc, pi=pi, hh=hh: h.matmul(PS[pi][:], lhsT=w_[:, kc, :], rhs=srct[:, kc, hh * 512:(hh + 1) * 512],
                                                                                   start=(kc == 0), stop=(kc == KCn - 1)),
                               reads=[wb_, srcb], writes=[PSB[pi]], inc=(kc == KCn - 1))
                        op("dve", lambda h, y_=y_, pi=pi, hh=hh: h.tensor_copy(out=y_[:, hh * 512:(hh + 1) * 512], in_=PS[pi][:]), reads=[PSB[pi]], writes=[yb_])
                        q_, qb_ = sqr.next()
                        op("act", lambda h, q_=q_, y_=y_, hh=hh: h.activation(out=q_[:], in_=y_[:, hh * 512:(hh + 1) * 512], func=AF.Square), reads=[yb_], writes=[qb_])
                        op("pe", lambda h, q_=q_, hh=hh, c=c: h.matmul(PS[4 + hh][:], lhsT=onesb[:], rhs=q_[:], start=(c == 0), stop=(c == KC - 1)),
                           reads=[qb_, cb_b], writes=[PSB[4 + hh]])
                    dma("sp", yT[c * 128:(c + 1) * 128, ts], y_[:], reads=[yb_], writes=[db("yT", c)])
                for hh in range(NH):
                    hs = slice(hh * 512, (hh + 1) * 512)
                    op("dve", lambda h, hh=hh, hs=hs: h.tensor_scalar(out=rst[:, hs], in0=PS[4 + hh][:], scalar1=1.0 / D, scalar2=NORM_EPS, op0=ALU.mult, op1=ALU.add),
                       reads=[PSB[4 + hh]], writes=[rsb])
                op("act", lambda h: h.activation(out=rst[:], in_=rst[:], func=AF.Sqrt), reads=[rsb], writes=[rsb])
                op("dve", lambda h: h.reciprocal(out=rst[:], in_=rst[:]), reads=[rsb], writes=[rsb])
                def loadxy(c):
                    y_, yb_ = yr.next()
                    x_, xb_ = xr.next()
                    dma("sp", y_[:], yT[c * 128:(c + 1) * 128, ts], reads=[db("yT", c)], writes=[yb_])
                    dma("sp", x_[:], xT[c * 128:(c + 1) * 128, ts], reads=[db("xT", c)], writes=[xb_])
                    return y_, yb_, x_, xb_
                nxy = loadxy(0)
                for c in range(KC):
                    y_, yb_, x_, xb_ = nxy
                    if c + 1 < KC:
                        nxy = loadxy(c + 1)
                    op("dve", lambda h, y_=y_, c=c: h.scalar_tensor_tensor(out=y_[:], in0=y_[:], scalar=cvc(l, gname, c), in1=rst[:], op0=ALU.mult, op1=ALU.mult),
                       reads=[yb_, rsb, cv_b], writes=[yb_])
                    op("dve", lambda h, y_=y_, x_=x_: h.tensor_tensor(out=x_[:], in0=x_[:], in1=y_[:], op=ALU.add), reads=[yb_, xb_], writes=[xb_])
                    dma("sp", xT[c * 128:(c + 1) * 128, ts], x_[:], reads=[xb_], writes=[db("xT", c)])


        def attn_phase(es, l):
            C = sb(es, "arC", [128, S]); Cb = Buf()
            Sg = sb(es, "arS", [128, S]); Sgb = Buf()
            dma("sp", C[:], rotC[:, :], reads=[db("rot")], writes=[Cb])
            dma("sp", Sg[:], rotS[:, :], reads=[db("rot")], writes=[Sgb])
            dmf = sb(es, "admf", [128, 2048]); dmfb = Buf()
            dm = sb(es, "adm", [128, 4, 512], BF16); dmb = Buf()
            dma("sp", dmf[:], cn_in[:, CN["dmask"]:CN["dmask"] + 2048], writes=[dmfb])
            op("pool", lambda h: h.tensor_copy(out=dm[:], in_=dmf[:].rearrange("p (a b) -> p a b", a=4)), reads=[dmfb], writes=[dmb])
            raws = Ring(es, nc, "araw", 2, [128, 512], BF16)
            t1r = Ring(es, nc, "at1", 2, [128, 512], F32)
            t2r = Ring(es, nc, "at2", 2, [128, 512], F32)
            qk = Ring(es, nc, "aqk", 4, [128, S], BF16)
            Vr = Ring(es, nc, "aV", 2, [128, NT, 128], BF16)
            Er = Ring(es, nc, "aE", 4, [128, 512], BF16)
            wk = Ring(es, nc, "awk", 6, [128, 512], F32)
            sqr = Ring(es, nc, "asq", 2, [128, 512], BF16)
            outr = Ring(es, nc, "aout", 2, [128, 512], BF16)
            for hd in range(8):
                rot = []
                for src, nm in ((qT, "qT"), (kdT, "kdT")):
                    d_, db_ = qk.next()
                    for tb in range(NTB):
                        ts = slice(tb * 512, (tb + 1) * 512)
                        r_, rb_ = raws.next()
                        dma("sp", r_[:], src[hd * 128:(hd + 1) * 128, ts], reads=[db(nm, hd)], writes=[rb_])
                        op("pe", lambda h, r_=r_: h.matmul(PS[0][:], lhsT=permb[:], rhs=r_[:], start=True, stop=True), reads=[rb_, cb_b], writes=[PSB[0]])
                        a_, ab_ = t1r.next()
                        b_, bb_ = t2r.next()
                        op("pool", lambda h, r_=r_, a_=a_, ts=ts: h.tensor_tensor(out=a_[:], in0=r_[:], in1=C[:, ts], op=ALU.mult), reads=[rb_, Cb], writes=[ab_])
                        op("dve", lambda h, b_=b_, ts=ts: h.tensor_tensor(out=b_[:], in0=PS[0][:], in1=Sg[:, ts], op=ALU.mult), reads=[PSB[0], Sgb], writes=[bb_])
                        op("dve", lambda h, a_=a_, b_=b_, d_=d_, ts=ts: h.tensor_tensor(out=d_[:, ts], in0=a_[:], in1=b_[:], op=ALU.add), reads=[ab_, bb_], writes=[db_])
                    rot.append((d_, db_))
                (q_, qb_), (k_, kb_) = rot
                V_, Vb_ = Vr.next()
                dma("sp", V_[:], vd[:, hd * 128:(hd + 1) * 128].rearrange("(tt p) c -> p tt c", p=128), reads=[db("vd", hd)], writes=[Vb_])
                for qb in range(NTB):
                    qs = slice(qb * 512, (qb + 1) * 512)
                    nk = 4 * qb + 4
                    steps = [(kt, m) for kt in range(nk) for m in range(2)]

                    def emit_s(i):
                        kt, m = steps[i]
                        pi = i % 3
                        op("pe", lambda h: h.matmul(PS[pi][:], lhsT=k_[m * 64:(m + 1) * 64, kt * 128:(kt + 1) * 128], rhs=q_[m * 64:(m + 1) * 64, qs],
                                                    start=True, stop=True), reads=[kb_, qb_], writes=[PSB[pi]])
                    emit_s(0)
                    for i, (kt, m) in enumerate(steps):
                        pi = i % 3
                        if i + 1 < len(steps):
                            emit_s(i + 1)
                        e_, eb_ = Er.next()
                        op("act", lambda h, e_=e_, pi=pi: h.activation(out=e_[:], in_=PS[pi][:], func=AF.Exp, scale=0.125), reads=[PSB[pi]], writes=[eb_])
                        if kt >= 4 * qb:
                            op("pool", lambda h, e_=e_, j=kt - 4 * qb: h.tensor_tensor(out=e_[:], in0=e_[:], in1=dm[:, j, :], op=ALU.mult), reads=[eb_, dmb], writes=[eb_])
                        op("pe", lambda h, e_=e_, kt=kt, m=m: h.matmul(PS[3 + m][:], lhsT=V_[:, kt, :], rhs=e_[:], start=(kt == 0), stop=(kt == nk - 1)),
                           reads=[Vb_, eb_], writes=[PSB[3 + m]], inc=False)
                        op("pe", lambda h, e_=e_, kt=kt, m=m: h.matmul(PS[5 + m][:], lhsT=onesb[:], rhs=e_[:], start=(kt == 0), stop=(kt == nk - 1)),
                           reads=[cb_b, eb_], writes=[PSB[5 + m]])
                    w = [wk.next() for _ in range(6)]
                    for m in range(2):
                        op("dve", lambda h, m=m: h.reciprocal(out=w[m][0][:], in_=PS[5 + m][:]), reads=[PSB[5 + m]], writes=[w[m][1]])
                        op("dve", lambda h, m=m: h.tensor_tensor(out=w[2 + m][0][:], in0=PS[3 + m][:], in1=w[m][0][:], op=ALU.mult), reads=[PSB[3 + m], w[m][1]], writes=[w[2 + m][1]])
                    op("dve", lambda h: h.scalar_tensor_tensor(out=w[4][0][:], in0=w[3][0][:], scalar=dvc(l, "nlam"), in1=w[2][0][:], op0=ALU.mult, op1=ALU.add),
                       reads=[w[3][1], w[2][1], dv_b], writes=[w[4][1]])
                    s_, sb_ = sqr.next()
                    op("act", lambda h, s_=s_: h.activation(out=s_[:], in_=w[4][0][:], func=AF.Square), reads=[w[4][1]], writes=[sb_])
                    op("pe", lambda h, s_=s_: h.matmul(PS[0][:], lhsT=onesb[:], rhs=s_[:], start=True, stop=True), reads=[sb_, cb_b], writes=[PSB[0]])
                    op("dve", lambda h: h.tensor_scalar(out=w[5][0][:], in0=PS[0][:], scalar1=1.0 / 128, scalar2=SUBLN_EPS, op0=ALU.mult, op1=ALU.add), reads=[PSB[0]], writes=[w[5][1]])
                    op("act", lambda h: h.activation(out=w[5][0][:], in_=w[5][0][:], func=AF.Sqrt), reads=[w[5][1]], writes=[w[5][1]])
                    op("dve", lambda h: h.reciprocal(out=w[5][0][:], in_=w[5][0][:]), reads=[w[5][1]], writes=[w[5][1]])
                    o_, ob_ = outr.next()
                    op("dve", lambda h, o_=o_: h.scalar_tensor_tensor(out=o_[:], in0=w[4][0][:], scalar=dvc(l, "sub2"), in1=w[5][0][:], op0=ALU.mult, op1=ALU.mult),
                       reads=[w[4][1], w[5][1], dv_b], writes=[ob_])
                    dma("sp", mixT[1024 + hd * 128:1024 + (hd + 1) * 128, qs], o_[:], reads=[ob_], writes=[db("mixT", 8 + hd)])

        def rwkv_phase(es, l):
            f32t = lambda nm, sh=[128, 512]: (sb(es, nm, sh), Buf())
            bft = lambda nm, sh=[128, 512]: (sb(es, nm, sh, BF16), Buf())
            scanm, scb = f32t("scanm")
            dma("sp", scanm[:], cn_in[:, CN["scanm"]:CN["scanm"] + 512], writes=[scb])
            hm, hmb = f32t("hm", [128, 7, 256])
            dma("sp", hm[:], cn_in[:, CN["hm"]:CN["hm"] + 7 * 256].rearrange("p (k c) -> p k c", k=7), writes=[hmb])
            DG = [bft("DG%d" % i, [128, 256]) for i in range(2)]; ZZ, ZZb = bft("ZZ", [128, 256]); tW, tWb = bft("tW", [128, 256])
            m2b, m2bb = bft("m2b", [128, 256]); mslb, mslbb = bft("mslb", [128, 128])
            op("pool", lambda h: h.tensor_copy(out=m2b[:], in_=cn[:, CN["m2"]:CN["m2"] + 256]), reads=[cn_b], writes=[m2bb])
            op("pool", lambda h: h.tensor_copy(out=mslb[:], in_=cn[:, CN["msl"]:CN["msl"] + 128]), reads=[cn_b], writes=[mslbb])
            wst, wstb = f32t("lst", [128, 1024])
            wa2b, wa2bb = bft("wa2b", [128, 1024]); g2ab, g2abb = bft("g2ab", [128, 1024]); g2bv, g2bvb = bft("g2bv", [64, 1024])
            dma("sp", wst[0:64, :], w2_in[l], writes=[wstb]); dma("sp", wst[64:128, :], a2_in[l], writes=[wstb])
            op("pool", lambda h: h.tensor_copy(out=wa2b[:], in_=wst[:]), reads=[wstb], writes=[wa2bb])
            dma("sp", wst[:], g2_in[l][0:128, :], writes=[wstb])
            op("pool", lambda h: h.tensor_copy(out=g2ab[:], in_=wst[:]), reads=[wstb], writes=[g2abb])
            dma("sp", wst[0:32, :], g2_in[l][128:160, :], writes=[wstb])
            if l > 0:
                dma("sp", wst[32:64, :], v2_in[l - 1], writes=[wstb])
            op("pool", lambda h: h.tensor_copy(out=g2bv[0:64 if l > 0 else 32, :], in_=wst[0:64 if l > 0 else 32, :]), reads=[wstb], writes=[g2bvb])
            Hf, Hfb = f32t("Hf", [128, 8, 64]); Hb, Hbb = bft("Hb", [128, 8, 64]); HG, HGb = f32t("HG", [128, 64])
            op("pool", lambda h: h.memset(Hf[:], 0.0), writes=[Hfb]); op("pool", lambda h: h.memset(Hb[:], 0.0), writes=[Hbb])
            wa_t, wab = bft("wa_t"); g1_t, g1b = bft("g1_t"); g2_t, g2b_ = bft("g2_t", [64, 512])
            r_t, rb = bft("r_t"); k_t, kb_ = bft("k_t"); v_t, vb = bft("v_t"); vf_t, vfb = bft("vf_t")
            sg, sgb = f32t("sg"); cs, csb = f32t("cs"); ex, exb = f32t("ex"); eG, eGb = f32t("eG"); eGi, eGib = f32t("eGi"); eGx, eGxb = f32t("eGx")
            a_t, ab = f32t("a_t"); gt, gtb = f32t("gt"); v2t, v2b = f32t("v2t"); kk, kkb = f32t("kk"); sqb_, sqbb = bft("sqb"); rn, rnb = f32t("rn")
            k2, k2b = f32t("k2"); tmp, tmpb = f32t("tmp"); AR, ARb = bft("AR", [128, 2, 512]); BhT, BhTb = bft("BhT"); KhT, KhTb = bft("KhT"); vbf, vbfb = bft("vbf")
            rkb, rkbb = bft("rkb"); bon, bonb = f32t("bon"); OT, OTb = f32t("OT"); ob16, ob16b = bft("ob16"); mean, meanb = f32t("mean"); var, varb = f32t("var")
            Bh = [bft("Bh%d" % i, [128, 128]) for i in range(4)]; Kh = [bft("Kh%d" % i, [128, 128]) for i in range(4)]; Vt = [bft("Vt%d" % i, [128, 128]) for i in range(4)]
            T1, T1b = bft("T1", [128, 256]); T2, T2b = bft("T2", [128, 256]); T3, T3b = bft("T3", [128, 128])
            W1, W1b = bft("W1", [128, 128]); W2, W2b = bft("W2", [128, 256])
            PP = [bft("PP%d" % i, [128, 256]) for i in range(2)]; NTt = [bft("NT%d" % i, [128, 128]) for i in range(2)]
            Xb, Xbb = bft("Xb", [128, 64]); Ub, Ubb = bft("Ub", [128, 64]); mo, mob = bft("mo")
            tt_ = lambda e, o, a, b, opn, rd, wr: op(e, lambda h: h.tensor_tensor(out=o, in0=a, in1=b, op=opn), reads=rd, writes=wr)
            trc = [0]
            G2R = 64 if l > 0 else 32
            for tb in range(NTB):
                ts = slice(tb * 512, (tb + 1) * 512)
                dma("sp", wa_t[:], waT[:, ts], reads=[db("waT")], writes=[wab]); dma("sp", g1_t[:], g1T[:, ts], reads=[db("g1T")], writes=[g1b])
                dma("sp", g2_t[0:G2R, :], g2T[0:G2R, ts], reads=[db("g2T")], writes=[g2b_])
                for c in range(8):
                    cs_ = slice(c * 128, (c + 1) * 128)
                    dma("sp", r_t[:], rT[cs_, ts], reads=[db("rT", c)], writes=[rb]); dma("sp", k_t[:], kT[cs_, ts], reads=[db("kT", c)], writes=[kb_])
                    dma("sp", v_t[:], vT[cs_, ts], reads=[db("vT", c)], writes=[vb])
                    op("pe", lambda h: h.matmul(PS[0][:], lhsT=wa2b[0:64, cs_], rhs=wa_t[0:64, :], start=True, stop=True), reads=[wa2bb, wab], writes=[PSB[0]])
                    op("act", lambda h: h.activation(out=sg[:], in_=PS[0][:], func=AF.Sigmoid, bias=cvc(l, "w0", c)), reads=[PSB[0], cv_b], writes=[sgb])
                    op("dve", lambda h: h.tensor_tensor_scan(out=cs[:], data0=scanm[:], data1=sg[:], initial=0.0, op0=ALU.mult, op1=ALU.add), reads=[scb, sgb], writes=[csb])
                    tt_("dve", ex[:], cs[:], sg[:], ALU.subtract, [csb, sgb], [exb])
                    op("act", lambda h: h.activation(out=eG[:], in_=cs[:], func=AF.Exp, scale=-C0), reads=[csb], writes=[eGb])
                    op("act", lambda h: h.activation(out=eGi[:], in_=cs[:], func=AF.Exp, scale=C0), reads=[csb], writes=[eGib])
                    op("act", lambda h: h.activation(out=eGx[:], in_=ex[:], func=AF.Exp, scale=-C0), reads=[exb], writes=[eGxb])
                    op("pe", lambda h: h.matmul(PS[0][:], lhsT=wa2b[64:128, cs_], rhs=wa_t[64:128, :], start=True, stop=True), reads=[wa2bb, wab], writes=[PSB[0]])
                    op("act", lambda h: h.activation(out=a_t[:], in_=PS[0][:], func=AF.Sigmoid, bias=cvc(l, "a0", c)), reads=[PSB[0], cv_b], writes=[ab])
                    op("pe", lambda h: h.matmul(PS[0][:], lhsT=g2ab[:, cs_], rhs=g1_t[:], start=True, stop=False), reads=[g2abb, g1b], writes=[PSB[0]], inc=False)
                    op("pe", lambda h: h.matmul(PS[0][:], lhsT=g2bv[0:32, cs_], rhs=g2_t[0:32, :], start=False, stop=True), reads=[g2bvb, g2b_], writes=[PSB[0]])
                    op("act", lambda h: h.copy(out=gt[:], in_=PS[0][:]), reads=[PSB[0]], writes=[gtb])
                    if l > 0:
                        dma("sp", vf_t[:], vfT[cs_, ts], reads=[db("vfT", c)], writes=[vfb])
                        op("pe", lambda h: h.matmul(PS[0][:], lhsT=g2bv[32:64, cs_], rhs=g2_t[32:64, :], start=True, stop=True), reads=[g2bvb, g2b_], writes=[PSB[0]])
                        op("act", lambda h: h.activation(out=tmp[:], in_=PS[0][:], func=AF.Sigmoid, bias=cvc(l, "v0", c)), reads=[PSB[0], cv_b], writes=[tmpb])
                        tt_("dve", v2t[:], vf_t[:], v_t[:], ALU.subtract, [vfb, vb], [v2b])
                        tt_("dve", v2t[:], v2t[:], tmp[:], ALU.mult, [v2b, tmpb], [v2b])
                        tt_("dve", v2t[:], v2t[:], v_t[:], ALU.add, [v2b, vb], [v2b])
                    else:
                        op("dve", lambda h: h.tensor_copy(out=v2t[:], in_=v_t[:]), reads=[vb], writes=[v2b])
                    op("pool", lambda h: h.tensor_copy(out=vbf[:], in_=v2t[:]), reads=[v2b], writes=[vbfb])
                    op("dve", lambda h: h.tensor_scalar(out=kk[:], in0=k_t[:], scalar1=cvc(l, "k_k", c), scalar2=None, op0=ALU.mult), reads=[kb_, cv_b], writes=[kkb])
                    tt_("pool", sqb_[:], kk[:], kk[:], ALU.mult, [kkb], [sqbb])
                    op("pe", lambda h: h.matmul(PS[0][:], lhsT=bonesb[:], rhs=sqb_[:], start=True, stop=True), reads=[cb_b, sqbb], writes=[PSB[0]])
                    op("act", lambda h: h.activation(out=rn[:], in_=PS[0][:], func=AF.Sqrt, bias=1e-20), reads=[PSB[0]], writes=[rnb])
                    op("dve", lambda h: h.reciprocal(out=rn[:], in_=rn[:]), reads=[rnb], writes=[rnb])
                    tt_("dve", kk[:], kk[:], rn[:], ALU.mult, [kkb, rnb], [kkb])
                    op("dve", lambda h: h.tensor_scalar(out=tmp[:], in0=a_t[:], scalar1=cvc(l, "k_a", c), scalar2=dvc(l, "omka", c), op0=ALU.mult, op1=ALU.add),
                       reads=[ab, cv_b, dv_b], writes=[tmpb])
                    tt_("dve", k2[:], k_t[:], tmp[:], ALU.mult, [kb_, tmpb], [k2b])
                    op("dve", lambda h: h.scalar_tensor_tensor(out=AR[:, 0, :], in0=kk[:], scalar=-1.0, in1=eGx[:], op0=ALU.mult, op1=ALU.mult), reads=[kkb, eGxb], writes=[ARb])
                    tt_("pool", AR[:, 1, :], r_t[:], eG[:], ALU.mult, [rb, eGb], [ARb])
                    tt_("dve", tmp[:], kk[:], a_t[:], ALU.mult, [kkb, ab], [tmpb])
                    tt_("dve", BhT[:], tmp[:], eGi[:], ALU.mult, [tmpb, eGib], [BhTb])
                    tt_("pool", KhT[:], k2[:], eGi[:], ALU.mult, [k2b, eGib], [KhTb])
                    op("dve", lambda h: h.scalar_tensor_tensor(out=rkb[:], in0=r_t[:], scalar=cvc(l, "r_k", c), in1=k2[:], op0=ALU.mult, op1=ALU.mult), reads=[rb, k2b, cv_b], writes=[rkbb])
                    op("pe", lambda h: h.matmul(PS[0][:], lhsT=bonesb[:], rhs=rkb[:], start=True, stop=True), reads=[cb_b, rkbb], writes=[PSB[0]])
                    tt_("dve", bon[:], PS[0][:], v2t[:], ALU.mult, [PSB[0], v2b], [bonb])
                    for n in range(4):
                        tsl = slice(n * 128, (n + 1) * 128)
                        for src, srcb, dst in ((BhT, BhTb, Bh[n]), (KhT, KhTb, Kh[n]), (vbf, vbfb, Vt[n])):
                            ri = trc[0] % 4
                            trc[0] += 1
                            op("pe", lambda h, src=src, ri=ri: h.transpose(out=PSH[:, ri * 128:(ri + 1) * 128], in_=src[:, tsl], identity=identb[:]), reads=[srcb, cb_b], writes=[PSH_b[ri]])
                            op("act" if ri % 2 else "dve", lambda h, dst=dst, ri=ri: (h.copy if ri % 2 else h.tensor_copy)(out=dst[0][:], in_=PSH[:, ri * 128:(ri + 1) * 128]),
                               reads=[PSH_b[ri]], writes=[dst[1]])
                    for n in range(4):
                        tsl = slice(n * 128, (n + 1) * 128)
                        for hh in range(2):
                            pb = 64 * hh
                            P_ = slice(pb, pb + 64)
                            mm = lambda out, lhsT, rhs, rd, wr, st=True, sp=True, inc=True: op("pe", lambda h: h.matmul(out, lhsT=lhsT, rhs=rhs, start=st, stop=sp), reads=rd, writes=wr, inc=inc)
                            mm(PS[1][:, 0:256], BhT[P_, tsl], AR[P_, :, tsl], [BhTb, ARb], [PSB[1]])
                            mm(PS[2][:, 0:256], KhT[P_, tsl], AR[P_, :, tsl], [KhTb, ARb], [PSB[2]])
                            mm(PS[3][:, 0:128], AR[P_, 0, tsl], BhT[P_, tsl], [ARb, BhTb], [PSB[3]])
                            op("act", lambda h: h.copy(out=T1[:], in_=PS[1][:, 0:256]), reads=[PSB[1]], writes=[T1b])
                            op("dve", lambda h: h.tensor_copy(out=T2[:], in_=PS[2][:, 0:256]), reads=[PSB[2]], writes=[T2b])
                            op("act", lambda h: h.copy(out=T3[:], in_=PS[3][:, 0:128]), reads=[PSB[3]], writes=[T3b])
                            p0, p0b = PP[0]
                            tt_("pool", p0[:, 0:128], T3[:], mslb[:], ALU.mult, [T3b, mslbb], [p0b])
                            tt_("pool", p0[:, 128:256], T1[:, 0:128], m2b[:, 0:128], ALU.mult, [T1b, m2bb], [p0b])
                            tt_("pool", W1[:], T1[:, 128:256], m2b[:, 128:256], ALU.mult, [T1b, m2bb], [W1b])
                            tt_("pool", W2[:], T2[:], m2b[:], ALU.mult, [T2b, m2bb], [W2b])
                            d0, d0b = DG[0]
                            tt_("pool", d0[:, 0:128], T3[:], hm[:, 0, 0:128], ALU.mult, [T3b, hmb], [d0b])
                            tt_("pool", d0[:, 128:256], T1[:, 0:128], hm[:, 0, 128:256], ALU.mult, [T1b, hmb], [d0b])
                            tt_("pool", d0[:, 0:128], d0[:, 0:128], identb[:], ALU.add, [d0b, cb_b], [d0b])
                            tt_("pool", d0[:, 128:256], d0[:, 128:256], identb[:], ALU.add, [d0b, cb_b], [d0b])
                            for k in range(1, 7):
                                dc, dcb = DG[(k - 1) % 2]
                                dn, dnb = DG[k % 2]
                                mm(PS[1][:, 0:128], p0[:, 128:256], dc[:, 0:128], [p0b, dcb], [PSB[1]], inc=False)
                                mm(PS[1][:, 128:256], p0[:, 0:128], dc[:, 128:256], [p0b, dcb], [PSB[1]])
                                op("act" if k % 2 else "dve", lambda h, k=k: (h.copy if k % 2 else h.tensor_copy)(out=ZZ[:], in_=PS[1][:, 0:256]), reads=[PSB[1]], writes=[ZZb])
                                mm(PS[4][:, 0:128], dc[:, 128:256], ZZ[:, 0:128], [dcb, ZZb], [PSB[4]], inc=False)
                                mm(PS[4][:, 128:256], dc[:, 0:128], ZZ[:, 128:256], [dcb, ZZb], [PSB[4]])
                                tt_("dve", tW[:], PS[4][:, 0:256], hm[:, k, :], ALU.mult, [PSB[4], hmb], [tWb])
                                tt_("pool", dn[:], dc[:], tW[:], ALU.add, [dcb, tWb], [dnb])
                            ntf, ntfb = DG[0][0][:, 128:256], DG[0][1]
                            vt, vtb = Vt[n]
                            mm(PS[5][:, 0:64], AR[P_, 0, tsl], Hb[P_, c, :], [ARb, Hbb], [PSB[5]], True, False, False)
                            mm(PS[5][:, 0:64], W2[:, 0:128], vt[:, P_], [W2b, vtb], [PSB[5]], False, True)
                            op("act", lambda h: h.copy(out=Xb[:], in_=PS[5][:, 0:64]), reads=[PSB[5]], writes=[Xbb])
                            mm(PS[5][:, 64:128], ntf, Xb[:], [ntfb, Xbb], [PSB[5]])
                            op("dve", lambda h: h.tensor_copy(out=Ub[:], in_=PS[5][:, 64:128]), reads=[PSB[5]], writes=[Ubb])
                            mm(PS[6][P_, 0:128], Hb[P_, c, :], AR[P_, 1, tsl], [Hbb, ARb], [PSB[6]], True, False, False)
                            mm(PS[6][P_, 0:128], Ub[:], W1[:], [Ubb, W1b], [PSB[6]], False, False, False)
                            mm(PS[6][P_, 0:128], vt[:, P_], W2[:, 128:256], [vtb, W2b], [PSB[6]], False, True)
                            op("act", lambda h: h.copy(out=OT[P_, tsl], in_=PS[6][P_, 0:128]), reads=[PSB[6]], writes=[OTb])
                            mm(PS[3][P_, 128:192], Bh[n][0][:, P_], Ub[:], [Bh[n][1], Ubb], [PSB[3]], True, False, False)
                            mm(PS[3][P_, 128:192], Kh[n][0][:, P_], vt[:, P_], [Kh[n][1], vtb], [PSB[3]], False, True)
                            gcol = eG[P_, n * 128 + 127:n * 128 + 128]
                            op("dve", lambda h: h.tensor_scalar(out=HG[P_, :], in0=Hf[P_, c, :], scalar1=gcol, scalar2=None, op0=ALU.mult), reads=[Hfb, eGb], writes=[HGb])
                            op("dve", lambda h: h.scalar_tensor_tensor(out=Hf[P_, c, :], in0=PS[3][P_, 128:192], scalar=gcol, in1=HG[P_, :], op0=ALU.mult, op1=ALU.add),
                               reads=[PSB[3], eGb, HGb], writes=[Hfb])
                            op("act", lambda h: h.copy(out=Hb[P_, c, :], in_=Hf[P_, c, :]), reads=[Hfb], writes=[Hbb])
                    op("pool", lambda h: h.tensor_copy(out=ob16[:], in_=OT[:]), reads=[OTb], writes=[ob16b])
                    op("pe", lambda h: h.matmul(PS[0][:], lhsT=bonesb[:], rhs=ob16[:], start=True, stop=True), reads=[cb_b, ob16b], writes=[PSB[0]])
                    op("dve", lambda h: h.tensor_scalar(out=mean[:], in0=PS[0][:], scalar1=1.0 / 64, scalar2=None, op0=ALU.mult), reads=[PSB[0]], writes=[meanb])
                    tt_("dve", OT[:], OT[:], mean[:], ALU.subtract, [OTb, meanb], [OTb])
                    op("act", lambda h: h.activation(out=sqb_[:], in_=OT[:], func=AF.Square), reads=[OTb], writes=[sqbb])
                    op("pe", lambda h: h.matmul(PS[0][:], lhsT=bonesb[:], rhs=sqb_[:], start=True, stop=True), reads=[cb_b, sqbb], writes=[PSB[0]])
                    op("act", lambda h: h.activation(out=var[:], in_=PS[0][:], func=AF.Sqrt, scale=1.0 / 64, bias=GN_EPS), reads=[PSB[0]], writes=[varb])
                    op("dve", lambda h: h.reciprocal(out=var[:], in_=var[:]), reads=[varb], writes=[varb])
                    tt_("dve", OT[:], OT[:], var[:], ALU.mult, [OTb, varb], [OTb])
                    op("act", lambda h: h.activation(out=OT[:], in_=OT[:], func=AF.Identity, scale=cvc(l, "gn_w", c), bias=cvc(l, "gn_b", c)), reads=[OTb, cv_b], writes=[OTb])
                    tt_("dve", OT[:], OT[:], bon[:], ALU.add, [OTb, bonb], [OTb])
                    tt_("dve", mo[:], OT[:], gt[:], ALU.mult, [OTb, gtb], [mob])
                    dma("sp", mixT[cs_, ts], mo[:], reads=[mob], writes=[db("mixT", c)])

        def lockstep(gens):
            gens = list(gens)
            while gens:
                nxt = []
                for g in gens:
                    try:
                        next(g)
                        nxt.append(g)
                    except StopIteration:
                        pass
                gens = nxt

        for l in range(NL if 'nolayers' not in parts else 0):
            kb.new_epoch()
            if 'mix' in parts:
                with scope() as es:
                    hT = sb(es, "hT", [128, KC, S], BF16)
                    hTb = [Buf() for _ in range(NTB)]
                    with scope() as es2:
                        norm_to_hT(es2, l, "g_pre", hT, hTb)
                    with scope() as es2:
                        W = w_in[l]
                        R = mk_rings(es2)
                        tasks = []
                        for c in range(8):
                            tasks.append(([(W[:, c * 128:(c + 1) * 128], 128)], 128,
                                          shift_epi(R, "er%d" % (c % 2), 128, cvc(l, "mu_r", c), dvc(l, "om_r", c), [(rT[c * 128:(c + 1) * 128], db("rT", c))])))
                        for c in range(8):
                            tasks.append(([(W[:, 1088 + c * 128:1088 + (c + 1) * 128], 128)], 128,
                                          shift_epi(R, "ek%d" % (c % 2), 128, cvc(l, "mu_k", c), dvc(l, "om_k", c), [(kT[c * 128:(c + 1) * 128], db("kT", c))])))
                        for c in range(8):
                            dsts = [(vT[c * 128:(c + 1) * 128], db("vT", c))]
                            if l == 0:
                                dsts.append((vfT[c * 128:(c + 1) * 128], db("vfT", c)))
                            tasks.append(([(W[:, 2112 + c * 128:2112 + (c + 1) * 128], 128)], 128,
                                          shift_epi(R, "ev%d" % (c % 2), 128, cvc(l, "mu_v", c), dvc(l, "om_v", c), dsts)))
                        tasks.append(([(W[:, 1024:1088], 64), (W[:, 3136:3200], 64)], 128,
                                      shift_epi(R, "ewa", 128, cvc(l, "mu_wa"), dvc(l, "om_wa"), [(waT, db("waT"))], act=[(0, 64, AF.Tanh), (64, 128, None)])))
                        tasks.append(([(W[:, 3200:3328], 128)], 128,
                                      shift_epi(R, "eg1", 128, cvc(l, "mu_g1"), dvc(l, "om_g1"), [(g1T, db("g1T"))], act=[(0, 128, AF.Sigmoid)])))
                        if l == 0:
                            tasks.append(([(W[:, 3328:3360], 32)], 32,
                                          shift_epi(R, "eg2", 32, cvc(l, "mu_g2", 0, 1, 0, 32), dvc(l, "om_g2", 0, 1, 0, 32), [(g2T[0:32], db("g2T"))], act=[(0, 32, AF.Sigmoid)])))
                        else:
                            tasks.append(([(W[:, 3328:3360], 32), (w_mv[l - 1], 32)], 64,
                                          shift_epi(R, "eg2", 64, cvc(l, "mu_g2", 0, 1, 0, 64), dvc(l, "om_g2", 0, 1, 0, 64), [(g2T, db("g2T"))],
                                                    act=[(0, 32, AF.Sigmoid), (32, 64, None)])))
                        for c in range(8):
                            tasks.append(([(W[:, 3360 + c * 128:3360 + (c + 1) * 128], 128)], 128, plain_epi(R, "eq%d" % (c % 2), qT[c * 128:(c + 1) * 128], db("qT", c), 0)))
                        for c in range(8):
                            tasks.append(([(W[:, 4384 + c * 128:4384 + (c + 1) * 128], 128)], 128, plain_epi(R, "ekd%d" % (c % 2), kdT[c * 128:(c + 1) * 128], db("kdT", c), 1)))
                        proj_fm(es2, hT, hTb, tasks)
                    with scope() as es2:
                        st = Ring(es2, nc, "vst", 2, [128, KC, 128], F32)
                        wbr = Ring(es2, nc, "vwb", 2, [128, KC, 128], BF16)
                        orr = Ring(es2, nc, "vo", 2, [128, 128], BF16)
                        for c in range(8):
                            s_, sb_ = st.next()
                            dma("sp", s_[:], w_in[l][:, 5408 + c * 128:5408 + (c + 1) * 128].rearrange("(kc p) n -> p kc n", p=128), writes=[sb_])
                            w_, wb_ = wbr.next()
                            op("pool", lambda h, s_=s_, w_=w_: h.tensor_copy(out=w_[:], in_=s_[:]), reads=[sb_], writes=[wb_])
                            for tt in range(NT):
                                pi = tt % 4
                                for kc in range(KC):
                                    op("pe", lambda h, w_=w_, kc=kc, pi=pi, tt=tt: h.matmul(PS[pi][:, 0:128], lhsT=hT[:, kc, tt * 128:(tt + 1) * 128], rhs=w_[:, kc, :],
                                                                                           start=(kc == 0), stop=(kc == KC - 1)),
                                       reads=[wb_, hTb[tt // 4]], writes=[PSB[pi]], inc=(kc == KC - 1))
                                o_, ob_ = orr.next()
                                op("act" if tt % 2 else "dve", lambda h, o_=o_, pi=pi, tt=tt: (h.copy if tt % 2 else h.tensor_copy)(out=o_[:], in_=PS[pi][:, 0:128]),
                                   reads=[PSB[pi]], writes=[ob_])
                                dma("sp", vd[tt * 128:(tt + 1) * 128, c * 128:(c + 1) * 128], o_[:], reads=[ob_], writes=[db("vd", c)])

                with scope() as es:
                    if 'norwkv' not in parts:
                        rwkv_phase(es, l)
                with scope() as es:
                    if 'noattn' not in parts:
                        attn_phase(es, l)
                with scope() as es:
                    proj_res(es, l, mixT, "mixT", KC, w_out[l], "g_pm")
            with scope() as es:
                hT = sb(es, "hT2", [128, KC, S], BF16)
                hTb = [Buf() for _ in range(NTB)]
                with scope() as es2:
                    norm_to_hT(es2, l, "g_pf", hT, hTb)
                with scope() as es2:
                    tr = Ring(es2, nc, "ft", 2, [128, 512], F32)
                    gr = Ring(es2, nc, "fg", 2, [128, 512], F32)
                    orr = Ring(es2, nc, "fo", 2, [128, 512], BF16)
                    carr = [(sb(es2, "fc%d" % i, [128, 2]), Buf()) for i in range(2)]

                    def ffn_epi(c):
                        car, carb = carr[c % 2]

                        def epi(tb, pss):
                            pg, pu = pss
                            ts = slice(tb * 512, (tb + 1) * 512)
                            t_, tb_ = tr.next()
                            g_, gb_ = gr.next()
                            o_, ob_ = orr.next()
                            op("act", lambda h: h.activation(out=t_[:], in_=PS[pg][:], func=AF.Identity, scale=cvc(l, "cw2", c), bias=cvc(l, "cb", c)),
                               reads=[PSB[pg], cv_b], writes=[tb_])
                            op("dve", lambda h: h.scalar_tensor_tensor(out=t_[:, 1:512], in0=PS[pg][:, 0:511], scalar=cvc(l, "cw1", c), in1=t_[:, 1:512], op0=ALU.mult, op1=ALU.add),
                               reads=[PSB[pg], tb_, cv_b], writes=[tb_])
                            op("dve", lambda h: h.scalar_tensor_tensor(out=t_[:, 2:512], in0=PS[pg][:, 0:510], scalar=cvc(l, "cw0", c), in1=t_[:, 2:512], op0=ALU.mult, op1=ALU.add),
                               reads=[PSB[pg], tb_, cv_b], writes=[tb_])
                            if tb > 0:
                                op("dve", lambda h: h.scalar_tensor_tensor(out=t_[:, 0:2], in0=car[:, 0:2], scalar=cvc(l, "cw0", c), in1=t_[:, 0:2], op0=ALU.mult, op1=ALU.add),
                                   reads=[carb, tb_, cv_b], writes=[tb_])
                                op("dve", lambda h: h.scalar_tensor_tensor(out=t_[:, 0:1], in0=car[:, 1:2], scalar=cvc(l, "cw1", c), in1=t_[:, 0:1], op0=ALU.mult, op1=ALU.add),
                                   reads=[carb, tb_, cv_b], writes=[tb_])
                            op("act", lambda h: h.copy(out=car[:, 0:2], in_=PS[pg][:, 510:512]), reads=[PSB[pg]], writes=[carb])
                            op("act", lambda h: h.activation(out=g_[:], in_=t_[:], func=AF.Gelu_apprx_tanh), reads=[tb_], writes=[gb_])
                            op("dve", lambda h: h.tensor_tensor(out=o_[:], in0=PS[pu][:], in1=g_[:], op=ALU.mult), reads=[PSB[pu], gb_], writes=[ob_])
                            dma("sp", actT[c * 128:(c + 1) * 128, ts], o_[:], reads=[ob_], writes=[db("actT", c)])
                        return epi
                    tasks = []
                    for c in range(FC):
                        e = ffn_epi(c)
                        tasks.append(([(w_up[l][:, c * 128:(c + 1) * 128], 128)], 128, e))
                        tasks.append(([(w_up[l][:, FF + c * 128:FF + (c + 1) * 128], 128)], 128, e))
                    if 'noproj' not in parts:
                        proj_fm(es2, hT, hTb, tasks, nper=2)
            with scope() as es:
                if 'nores' not in parts:
                    proj_res(es, l, actT, "actT", FC, w_down[l], "g_ff")

        with scope() as es:
            xr = Ring(es, nc, "oxi", 2, [128, KC, 128], F32)
            xo = Ring(es, nc, "oxo", 2, [128, D], F32)
            for tt in range(NT):
                xi, xib = xr.next()
                dma("sp", xi[:], xT[:, tt * 128:(tt + 1) * 128].rearrange("(kc p) t -> p kc t", p=128), reads=[db("xT", kc) for kc in range(KC)], writes=[xib])
                o_, ob_ = xo.next()
                for kc in range(KC):
                    pi = kc % 4
                    op("pe", lambda h, kc=kc, pi=pi: h.transpose(out=PS[pi][:, 0:128], in_=xi[:, kc, :], identity=cn[:, 0:128]), reads=[xib, cn_b], writes=[PSB[pi]])
                    op("act" if kc % 2 else "dve", lambda h, kc=kc, pi=pi: (h.copy if kc % 2 else h.tensor_copy)(out=o_[:, kc * 128:(kc + 1) * 128], in_=PS[pi][:, 0:128]),
                       reads=[PSB[pi]], writes=[ob_])
                dma("sp", y_out[tt * 128:(tt + 1) * 128, :], o_[:], reads=[ob_], writes=[db("yout")])
        kb.finish()
    return nc


def pack_inputs(inp, S, NL):
    f = np.float32
    cv = np.zeros((128, NL, NCV), f)

    def put(l, name, vec):
        vec = np.asarray(vec, f).reshape(-1)
        n = (vec.size + 127) // 128
        pad = np.zeros(n * 128, f)
        pad[:vec.size] = vec
        cv[:, l, CV[name]:CV[name] + n] = pad.reshape(n, 128).T
    for l in range(NL):
        put(l, "g_pre", inp["pre_mix_norm"][l]); put(l, "g_pm", inp["post_mix_norm"][l])
        put(l, "g_pf", inp["pre_ffn_norm"][l]); put(l, "g_ff", inp["post_ffn_norm"][l])
        mu = np.asarray(inp["shift_mu"][l], f)
        put(l, "mu_r", mu[0:1024]); put(l, "mu_k", mu[1088:2112]); put(l, "mu_v", mu[2112:3136])
        put(l, "mu_wa", np.concatenate([mu[1024:1088], mu[3136:3200]]))
        put(l, "mu_g1", mu[3200:3328])
        g2 = np.zeros(128, f)
        g2[0:32] = mu[3328:3360]
        if l > 0:
            g2[32:64] = np.asarray(inp["shift_mu_mv"][l - 1], f)
        put(l, "mu_g2", g2)
        for nm in ("w0", "a0", "k_k", "k_a", "r_k", "gn_w", "gn_b"):
            put(l, nm, inp[nm][l])
        if l > 0:
            put(l, "v0", inp["v0"][l - 1])
        put(l, "subln", inp["subln_w"][l])
        cw = np.asarray(inp["conv_w"][l], f)
        put(l, "cw0", cw[0]); put(l, "cw1", cw[1]); put(l, "cw2", cw[2]); put(l, "cb", inp["conv_b"][l])
        for nm, k in (("lq1", "lam_q1"), ("lk1", "lam_k1"), ("lq2", "lam_q2"), ("lk2", "lam_k2")):
            cv[:, l, CV[nm]:CV[nm] + 64] = np.asarray(inp[k][l], f)[None, :]
    shared = {
        "cv": np.ascontiguousarray(cv.reshape(128, NL * NCV)),
        "cn": make_consts(),
        "w_in": np.ascontiguousarray(inp["w_in"][:NL], dtype=f),
        "w_mv": np.ascontiguousarray(inp["w_mv_down"][:max(NL - 1, 1)], dtype=f),
        "w2": np.ascontiguousarray(inp["w2"][:NL], dtype=f), "a2": np.ascontiguousarray(inp["a2"][:NL], dtype=f),
        "g2": np.ascontiguousarray(inp["g2"][:NL], dtype=f), "v2": np.ascontiguousarray(inp["v2"][:max(NL - 1, 1)], dtype=f),
        "w_out": np.ascontiguousarray(inp["w_out"][:NL], dtype=f), "w_up": np.ascontiguousarray(inp["w_up"][:NL], dtype=f),
        "w_down": np.ascontiguousarray(inp["w_down"][:NL], dtype=f),
    }
    return shared


def kernel(**inp):
    x = np.asarray(inp["x"], np.float32)
    B, S, _ = x.shape
    NL = 4
    nc = build(S, NL)
    shared = pack_inputs(inp, S, NL)
    pos = np.asarray(inp["positions"], np.int32)
    in_maps = []
    for b in range(B):
        m = dict(shared)
        m["x"] = np.ascontiguousarray(x[b])
        m["pos"] = np.ascontiguousarray(pos[b:b + 1])
        in_maps.append(m)
    res = run_bass_kernel_spmd(nc, in_maps, core_ids=list(range(B)))
    return np.stack([np.asarray(r["y"], np.float32) for r in res.results], 0)
```

```python
import math
from contextlib import ExitStack, contextmanager
import numpy as np
import concourse.bass as bass
import concourse.mybir as mybir
from concourse.bass_utils import run_bass_kernel_spmd

F32 = mybir.dt.float32
BF16 = mybir.dt.bfloat16
I32 = mybir.dt.int32
AF = mybir.ActivationFunctionType
ALU = mybir.AluOpType

D = 2048
KC = 16
FF = 5632
FC = 44
RW = 1024
RCOLS = 3360
INC = 6432
C0 = math.exp(-0.5)
NORM_EPS = 1e-6
GN_EPS = 64e-5
SUBLN_EPS = 1e-5

CV = {}
_o = 0
for _n, _w in (("g_pre", 16), ("g_pm", 16), ("g_pf", 16), ("g_ff", 16), ("mu_r", 8), ("mu_k", 8), ("mu_v", 8),
               ("mu_wa", 1), ("mu_g1", 1), ("mu_g2", 1), ("w0", 8), ("a0", 8), ("k_k", 8), ("k_a", 8), ("r_k", 8),
               ("gn_w", 8), ("gn_b", 8), ("v0", 8), ("subln", 1), ("cw0", 44), ("cw1", 44), ("cw2", 44), ("cb", 44),
               ("lq1", 64), ("lk1", 64), ("lq2", 64), ("lk2", 64)):
    CV[_n] = _o
    _o += _w
NCV = _o
DV = {}
_o = 0
for _n, _w in (("om_r", 8), ("om_k", 8), ("om_v", 8), ("om_wa", 1), ("om_g1", 1), ("om_g2", 1), ("omka", 8), ("lam", 1), ("nlam", 1), ("sub2", 1)):
    DV[_n] = _o
    _o += _w
NDV = _o

CN = {"ident": 0, "bones": 128, "ones": 256, "perm": 384, "m2": 512, "msl": 768, "invf": 896, "sign": 897, "dmask": 898, "scanm": 2946, "hm": 3458}
NCNP = 898
NCN = 3458 + 7 * 256


def make_consts():
    c = np.zeros((128, NCN), np.float32)
    p = np.arange(128)
    c[:, 0:128] = np.eye(128)
    c[:, 128:256] = (p[:, None] // 64 == p[None, :] // 64)
    c[:, 256:384] = 1.0
    part = p.copy()
    for b in (0, 64):
        for i in range(8):
            part[b + i] = b + i + 8
            part[b + 8 + i] = b + i
    perm = np.zeros((128, 128), np.float32)
    for m in range(128):
        if part[m] != m:
            perm[part[m], m] = 1.0
    c[:, 384:512] = perm
    c[:, 512:640] = (p[:, None] < p[None, :])
    c[:, 640:768] = (p[:, None] <= p[None, :])
    c[:, 768:896] = (p[None, :] < p[:, None])
    q = np.arange(512)
    for j in range(4):
        c[:, 898 + j * 512:898 + (j + 1) * 512] = ((j * 128 + p)[:, None] <= q[None, :])
    invf = np.zeros(128, np.float64)
    sign = np.zeros(128, np.float32)
    fr = 500000.0 ** (-np.arange(0, 16, 2, dtype=np.float32) / 16)
    for b in (0, 64):
        for i in range(8):
            invf[b + i] = fr[i]
            invf[b + 8 + i] = fr[i]
            sign[b + i] = -1.0
            sign[b + 8 + i] = 1.0
    c[:, 896] = invf.astype(np.float32)
    c[:, 897] = sign
    sm = np.ones(512, np.float32)
    sm[::128] = 0.0
    c[:, 2946:2946 + 512] = sm[None, :]
    for k in range(7):
        t = p[:, None]
        s_ = p[None, :]
        mk = ((t >> (k + 1)) == (s_ >> (k + 1))) & (((t >> k) & 1) == 1) & (((s_ >> k) & 1) == 0)
        c[:, 3458 + k * 256:3458 + k * 256 + 128] = mk
        c[:, 3458 + k * 256 + 128:3458 + (k + 1) * 256] = mk.T
    return c


class Buf:
    __slots__ = ("w", "r")

    def __init__(self):
        self.w = {}
        self.r = {}


class Eng:
    def __init__(self, name, h, sem):
        self.name, self.h, self.sem, self.cnt, self.seen = name, h, sem, 0, {}


class KB:
    def __init__(self, nc, es, n_epochs=1):
        self.nc = nc
        self.E = {}
        self.sems = {}
        self.keyeng = {}
        self.dead = set()
        self.pool_sems = {}
        hs = (("pe", nc.tensor), ("act", nc.scalar), ("dve", nc.vector), ("pool", nc.gpsimd), ("sp", nc.sync))
        for name, h in hs:
            self.pool_sems[name] = [es.enter_context(nc.semaphore("s_%s%d" % (name, i))) for i in range(n_epochs if name != "sp" else 1)]
            e = Eng(name, h, self.pool_sems[name][0])
            e.key = name + "#0"
            e.epoch = 0
            self.E[name] = e
            self.sems[e.key] = (e.sem, 1)
            self.keyeng[e.key] = e
        self.slots = {}
        for q, n in (("sp", 10),):
            sl = []
            for i in range(n):
                key = "d%s%d" % (q, i)
                sem = es.enter_context(nc.semaphore("s_" + key))
                self.sems[key] = (sem, 16)
                sl.append([key, 0])
            self.slots[q] = [sl, 0]

    def new_epoch(self):
        self.barrier()
        for name in ("pe", "act", "dve", "pool"):
            e = self.E[name]
            if e.epoch + 1 >= len(self.pool_sems[name]):
                continue
            self.dead.add(e.key)
            e.epoch += 1
            e.sem = self.pool_sems[name][e.epoch]
            e.cnt = 0
            e.key = "%s#%d" % (name, e.epoch)
            self.sems[e.key] = (e.sem, 1)
            self.keyeng[e.key] = e

    def _need(self, e, key, n, raw):
        if key in self.dead:
            return
        if key == e.key and (not raw) and e.name == "pe":
            return
        if e.seen.get(key, 0) >= n:
            return
        sem, unit = self.sems[key]
        if key in self.keyeng and key != e.key:
            assert n <= self.keyeng[key].cnt, ("pending ticket", key, n)
        e.h.wait_ge(sem, n * unit)
        e.seen[key] = n

    def _deps(self, e, reads, writes):
        for b in reads:
            for k, n in b.w.items():
                self._need(e, k, n, True)
        for b in writes:
            for k, n in b.w.items():
                self._need(e, k, n, False)
            for k, n in b.r.items():
                self._need(e, k, n, False)

    def op(self, eng, fn, reads=(), writes=(), inc=True):
        e = self.E[eng]
        self._deps(e, reads, writes)
        ins = fn(e.h)
        if inc:
            ins.then_inc(e.sem, 1)
            e.cnt += 1
            t = e.cnt
        else:
            t = e.cnt + 1
        for b in writes:
            b.w = {e.key: t}
            b.r = {}
        for b in reads:
            b.r[e.key] = t
        return ins

    def dma(self, q, out, in_, reads=(), writes=()):
        e = self.E[q]
        self._deps(e, reads, writes)
        sl, idx = self.slots[q]
        key, cnt = sl[idx]
        self.slots[q][1] = (idx + 1) % len(sl)
        if cnt > 0:
            self._need(e, key, cnt, True)
        sem, unit = self.sems[key]
        e.h.dma_start(out=out, in_=in_).then_inc(sem, 16)
        sl[idx][1] = cnt + 1
        for b in writes:
            b.w = {key: cnt + 1}
            b.r = {}
        for b in reads:
            b.r[key] = cnt + 1

    def barrier(self):
        for en in ("pe", "act", "dve", "pool", "sp"):
            e = self.E[en]
            for k2 in ("pe", "act", "dve", "pool"):
                o = self.E[k2]
                if k2 != en and o.cnt > 0:
                    self._need(e, o.key, o.cnt, True)
            for q in self.slots:
                for key, cnt in self.slots[q][0]:
                    if cnt > 0:
                        self._need(e, key, cnt, True)

    def finish(self):
        e = self.E["sp"]
        for q in self.slots:
            for key, cnt in self.slots[q][0]:
                if cnt > 0:
                    self._need(e, key, cnt, True)


_UID = [0]


class Ring:
    def __init__(self, es, nc, name, n, shape, dt, psum=False):
        self.t = []
        for i in range(n):
            _UID[0] += 1
            t = es.enter_context((nc.psum_tensor if psum else nc.sbuf_tensor)("rg_%s_%d" % (name, _UID[0]), shape, dt))
            self.t.append((t, Buf()))
        self.i = 0

    def next(self):
        r = self.t[self.i]
        self.i = (self.i + 1) % len(self.t)
        return r


def build(S, NL, dbg=False, parts=('mix', 'ffn')):
    nc = bass.Bass("TRN2", target_bir_lowering=False)
    NTB = S // 512
    NT = S // 128

    def din(name, shape, dt=F32):
        return nc.dram_tensor(name, shape, dt, kind="ExternalInput").ap()

    x_in = din("x", [S, D])
    pos_in = din("pos", [1, S], I32)
    cv_in = din("cv", [128, NL * NCV])
    cn_in = din("cn", [128, NCN])
    w_in = din("w_in", [NL, D, INC])
    w_mv = din("w_mv", [max(NL - 1, 1), D, 32])
    w2_in = din("w2", [NL, 64, RW])
    a2_in = din("a2", [NL, 64, RW])
    g2_in = din("g2", [NL, 160, RW])
    v2_in = din("v2", [max(NL - 1, 1), 32, RW])
    w_out = din("w_out", [NL, D, D])
    w_up = din("w_up", [NL, D, 2 * FF])
    w_down = din("w_down", [NL, FF, D])
    y_out = nc.dram_tensor("y", [S, D], F32, kind="ExternalOutput").ap()

    kindS = "ExternalOutput" if dbg else "Internal"

    def dsc(name, shape, dt):
        return nc.dram_tensor(name, shape, dt, kind=kindS).ap()

    xT = dsc("xT", [D, S], F32)
    yT = dsc("yT", [D, S], F32)
    rT = dsc("rT", [RW, S], BF16)
    kT = dsc("kT", [RW, S], BF16)
    vT = dsc("vT", [RW, S], BF16)
    vfT = dsc("vfT", [RW, S], BF16)
    waT = dsc("waT", [128, S], BF16)
    g1T = dsc("g1T", [128, S], BF16)
    g2T = dsc("g2T", [64, S], BF16)
    qT = dsc("qT", [RW, S], BF16)
    kdT = dsc("kdT", [RW, S], BF16)
    vd = dsc("vd", [S, RW], BF16)
    mixT = dsc("mixT", [D, S], BF16)
    actT = dsc("actT", [FF, S], BF16)
    rotC = dsc("rotC", [128, S], F32)
    rotS = dsc("rotS", [128, S], F32)
    DB = {}

    def db(name, i=0):
        k = (name, i)
        if k not in DB:
            DB[k] = Buf()
        return DB[k]

    with ExitStack() as es0:
        kb = KB(nc, es0, n_epochs=NL + 1)
        op, dma = kb.op, kb.dma

        @contextmanager
        def scope():
            with ExitStack() as e_:
                yield e_
                kb.barrier()

        def sb(es, name, shape, dt=F32):
            _UID[0] += 1
            return es.enter_context(nc.sbuf_tensor("sb_%s_%d" % (name, _UID[0]), shape, dt))

        cn = sb(es0, "cn", [128, NCNP]); cn_b = Buf()
        cv = sb(es0, "cv", [128, NL * NCV]); cv_b = Buf()
        dv = sb(es0, "dv", [128, NL * NDV]); dv_b = Buf()
        identb = sb(es0, "identb", [128, 128], BF16)
        bonesb = sb(es0, "bonesb", [128, 128], BF16)
        onesb = sb(es0, "onesb", [128, 128], BF16)
        permb = sb(es0, "permb", [128, 128], BF16)
        cb_b = Buf()
        PS = [es0.enter_context(nc.psum_tensor("ps%d" % i, [128, 512], F32)) for i in range(7)]
        PSB = [Buf() for _ in range(7)]
        PSH = es0.enter_context(nc.psum_tensor("psh", [128, 1024], BF16)); PSH_b = [Buf()] * 4

        dma("sp", cn[:], cn_in[:, 0:NCNP], writes=[cn_b])
        dma("sp", cv[:], cv_in[:, :], writes=[cv_b])
        for t, o in ((identb, CN["ident"]), (bonesb, CN["bones"]), (onesb, CN["ones"]), (permb, CN["perm"])):
            op("dve", lambda h, t=t, o=o: h.tensor_copy(out=t[:], in_=cn[:, o:o + 128]), reads=[cn_b], writes=[cb_b])

        def cvc(l, name, i=0, n=1, p0=0, p1=128):
            o = l * NCV + CV[name] + i
            return cv[p0:p1, o:o + n]

        def dvc(l, name, i=0, n=1, p0=0, p1=128):
            o = l * NDV + DV[name] + i
            return dv[p0:p1, o:o + n]

        for l in range(NL):
            for a, b_, n in (("om_r", "mu_r", 8), ("om_k", "mu_k", 8), ("om_v", "mu_v", 8), ("om_wa", "mu_wa", 1),
                             ("om_g1", "mu_g1", 1), ("om_g2", "mu_g2", 1), ("omka", "k_a", 8)):
                op("dve", lambda h, l=l, a=a, b_=b_, n=n: h.tensor_scalar(out=dvc(l, a, 0, n), in0=cvc(l, b_, 0, n), scalar1=-1.0, scalar2=1.0,
                                                                        op0=ALU.mult, op1=ALU.add), reads=[cv_b], writes=[dv_b])
        with scope() as es:
            tmp = sb(es, "lamtmp", [128, 64]); tb_ = Buf()
            acc = sb(es, "lamacc", [128, 4]); ab_ = Buf()
            for l in range(NL):
                li = 0.8 - 0.6 * math.exp(-0.3 * l)
                for j, (qa, ka) in enumerate((("lq1", "lk1"), ("lq2", "lk2"))):
                    op("dve", lambda h, l=l, qa=qa, ka=ka: h.tensor_tensor(out=tmp[:], in0=cvc(l, qa, 0, 64), in1=cvc(l, ka, 0, 64), op=ALU.mult),
                       reads=[cv_b], writes=[tb_])
                    op("dve", lambda h, j=j: h.reduce_sum(out=acc[:, j:j + 1], in_=tmp[:], axis=mybir.AxisListType.X), reads=[tb_], writes=[ab_])
                op("act", lambda h: h.activation(out=acc[:, 2:4], in_=acc[:, 0:2], func=AF.Exp), reads=[ab_], writes=[ab_])
                op("dve", lambda h, l=l: h.tensor_tensor(out=dvc(l, "lam"), in0=acc[:, 2:3], in1=acc[:, 3:4], op=ALU.subtract), reads=[ab_, dv_b], writes=[dv_b])
                op("dve", lambda h, l=l, li=li: h.tensor_scalar(out=dvc(l, "nlam"), in0=dvc(l, "lam"), scalar1=float(li), scalar2=-1.0, op0=ALU.add, op1=ALU.mult),
                   reads=[dv_b], writes=[dv_b])
                op("dve", lambda h, l=l, li=li: h.tensor_scalar(out=dvc(l, "sub2"), in0=cvc(l, "subln"), scalar1=float(1.0 - li), scalar2=None, op0=ALU.mult),
                   reads=[cv_b, dv_b], writes=[dv_b])

        with scope() as es:
          if 'norot' not in parts:
              pi_ = sb(es, "posi", [128, 512], I32); pib = Buf()
              pf = sb(es, "posf", [128, 512]); pfb = Buf()
              t1 = sb(es, "rt1", [128, 512]); t1b = Buf()
              t2 = sb(es, "rt2", [128, 512]); t2b = Buf()
              ki = sb(es, "rki", [128, 512], I32); kib = Buf()
              ro = Ring(es, nc, "rto", 2, [128, 512], F32)
              TWO_PI = float(2 * np.pi)
              for tb in range(NTB):
                  ts = slice(tb * 512, (tb + 1) * 512)
                  dma("sp", pi_[:], pos_in[0:1, ts].partition_broadcast(128), writes=[pib])
                  op("dve", lambda h: h.tensor_copy(out=pf[:], in_=pi_[:]), reads=[pib], writes=[pfb])
                  op("dve", lambda h: h.tensor_scalar(out=pf[:], in0=pf[:], scalar1=cn[:, CN["invf"]:CN["invf"] + 1], scalar2=None, op0=ALU.mult),
                     reads=[pfb, cn_b], writes=[pfb])
                  for which, off, dst in (("c", float(np.pi / 2), rotC), ("s", 0.0, rotS)):
                      op("dve", lambda h, off=off: h.tensor_scalar(out=t1[:], in0=pf[:], scalar1=off, scalar2=None, op0=ALU.add), reads=[pfb], writes=[t1b])
                      op("dve", lambda h: h.tensor_scalar(out=t2[:], in0=t1[:], scalar1=float(1 / (2 * np.pi)), scalar2=None, op0=ALU.mult), reads=[t1b], writes=[t2b])
                      op("dve", lambda h: h.tensor_copy(out=ki[:], in_=t2[:]), reads=[t2b], writes=[kib])
                      op("dve", lambda h: h.tensor_copy(out=t2[:], in_=ki[:]), reads=[kib], writes=[t2b])
                      op("dve", lambda h: h.scalar_tensor_tensor(out=t1[:], in0=t2[:], scalar=-TWO_PI, in1=t1[:], op0=ALU.mult, op1=ALU.add), reads=[t2b, t1b], writes=[t1b])
                      op("dve", lambda h: h.tensor_scalar(out=t2[:], in0=t1[:], scalar1=float(np.pi), scalar2=-TWO_PI, op0=ALU.is_gt, op1=ALU.mult), reads=[t1b], writes=[t2b])
                      op("dve", lambda h: h.tensor_tensor(out=t1[:], in0=t1[:], in1=t2[:], op=ALU.add), reads=[t1b, t2b], writes=[t1b])
                      op("dve", lambda h: h.tensor_scalar(out=t2[:], in0=t1[:], scalar1=float(-np.pi), scalar2=TWO_PI, op0=ALU.is_lt, op1=ALU.mult), reads=[t1b], writes=[t2b])
                      op("dve", lambda h: h.tensor_tensor(out=t1[:], in0=t1[:], in1=t2[:], op=ALU.add), reads=[t1b, t2b], writes=[t1b])
                      o_, ob_ = ro.next()
                      op("act", lambda h, o_=o_: h.activation(out=o_[:], in_=t1[:], func=AF.Sin), reads=[t1b], writes=[ob_])
                      if which == "s":
                          op("dve", lambda h, o_=o_: h.tensor_scalar(out=o_[:], in0=o_[:], scalar1=cn[:, CN["sign"]:CN["sign"] + 1], scalar2=None, op0=ALU.mult),
                             reads=[ob_, cn_b], writes=[ob_])
                      dma("sp", dst[:, ts], o_[:], reads=[ob_], writes=[db("rot")])

        with scope() as es:
            xr = Ring(es, nc, "xin", 2, [128, D], F32)
            xo = Ring(es, nc, "xto", 2, [128, KC, 128], F32)
            for tt in range(NT):
                xi, xib = xr.next()
                dma("sp", xi[:], x_in[tt * 128:(tt + 1) * 128, :], writes=[xib])
                o_, ob_ = xo.next()
                for kc in range(KC):
                    pi = kc % 4
                    op("pe", lambda h, kc=kc, pi=pi: h.transpose(out=PS[pi][:, 0:128], in_=xi[:, kc * 128:(kc + 1) * 128], identity=cn[:, 0:128]),
                       reads=[xib, cn_b], writes=[PSB[pi]])
                    op("act" if kc % 2 else "dve", lambda h, kc=kc, pi=pi: (h.copy if kc % 2 else h.tensor_copy)(out=o_[:, kc, :], in_=PS[pi][:, 0:128]),
                       reads=[PSB[pi]], writes=[ob_])
                dma("sp", xT[:, tt * 128:(tt + 1) * 128].rearrange("(kc p) t -> p kc t", p=128), o_[:], reads=[ob_], writes=[db("xT", kc) for kc in range(KC)])

        def norm_to_hT(es, l, gname, hT, hTb):
            xs = Ring(es, nc, "nxs", 16, [128, 512], F32)
            sq = Ring(es, nc, "nsq", 2, [128, 512], BF16)
            rs = sb(es, "nrs", [128, 512]); rsb = Buf()
            for tb in range(NTB):
                ts = slice(tb * 512, (tb + 1) * 512)
                tl = []
                for kc in range(KC):
                    x_, xb_ = xs.next()
                    dma("sp", x_[:], xT[kc * 128:(kc + 1) * 128, ts], reads=[db("xT", kc)], writes=[xb_])
                    s_, sb_ = sq.next()
                    op("act", lambda h, x_=x_, s_=s_: h.activation(out=s_[:], in_=x_[:], func=AF.Square), reads=[xb_], writes=[sb_])
                    op("pe", lambda h, s_=s_, kc=kc: h.matmul(PS[6][:], lhsT=onesb[:], rhs=s_[:], start=(kc == 0), stop=(kc == KC - 1)),
                       reads=[sb_, cb_b], writes=[PSB[6]])
                    tl.append((x_, xb_))
                op("dve", lambda h: h.tensor_scalar(out=rs[:], in0=PS[6][:], scalar1=1.0 / D, scalar2=NORM_EPS, op0=ALU.mult, op1=ALU.add), reads=[PSB[6]], writes=[rsb])
                op("act", lambda h: h.activation(out=rs[:], in_=rs[:], func=AF.Sqrt), reads=[rsb], writes=[rsb])
                op("dve", lambda h: h.reciprocal(out=rs[:], in_=rs[:]), reads=[rsb], writes=[rsb])
                for kc in range(KC):
                    x_, xb_ = tl[kc]
                    op("dve", lambda h, x_=x_, kc=kc: h.scalar_tensor_tensor(out=hT[:, kc, ts], in0=x_[:], scalar=cvc(l, gname, kc), in1=rs[:],
                                                                                                   op0=ALU.mult, op1=ALU.mult),
                       reads=[xb_, rsb, cv_b], writes=[hTb[tb]])

        def proj_fm(es, hT, hTb, tasks, nper=1):
            st = Ring(es, nc, "wst", 2 if nper == 1 else 3, [128, KC, 128], F32)
            wb = Ring(es, nc, "wbf", 3 if nper == 1 else 4, [128, KC, 128], BF16)
            psr = [0]
            groups = [tasks[ti:ti + nper] for ti in range(0, len(tasks), nper)]

            def load(grp):
                wts = []
                for segs, M, epi in grp:
                    s_, sb_ = st.next()
                    o = 0
                    for ap, n in segs:
                        dma("sp", s_[:, :, o:o + n], ap.rearrange("(kc p) n -> p kc n", p=128), writes=[sb_])
                        o += n
                    w_, wb_ = wb.next()
                    op("pool", lambda h, s_=s_, w_=w_, M=M: h.tensor_copy(out=w_[:, :, 0:M], in_=s_[:, :, 0:M]), reads=[sb_], writes=[wb_])
                    wts.append((w_, wb_, M))
                return wts
            nxt = load(groups[0])
            for gi, grp in enumerate(groups):
                wts = nxt
                if gi + 1 < len(groups):
                    nxt = load(groups[gi + 1])
                for tb in range(NTB):
                    ts = slice(tb * 512, (tb + 1) * 512)
                    pss = []
                    for w_, wb_, M in wts:
                        pi = psr[0] % 4
                        psr[0] += 1
                        for kc in range(KC):
                            op("pe", lambda h, w_=w_, M=M, kc=kc, pi=pi: h.matmul(PS[pi][0:M, :], lhsT=w_[:, kc, 0:M], rhs=hT[:, kc, ts], start=(kc == 0), stop=(kc == KC - 1)),
                               reads=[wb_, hTb[tb]], writes=[PSB[pi]], inc=(kc == KC - 1))
                        pss.append(pi)
                    grp[0][2](tb, pss)

        def mk_rings(es):
            return {"t": Ring(es, nc, "ept", 2, [128, 512], F32), "of": Ring(es, nc, "epof", 2, [128, 512], F32),
                    "ob": Ring(es, nc, "epob", 3, [128, 512], BF16), "car": sb(es, "epcar", [128, 64]), "ncar": [0]}

        def shift_epi(R, name, M, mu_ap, om_ap, dests, act=None):
            tr = R["t"]
            orr = R["of"] if act else R["ob"]
            obr = R["ob"] if act else None
            ci = R["ncar"][0]
            R["ncar"][0] += 1
            carry = R["car"][:, ci:ci + 1]; cb = Buf()

            def epi(tb, pss):
                pi = pss[0]
                ts = slice(tb * 512, (tb + 1) * 512)
                t_, tb_ = tr.next()
                o_, ob_ = orr.next()
                op("act", lambda h: h.activation(out=t_[0:M, :], in_=PS[pi][0:M, :], func=AF.Identity, scale=om_ap), reads=[PSB[pi], dv_b], writes=[tb_])
                op("dve", lambda h: h.scalar_tensor_tensor(out=o_[0:M, 1:512], in0=PS[pi][0:M, 0:511], scalar=mu_ap, in1=t_[0:M, 1:512], op0=ALU.mult, op1=ALU.add),
                   reads=[PSB[pi], tb_, cv_b], writes=[ob_])
                if tb == 0:
                    op("dve", lambda h: h.tensor_copy(out=o_[0:M, 0:1], in_=t_[0:M, 0:1]), reads=[tb_], writes=[ob_])
                else:
                    op("dve", lambda h: h.scalar_tensor_tensor(out=o_[0:M, 0:1], in0=carry[0:M, :], scalar=mu_ap, in1=t_[0:M, 0:1], op0=ALU.mult, op1=ALU.add),
                       reads=[cb, tb_, cv_b], writes=[ob_])
                op("act", lambda h: h.copy(out=carry[0:M, :], in_=PS[pi][0:M, 511:512]), reads=[PSB[pi]], writes=[cb])
                if act:
                    f_, fb_ = obr.next()
                    for p0, p1, fn in act:
                        if fn is None:
                            op("dve", lambda h, p0=p0, p1=p1: h.tensor_copy(out=f_[p0:p1, :], in_=o_[p0:p1, :]), reads=[ob_], writes=[fb_])
                        else:
                            op("act", lambda h, p0=p0, p1=p1, fn=fn: h.activation(out=f_[p0:p1, :], in_=o_[p0:p1, :], func=fn), reads=[ob_], writes=[fb_])
                    o_, ob_ = f_, fb_
                for dap, dbuf in dests:
                    dma("sp", dap[:, ts], o_[0:M, :], reads=[ob_], writes=[dbuf])
            return epi

        def plain_epi(R, name, dest, dbuf, flip):
            orr = R["ob"]

            def epi(tb, pss):
                pi = pss[0]
                o_, ob_ = orr.next()
                if (tb + flip) % 2:
                    op("act", lambda h: h.copy(out=o_[:], in_=PS[pi][:]), reads=[PSB[pi]], writes=[ob_])
                else:
                    op("dve", lambda h: h.tensor_copy(out=o_[:], in_=PS[pi][:]), reads=[PSB[pi]], writes=[ob_])
                dma("sp", dest[:, tb * 512:(tb + 1) * 512], o_[:], reads=[ob_], writes=[dbuf])
            return epi

        def proj_res(es, l, src, srcname, KCn, W, gname):
            TBD = min(1024, S)
            NH = TBD // 512
            srct = sb(es, "prs", [128, KCn, TBD], BF16); srcb = Buf()
            KH = KCn // 4
            st = Ring(es, nc, "prst", 4, [128, KH, 128], F32)
            wbr = Ring(es, nc, "prwb", 3, [128, KCn, 128], BF16)
            yr = Ring(es, nc, "pry", 3, [128, TBD], F32)
            sqr = Ring(es, nc, "prsq", 2, [128, 512], BF16)
            rst = sb(es, "prrs", [128, TBD]); rsb = Buf()
            xr = Ring(es, nc, "prx", 3, [128, TBD], F32)
            for tbd in range(S // TBD):
                ts = slice(tbd * TBD, (tbd + 1) * TBD)
                for kc in range(KCn):
                    dma("sp", srct[:, kc, :], src[kc * 128:(kc + 1) * 128, ts], reads=[db(srcname, kc)], writes=[srcb])
                def loadw(c):
                    w_, wb_ = wbr.next()
                    for hf in range(4):
                        s_, sb_ = st.next()
                        dma("sp", s_[:], W[hf * KH * 128:(hf + 1) * KH * 128, c * 128:(c + 1) * 128].rearrange("(kc p) n -> p kc n", p=128), writes=[sb_])
                        op("pool", lambda h, s_=s_, w_=w_, hf=hf: h.tensor_copy(out=w_[:, hf * KH:(hf + 1) * KH, :], in_=s_[:]), reads=[sb_], writes=[wb_])
                    return w_, wb_
                nxtw = loadw(0)
                for c in range(KC):
                    w_, wb_ = nxtw
                    if c + 1 < KC:
                        nxtw = loadw(c + 1)
                    y_, yb_ = yr.next()
                    for hh in range(NH):
                        pi = (c * NH + hh) % 4
                        for kc in range(KCn):
                            op("pe", lambda h, w_=w_, kc=kc, pi=pi, hh=hh: h.matmul(PS[pi][:], lhsT=w_[:, kc, :], rhs=srct[:, kc, hh * 512:(hh + 1) * 512],
                                                                                   start=(kc == 0), stop=(kc == KCn - 1)),
                               reads=[wb_, srcb], writes=[PSB[pi]], inc=(kc == KCn - 1))
                        op("dve", lambda h, y_=y_, pi=pi, hh=hh: h.tensor_copy(out=y_[:, hh * 512:(hh + 1) * 512], in_=PS[pi][:]), reads=[PSB[pi]], writes=[yb_])
                        q_, qb_ = sqr.next()
                        op("act", lambda h, q_=q_, y_=y_, hh=hh: h.activation(out=q_[:], in_=y_[:, hh * 512:(hh + 1) * 512], func=AF.Square), reads=[yb_], writes=[qb_])
                        op("pe", lambda h, q_=q_, hh=hh, c=c: h.matmul(PS[4 + hh][:], lhsT=onesb[:], rhs=q_[:], start=(c == 0), stop=(c == KC - 1)),
                           reads=[qb_, cb_b], writes=[PSB[4 + hh]])
                    dma("sp", yT[c * 128:(c + 1) * 128, ts], y_[:], reads=[yb_], writes=[db("yT", c)])
                for hh in range(NH):
                    hs = slice(hh * 512, (hh + 1) * 512)
                    op("dve", lambda h, hh=hh, hs=hs: h.tensor_scalar(out=rst[:, hs], in0=PS[4 + hh][:], scalar1=1.0 / D, scalar2=NORM_EPS, op0=ALU.mult, op1=ALU.add),
                       reads=[PSB[4 + hh]], writes=[rsb])
                op("act", lambda h: h.activation(out=rst[:], in_=rst[:], func=AF.Sqrt), reads=[rsb], writes=[rsb])
                op("dve", lambda h: h.reciprocal(out=rst[:], in_=rst[:]), reads=[rsb], writes=[rsb])
                def loadxy(c):
                    y_, yb_ = yr.next()
                    x_, xb_ = xr.next()
                    dma("sp", y_[:], yT[c * 128:(c + 1) * 128, ts], reads=[db("yT", c)], writes=[yb_])
                    dma("sp", x_[:], xT[c * 128:(c + 1) * 128, ts], reads=[db("xT", c)], writes=[xb_])
                    return y_, yb_, x_, xb_
                nxy = loadxy(0)
                for c in range(KC):
                    y_, yb_, x_, xb_ = nxy
                    if c + 1 < KC:
                        nxy = loadxy(c + 1)
                    op("dve", lambda h, y_=y_, c=c: h.scalar_tensor_tensor(out=y_[:], in0=y_[:], scalar=cvc(l, gname, c), in1=rst[:], op0=ALU.mult, op1=ALU.mult),
                       reads=[yb_, rsb, cv_b], writes=[yb_])
                    op("dve", lambda h, y_=y_, x_=x_: h.tensor_tensor(out=x_[:], in0=x_[:], in1=y_[:], op=ALU.add), reads=[yb_, xb_], writes=[xb_])
                    dma("sp", xT[c * 128:(c + 1) * 128, ts], x_[:], reads=[xb_], writes=[db("xT", c)])


        def attn_phase(es, l):
            C = sb(es, "arC", [128, S]); Cb = Buf()
            Sg = sb(es, "arS", [128, S]); Sgb = Buf()
            dma("sp", C[:], rotC[:, :], reads=[db("rot")], writes=[Cb])
            dma("sp", Sg[:], rotS[:, :], reads=[db("rot")], writes=[Sgb])
            dmf = sb(es, "admf", [128, 2048]); dmfb = Buf()
            dm = sb(es, "adm", [128, 4, 512], BF16); dmb = Buf()
            dma("sp", dmf[:], cn_in[:, CN["dmask"]:CN["dmask"] + 2048], writes=[dmfb])
            op("pool", lambda h: h.tensor_copy(out=dm[:], in_=dmf[:].rearrange("p (a b) -> p a b", a=4)), reads=[dmfb], writes=[dmb])
            raws = Ring(es, nc, "araw", 2, [128, 512], BF16)
            t1r = Ring(es, nc, "at1", 2, [128, 512], F32)
            t2r = Ring(es, nc, "at2", 2, [128, 512], F32)
            qk = Ring(es, nc, "aqk", 4, [128, S], BF16)
            Vr = Ring(es, nc, "aV", 2, [128, NT, 128], BF16)
            Er = Ring(es, nc, "aE", 4, [128, 512], BF16)
            wk = Ring(es, nc, "awk", 6, [128, 512], F32)
            sqr = Ring(es, nc, "asq", 2, [128, 512], BF16)
            outr = Ring(es, nc, "aout", 2, [128, 512], BF16)
            for hd in range(8):
                rot = []
                for src, nm in ((qT, "qT"), (kdT, "kdT")):
                    d_, db_ = qk.next()
                    for tb in range(NTB):
                        ts = slice(tb * 512, (tb + 1) * 512)
                        r_, rb_ = raws.next()
                        dma("sp", r_[:], src[hd * 128:(hd + 1) * 128, ts], reads=[db(nm, hd)], writes=[rb_])
                        op("pe", lambda h, r_=r_: h.matmul(PS[0][:], lhsT=permb[:], rhs=r_[:], start=True, stop=True), reads=[rb_, cb_b], writes=[PSB[0]])
                        a_, ab_ = t1r.next()
                        b_, bb_ = t2r.next()
                        op("pool", lambda h, r_=r_, a_=a_, ts=ts: h.tensor_tensor(out=a_[:], in0=r_[:], in1=C[:, ts], op=ALU.mult), reads=[rb_, Cb], writes=[ab_])
                        op("dve", lambda h, b_=b_, ts=ts: h.tensor_tensor(out=b_[:], in0=PS[0][:], in1=Sg[:, ts], op=ALU.mult), reads=[PSB[0], Sgb], writes=[bb_])
                        op("dve", lambda h, a_=a_, b_=b_, d_=d_, ts=ts: h.tensor_tensor(out=d_[:, ts], in0=a_[:], in1=b_[:], op=ALU.add), reads=[ab_, bb_], writes=[db_])
                    rot.append((d_, db_))
                (q_, qb_), (k_, kb_) = rot
                V_, Vb_ = Vr.next()
                dma("sp", V_[:], vd[:, hd * 128:(hd + 1) * 128].rearrange("(tt p) c -> p tt c", p=128), reads=[db("vd", hd)], writes=[Vb_])
                for qb in range(NTB):
                    qs = slice(qb * 512, (qb + 1) * 512)
                    nk = 4 * qb + 4
                    steps = [(kt, m) for kt in range(nk) for m in range(2)]

                    def emit_s(i):
                        kt, m = steps[i]
                        pi = i % 3
                        op("pe", lambda h: h.matmul(PS[pi][:], lhsT=k_[m * 64:(m + 1) * 64, kt * 128:(kt + 1) * 128], rhs=q_[m * 64:(m + 1) * 64, qs],
                                                    start=True, stop=True), reads=[kb_, qb_], writes=[PSB[pi]])
                    emit_s(0)
                    for i, (kt, m) in enumerate(steps):
                        pi = i % 3
                        if i + 1 < len(steps):
                            emit_s(i + 1)
                        e_, eb_ = Er.next()
                        op("act", lambda h, e_=e_, pi=pi: h.activation(out=e_[:], in_=PS[pi][:], func=AF.Exp, scale=0.125), reads=[PSB[pi]], writes=[eb_])
                        if kt >= 4 * qb:
                            op("pool", lambda h, e_=e_, j=kt - 4 * qb: h.tensor_tensor(out=e_[:], in0=e_[:], in1=dm[:, j, :], op=ALU.mult), reads=[eb_, dmb], writes=[eb_])
                        op("pe", lambda h, e_=e_, kt=kt, m=m: h.matmul(PS[3 + m][:], lhsT=V_[:, kt, :], rhs=e_[:], start=(kt == 0), stop=(kt == nk - 1)),
                           reads=[Vb_, eb_], writes=[PSB[3 + m]], inc=False)
                        op("pe", lambda h, e_=e_, kt=kt, m=m: h.matmul(PS[5 + m][:], lhsT=onesb[:], rhs=e_[:], start=(kt == 0), stop=(kt == nk - 1)),
                           reads=[cb_b, eb_], writes=[PSB[5 + m]])
                    w = [wk.next() for _ in range(6)]
                    for m in range(2):
                        op("dve", lambda h, m=m: h.reciprocal(out=w[m][0][:], in_=PS[5 + m][:]), reads=[PSB[5 + m]], writes=[w[m][1]])
                        op("dve", lambda h, m=m: h.tensor_tensor(out=w[2 + m][0][:], in0=PS[3 + m][:], in1=w[m][0][:], op=ALU.mult), reads=[PSB[3 + m], w[m][1]], writes=[w[2 + m][1]])
                    op("dve", lambda h: h.scalar_tensor_tensor(out=w[4][0][:], in0=w[3][0][:], scalar=dvc(l, "nlam"), in1=w[2][0][:], op0=ALU.mult, op1=ALU.add),
                       reads=[w[3][1], w[2][1], dv_b], writes=[w[4][1]])
                    s_, sb_ = sqr.next()
                    op("act", lambda h, s_=s_: h.activation(out=s_[:], in_=w[4][0][:], func=AF.Square), reads=[w[4][1]], writes=[sb_])
                    op("pe", lambda h, s_=s_: h.matmul(PS[0][:], lhsT=onesb[:], rhs=s_[:], start=True, stop=True), reads=[sb_, cb_b], writes=[PSB[0]])
                    op("dve", lambda h: h.tensor_scalar(out=w[5][0][:], in0=PS[0][:], scalar1=1.0 / 128, scalar2=SUBLN_EPS, op0=ALU.mult, op1=ALU.add), reads=[PSB[0]], writes=[w[5][1]])
                    op("act", lambda h: h.activation(out=w[5][0][:], in_=w[5][0][:], func=AF.Sqrt), reads=[w[5][1]], writes=[w[5][1]])
                    op("dve", lambda h: h.reciprocal(out=w[5][0][:], in_=w[5][0][:]), reads=[w[5][1]], writes=[w[5][1]])
                    o_, ob_ = outr.next()
                    op("dve", lambda h, o_=o_: h.scalar_tensor_tensor(out=o_[:], in0=w[4][0][:], scalar=dvc(l, "sub2"), in1=w[5][0][:], op0=ALU.mult, op1=ALU.mult),
                       reads=[w[4][1], w[5][1], dv_b], writes=[ob_])
                    dma("sp", mixT[1024 + hd * 128:1024 + (hd + 1) * 128, qs], o_[:], reads=[ob_], writes=[db("mixT", 8 + hd)])

        def rwkv_phase(es, l):
            f32t = lambda nm, sh=[128, 512]: (sb(es, nm, sh), Buf())
            bft = lambda nm, sh=[128, 512]: (sb(es, nm, sh, BF16), Buf())
            scanm, scb = f32t("scanm")
            dma("sp", scanm[:], cn_in[:, CN["scanm"]:CN["scanm"] + 512], writes=[scb])
            hm, hmb = f32t("hm", [128, 7, 256])
            dma("sp", hm[:], cn_in[:, CN["hm"]:CN["hm"] + 7 * 256].rearrange("p (k c) -> p k c", k=7), writes=[hmb])
            SL = []
            for i in range(8):
                SL.append({"T12": bft("T12_%d" % i, [128, 512]), "T3": bft("T3_%d" % i, [128, 128]), "W1": bft("W1_%d" % i, [128, 128]), "W2": bft("W2_%d" % i, [128, 256]),
                           "p0": bft("p0_%d" % i, [128, 256]), "DG": [bft("DG%d_%d" % (j, i), [128, 256]) for j in range(2)], "ZZ": bft("ZZ_%d" % i, [128, 256]),
                           "tW": bft("tW_%d" % i, [128, 256]), "Xb": bft("Xb_%d" % i, [128, 64]), "Ub": bft("Ub_%d" % i, [128, 64]), "HG": f32t("HG_%d" % i, [128, 64])})
            m2b, m2bb = bft("m2b", [128, 256]); mslb, mslbb = bft("mslb", [128, 128])
            op("pool", lambda h: h.tensor_copy(out=m2b[:], in_=cn[:, CN["m2"]:CN["m2"] + 256]), reads=[cn_b], writes=[m2bb])
            op("pool", lambda h: h.tensor_copy(out=mslb[:], in_=cn[:, CN["msl"]:CN["msl"] + 128]), reads=[cn_b], writes=[mslbb])
            wst, wstb = f32t("lst", [128, 1024])
            wa2b, wa2bb = bft("wa2b", [128, 1024]); g2ab, g2abb = bft("g2ab", [128, 1024]); g2bv, g2bvb = bft("g2bv", [64, 1024])
            dma("sp", wst[0:64, :], w2_in[l], writes=[wstb]); dma("sp", wst[64:128, :], a2_in[l], writes=[wstb])
            op("pool", lambda h: h.tensor_copy(out=wa2b[:], in_=wst[:]), reads=[wstb], writes=[wa2bb])
            dma("sp", wst[:], g2_in[l][0:128, :], writes=[wstb])
            op("pool", lambda h: h.tensor_copy(out=g2ab[:], in_=wst[:]), reads=[wstb], writes=[g2abb])
            dma("sp", wst[0:32, :], g2_in[l][128:160, :], writes=[wstb])
            if l > 0:
                dma("sp", wst[32:64, :], v2_in[l - 1], writes=[wstb])
            op("pool", lambda h: h.tensor_copy(out=g2bv[0:64 if l > 0 else 32, :], in_=wst[0:64 if l > 0 else 32, :]), reads=[wstb], writes=[g2bvb])
            Hf, _ = f32t("Hf", [128, 8, 64]); Hb, _ = bft("Hb", [128, 8, 64])
            Hfb = [Buf(), Buf()]; Hbb = [Buf(), Buf()]
            op("pool", lambda h: h.memset(Hf[:], 0.0), writes=Hfb); op("pool", lambda h: h.memset(Hb[:], 0.0), writes=Hbb)
            wa_t, wab = bft("wa_t"); g1_t, g1b = bft("g1_t"); g2_t, g2b_ = bft("g2_t", [64, 512])
            r_t, rb = bft("r_t"); k_t, kb_ = bft("k_t"); v_t, vb = bft("v_t"); vf_t, vfb = bft("vf_t")
            sg, sgb = f32t("sg"); cs, csb = f32t("cs"); ex, exb = f32t("ex"); eG, eGb = f32t("eG"); eGi, eGib = f32t("eGi"); eGx, eGxb = f32t("eGx")
            a_t, ab = f32t("a_t"); gt, gtb = f32t("gt"); v2t, v2b = f32t("v2t"); kk, kkb = f32t("kk"); sqb_, sqbb = bft("sqb"); rn, rnb = f32t("rn")
            k2, k2b = f32t("k2"); tmp, tmpb = f32t("tmp"); AR, ARb = bft("AR", [128, 2, 512]); BhT, BhTb = bft("BhT"); KhT, KhTb = bft("KhT"); vbf, vbfb = bft("vbf")
            rkb, rkbb = bft("rkb"); bon, bonb = f32t("bon"); OT, _ = f32t("OT"); OTb = [Buf(), Buf()]; ob16, ob16b = bft("ob16"); mean, meanb = f32t("mean"); var, varb = f32t("var")
            Bh = [bft("Bh%d" % i, [128, 128]) for i in range(4)]; Kh = [bft("Kh%d" % i, [128, 128]) for i in range(4)]; Vt = [bft("Vt%d" % i, [128, 128]) for i in range(4)]
            T1, T1b = bft("T1", [128, 256]); T2, T2b = bft("T2", [128, 256]); T3, T3b = bft("T3", [128, 128])
            W1, W1b = bft("W1", [128, 128]); W2, W2b = bft("W2", [128, 256])
            PP = [bft("PP%d" % i, [128, 256]) for i in range(2)]; NTt = [bft("NT%d" % i, [128, 128]) for i in range(2)]
            Xb, Xbb = bft("Xb", [128, 64]); Ub, Ubb = bft("Ub", [128, 64]); mo, mob = bft("mo")
            tt_ = lambda e, o, a, b, opn, rd, wr: op(e, lambda h: h.tensor_tensor(out=o, in0=a, in1=b, op=opn), reads=rd, writes=wr)
            trc = [0]
            G2R = 64 if l > 0 else 32
            for tb in range(NTB):
                ts = slice(tb * 512, (tb + 1) * 512)
                dma("sp", wa_t[:], waT[:, ts], reads=[db("waT")], writes=[wab]); dma("sp", g1_t[:], g1T[:, ts], reads=[db("g1T")], writes=[g1b])
                dma("sp", g2_t[0:G2R, :], g2T[0:G2R, ts], reads=[db("g2T")], writes=[g2b_])
                for c in range(8):
                    cs_ = slice(c * 128, (c + 1) * 128)
                    dma("sp", r_t[:], rT[cs_, ts], reads=[db("rT", c)], writes=[rb]); dma("sp", k_t[:], kT[cs_, ts], reads=[db("kT", c)], writes=[kb_])
                    dma("sp", v_t[:], vT[cs_, ts], reads=[db("vT", c)], writes=[vb])
                    op("pe", lambda h: h.matmul(PS[0][:], lhsT=wa2b[0:64, cs_], rhs=wa_t[0:64, :], start=True, stop=True), reads=[wa2bb, wab], writes=[PSB[0]])
                    op("act", lambda h: h.activation(out=sg[:], in_=PS[0][:], func=AF.Sigmoid, bias=cvc(l, "w0", c)), reads=[PSB[0], cv_b], writes=[sgb])
                    op("dve", lambda h: h.tensor_tensor_scan(out=cs[:], data0=scanm[:], data1=sg[:], initial=0.0, op0=ALU.mult, op1=ALU.add), reads=[scb, sgb], writes=[csb])
                    tt_("dve", ex[:], cs[:], sg[:], ALU.subtract, [csb, sgb], [exb])
                    op("act", lambda h: h.activation(out=eG[:], in_=cs[:], func=AF.Exp, scale=-C0), reads=[csb], writes=[eGb])
                    op("act", lambda h: h.activation(out=eGi[:], in_=cs[:], func=AF.Exp, scale=C0), reads=[csb], writes=[eGib])
                    op("act", lambda h: h.activation(out=eGx[:], in_=ex[:], func=AF.Exp, scale=-C0), reads=[exb], writes=[eGxb])
                    op("pe", lambda h: h.matmul(PS[0][:], lhsT=wa2b[64:128, cs_], rhs=wa_t[64:128, :], start=True, stop=True), reads=[wa2bb, wab], writes=[PSB[0]])
                    op("act", lambda h: h.activation(out=a_t[:], in_=PS[0][:], func=AF.Sigmoid, bias=cvc(l, "a0", c)), reads=[PSB[0], cv_b], writes=[ab])
                    op("pe", lambda h: h.matmul(PS[0][:], lhsT=g2ab[:, cs_], rhs=g1_t[:], start=True, stop=False), reads=[g2abb, g1b], writes=[PSB[0]], inc=False)
                    op("pe", lambda h: h.matmul(PS[0][:], lhsT=g2bv[0:32, cs_], rhs=g2_t[0:32, :], start=False, stop=True), reads=[g2bvb, g2b_], writes=[PSB[0]])
                    op("act", lambda h: h.copy(out=gt[:], in_=PS[0][:]), reads=[PSB[0]], writes=[gtb])
                    if l > 0:
                        dma("sp", vf_t[:], vfT[cs_, ts], reads=[db("vfT", c)], writes=[vfb])
                        op("pe", lambda h: h.matmul(PS[0][:], lhsT=g2bv[32:64, cs_], rhs=g2_t[32:64, :], start=True, stop=True), reads=[g2bvb, g2b_], writes=[PSB[0]])
                        op("act", lambda h: h.activation(out=tmp[:], in_=PS[0][:], func=AF.Sigmoid, bias=cvc(l, "v0", c)), reads=[PSB[0], cv_b], writes=[tmpb])
                        tt_("dve", v2t[:], vf_t[:], v_t[:], ALU.subtract, [vfb, vb], [v2b])
                        tt_("dve", v2t[:], v2t[:], tmp[:], ALU.mult, [v2b, tmpb], [v2b])
                        tt_("dve", v2t[:], v2t[:], v_t[:], ALU.add, [v2b, vb], [v2b])
                    else:
                        op("dve", lambda h: h.tensor_copy(out=v2t[:], in_=v_t[:]), reads=[vb], writes=[v2b])
                    op("pool", lambda h: h.tensor_copy(out=vbf[:], in_=v2t[:]), reads=[v2b], writes=[vbfb])
                    op("dve", lambda h: h.tensor_scalar(out=kk[:], in0=k_t[:], scalar1=cvc(l, "k_k", c), scalar2=None, op0=ALU.mult), reads=[kb_, cv_b], writes=[kkb])
                    tt_("pool", sqb_[:], kk[:], kk[:], ALU.mult, [kkb], [sqbb])
                    op("pe", lambda h: h.matmul(PS[0][:], lhsT=bonesb[:], rhs=sqb_[:], start=True, stop=True), reads=[cb_b, sqbb], writes=[PSB[0]])
                    op("act", lambda h: h.activation(out=rn[:], in_=PS[0][:], func=AF.Sqrt, bias=1e-20), reads=[PSB[0]], writes=[rnb])
                    op("dve", lambda h: h.reciprocal(out=rn[:], in_=rn[:]), reads=[rnb], writes=[rnb])
                    tt_("dve", kk[:], kk[:], rn[:], ALU.mult, [kkb, rnb], [kkb])
                    op("dve", lambda h: h.tensor_scalar(out=tmp[:], in0=a_t[:], scalar1=cvc(l, "k_a", c), scalar2=dvc(l, "omka", c), op0=ALU.mult, op1=ALU.add),
                       reads=[ab, cv_b, dv_b], writes=[tmpb])
                    tt_("dve", k2[:], k_t[:], tmp[:], ALU.mult, [kb_, tmpb], [k2b])
                    op("dve", lambda h: h.scalar_tensor_tensor(out=AR[:, 0, :], in0=kk[:], scalar=-1.0, in1=eGx[:], op0=ALU.mult, op1=ALU.mult), reads=[kkb, eGxb], writes=[ARb])
                    tt_("pool", AR[:, 1, :], r_t[:], eG[:], ALU.mult, [rb, eGb], [ARb])
                    tt_("dve", tmp[:], kk[:], a_t[:], ALU.mult, [kkb, ab], [tmpb])
                    tt_("dve", BhT[:], tmp[:], eGi[:], ALU.mult, [tmpb, eGib], [BhTb])
                    tt_("pool", KhT[:], k2[:], eGi[:], ALU.mult, [k2b, eGib], [KhTb])
                    op("dve", lambda h: h.scalar_tensor_tensor(out=rkb[:], in0=r_t[:], scalar=cvc(l, "r_k", c), in1=k2[:], op0=ALU.mult, op1=ALU.mult), reads=[rb, k2b, cv_b], writes=[rkbb])
                    op("pe", lambda h: h.matmul(PS[0][:], lhsT=bonesb[:], rhs=rkb[:], start=True, stop=True), reads=[cb_b, rkbb], writes=[PSB[0]])
                    tt_("dve", bon[:], PS[0][:], v2t[:], ALU.mult, [PSB[0], v2b], [bonb])
                    for n in range(4):
                        tsl = slice(n * 128, (n + 1) * 128)
                        for src, srcb, dst in ((BhT, BhTb, Bh[n]), (KhT, KhTb, Kh[n]), (vbf, vbfb, Vt[n])):
                            ri = trc[0] % 4
                            trc[0] += 1
                            op("pe", lambda h, src=src, ri=ri: h.transpose(out=PSH[:, ri * 128:(ri + 1) * 128], in_=src[:, tsl], identity=identb[:]), reads=[srcb, cb_b], writes=[PSH_b[ri]])
                            op("act" if ri % 2 else "dve", lambda h, dst=dst, ri=ri: (h.copy if ri % 2 else h.tensor_copy)(out=dst[0][:], in_=PSH[:, ri * 128:(ri + 1) * 128]),
                               reads=[PSH_b[ri]], writes=[dst[1]])
                    def mmq(out, lhsT, rhs, rd, wr, st=True, sp=True, inc=True):
                        op("pe", lambda h: h.matmul(out, lhsT=lhsT, rhs=rhs, start=st, stop=sp), reads=rd, writes=wr, inc=inc)

                    def stage1(n, hh, lane, sl):
                        tsl = slice(n * 128, (n + 1) * 128)
                        P_ = slice(64 * hh, 64 * hh + 64)
                        A, Ab = PS[1 + lane], PSB[1 + lane]
                        e1 = "act" if lane % 2 == 0 else "dve"
                        cp = lambda eng, o, i, rd, wr: op(eng, lambda h: (h.copy if eng == "act" else h.tensor_copy)(out=o, in_=i), reads=rd, writes=wr)
                        T12, T12b = sl["T12"]; T3, T3b = sl["T3"]; W1, W1b = sl["W1"]; W2, W2b = sl["W2"]; p0, p0b = sl["p0"]
                        DGs = sl["DG"]; ZZ, ZZb = sl["ZZ"]; tW, tWb = sl["tW"]
                        mmq(A[:, 0:256], BhT[P_, tsl], AR[P_, :, tsl], [BhTb, ARb], [Ab], inc=False)
                        mmq(A[:, 256:512], KhT[P_, tsl], AR[P_, :, tsl], [KhTb, ARb], [Ab])
                        yield
                        cp(e1, T12[:], A[:, 0:512], [Ab], [T12b, Ab])
                        yield
                        mmq(A[:, 0:128], AR[P_, 0, tsl], BhT[P_, tsl], [ARb, BhTb], [Ab])
                        d0, d0b = DGs[0]
                        tt_("pool", p0[:, 128:256], T12[:, 0:128], m2b[:, 0:128], ALU.mult, [T12b, m2bb], [p0b])
                        tt_("pool", W1[:], T12[:, 128:256], m2b[:, 128:256], ALU.mult, [T12b, m2bb], [W1b])
                        tt_("pool", W2[:], T12[:, 256:512], m2b[:], ALU.mult, [T12b, m2bb], [W2b])
                        tt_("pool", d0[:, 128:256], T12[:, 0:128], hm[:, 0, 128:256], ALU.mult, [T12b, hmb], [d0b])
                        yield
                        cp(e1, T3[:], A[:, 0:128], [Ab], [T3b, Ab])
                        tt_("pool", d0[:, 128:256], d0[:, 128:256], identb[:], ALU.add, [d0b, cb_b], [d0b])
                        yield
                        tt_("pool", p0[:, 0:128], T3[:], mslb[:], ALU.mult, [T3b, mslbb], [p0b])
                        tt_("pool", d0[:, 0:128], T3[:], hm[:, 0, 0:128], ALU.mult, [T3b, hmb], [d0b])
                        tt_("pool", d0[:, 0:128], d0[:, 0:128], identb[:], ALU.add, [d0b, cb_b], [d0b])
                        yield
                        for k in range(1, 7):
                            dc, dcb = DGs[(k - 1) % 2]
                            dn, dnb = DGs[k % 2]
                            mmq(A[:, 0:128], p0[:, 128:256], dc[:, 0:128], [p0b, dcb], [Ab], inc=False)
                            mmq(A[:, 128:256], p0[:, 0:128], dc[:, 128:256], [p0b, dcb], [Ab])
                            yield
                            cp("act" if (k + lane) % 2 else "dve", ZZ[:], A[:, 0:256], [Ab], [ZZb, Ab])
                            yield
                            mmq(A[:, 256:384], dc[:, 128:256], ZZ[:, 0:128], [dcb, ZZb], [Ab], inc=False)
                            mmq(A[:, 384:512], dc[:, 0:128], ZZ[:, 128:256], [dcb, ZZb], [Ab])
                            yield
                            tt_("dve", tW[:], A[:, 256:512], hm[:, k, :], ALU.mult, [Ab, hmb], [tWb, Ab])
                            yield
                            tt_("pool", dn[:], dc[:], tW[:], ALU.add, [dcb, tWb], [dnb])
                            yield

                    def stage2(n, hh, lane, sl):
                        tsl = slice(n * 128, (n + 1) * 128)
                        P_ = slice(64 * hh, 64 * hh + 64)
                        B, Bb = PS[1 + lane], PSB[1 + lane]
                        W1, W1b = sl["W1"]; W2, W2b = sl["W2"]; Xb, Xbb = sl["Xb"]; Ub, Ubb = sl["Ub"]; HG, HGb = sl["HG"]
                        ntf, ntfb = sl["DG"][0][0][:, 128:256], sl["DG"][0][1]
                        vt, vtb = Vt[n]
                        mmq(B[:, 0:64], AR[P_, 0, tsl], Hb[P_, c, :], [ARb, Hbb[hh]], [Bb], True, False, False)
                        mmq(B[:, 0:64], W2[:, 0:128], vt[:, P_], [W2b, vtb], [Bb], False, True)
                        yield
                        op("act", lambda h: h.copy(out=Xb[:], in_=B[:, 0:64]), reads=[Bb], writes=[Xbb, Bb])
                        yield
                        mmq(B[:, 64:128], ntf, Xb[:], [ntfb, Xbb], [Bb])
                        yield
                        op("dve", lambda h: h.tensor_copy(out=Ub[:], in_=B[:, 64:128]), reads=[Bb], writes=[Ubb, Bb])
                        gcol = eG[P_, n * 128 + 127:n * 128 + 128]
                        op("dve", lambda h: h.tensor_scalar(out=HG[P_, :], in0=Hf[P_, c, :], scalar1=gcol, scalar2=None, op0=ALU.mult), reads=[Hfb[hh], eGb], writes=[HGb])
                        yield
                        mmq(B[P_, 128:256], Hb[P_, c, :], AR[P_, 1, tsl], [Hbb[hh], ARb], [Bb], True, False, False)
                        mmq(B[P_, 128:256], Ub[:], W1[:], [Ubb, W1b], [Bb], False, False, False)
                        mmq(B[P_, 128:256], vt[:, P_], W2[:, 128:256], [vtb, W2b], [Bb], False, True, False)
                        mmq(B[P_, 256:320], Bh[n][0][:, P_], Ub[:], [Bh[n][1], Ubb], [Bb], True, False, False)
                        mmq(B[P_, 256:320], Kh[n][0][:, P_], vt[:, P_], [Kh[n][1], vtb], [Bb], False, True)
                        yield
                        op("dve", lambda h: h.scalar_tensor_tensor(out=Hf[P_, c, :], in0=B[P_, 256:320], scalar=gcol, in1=HG[P_, :], op0=ALU.mult, op1=ALU.add),
                           reads=[Bb, eGb, HGb], writes=[Hfb[hh], Bb])
                        op("act", lambda h: h.copy(out=OT[P_, tsl], in_=B[P_, 128:256]), reads=[Bb], writes=[OTb[hh], Bb])
                        yield
                        op("act", lambda h: h.copy(out=Hb[P_, c, :], in_=Hf[P_, c, :]), reads=[Hfb[hh]], writes=[Hbb[hh]])
                        yield

                    units = [(n, hh) for n in range(4) for hh in range(2)]
                    for r0 in range(0, 8, 4):
                        lockstep([stage1(n, hh, lane, SL[r0 + lane]) for lane, (n, hh) in enumerate(units[r0:r0 + 4])])
                    for n in range(4):
                        lockstep([stage2(n, hh, hh, SL[2 * n + hh]) for hh in range(2)])
                    op("pool", lambda h: h.tensor_copy(out=ob16[:], in_=OT[:]), reads=OTb, writes=[ob16b])
                    op("pe", lambda h: h.matmul(PS[0][:], lhsT=bonesb[:], rhs=ob16[:], start=True, stop=True), reads=[cb_b, ob16b], writes=[PSB[0]])
                    op("dve", lambda h: h.tensor_scalar(out=mean[:], in0=PS[0][:], scalar1=1.0 / 64, scalar2=None, op0=ALU.mult), reads=[PSB[0]], writes=[meanb])
                    tt_("dve", OT[:], OT[:], mean[:], ALU.subtract, OTb + [meanb], OTb)
                    op("act", lambda h: h.activation(out=sqb_[:], in_=OT[:], func=AF.Square), reads=OTb, writes=[sqbb])
                    op("pe", lambda h: h.matmul(PS[0][:], lhsT=bonesb[:], rhs=sqb_[:], start=True, stop=True), reads=[cb_b, sqbb], writes=[PSB[0]])
                    op("act", lambda h: h.activation(out=var[:], in_=PS[0][:], func=AF.Sqrt, scale=1.0 / 64, bias=GN_EPS), reads=[PSB[0]], writes=[varb])
                    op("dve", lambda h: h.reciprocal(out=var[:], in_=var[:]), reads=[varb], writes=[varb])
                    tt_("dve", OT[:], OT[:], var[:], ALU.mult, OTb + [varb], OTb)
                    op("act", lambda h: h.activation(out=OT[:], in_=OT[:], func=AF.Identity, scale=cvc(l, "gn_w", c), bias=cvc(l, "gn_b", c)), reads=OTb + [cv_b], writes=OTb)
                    tt_("dve", OT[:], OT[:], bon[:], ALU.add, OTb + [bonb], OTb)
                    tt_("dve", mo[:], OT[:], gt[:], ALU.mult, OTb + [gtb], [mob])
                    dma("sp", mixT[cs_, ts], mo[:], reads=[mob], writes=[db("mixT", c)])

        def lockstep(gens):
            gens = list(gens)
            while gens:
                nxt = []
                for g in gens:
                    try:
                        next(g)
                        nxt.append(g)
                    except StopIteration:
                        pass
                gens = nxt

        for l in range(NL if 'nolayers' not in parts else 0):
            kb.new_epoch()
            if 'mix' in parts:
                with scope() as es:
                    hT = sb(es, "hT", [128, KC, S], BF16)
                    hTb = [Buf() for _ in range(NTB)]
                    with scope() as es2:
                        norm_to_hT(es2, l, "g_pre", hT, hTb)
                    with scope() as es2:
                        W = w_in[l]
                        R = mk_rings(es2)
                        tasks = []
                        for c in range(8):
                            tasks.append(([(W[:, c * 128:(c + 1) * 128], 128)], 128,
                                          shift_epi(R, "er%d" % (c % 2), 128, cvc(l, "mu_r", c), dvc(l, "om_r", c), [(rT[c * 128:(c + 1) * 128], db("rT", c))])))
                        for c in range(8):
                            tasks.append(([(W[:, 1088 + c * 128:1088 + (c + 1) * 128], 128)], 128,
                                          shift_epi(R, "ek%d" % (c % 2), 128, cvc(l, "mu_k", c), dvc(l, "om_k", c), [(kT[c * 128:(c + 1) * 128], db("kT", c))])))
                        for c in range(8):
                            dsts = [(vT[c * 128:(c + 1) * 128], db("vT", c))]
                            if l == 0:
                                dsts.append((vfT[c * 128:(c + 1) * 128], db("vfT", c)))
                            tasks.append(([(W[:, 2112 + c * 128:2112 + (c + 1) * 128], 128)], 128,
                                          shift_epi(R, "ev%d" % (c % 2), 128, cvc(l, "mu_v", c), dvc(l, "om_v", c), dsts)))
                        tasks.append(([(W[:, 1024:1088], 64), (W[:, 3136:3200], 64)], 128,
                                      shift_epi(R, "ewa", 128, cvc(l, "mu_wa"), dvc(l, "om_wa"), [(waT, db("waT"))], act=[(0, 64, AF.Tanh), (64, 128, None)])))
                        tasks.append(([(W[:, 3200:3328], 128)], 128,
                                      shift_epi(R, "eg1", 128, cvc(l, "mu_g1"), dvc(l, "om_g1"), [(g1T, db("g1T"))], act=[(0, 128, AF.Sigmoid)])))
                        if l == 0:
                            tasks.append(([(W[:, 3328:3360], 32)], 32,
                                          shift_epi(R, "eg2", 32, cvc(l, "mu_g2", 0, 1, 0, 32), dvc(l, "om_g2", 0, 1, 0, 32), [(g2T[0:32], db("g2T"))], act=[(0, 32, AF.Sigmoid)])))
                        else:
                            tasks.append(([(W[:, 3328:3360], 32), (w_mv[l - 1], 32)], 64,
                                          shift_epi(R, "eg2", 64, cvc(l, "mu_g2", 0, 1, 0, 64), dvc(l, "om_g2", 0, 1, 0, 64), [(g2T, db("g2T"))],
                                                    act=[(0, 32, AF.Sigmoid), (32, 64, None)])))
                        for c in range(8):
                            tasks.append(([(W[:, 3360 + c * 128:3360 + (c + 1) * 128], 128)], 128, plain_epi(R, "eq%d" % (c % 2), qT[c * 128:(c + 1) * 128], db("qT", c), 0)))
                        for c in range(8):
                            tasks.append(([(W[:, 4384 + c * 128:4384 + (c + 1) * 128], 128)], 128, plain_epi(R, "ekd%d" % (c % 2), kdT[c * 128:(c + 1) * 128], db("kdT", c), 1)))
                        proj_fm(es2, hT, hTb, tasks)
                    with scope() as es2:
                        st = Ring(es2, nc, "vst", 2, [128, KC, 128], F32)
                        wbr = Ring(es2, nc, "vwb", 2, [128, KC, 128], BF16)
                        orr = Ring(es2, nc, "vo", 2, [128, 128], BF16)
                        for c in range(8):
                            s_, sb_ = st.next()
                            dma("sp", s_[:], w_in[l][:, 5408 + c * 128:5408 + (c + 1) * 128].rearrange("(kc p) n -> p kc n", p=128), writes=[sb_])
                            w_, wb_ = wbr.next()
                            op("pool", lambda h, s_=s_, w_=w_: h.tensor_copy(out=w_[:], in_=s_[:]), reads=[sb_], writes=[wb_])
                            for tt in range(NT):
                                pi = tt % 4
                                for kc in range(KC):
                                    op("pe", lambda h, w_=w_, kc=kc, pi=pi, tt=tt: h.matmul(PS[pi][:, 0:128], lhsT=hT[:, kc, tt * 128:(tt + 1) * 128], rhs=w_[:, kc, :],
                                                                                           start=(kc == 0), stop=(kc == KC - 1)),
                                       reads=[wb_, hTb[tt // 4]], writes=[PSB[pi]], inc=(kc == KC - 1))
                                o_, ob_ = orr.next()
                                op("act" if tt % 2 else "dve", lambda h, o_=o_, pi=pi, tt=tt: (h.copy if tt % 2 else h.tensor_copy)(out=o_[:], in_=PS[pi][:, 0:128]),
                                   reads=[PSB[pi]], writes=[ob_])
                                dma("sp", vd[tt * 128:(tt + 1) * 128, c * 128:(c + 1) * 128], o_[:], reads=[ob_], writes=[db("vd", c)])

                with scope() as es:
                    if 'norwkv' not in parts:
                        rwkv_phase(es, l)
                with scope() as es:
                    if 'noattn' not in parts:
                        attn_phase(es, l)
                with scope() as es:
                    proj_res(es, l, mixT, "mixT", KC, w_out[l], "g_pm")
            with scope() as es:
                hT = sb(es, "hT2", [128, KC, S], BF16)
                hTb = [Buf() for _ in range(NTB)]
                with scope() as es2:
                    norm_to_hT(es2, l, "g_pf", hT, hTb)
                with scope() as es2:
                    tr = Ring(es2, nc, "ft", 2, [128, 512], F32)
                    gr = Ring(es2, nc, "fg", 2, [128, 512], F32)
                    orr = Ring(es2, nc, "fo", 2, [128, 512], BF16)
                    carr = [(sb(es2, "fc%d" % i, [128, 2]), Buf()) for i in range(2)]

                    def ffn_epi(c):
                        car, carb = carr[c % 2]

                        def epi(tb, pss):
                            pg, pu = pss
                            ts = slice(tb * 512, (tb + 1) * 512)
                            t_, tb_ = tr.next()
                            g_, gb_ = gr.next()
                            o_, ob_ = orr.next()
                            op("act", lambda h: h.activation(out=t_[:], in_=PS[pg][:], func=AF.Identity, scale=cvc(l, "cw2", c), bias=cvc(l, "cb", c)),
                               reads=[PSB[pg], cv_b], writes=[tb_])
                            op("dve", lambda h: h.scalar_tensor_tensor(out=t_[:, 1:512], in0=PS[pg][:, 0:511], scalar=cvc(l, "cw1", c), in1=t_[:, 1:512], op0=ALU.mult, op1=ALU.add),
                               reads=[PSB[pg], tb_, cv_b], writes=[tb_])
                            op("dve", lambda h: h.scalar_tensor_tensor(out=t_[:, 2:512], in0=PS[pg][:, 0:510], scalar=cvc(l, "cw0", c), in1=t_[:, 2:512], op0=ALU.mult, op1=ALU.add),
                               reads=[PSB[pg], tb_, cv_b], writes=[tb_])
                            if tb > 0:
                                op("dve", lambda h: h.scalar_tensor_tensor(out=t_[:, 0:2], in0=car[:, 0:2], scalar=cvc(l, "cw0", c), in1=t_[:, 0:2], op0=ALU.mult, op1=ALU.add),
                                   reads=[carb, tb_, cv_b], writes=[tb_])
                                op("dve", lambda h: h.scalar_tensor_tensor(out=t_[:, 0:1], in0=car[:, 1:2], scalar=cvc(l, "cw1", c), in1=t_[:, 0:1], op0=ALU.mult, op1=ALU.add),
                                   reads=[carb, tb_, cv_b], writes=[tb_])
                            op("act", lambda h: h.copy(out=car[:, 0:2], in_=PS[pg][:, 510:512]), reads=[PSB[pg]], writes=[carb])
                            op("act", lambda h: h.activation(out=g_[:], in_=t_[:], func=AF.Gelu_apprx_tanh), reads=[tb_], writes=[gb_])
                            op("dve", lambda h: h.tensor_tensor(out=o_[:], in0=PS[pu][:], in1=g_[:], op=ALU.mult), reads=[PSB[pu], gb_], writes=[ob_])
                            dma("sp", actT[c * 128:(c + 1) * 128, ts], o_[:], reads=[ob_], writes=[db("actT", c)])
                        return epi
                    tasks = []
                    for c in range(FC):
                        e = ffn_epi(c)
                        tasks.append(([(w_up[l][:, c * 128:(c + 1) * 128], 128)], 128, e))
                        tasks.append(([(w_up[l][:, FF + c * 128:FF + (c + 1) * 128], 128)], 128, e))
                    if 'noproj' not in parts:
                        proj_fm(es2, hT, hTb, tasks, nper=2)
            with scope() as es:
                if 'nores' not in parts:
                    proj_res(es, l, actT, "actT", FC, w_down[l], "g_ff")

        with scope() as es:
            xr = Ring(es, nc, "oxi", 2, [128, KC, 128], F32)
            xo = Ring(es, nc, "oxo", 2, [128, D], F32)
            for tt in range(NT):
                xi, xib = xr.next()
                dma("sp", xi[:], xT[:, tt * 128:(tt + 1) * 128].rearrange("(kc p) t -> p kc t", p=128), reads=[db("xT", kc) for kc in range(KC)], writes=[xib])
                o_, ob_ = xo.next()
                for kc in range(KC):
                    pi = kc % 4
                    op("pe", lambda h, kc=kc, pi=pi: h.transpose(out=PS[pi][:, 0:128], in_=xi[:, kc, :], identity=cn[:, 0:128]), reads=[xib, cn_b], writes=[PSB[pi]])
                    op("act" if kc % 2 else "dve", lambda h, kc=kc, pi=pi: (h.copy if kc % 2 else h.tensor_copy)(out=o_[:, kc * 128:(kc + 1) * 128], in_=PS[pi][:, 0:128]),
                       reads=[PSB[pi]], writes=[ob_])
                dma("sp", y_out[tt * 128:(tt + 1) * 128, :], o_[:], reads=[ob_], writes=[db("yout")])
        kb.finish()
    return nc


def pack_inputs(inp, S, NL):
    f = np.float32
    cv = np.zeros((128, NL, NCV), f)

    def put(l, name, vec):
        vec = np.asarray(vec, f).reshape(-1)
        n = (vec.size + 127) // 128
        pad = np.zeros(n * 128, f)
        pad[:vec.size] = vec
        cv[:, l, CV[name]:CV[name] + n] = pad.reshape(n, 128).T
    for l in range(NL):
        put(l, "g_pre", inp["pre_mix_norm"][l]); put(l, "g_pm", inp["post_mix_norm"][l])
        put(l, "g_pf", inp["pre_ffn_norm"][l]); put(l, "g_ff", inp["post_ffn_norm"][l])
        mu = np.asarray(inp["shift_mu"][l], f)
        put(l, "mu_r", mu[0:1024]); put(l, "mu_k", mu[1088:2112]); put(l, "mu_v", mu[2112:3136])
        put(l, "mu_wa", np.concatenate([mu[1024:1088], mu[3136:3200]]))
        put(l, "mu_g1", mu[3200:3328])
        g2 = np.zeros(128, f)
        g2[0:32] = mu[3328:3360]
        if l > 0:
            g2[32:64] = np.asarray(inp["shift_mu_mv"][l - 1], f)
        put(l, "mu_g2", g2)
        for nm in ("w0", "a0", "k_k", "k_a", "r_k", "gn_w", "gn_b"):
            put(l, nm, inp[nm][l])
        if l > 0:
            put(l, "v0", inp["v0"][l - 1])
        put(l, "subln", inp["subln_w"][l])
        cw = np.asarray(inp["conv_w"][l], f)
        put(l, "cw0", cw[0]); put(l, "cw1", cw[1]); put(l, "cw2", cw[2]); put(l, "cb", inp["conv_b"][l])
        for nm, k in (("lq1", "lam_q1"), ("lk1", "lam_k1"), ("lq2", "lam_q2"), ("lk2", "lam_k2")):
            cv[:, l, CV[nm]:CV[nm] + 64] = np.asarray(inp[k][l], f)[None, :]
    shared = {
        "cv": np.ascontiguousarray(cv.reshape(128, NL * NCV)),
        "cn": make_consts(),
        "w_in": np.ascontiguousarray(inp["w_in"][:NL], dtype=f),
        "w_mv": np.ascontiguousarray(inp["w_mv_down"][:max(NL - 1, 1)], dtype=f),
        "w2": np.ascontiguousarray(inp["w2"][:NL], dtype=f), "a2": np.ascontiguousarray(inp["a2"][:NL], dtype=f),
        "g2": np.ascontiguousarray(inp["g2"][:NL], dtype=f), "v2": np.ascontiguousarray(inp["v2"][:max(NL - 1, 1)], dtype=f),
        "w_out": np.ascontiguousarray(inp["w_out"][:NL], dtype=f), "w_up": np.ascontiguousarray(inp["w_up"][:NL], dtype=f),
        "w_down": np.ascontiguousarray(inp["w_down"][:NL], dtype=f),
    }
    return shared


def kernel(**inp):
    x = np.asarray(inp["x"], np.float32)
    B, S, _ = x.shape
    NL = 4
    nc = build(S, NL)
    shared = pack_inputs(inp, S, NL)
    pos = np.asarray(inp["positions"], np.int32)
    in_maps = []
    for b in range(B):
        m = dict(shared)
        m["x"] = np.ascontiguousarray(x[b])
        m["pos"] = np.ascontiguousarray(pos[b:b + 1])
        in_maps.append(m)
    res = run_bass_kernel_spmd(nc, in_maps, core_ids=list(range(B)))
    return np.stack([np.asarray(r["y"], np.float32) for r in res.results], 0)
```

```python
import math
from contextlib import ExitStack, contextmanager
import numpy as np
import concourse.bass as bass
import concourse.mybir as mybir
from concourse.bass_utils import run_bass_kernel_spmd

F32 = mybir.dt.float32
BF16 = mybir.dt.bfloat16
I32 = mybir.dt.int32
AF = mybir.ActivationFunctionType
ALU = mybir.AluOpType

D = 2048
KC = 16
FF = 5632
FC = 44
RW = 1024
RCOLS = 3360
INC = 6432
C0 = math.exp(-0.5)
NORM_EPS = 1e-6
GN_EPS = 64e-5
SUBLN_EPS = 1e-5

CV = {}
_o = 0
for _n, _w in (("g_pre", 16), ("g_pm", 16), ("g_pf", 16), ("g_ff", 16), ("mu_r", 8), ("mu_k", 8), ("mu_v", 8),
               ("mu_wa", 1), ("mu_g1", 1), ("mu_g2", 1), ("w0", 8), ("a0", 8), ("k_k", 8), ("k_a", 8), ("r_k", 8),
               ("gn_w", 8), ("gn_b", 8), ("v0", 8), ("subln", 1), ("cw0", 44), ("cw1", 44), ("cw2", 44), ("cb", 44),
               ("lq1", 64), ("lk1", 64), ("lq2", 64), ("lk2", 64)):
    CV[_n] = _o
    _o += _w
NCV = _o
DV = {}
_o = 0
for _n, _w in (("om_r", 8), ("om_k", 8), ("om_v", 8), ("om_wa", 1), ("om_g1", 1), ("om_g2", 1), ("omka", 8), ("lam", 1), ("nlam", 1), ("sub2", 1)):
    DV[_n] = _o
    _o += _w
NDV = _o

CN = {"ident": 0, "bones": 128, "ones": 256, "perm": 384, "m2": 512, "msl": 768, "invf": 896, "sign": 897, "dmask": 898, "scanm": 2946, "hm": 3458}
NCNP = 898
NCN = 3458 + 7 * 256


def make_consts():
    c = np.zeros((128, NCN), np.float32)
    p = np.arange(128)
    c[:, 0:128] = np.eye(128)
    c[:, 128:256] = (p[:, None] // 64 == p[None, :] // 64)
    c[:, 256:384] = 1.0
    part = p.copy()
    for b in (0, 64):
        for i in range(8):
            part[b + i] = b + i + 8
            part[b + 8 + i] = b + i
    perm = np.zeros((128, 128), np.float32)
    for m in range(128):
        if part[m] != m:
            perm[part[m], m] = 1.0
    c[:, 384:512] = perm
    c[:, 512:640] = (p[:, None] < p[None, :])
    c[:, 640:768] = (p[:, None] <= p[None, :])
    c[:, 768:896] = (p[None, :] < p[:, None])
    q = np.arange(512)
    for j in range(4):
        c[:, 898 + j * 512:898 + (j + 1) * 512] = ((j * 128 + p)[:, None] <= q[None, :])
    invf = np.zeros(128, np.float64)
    sign = np.zeros(128, np.float32)
    fr = 500000.0 ** (-np.arange(0, 16, 2, dtype=np.float32) / 16)
    for b in (0, 64):
        for i in range(8):
            invf[b + i] = fr[i]
            invf[b + 8 + i] = fr[i]
            sign[b + i] = -1.0
            sign[b + 8 + i] = 1.0
    c[:, 896] = invf.astype(np.float32)
    c[:, 897] = sign
    sm = np.ones(512, np.float32)
    sm[::128] = 0.0
    c[:, 2946:2946 + 512] = sm[None, :]
    for k in range(7):
        t = p[:, None]
        s_ = p[None, :]
        mk = ((t >> (k + 1)) == (s_ >> (k + 1))) & (((t >> k) & 1) == 1) & (((s_ >> k) & 1) == 0)
        c[:, 3458 + k * 256:3458 + k * 256 + 128] = mk
        c[:, 3458 + k * 256 + 128:3458 + (k + 1) * 256] = mk.T
    return c


class Buf:
    __slots__ = ("w", "r")

    def __init__(self):
        self.w = {}
        self.r = {}


class Eng:
    def __init__(self, name, h, sem):
        self.name, self.h, self.sem, self.cnt, self.seen = name, h, sem, 0, {}


class KB:
    def __init__(self, nc, es, n_epochs=1):
        self.nc = nc
        self.E = {}
        self.sems = {}
        self.keyeng = {}
        self.dead = set()
        self.pool_sems = {}
        hs = (("pe", nc.tensor), ("act", nc.scalar), ("dve", nc.vector), ("pool", nc.gpsimd), ("sp", nc.sync))
        for name, h in hs:
            self.pool_sems[name] = [es.enter_context(nc.semaphore("s_%s%d" % (name, i))) for i in range(n_epochs if name != "sp" else 1)]
            e = Eng(name, h, self.pool_sems[name][0])
            e.key = name + "#0"
            e.epoch = 0
            self.E[name] = e
            self.sems[e.key] = (e.sem, 1)
            self.keyeng[e.key] = e
        self.slots = {}
        for q, n in (("sp", 10),):
            sl = []
            for i in range(n):
                key = "d%s%d" % (q, i)
                sem = es.enter_context(nc.semaphore("s_" + key))
                self.sems[key] = (sem, 16)
                sl.append([key, 0])
            self.slots[q] = [sl, 0]

    def new_epoch(self):
        self.barrier()
        for name in ("pe", "act", "dve", "pool"):
            e = self.E[name]
            if e.epoch + 1 >= len(self.pool_sems[name]):
                continue
            self.dead.add(e.key)
            e.epoch += 1
            e.sem = self.pool_sems[name][e.epoch]
            e.cnt = 0
            e.key = "%s#%d" % (name, e.epoch)
            self.sems[e.key] = (e.sem, 1)
            self.keyeng[e.key] = e

    def _need(self, e, key, n, raw):
        if key in self.dead:
            return
        if key == e.key and (not raw) and e.name == "pe":
            return
        if e.seen.get(key, 0) >= n:
            return
        sem, unit = self.sems[key]
        if key in self.keyeng and key != e.key:
            assert n <= self.keyeng[key].cnt, ("pending ticket", key, n)
        e.h.wait_ge(sem, n * unit)
        e.seen[key] = n

    def _deps(self, e, reads, writes):
        for b in reads:
            for k, n in b.w.items():
                self._need(e, k, n, True)
        for b in writes:
            for k, n in b.w.items():
                self._need(e, k, n, False)
            for k, n in b.r.items():
                self._need(e, k, n, False)

    def op(self, eng, fn, reads=(), writes=(), inc=True):
        e = self.E[eng]
        self._deps(e, reads, writes)
        ins = fn(e.h)
        if inc:
            ins.then_inc(e.sem, 1)
            e.cnt += 1
            t = e.cnt
        else:
            t = e.cnt + 1
        for b in writes:
            b.w = {e.key: t}
            b.r = {}
        for b in reads:
            b.r[e.key] = t
        return ins

    def dma(self, q, out, in_, reads=(), writes=(), store=False):
        e = self.E[q]
        if store:
            for b in reads:
                for k, n in b.w.items():
                    self._need(e, k, n, True)
            for b in writes:
                for k, n in b.r.items():
                    self._need(e, k, n, False)
        else:
            self._deps(e, reads, writes)
        sl, idx = self.slots[q]
        key, cnt = sl[idx]
        self.slots[q][1] = (idx + 1) % len(sl)
        if cnt > 0:
            self._need(e, key, cnt, True)
        sem, unit = self.sems[key]
        e.h.dma_start(out=out, in_=in_).then_inc(sem, 16)
        sl[idx][1] = cnt + 1
        for b in writes:
            if store:
                b.w = {k: v for k, v in b.w.items() if k.startswith("d")}
                b.w[key] = cnt + 1
            else:
                b.w = {key: cnt + 1}
            b.r = {}
        for b in reads:
            b.r[key] = cnt + 1

    def barrier(self):
        for en in ("pe", "act", "dve", "pool", "sp"):
            e = self.E[en]
            for k2 in ("pe", "act", "dve", "pool"):
                o = self.E[k2]
                if k2 != en and o.cnt > 0:
                    self._need(e, o.key, o.cnt, True)
            for q in self.slots:
                for key, cnt in self.slots[q][0]:
                    if cnt > 0:
                        self._need(e, key, cnt, True)

    def finish(self):
        e = self.E["sp"]
        for q in self.slots:
            for key, cnt in self.slots[q][0]:
                if cnt > 0:
                    self._need(e, key, cnt, True)


_UID = [0]


class Ring:
    def __init__(self, es, nc, name, n, shape, dt, psum=False):
        self.t = []
        for i in range(n):
            _UID[0] += 1
            t = es.enter_context((nc.psum_tensor if psum else nc.sbuf_tensor)("rg_%s_%d" % (name, _UID[0]), shape, dt))
            self.t.append((t, Buf()))
        self.i = 0

    def next(self):
        r = self.t[self.i]
        self.i = (self.i + 1) % len(self.t)
        return r


def build(S, NL, dbg=False, parts=('mix', 'ffn')):
    nc = bass.Bass("TRN2", target_bir_lowering=False)
    NTB = S // 512
    NT = S // 128

    def din(name, shape, dt=F32):
        return nc.dram_tensor(name, shape, dt, kind="ExternalInput").ap()

    x_in = din("x", [S, D])
    pos_in = din("pos", [1, S], I32)
    cv_in = din("cv", [128, NL * NCV])
    cn_in = din("cn", [128, NCN])
    w_in = din("w_in", [NL, D, INC])
    w_mv = din("w_mv", [max(NL - 1, 1), D, 32])
    w2_in = din("w2", [NL, 64, RW])
    a2_in = din("a2", [NL, 64, RW])
    g2_in = din("g2", [NL, 160, RW])
    v2_in = din("v2", [max(NL - 1, 1), 32, RW])
    w_out = din("w_out", [NL, D, D])
    w_up = din("w_up", [NL, D, 2 * FF])
    w_down = din("w_down", [NL, FF, D])
    y_out = nc.dram_tensor("y", [S, D], F32, kind="ExternalOutput").ap()

    kindS = "ExternalOutput" if dbg else "Internal"

    def dsc(name, shape, dt):
        return nc.dram_tensor(name, shape, dt, kind=kindS).ap()

    xT = dsc("xT", [D, S], F32)
    yT = dsc("yT", [D, S], F32)
    rT = dsc("rT", [RW, S], BF16)
    kT = dsc("kT", [RW, S], BF16)
    vT = dsc("vT", [RW, S], BF16)
    vfT = dsc("vfT", [RW, S], BF16)
    waT = dsc("waT", [128, S], BF16)
    g1T = dsc("g1T", [128, S], BF16)
    g2T = dsc("g2T", [64, S], BF16)
    qT = dsc("qT", [RW, S], BF16)
    kdT = dsc("kdT", [RW, S], BF16)
    vd = dsc("vd", [S, RW], BF16)
    mixT = dsc("mixT", [D, S], BF16)
    actT = dsc("actT", [FF, S], BF16)
    rotC = dsc("rotC", [128, S], F32)
    rotS = dsc("rotS", [128, S], F32)
    DB = {}

    def db(name, i=0):
        k = (name, i)
        if k not in DB:
            DB[k] = Buf()
        return DB[k]

    with ExitStack() as es0:
        kb = KB(nc, es0, n_epochs=NL + 1)
        op, dma = kb.op, kb.dma

        @contextmanager
        def scope():
            with ExitStack() as e_:
                yield e_
                kb.barrier()

        def sb(es, name, shape, dt=F32):
            _UID[0] += 1
            return es.enter_context(nc.sbuf_tensor("sb_%s_%d" % (name, _UID[0]), shape, dt))

        cn = sb(es0, "cn", [128, NCNP]); cn_b = Buf()
        cv = sb(es0, "cv", [128, NL * NCV]); cv_b = Buf()
        dv = sb(es0, "dv", [128, NL * NDV]); dv_b = Buf()
        identb = sb(es0, "identb", [128, 128], BF16)
        bonesb = sb(es0, "bonesb", [128, 128], BF16)
        onesb = sb(es0, "onesb", [128, 128], BF16)
        permb = sb(es0, "permb", [128, 128], BF16)
        cb_b = Buf()
        PS = [es0.enter_context(nc.psum_tensor("ps%d" % i, [128, 512], F32)) for i in range(7)]
        PSB = [Buf() for _ in range(7)]
        PSH = es0.enter_context(nc.psum_tensor("psh", [128, 1024], BF16)); PSH_b = [Buf()] * 4

        dma("sp", cn[:], cn_in[:, 0:NCNP], writes=[cn_b])
        dma("sp", cv[:], cv_in[:, :], writes=[cv_b])
        for t, o in ((identb, CN["ident"]), (bonesb, CN["bones"]), (onesb, CN["ones"]), (permb, CN["perm"])):
            op("dve", lambda h, t=t, o=o: h.tensor_copy(out=t[:], in_=cn[:, o:o + 128]), reads=[cn_b], writes=[cb_b])

        def cvc(l, name, i=0, n=1, p0=0, p1=128):
            o = l * NCV + CV[name] + i
            return cv[p0:p1, o:o + n]

        def dvc(l, name, i=0, n=1, p0=0, p1=128):
            o = l * NDV + DV[name] + i
            return dv[p0:p1, o:o + n]

        for l in range(NL):
            for a, b_, n in (("om_r", "mu_r", 8), ("om_k", "mu_k", 8), ("om_v", "mu_v", 8), ("om_wa", "mu_wa", 1),
                             ("om_g1", "mu_g1", 1), ("om_g2", "mu_g2", 1), ("omka", "k_a", 8)):
                op("dve", lambda h, l=l, a=a, b_=b_, n=n: h.tensor_scalar(out=dvc(l, a, 0, n), in0=cvc(l, b_, 0, n), scalar1=-1.0, scalar2=1.0,
                                                                        op0=ALU.mult, op1=ALU.add), reads=[cv_b], writes=[dv_b])
        with scope() as es:
            tmp = sb(es, "lamtmp", [128, 64]); tb_ = Buf()
            acc = sb(es, "lamacc", [128, 4]); ab_ = Buf()
            for l in range(NL):
                li = 0.8 - 0.6 * math.exp(-0.3 * l)
                for j, (qa, ka) in enumerate((("lq1", "lk1"), ("lq2", "lk2"))):
                    op("dve", lambda h, l=l, qa=qa, ka=ka: h.tensor_tensor(out=tmp[:], in0=cvc(l, qa, 0, 64), in1=cvc(l, ka, 0, 64), op=ALU.mult),
                       reads=[cv_b], writes=[tb_])
                    op("dve", lambda h, j=j: h.reduce_sum(out=acc[:, j:j + 1], in_=tmp[:], axis=mybir.AxisListType.X), reads=[tb_], writes=[ab_])
                op("act", lambda h: h.activation(out=acc[:, 2:4], in_=acc[:, 0:2], func=AF.Exp), reads=[ab_], writes=[ab_])
                op("dve", lambda h, l=l: h.tensor_tensor(out=dvc(l, "lam"), in0=acc[:, 2:3], in1=acc[:, 3:4], op=ALU.subtract), reads=[ab_, dv_b], writes=[dv_b])
                op("dve", lambda h, l=l, li=li: h.tensor_scalar(out=dvc(l, "nlam"), in0=dvc(l, "lam"), scalar1=float(li), scalar2=-1.0, op0=ALU.add, op1=ALU.mult),
                   reads=[dv_b], writes=[dv_b])
                op("dve", lambda h, l=l, li=li: h.tensor_scalar(out=dvc(l, "sub2"), in0=cvc(l, "subln"), scalar1=float(1.0 - li), scalar2=None, op0=ALU.mult),
                   reads=[cv_b, dv_b], writes=[dv_b])

        with scope() as es:
          if 'norot' not in parts:
              pi_ = sb(es, "posi", [128, 512], I32); pib = Buf()
              pf = sb(es, "posf", [128, 512]); pfb = Buf()
              t1 = sb(es, "rt1", [128, 512]); t1b = Buf()
              t2 = sb(es, "rt2", [128, 512]); t2b = Buf()
              ki = sb(es, "rki", [128, 512], I32); kib = Buf()
              ro = Ring(es, nc, "rto", 2, [128, 512], F32)
              TWO_PI = float(2 * np.pi)
              for tb in range(NTB):
                  ts = slice(tb * 512, (tb + 1) * 512)
                  dma("sp", pi_[:], pos_in[0:1, ts].partition_broadcast(128), writes=[pib])
                  op("dve", lambda h: h.tensor_copy(out=pf[:], in_=pi_[:]), reads=[pib], writes=[pfb])
                  op("dve", lambda h: h.tensor_scalar(out=pf[:], in0=pf[:], scalar1=cn[:, CN["invf"]:CN["invf"] + 1], scalar2=None, op0=ALU.mult),
                     reads=[pfb, cn_b], writes=[pfb])
                  for which, off, dst in (("c", float(np.pi / 2), rotC), ("s", 0.0, rotS)):
                      op("dve", lambda h, off=off: h.tensor_scalar(out=t1[:], in0=pf[:], scalar1=off, scalar2=None, op0=ALU.add), reads=[pfb], writes=[t1b])
                      op("dve", lambda h: h.tensor_scalar(out=t2[:], in0=t1[:], scalar1=float(1 / (2 * np.pi)), scalar2=None, op0=ALU.mult), reads=[t1b], writes=[t2b])
                      op("dve", lambda h: h.tensor_copy(out=ki[:], in_=t2[:]), reads=[t2b], writes=[kib])
                      op("dve", lambda h: h.tensor_copy(out=t2[:], in_=ki[:]), reads=[kib], writes=[t2b])
                      op("dve", lambda h: h.scalar_tensor_tensor(out=t1[:], in0=t2[:], scalar=-TWO_PI, in1=t1[:], op0=ALU.mult, op1=ALU.add), reads=[t2b, t1b], writes=[t1b])
                      op("dve", lambda h: h.tensor_scalar(out=t2[:], in0=t1[:], scalar1=float(np.pi), scalar2=-TWO_PI, op0=ALU.is_gt, op1=ALU.mult), reads=[t1b], writes=[t2b])
                      op("dve", lambda h: h.tensor_tensor(out=t1[:], in0=t1[:], in1=t2[:], op=ALU.add), reads=[t1b, t2b], writes=[t1b])
                      op("dve", lambda h: h.tensor_scalar(out=t2[:], in0=t1[:], scalar1=float(-np.pi), scalar2=TWO_PI, op0=ALU.is_lt, op1=ALU.mult), reads=[t1b], writes=[t2b])
                      op("dve", lambda h: h.tensor_tensor(out=t1[:], in0=t1[:], in1=t2[:], op=ALU.add), reads=[t1b, t2b], writes=[t1b])
                      o_, ob_ = ro.next()
                      op("act", lambda h, o_=o_: h.activation(out=o_[:], in_=t1[:], func=AF.Sin), reads=[t1b], writes=[ob_])
                      if which == "s":
                          op("dve", lambda h, o_=o_: h.tensor_scalar(out=o_[:], in0=o_[:], scalar1=cn[:, CN["sign"]:CN["sign"] + 1], scalar2=None, op0=ALU.mult),
                             reads=[ob_, cn_b], writes=[ob_])
                      dma("sp", dst[:, ts], o_[:], reads=[ob_], writes=[db("rot")], store=True)

        with scope() as es:
            xr = Ring(es, nc, "xin", 2, [128, D], F32)
            xo = Ring(es, nc, "xto", 2, [128, KC, 128], F32)
            for tt in range(NT):
                xi, xib = xr.next()
                dma("sp", xi[:], x_in[tt * 128:(tt + 1) * 128, :], writes=[xib])
                o_, ob_ = xo.next()
                for kc in range(KC):
                    pi = kc % 4
                    op("pe", lambda h, kc=kc, pi=pi: h.transpose(out=PS[pi][:, 0:128], in_=xi[:, kc * 128:(kc + 1) * 128], identity=cn[:, 0:128]),
                       reads=[xib, cn_b], writes=[PSB[pi]])
                    op("act" if kc % 2 else "dve", lambda h, kc=kc, pi=pi: (h.copy if kc % 2 else h.tensor_copy)(out=o_[:, kc, :], in_=PS[pi][:, 0:128]),
                       reads=[PSB[pi]], writes=[ob_])
                dma("sp", xT[:, tt * 128:(tt + 1) * 128].rearrange("(kc p) t -> p kc t", p=128), o_[:], reads=[ob_], writes=[db("xT", kc) for kc in range(KC)], store=True)

        def norm_to_hT(es, l, gname, hT, hTb):
            xs = Ring(es, nc, "nxs", 16, [128, 512], F32)
            sq = Ring(es, nc, "nsq", 2, [128, 512], BF16)
            rs = sb(es, "nrs", [128, 512]); rsb = Buf()
            for tb in range(NTB):
                ts = slice(tb * 512, (tb + 1) * 512)
                tl = []
                for kc in range(KC):
                    x_, xb_ = xs.next()
                    dma("sp", x_[:], xT[kc * 128:(kc + 1) * 128, ts], reads=[db("xT", kc)], writes=[xb_])
                    s_, sb_ = sq.next()
                    op("act", lambda h, x_=x_, s_=s_: h.activation(out=s_[:], in_=x_[:], func=AF.Square), reads=[xb_], writes=[sb_])
                    op("pe", lambda h, s_=s_, kc=kc: h.matmul(PS[6][:], lhsT=onesb[:], rhs=s_[:], start=(kc == 0), stop=(kc == KC - 1)),
                       reads=[sb_, cb_b], writes=[PSB[6]])
                    tl.append((x_, xb_))
                op("dve", lambda h: h.tensor_scalar(out=rs[:], in0=PS[6][:], scalar1=1.0 / D, scalar2=NORM_EPS, op0=ALU.mult, op1=ALU.add), reads=[PSB[6]], writes=[rsb])
                op("act", lambda h: h.activation(out=rs[:], in_=rs[:], func=AF.Sqrt), reads=[rsb], writes=[rsb])
                op("dve", lambda h: h.reciprocal(out=rs[:], in_=rs[:]), reads=[rsb], writes=[rsb])
                for kc in range(KC):
                    x_, xb_ = tl[kc]
                    op("dve", lambda h, x_=x_, kc=kc: h.scalar_tensor_tensor(out=hT[:, kc, ts], in0=x_[:], scalar=cvc(l, gname, kc), in1=rs[:],
                                                                                                   op0=ALU.mult, op1=ALU.mult),
                       reads=[xb_, rsb, cv_b], writes=[hTb[tb]])

        def proj_fm(es, hT, hTb, tasks, nper=1, each=False):
            st = Ring(es, nc, "wst", 2 if nper == 1 else 3, [128, KC, 128], F32)
            wb = Ring(es, nc, "wbf", 3 if nper == 1 else 4, [128, KC, 128], BF16)
            psr = [0]
            cvi = [0]
            groups = [tasks[ti:ti + nper] for ti in range(0, len(tasks), nper)]

            def load(grp):
                wts = []
                for segs, M, epi in grp:
                    s_, sb_ = st.next()
                    o = 0
                    for ap, n in segs:
                        dma("sp", s_[:, :, o:o + n], ap.rearrange("(kc p) n -> p kc n", p=128), writes=[sb_])
                        o += n
                    w_, wb_ = wb.next()
                    cvi[0] += 1
                    if cvi[0] % 2:
                        op("act", lambda h, s_=s_, w_=w_, M=M: h.copy(out=w_[:, :, 0:M], in_=s_[:, :, 0:M]), reads=[sb_], writes=[wb_])
                    else:
                        op("dve", lambda h, s_=s_, w_=w_, M=M: h.tensor_copy(out=w_[:, :, 0:M], in_=s_[:, :, 0:M]), reads=[sb_], writes=[wb_])
                    wts.append((w_, wb_, M))
                return wts
            nxt = load(groups[0])
            for gi, grp in enumerate(groups):
                wts = nxt
                if gi + 1 < len(groups):
                    nxt = load(groups[gi + 1])
                for tb in range(NTB):
                    ts = slice(tb * 512, (tb + 1) * 512)
                    pss = []
                    for w_, wb_, M in wts:
                        pi = psr[0] % 4
                        psr[0] += 1
                        for kc in range(KC):
                            op("pe", lambda h, w_=w_, M=M, kc=kc, pi=pi: h.matmul(PS[pi][0:M, :], lhsT=w_[:, kc, 0:M], rhs=hT[:, kc, ts], start=(kc == 0), stop=(kc == KC - 1)),
                               reads=[wb_, hTb[tb]], writes=[PSB[pi]], inc=(kc == KC - 1))
                        pss.append(pi)
                    if each:
                        for t_, pi_ in zip(grp, pss):
                            t_[2](tb, [pi_])
                    else:
                        grp[0][2](tb, pss)

        def mk_rings(es):
            return {"t": Ring(es, nc, "ept", 2, [128, 512], F32), "of": Ring(es, nc, "epof", 2, [128, 512], F32),
                    "ob": Ring(es, nc, "epob", 3, [128, 512], BF16), "car": sb(es, "epcar", [128, 64]), "ncar": [0]}

        def shift_epi(R, name, M, mu_ap, om_ap, dests, act=None):
            tr = R["t"]
            orr = R["of"] if act else R["ob"]
            obr = R["ob"] if act else None
            ci = R["ncar"][0]
            R["ncar"][0] += 1
            carry = R["car"][:, ci:ci + 1]; cb = Buf()

            def epi(tb, pss):
                pi = pss[0]
                ts = slice(tb * 512, (tb + 1) * 512)
                t_, tb_ = tr.next()
                o_, ob_ = orr.next()
                op("act", lambda h: h.activation(out=t_[0:M, :], in_=PS[pi][0:M, :], func=AF.Identity, scale=om_ap), reads=[PSB[pi], dv_b], writes=[tb_])
                op("dve", lambda h: h.scalar_tensor_tensor(out=o_[0:M, 1:512], in0=PS[pi][0:M, 0:511], scalar=mu_ap, in1=t_[0:M, 1:512], op0=ALU.mult, op1=ALU.add),
                   reads=[PSB[pi], tb_, cv_b], writes=[ob_])
                if tb == 0:
                    op("dve", lambda h: h.tensor_copy(out=o_[0:M, 0:1], in_=t_[0:M, 0:1]), reads=[tb_], writes=[ob_])
                else:
                    op("dve", lambda h: h.scalar_tensor_tensor(out=o_[0:M, 0:1], in0=carry[0:M, :], scalar=mu_ap, in1=t_[0:M, 0:1], op0=ALU.mult, op1=ALU.add),
                       reads=[cb, tb_, cv_b], writes=[ob_])
                op("act", lambda h: h.copy(out=carry[0:M, :], in_=PS[pi][0:M, 511:512]), reads=[PSB[pi]], writes=[cb])
                if act:
                    f_, fb_ = obr.next()
                    for p0, p1, fn in act:
                        if fn is None:
                            op("dve", lambda h, p0=p0, p1=p1: h.tensor_copy(out=f_[p0:p1, :], in_=o_[p0:p1, :]), reads=[ob_], writes=[fb_])
                        else:
                            op("act", lambda h, p0=p0, p1=p1, fn=fn: h.activation(out=f_[p0:p1, :], in_=o_[p0:p1, :], func=fn), reads=[ob_], writes=[fb_])
                    o_, ob_ = f_, fb_
                for dap, dbuf in dests:
                    dma("sp", dap[:, ts], o_[0:M, :], reads=[ob_], writes=[dbuf], store=True)
            return epi

        def plain_epi(R, name, dest, dbuf, flip):
            orr = R["ob"]

            def epi(tb, pss):
                pi = pss[0]
                o_, ob_ = orr.next()
                if (tb + flip) % 2:
                    op("act", lambda h: h.copy(out=o_[:], in_=PS[pi][:]), reads=[PSB[pi]], writes=[ob_])
                else:
                    op("dve", lambda h: h.tensor_copy(out=o_[:], in_=PS[pi][:]), reads=[PSB[pi]], writes=[ob_])
                dma("sp", dest[:, tb * 512:(tb + 1) * 512], o_[:], reads=[ob_], writes=[dbuf], store=True)
            return epi

        def proj_res(es, l, src, srcname, KCn, W, gname):
            TBD = min(1024, S)
            NH = TBD // 512
            srct = sb(es, "prs", [128, KCn, TBD], BF16); srcb = Buf()
            KH = KCn // 4
            st = Ring(es, nc, "prst", 4, [128, KH, 128], F32)
            wbr = Ring(es, nc, "prwb", 3, [128, KCn, 128], BF16)
            yr = Ring(es, nc, "pry", 3, [128, TBD], F32)
            sqr = Ring(es, nc, "prsq", 2, [128, 512], BF16)
            rst = sb(es, "prrs", [128, TBD]); rsb = Buf()
            xr = Ring(es, nc, "prx", 3, [128, TBD], F32)
            for tbd in range(S // TBD):
                ts = slice(tbd * TBD, (tbd + 1) * TBD)
                for kc in range(KCn):
                    dma("sp", srct[:, kc, :], src[kc * 128:(kc + 1) * 128, ts], reads=[db(srcname, kc)], writes=[srcb])
                def loadw(c):
                    w_, wb_ = wbr.next()
                    for hf in range(4):
                        s_, sb_ = st.next()
                        dma("sp", s_[:], W[hf * KH * 128:(hf + 1) * KH * 128, c * 128:(c + 1) * 128].rearrange("(kc p) n -> p kc n", p=128), writes=[sb_])
                        if hf % 2:
                            op("act", lambda h, s_=s_, w_=w_, hf=hf: h.copy(out=w_[:, hf * KH:(hf + 1) * KH, :], in_=s_[:]), reads=[sb_], writes=[wb_])
                        else:
                            op("dve", lambda h, s_=s_, w_=w_, hf=hf: h.tensor_copy(out=w_[:, hf * KH:(hf + 1) * KH, :], in_=s_[:]), reads=[sb_], writes=[wb_])
                    return w_, wb_
                nxtw = loadw(0)
                for c in range(KC):
                    w_, wb_ = nxtw
                    if c + 1 < KC:
                        nxtw = loadw(c + 1)
                    y_, yb_ = yr.next()
                    for hh in range(NH):
                        pi = (c * NH + hh) % 4
                        for kc in range(KCn):
                            op("pe", lambda h, w_=w_, kc=kc, pi=pi, hh=hh: h.matmul(PS[pi][:], lhsT=w_[:, kc, :], rhs=srct[:, kc, hh * 512:(hh + 1) * 512],
                                                                                   start=(kc == 0), stop=(kc == KCn - 1)),
                               reads=[wb_, srcb], writes=[PSB[pi]], inc=(kc == KCn - 1))
                        op("dve", lambda h, y_=y_, pi=pi, hh=hh: h.tensor_copy(out=y_[:, hh * 512:(hh + 1) * 512], in_=PS[pi][:]), reads=[PSB[pi]], writes=[yb_])
                        q_, qb_ = sqr.next()
                        op("act", lambda h, q_=q_, y_=y_, hh=hh: h.activation(out=q_[:], in_=y_[:, hh * 512:(hh + 1) * 512], func=AF.Square), reads=[yb_], writes=[qb_])
                        op("pe", lambda h, q_=q_, hh=hh, c=c: h.matmul(PS[4 + hh][:], lhsT=onesb[:], rhs=q_[:], start=(c == 0), stop=(c == KC - 1)),
                           reads=[qb_, cb_b], writes=[PSB[4 + hh]])
                    dma("sp", yT[c * 128:(c + 1) * 128, ts], y_[:], reads=[yb_], writes=[db("yT", c)], store=True)
                for hh in range(NH):
                    hs = slice(hh * 512, (hh + 1) * 512)
                    op("dve", lambda h, hh=hh, hs=hs: h.tensor_scalar(out=rst[:, hs], in0=PS[4 + hh][:], scalar1=1.0 / D, scalar2=NORM_EPS, op0=ALU.mult, op1=ALU.add),
                       reads=[PSB[4 + hh]], writes=[rsb])
                op("act", lambda h: h.activation(out=rst[:], in_=rst[:], func=AF.Sqrt), reads=[rsb], writes=[rsb])
                op("dve", lambda h: h.reciprocal(out=rst[:], in_=rst[:]), reads=[rsb], writes=[rsb])
                def loadxy(c):
                    y_, yb_ = yr.next()
                    x_, xb_ = xr.next()
                    dma("sp", y_[:], yT[c * 128:(c + 1) * 128, ts], reads=[db("yT", c)], writes=[yb_])
                    dma("sp", x_[:], xT[c * 128:(c + 1) * 128, ts], reads=[db("xT", c)], writes=[xb_])
                    return y_, yb_, x_, xb_
                nxy = loadxy(0)
                for c in range(KC):
                    y_, yb_, x_, xb_ = nxy
                    if c + 1 < KC:
                        nxy = loadxy(c + 1)
                    op("dve", lambda h, y_=y_, c=c: h.scalar_tensor_tensor(out=y_[:], in0=y_[:], scalar=cvc(l, gname, c), in1=rst[:], op0=ALU.mult, op1=ALU.mult),
                       reads=[yb_, rsb, cv_b], writes=[yb_])
                    op("dve", lambda h, y_=y_, x_=x_: h.tensor_tensor(out=x_[:], in0=x_[:], in1=y_[:], op=ALU.add), reads=[yb_, xb_], writes=[xb_])
                    dma("sp", xT[c * 128:(c + 1) * 128, ts], x_[:], reads=[xb_], writes=[db("xT", c)], store=True)


        def attn_phase(es, l):
            C = sb(es, "arC", [128, S]); Cb = Buf()
            Sg = sb(es, "arS", [128, S]); Sgb = Buf()
            dma("sp", C[:], rotC[:, :], reads=[db("rot")], writes=[Cb])
            dma("sp", Sg[:], rotS[:, :], reads=[db("rot")], writes=[Sgb])
            dmf = sb(es, "admf", [128, 2048]); dmfb = Buf()
            dm = sb(es, "adm", [128, 4, 512], BF16); dmb = Buf()
            dma("sp", dmf[:], cn_in[:, CN["dmask"]:CN["dmask"] + 2048], writes=[dmfb])
            op("pool", lambda h: h.tensor_copy(out=dm[:], in_=dmf[:].rearrange("p (a b) -> p a b", a=4)), reads=[dmfb], writes=[dmb])
            raws = Ring(es, nc, "araw", 2, [128, 512], BF16)
            t1r = Ring(es, nc, "at1", 2, [128, 512], F32)
            t2r = Ring(es, nc, "at2", 2, [128, 512], F32)
            qk = Ring(es, nc, "aqk", 4, [128, S], BF16)
            Vr = Ring(es, nc, "aV", 2, [128, NT, 128], BF16)
            Er = Ring(es, nc, "aE", 4, [128, 512], BF16)
            wk = Ring(es, nc, "awk", 6, [128, 512], F32)
            sqr = Ring(es, nc, "asq", 2, [128, 512], BF16)
            outr = Ring(es, nc, "aout", 2, [128, 512], BF16)
            for hd in range(8):
                rot = []
                for src, nm in ((qT, "qT"), (kdT, "kdT")):
                    d_, db_ = qk.next()
                    for tb in range(NTB):
                        ts = slice(tb * 512, (tb + 1) * 512)
                        r_, rb_ = raws.next()
                        dma("sp", r_[:], src[hd * 128:(hd + 1) * 128, ts], reads=[db(nm, hd)], writes=[rb_])
                        op("pe", lambda h, r_=r_: h.matmul(PS[0][:], lhsT=permb[:], rhs=r_[:], start=True, stop=True), reads=[rb_, cb_b], writes=[PSB[0]])
                        a_, ab_ = t1r.next()
                        b_, bb_ = t2r.next()
                        op("dve", lambda h, r_=r_, a_=a_, ts=ts: h.tensor_tensor(out=a_[:], in0=r_[:], in1=C[:, ts], op=ALU.mult), reads=[rb_, Cb], writes=[ab_])
                        op("dve", lambda h, b_=b_, ts=ts: h.tensor_tensor(out=b_[:], in0=PS[0][:], in1=Sg[:, ts], op=ALU.mult), reads=[PSB[0], Sgb], writes=[bb_])
                        op("dve", lambda h, a_=a_, b_=b_, d_=d_, ts=ts: h.tensor_tensor(out=d_[:, ts], in0=a_[:], in1=b_[:], op=ALU.add), reads=[ab_, bb_], writes=[db_])
                    rot.append((d_, db_))
                (q_, qb_), (k_, kb_) = rot
                V_, Vb_ = Vr.next()
                dma("sp", V_[:], vd[:, hd * 128:(hd + 1) * 128].rearrange("(tt p) c -> p tt c", p=128), reads=[db("vd", hd)], writes=[Vb_])
                for qb in range(NTB):
                    qs = slice(qb * 512, (qb + 1) * 512)
                    nk = 4 * qb + 4
                    steps = [(kt, m) for kt in range(nk) for m in range(2)]

                    def emit_s(i):
                        kt, m = steps[i]
                        pi = i % 3
                        op("pe", lambda h: h.matmul(PS[pi][:], lhsT=k_[m * 64:(m + 1) * 64, kt * 128:(kt + 1) * 128], rhs=q_[m * 64:(m + 1) * 64, qs],
                                                    start=True, stop=True), reads=[kb_, qb_], writes=[PSB[pi]])
                    emit_s(0)
                    emit_s(1)
                    for i, (kt, m) in enumerate(steps):
                        pi = i % 3
                        if i + 2 < len(steps):
                            emit_s(i + 2)
                        e_, eb_ = Er.next()
                        op("act", lambda h, e_=e_, pi=pi: h.activation(out=e_[:], in_=PS[pi][:], func=AF.Exp, scale=0.125), reads=[PSB[pi]], writes=[eb_])
                        if kt >= 4 * qb:
                            op("dve", lambda h, e_=e_, j=kt - 4 * qb: h.tensor_tensor(out=e_[:], in0=e_[:], in1=dm[:, j, :], op=ALU.mult), reads=[eb_, dmb], writes=[eb_])
                        op("pe", lambda h, e_=e_, kt=kt, m=m: h.matmul(PS[3 + m][:], lhsT=V_[:, kt, :], rhs=e_[:], start=(kt == 0), stop=(kt == nk - 1)),
                           reads=[Vb_, eb_], writes=[PSB[3 + m]], inc=False)
                        op("pe", lambda h, e_=e_, kt=kt, m=m: h.matmul(PS[5 + m][:], lhsT=onesb[:], rhs=e_[:], start=(kt == 0), stop=(kt == nk - 1)),
                           reads=[cb_b, eb_], writes=[PSB[5 + m]])
                    w = [wk.next() for _ in range(6)]
                    for m in range(2):
                        op("dve", lambda h, m=m: h.reciprocal(out=w[m][0][:], in_=PS[5 + m][:]), reads=[PSB[5 + m]], writes=[w[m][1]])
                        op("dve", lambda h, m=m: h.tensor_tensor(out=w[2 + m][0][:], in0=PS[3 + m][:], in1=w[m][0][:], op=ALU.mult), reads=[PSB[3 + m], w[m][1]], writes=[w[2 + m][1]])
                    op("dve", lambda h: h.scalar_tensor_tensor(out=w[4][0][:], in0=w[3][0][:], scalar=dvc(l, "nlam"), in1=w[2][0][:], op0=ALU.mult, op1=ALU.add),
                       reads=[w[3][1], w[2][1], dv_b], writes=[w[4][1]])
                    s_, sb_ = sqr.next()
                    op("act", lambda h, s_=s_: h.activation(out=s_[:], in_=w[4][0][:], func=AF.Square), reads=[w[4][1]], writes=[sb_])
                    op("pe", lambda h, s_=s_: h.matmul(PS[0][:], lhsT=onesb[:], rhs=s_[:], start=True, stop=True), reads=[sb_, cb_b], writes=[PSB[0]])
                    op("dve", lambda h: h.tensor_scalar(out=w[5][0][:], in0=PS[0][:], scalar1=1.0 / 128, scalar2=SUBLN_EPS, op0=ALU.mult, op1=ALU.add), reads=[PSB[0]], writes=[w[5][1]])
                    op("act", lambda h: h.activation(out=w[5][0][:], in_=w[5][0][:], func=AF.Sqrt), reads=[w[5][1]], writes=[w[5][1]])
                    op("dve", lambda h: h.reciprocal(out=w[5][0][:], in_=w[5][0][:]), reads=[w[5][1]], writes=[w[5][1]])
                    o_, ob_ = outr.next()
                    op("dve", lambda h, o_=o_: h.scalar_tensor_tensor(out=o_[:], in0=w[4][0][:], scalar=dvc(l, "sub2"), in1=w[5][0][:], op0=ALU.mult, op1=ALU.mult),
                       reads=[w[4][1], w[5][1], dv_b], writes=[ob_])
                    dma("sp", mixT[1024 + hd * 128:1024 + (hd + 1) * 128, qs], o_[:], reads=[ob_], writes=[db("mixT", 8 + hd)], store=True)

        def rwkv_phase(es, l):
            f32t = lambda nm, sh=[128, 512]: (sb(es, nm, sh), Buf())
            bft = lambda nm, sh=[128, 512]: (sb(es, nm, sh, BF16), Buf())
            scanm, scb = f32t("scanm")
            dma("sp", scanm[:], cn_in[:, CN["scanm"]:CN["scanm"] + 512], writes=[scb])
            hm, hmb = f32t("hm", [128, 7, 256])
            dma("sp", hm[:], cn_in[:, CN["hm"]:CN["hm"] + 7 * 256].rearrange("p (k c) -> p k c", k=7), writes=[hmb])
            SL = []
            for i in range(8):
                SL.append({"T12": bft("T12_%d" % i, [128, 512]), "T3": bft("T3_%d" % i, [128, 128]), "W1": bft("W1_%d" % i, [128, 128]), "W2": bft("W2_%d" % i, [128, 256]),
                           "p0": bft("p0_%d" % i, [128, 256]), "DG": [bft("DG%d_%d" % (j, i), [128, 256]) for j in range(2)], "ZZ": bft("ZZ_%d" % i, [128, 256]),
                           "tW": bft("tW_%d" % i, [128, 256]), "Xb": bft("Xb_%d" % i, [128, 64]), "Ub": bft("Ub_%d" % i, [128, 64]), "HG": f32t("HG_%d" % i, [128, 64])})
            m2b, m2bb = bft("m2b", [128, 256]); mslb, mslbb = bft("mslb", [128, 128])
            op("pool", lambda h: h.tensor_copy(out=m2b[:], in_=cn[:, CN["m2"]:CN["m2"] + 256]), reads=[cn_b], writes=[m2bb])
            op("pool", lambda h: h.tensor_copy(out=mslb[:], in_=cn[:, CN["msl"]:CN["msl"] + 128]), reads=[cn_b], writes=[mslbb])
            wst, wstb = f32t("lst", [128, 1024])
            wa2b, wa2bb = bft("wa2b", [128, 1024]); g2ab, g2abb = bft("g2ab", [128, 1024]); g2bv, g2bvb = bft("g2bv", [64, 1024])
            dma("sp", wst[0:64, :], w2_in[l], writes=[wstb]); dma("sp", wst[64:128, :], a2_in[l], writes=[wstb])
            op("pool", lambda h: h.tensor_copy(out=wa2b[:], in_=wst[:]), reads=[wstb], writes=[wa2bb])
            dma("sp", wst[:], g2_in[l][0:128, :], writes=[wstb])
            op("pool", lambda h: h.tensor_copy(out=g2ab[:], in_=wst[:]), reads=[wstb], writes=[g2abb])
            dma("sp", wst[0:32, :], g2_in[l][128:160, :], writes=[wstb])
            if l > 0:
                dma("sp", wst[32:64, :], v2_in[l - 1], writes=[wstb])
            op("pool", lambda h: h.tensor_copy(out=g2bv[0:64 if l > 0 else 32, :], in_=wst[0:64 if l > 0 else 32, :]), reads=[wstb], writes=[g2bvb])
            Hf, _ = f32t("Hf", [128, 8, 64]); Hb, _ = bft("Hb", [128, 8, 64])
            Hfb = [Buf(), Buf()]; Hbb = [Buf(), Buf()]
            op("pool", lambda h: h.memset(Hf[:], 0.0), writes=Hfb); op("pool", lambda h: h.memset(Hb[:], 0.0), writes=Hbb)
            wa_t, wab = bft("wa_t"); g1_t, g1b = bft("g1_t"); g2_t, g2b_ = bft("g2_t", [64, 512])
            r_t, rb = bft("r_t"); k_t, kb_ = bft("k_t"); v_t, vb = bft("v_t"); vf_t, vfb = bft("vf_t")
            sg, sgb = f32t("sg"); cs, csb = f32t("cs"); ex, exb = f32t("ex"); eG, eGb = f32t("eG"); eGi, eGib = f32t("eGi"); eGx, eGxb = f32t("eGx")
            a_t, ab = f32t("a_t"); gt, gtb = f32t("gt"); v2t, v2b = f32t("v2t"); kk, kkb = f32t("kk"); sqb_, sqbb = bft("sqb"); rn, rnb = f32t("rn")
            k2, k2b = f32t("k2"); tmp, tmpb = f32t("tmp"); AR, ARb = bft("AR", [128, 2, 512]); BhT, BhTb = bft("BhT"); KhT, KhTb = bft("KhT"); vbf, vbfb = bft("vbf")
            rkb, rkbb = bft("rkb"); bon, bonb = f32t("bon"); OT, _ = f32t("OT"); OTb = [Buf(), Buf()]; ob16, ob16b = bft("ob16"); mean, meanb = f32t("mean"); var, varb = f32t("var")
            Bh = [bft("Bh%d" % i, [128, 128]) for i in range(4)]; Kh = [bft("Kh%d" % i, [128, 128]) for i in range(4)]; Vt = [bft("Vt%d" % i, [128, 128]) for i in range(4)]
            T1, T1b = bft("T1", [128, 256]); T2, T2b = bft("T2", [128, 256]); T3, T3b = bft("T3", [128, 128])
            W1, W1b = bft("W1", [128, 128]); W2, W2b = bft("W2", [128, 256])
            PP = [bft("PP%d" % i, [128, 256]) for i in range(2)]; NTt = [bft("NT%d" % i, [128, 128]) for i in range(2)]
            Xb, Xbb = bft("Xb", [128, 64]); Ub, Ubb = bft("Ub", [128, 64]); mo, mob = bft("mo")
            tt_ = lambda e, o, a, b, opn, rd, wr: op(e, lambda h: h.tensor_tensor(out=o, in0=a, in1=b, op=opn), reads=rd, writes=wr)
            trc = [0]
            G2R = 64 if l > 0 else 32
            for tb in range(NTB):
                ts = slice(tb * 512, (tb + 1) * 512)
                dma("sp", wa_t[:], waT[:, ts], reads=[db("waT")], writes=[wab]); dma("sp", g1_t[:], g1T[:, ts], reads=[db("g1T")], writes=[g1b])
                dma("sp", g2_t[0:G2R, :], g2T[0:G2R, ts], reads=[db("g2T")], writes=[g2b_])
                for c in range(8):
                    cs_ = slice(c * 128, (c + 1) * 128)
                    dma("sp", r_t[:], rT[cs_, ts], reads=[db("rT", c)], writes=[rb]); dma("sp", k_t[:], kT[cs_, ts], reads=[db("kT", c)], writes=[kb_])
                    dma("sp", v_t[:], vT[cs_, ts], reads=[db("vT", c)], writes=[vb])
                    op("pe", lambda h: h.matmul(PS[0][:], lhsT=wa2b[0:64, cs_], rhs=wa_t[0:64, :], start=True, stop=True), reads=[wa2bb, wab], writes=[PSB[0]])
                    op("act", lambda h: h.activation(out=sg[:], in_=PS[0][:], func=AF.Sigmoid, bias=cvc(l, "w0", c)), reads=[PSB[0], cv_b], writes=[sgb])
                    op("dve", lambda h: h.tensor_tensor_scan(out=cs[:], data0=scanm[:], data1=sg[:], initial=0.0, op0=ALU.mult, op1=ALU.add), reads=[scb, sgb], writes=[csb])
                    tt_("dve", ex[:], cs[:], sg[:], ALU.subtract, [csb, sgb], [exb])
                    op("act", lambda h: h.activation(out=eG[:], in_=cs[:], func=AF.Exp, scale=-C0), reads=[csb], writes=[eGb])
                    op("act", lambda h: h.activation(out=eGi[:], in_=cs[:], func=AF.Exp, scale=C0), reads=[csb], writes=[eGib])
                    op("act", lambda h: h.activation(out=eGx[:], in_=ex[:], func=AF.Exp, scale=-C0), reads=[exb], writes=[eGxb])
                    op("pe", lambda h: h.matmul(PS[0][:], lhsT=wa2b[64:128, cs_], rhs=wa_t[64:128, :], start=True, stop=True), reads=[wa2bb, wab], writes=[PSB[0]])
                    op("act", lambda h: h.activation(out=a_t[:], in_=PS[0][:], func=AF.Sigmoid, bias=cvc(l, "a0", c)), reads=[PSB[0], cv_b], writes=[ab])
                    op("pe", lambda h: h.matmul(PS[0][:], lhsT=g2ab[:, cs_], rhs=g1_t[:], start=True, stop=False), reads=[g2abb, g1b], writes=[PSB[0]], inc=False)
                    op("pe", lambda h: h.matmul(PS[0][:], lhsT=g2bv[0:32, cs_], rhs=g2_t[0:32, :], start=False, stop=True), reads=[g2bvb, g2b_], writes=[PSB[0]])
                    op("act", lambda h: h.copy(out=gt[:], in_=PS[0][:]), reads=[PSB[0]], writes=[gtb])
                    if l > 0:
                        dma("sp", vf_t[:], vfT[cs_, ts], reads=[db("vfT", c)], writes=[vfb])
                        op("pe", lambda h: h.matmul(PS[0][:], lhsT=g2bv[32:64, cs_], rhs=g2_t[32:64, :], start=True, stop=True), reads=[g2bvb, g2b_], writes=[PSB[0]])
                        op("act", lambda h: h.activation(out=tmp[:], in_=PS[0][:], func=AF.Sigmoid, bias=cvc(l, "v0", c)), reads=[PSB[0], cv_b], writes=[tmpb])
                        tt_("dve", v2t[:], vf_t[:], v_t[:], ALU.subtract, [vfb, vb], [v2b])
                        tt_("dve", v2t[:], v2t[:], tmp[:], ALU.mult, [v2b, tmpb], [v2b])
                        tt_("dve", v2t[:], v2t[:], v_t[:], ALU.add, [v2b, vb], [v2b])
                    else:
                        op("dve", lambda h: h.tensor_copy(out=v2t[:], in_=v_t[:]), reads=[vb], writes=[v2b])
                    op("pool", lambda h: h.tensor_copy(out=vbf[:], in_=v2t[:]), reads=[v2b], writes=[vbfb])
                    op("dve", lambda h: h.tensor_scalar(out=kk[:], in0=k_t[:], scalar1=cvc(l, "k_k", c), scalar2=None, op0=ALU.mult), reads=[kb_, cv_b], writes=[kkb])
                    tt_("pool", sqb_[:], kk[:], kk[:], ALU.mult, [kkb], [sqbb])
                    op("pe", lambda h: h.matmul(PS[0][:], lhsT=bonesb[:], rhs=sqb_[:], start=True, stop=True), reads=[cb_b, sqbb], writes=[PSB[0]])
                    op("act", lambda h: h.activation(out=rn[:], in_=PS[0][:], func=AF.Sqrt, bias=1e-20), reads=[PSB[0]], writes=[rnb])
                    op("dve", lambda h: h.reciprocal(out=rn[:], in_=rn[:]), reads=[rnb], writes=[rnb])
                    tt_("dve", kk[:], kk[:], rn[:], ALU.mult, [kkb, rnb], [kkb])
                    op("dve", lambda h: h.tensor_scalar(out=tmp[:], in0=a_t[:], scalar1=cvc(l, "k_a", c), scalar2=dvc(l, "omka", c), op0=ALU.mult, op1=ALU.add),
                       reads=[ab, cv_b, dv_b], writes=[tmpb])
                    tt_("dve", k2[:], k_t[:], tmp[:], ALU.mult, [kb_, tmpb], [k2b])
                    op("dve", lambda h: h.scalar_tensor_tensor(out=AR[:, 0, :], in0=kk[:], scalar=-1.0, in1=eGx[:], op0=ALU.mult, op1=ALU.mult), reads=[kkb, eGxb], writes=[ARb])
                    tt_("pool", AR[:, 1, :], r_t[:], eG[:], ALU.mult, [rb, eGb], [ARb])
                    tt_("dve", tmp[:], kk[:], a_t[:], ALU.mult, [kkb, ab], [tmpb])
                    tt_("dve", BhT[:], tmp[:], eGi[:], ALU.mult, [tmpb, eGib], [BhTb])
                    tt_("pool", KhT[:], k2[:], eGi[:], ALU.mult, [k2b, eGib], [KhTb])
                    op("dve", lambda h: h.scalar_tensor_tensor(out=rkb[:], in0=r_t[:], scalar=cvc(l, "r_k", c), in1=k2[:], op0=ALU.mult, op1=ALU.mult), reads=[rb, k2b, cv_b], writes=[rkbb])
                    op("pe", lambda h: h.matmul(PS[0][:], lhsT=bonesb[:], rhs=rkb[:], start=True, stop=True), reads=[cb_b, rkbb], writes=[PSB[0]])
                    tt_("dve", bon[:], PS[0][:], v2t[:], ALU.mult, [PSB[0], v2b], [bonb])
                    for n in range(4):
                        tsl = slice(n * 128, (n + 1) * 128)
                        for src, srcb, dst in ((BhT, BhTb, Bh[n]), (KhT, KhTb, Kh[n]), (vbf, vbfb, Vt[n])):
                            ri = trc[0] % 4
                            trc[0] += 1
                            op("pe", lambda h, src=src, ri=ri: h.transpose(out=PSH[:, ri * 128:(ri + 1) * 128], in_=src[:, tsl], identity=identb[:]), reads=[srcb, cb_b], writes=[PSH_b[ri]])
                            op("act" if ri % 2 else "dve", lambda h, dst=dst, ri=ri: (h.copy if ri % 2 else h.tensor_copy)(out=dst[0][:], in_=PSH[:, ri * 128:(ri + 1) * 128]),
                               reads=[PSH_b[ri]], writes=[dst[1]])
                    def mmq(out, lhsT, rhs, rd, wr, st=True, sp=True, inc=True):
                        op("pe", lambda h: h.matmul(out, lhsT=lhsT, rhs=rhs, start=st, stop=sp), reads=rd, writes=wr, inc=inc)

                    def stage1(n, hh, lane, sl):
                        tsl = slice(n * 128, (n + 1) * 128)
                        P_ = slice(64 * hh, 64 * hh + 64)
                        A, Ab = PS[1 + lane], PSB[1 + lane]
                        e1 = "act" if lane % 2 == 0 else "dve"
                        cp = lambda eng, o, i, rd, wr: op(eng, lambda h: (h.copy if eng == "act" else h.tensor_copy)(out=o, in_=i), reads=rd, writes=wr)
                        T12, T12b = sl["T12"]; T3, T3b = sl["T3"]; W1, W1b = sl["W1"]; W2, W2b = sl["W2"]; p0, p0b = sl["p0"]
                        DGs = sl["DG"]; ZZ, ZZb = sl["ZZ"]; tW, tWb = sl["tW"]
                        mmq(A[:, 0:256], BhT[P_, tsl], AR[P_, :, tsl], [BhTb, ARb], [Ab], inc=False)
                        mmq(A[:, 256:512], KhT[P_, tsl], AR[P_, :, tsl], [KhTb, ARb], [Ab])
                        yield
                        cp(e1, T12[:], A[:, 0:512], [Ab], [T12b, Ab])
                        yield
                        mmq(A[:, 0:128], AR[P_, 0, tsl], BhT[P_, tsl], [ARb, BhTb], [Ab])
                        d0, d0b = DGs[0]
                        tt_("pool", p0[:, 128:256], T12[:, 0:128], m2b[:, 0:128], ALU.mult, [T12b, m2bb], [p0b])
                        tt_("pool", W1[:], T12[:, 128:256], m2b[:, 128:256], ALU.mult, [T12b, m2bb], [W1b])
                        tt_("pool", W2[:], T12[:, 256:512], m2b[:], ALU.mult, [T12b, m2bb], [W2b])
                        tt_("pool", d0[:, 128:256], T12[:, 0:128], hm[:, 0, 128:256], ALU.mult, [T12b, hmb], [d0b])
                        yield
                        cp(e1, T3[:], A[:, 0:128], [Ab], [T3b, Ab])
                        tt_("pool", d0[:, 128:256], d0[:, 128:256], identb[:], ALU.add, [d0b, cb_b], [d0b])
                        yield
                        tt_("pool", p0[:, 0:128], T3[:], mslb[:], ALU.mult, [T3b, mslbb], [p0b])
                        tt_("pool", d0[:, 0:128], T3[:], hm[:, 0, 0:128], ALU.mult, [T3b, hmb], [d0b])
                        tt_("pool", d0[:, 0:128], d0[:, 0:128], identb[:], ALU.add, [d0b, cb_b], [d0b])
                        yield
                        for k in range(1, 7):
                            dc, dcb = DGs[(k - 1) % 2]
                            dn, dnb = DGs[k % 2]
                            mmq(A[:, 0:128], p0[:, 128:256], dc[:, 0:128], [p0b, dcb], [Ab], inc=False)
                            mmq(A[:, 128:256], p0[:, 0:128], dc[:, 128:256], [p0b, dcb], [Ab])
                            yield
                            cp("act" if (k + lane) % 2 else "dve", ZZ[:], A[:, 0:256], [Ab], [ZZb, Ab])
                            yield
                            mmq(A[:, 256:384], dc[:, 128:256], ZZ[:, 0:128], [dcb, ZZb], [Ab], inc=False)
                            mmq(A[:, 384:512], dc[:, 0:128], ZZ[:, 128:256], [dcb, ZZb], [Ab])
                            yield
                            tt_("dve", tW[:], A[:, 256:512], hm[:, k, :], ALU.mult, [Ab, hmb], [tWb, Ab])
                            yield
                            tt_("pool", dn[:], dc[:], tW[:], ALU.add, [dcb, tWb], [dnb])
                            yield

                    def stage2(n, hh, lane, sl):
                        tsl = slice(n * 128, (n + 1) * 128)
                        P_ = slice(64 * hh, 64 * hh + 64)
                        B, Bb = PS[1 + lane], PSB[1 + lane]
                        W1, W1b = sl["W1"]; W2, W2b = sl["W2"]; Xb, Xbb = sl["Xb"]; Ub, Ubb = sl["Ub"]; HG, HGb = sl["HG"]
                        ntf, ntfb = sl["DG"][0][0][:, 128:256], sl["DG"][0][1]
                        vt, vtb = Vt[n]
                        mmq(B[:, 0:64], AR[P_, 0, tsl], Hb[P_, c, :], [ARb, Hbb[hh]], [Bb], True, False, False)
                        mmq(B[:, 0:64], W2[:, 0:128], vt[:, P_], [W2b, vtb], [Bb], False, True)
                        yield
                        op("act", lambda h: h.copy(out=Xb[:], in_=B[:, 0:64]), reads=[Bb], writes=[Xbb, Bb])
                        yield
                        mmq(B[:, 64:128], ntf, Xb[:], [ntfb, Xbb], [Bb])
                        yield
                        op("dve", lambda h: h.tensor_copy(out=Ub[:], in_=B[:, 64:128]), reads=[Bb], writes=[Ubb, Bb])
                        gcol = eG[P_, n * 128 + 127:n * 128 + 128]
                        op("dve", lambda h: h.tensor_scalar(out=HG[P_, :], in0=Hf[P_, c, :], scalar1=gcol, scalar2=None, op0=ALU.mult), reads=[Hfb[hh], eGb], writes=[HGb])
                        yield
                        mmq(B[P_, 128:256], Hb[P_, c, :], AR[P_, 1, tsl], [Hbb[hh], ARb], [Bb], True, False, False)
                        mmq(B[P_, 128:256], Ub[:], W1[:], [Ubb, W1b], [Bb], False, False, False)
                        mmq(B[P_, 128:256], vt[:, P_], W2[:, 128:256], [vtb, W2b], [Bb], False, True, False)
                        mmq(B[P_, 256:320], Bh[n][0][:, P_], Ub[:], [Bh[n][1], Ubb], [Bb], True, False, False)
                        mmq(B[P_, 256:320], Kh[n][0][:, P_], vt[:, P_], [Kh[n][1], vtb], [Bb], False, True)
                        yield
                        op("dve", lambda h: h.scalar_tensor_tensor(out=Hf[P_, c, :], in0=B[P_, 256:320], scalar=gcol, in1=HG[P_, :], op0=ALU.mult, op1=ALU.add),
                           reads=[Bb, eGb, HGb], writes=[Hfb[hh], Bb])
                        op("act", lambda h: h.copy(out=OT[P_, tsl], in_=B[P_, 128:256]), reads=[Bb], writes=[OTb[hh], Bb])
                        yield
                        op("act", lambda h: h.copy(out=Hb[P_, c, :], in_=Hf[P_, c, :]), reads=[Hfb[hh]], writes=[Hbb[hh]])
                        yield

                    units = [(n, hh) for n in range(4) for hh in range(2)]
                    for r0 in range(0, 8, 4):
                        lockstep([stage1(n, hh, lane, SL[r0 + lane]) for lane, (n, hh) in enumerate(units[r0:r0 + 4])])
                    for n in range(4):
                        lockstep([stage2(n, hh, hh, SL[2 * n + hh]) for hh in range(2)])
                    op("pool", lambda h: h.tensor_copy(out=ob16[:], in_=OT[:]), reads=OTb, writes=[ob16b])
                    op("pe", lambda h: h.matmul(PS[0][:], lhsT=bonesb[:], rhs=ob16[:], start=True, stop=True), reads=[cb_b, ob16b], writes=[PSB[0]])
                    op("dve", lambda h: h.tensor_scalar(out=mean[:], in0=PS[0][:], scalar1=1.0 / 64, scalar2=None, op0=ALU.mult), reads=[PSB[0]], writes=[meanb])
                    tt_("dve", OT[:], OT[:], mean[:], ALU.subtract, OTb + [meanb], OTb)
                    op("act", lambda h: h.activation(out=sqb_[:], in_=OT[:], func=AF.Square), reads=OTb, writes=[sqbb])
                    op("pe", lambda h: h.matmul(PS[0][:], lhsT=bonesb[:], rhs=sqb_[:], start=True, stop=True), reads=[cb_b, sqbb], writes=[PSB[0]])
                    op("act", lambda h: h.activation(out=var[:], in_=PS[0][:], func=AF.Sqrt, scale=1.0 / 64, bias=GN_EPS), reads=[PSB[0]], writes=[varb])
                    op("dve", lambda h: h.reciprocal(out=var[:], in_=var[:]), reads=[varb], writes=[varb])
                    tt_("dve", OT[:], OT[:], var[:], ALU.mult, OTb + [varb], OTb)
                    op("act", lambda h: h.activation(out=OT[:], in_=OT[:], func=AF.Identity, scale=cvc(l, "gn_w", c), bias=cvc(l, "gn_b", c)), reads=OTb + [cv_b], writes=OTb)
                    tt_("dve", OT[:], OT[:], bon[:], ALU.add, OTb + [bonb], OTb)
                    tt_("dve", mo[:], OT[:], gt[:], ALU.mult, OTb + [gtb], [mob])
                    dma("sp", mixT[cs_, ts], mo[:], reads=[mob], writes=[db("mixT", c)], store=True)

        def lockstep(gens):
            gens = list(gens)
            while gens:
                nxt = []
                for g in gens:
                    try:
                        next(g)
                        nxt.append(g)
                    except StopIteration:
                        pass
                gens = nxt

        for l in range(NL if 'nolayers' not in parts else 0):
            kb.new_epoch()
            if 'mix' in parts:
                with scope() as es:
                    hT = sb(es, "hT", [128, KC, S], BF16)
                    hTb = [Buf() for _ in range(NTB)]
                    with scope() as es2:
                        if "nonorm1" not in parts:
                            norm_to_hT(es2, l, "g_pre", hT, hTb)
                    with scope() as es2:
                        W = w_in[l]
                        R = mk_rings(es2)
                        tasks = []
                        for c in range(8):
                            tasks.append(([(W[:, c * 128:(c + 1) * 128], 128)], 128,
                                          shift_epi(R, "er%d" % (c % 2), 128, cvc(l, "mu_r", c), dvc(l, "om_r", c), [(rT[c * 128:(c + 1) * 128], db("rT", c))])))
                        for c in range(8):
                            tasks.append(([(W[:, 1088 + c * 128:1088 + (c + 1) * 128], 128)], 128,
                                          shift_epi(R, "ek%d" % (c % 2), 128, cvc(l, "mu_k", c), dvc(l, "om_k", c), [(kT[c * 128:(c + 1) * 128], db("kT", c))])))
                        for c in range(8):
                            dsts = [(vT[c * 128:(c + 1) * 128], db("vT", c))]
                            if l == 0:
                                dsts.append((vfT[c * 128:(c + 1) * 128], db("vfT", c)))
                            tasks.append(([(W[:, 2112 + c * 128:2112 + (c + 1) * 128], 128)], 128,
                                          shift_epi(R, "ev%d" % (c % 2), 128, cvc(l, "mu_v", c), dvc(l, "om_v", c), dsts)))
                        tasks.append(([(W[:, 1024:1088], 64), (W[:, 3136:3200], 64)], 128,
                                      shift_epi(R, "ewa", 128, cvc(l, "mu_wa"), dvc(l, "om_wa"), [(waT, db("waT"))], act=[(0, 64, AF.Tanh), (64, 128, None)])))
                        tasks.append(([(W[:, 3200:3328], 128)], 128,
                                      shift_epi(R, "eg1", 128, cvc(l, "mu_g1"), dvc(l, "om_g1"), [(g1T, db("g1T"))], act=[(0, 128, AF.Sigmoid)])))
                        if l == 0:
                            tasks.append(([(W[:, 3328:3360], 32)], 32,
                                          shift_epi(R, "eg2", 32, cvc(l, "mu_g2", 0, 1, 0, 32), dvc(l, "om_g2", 0, 1, 0, 32), [(g2T[0:32], db("g2T"))], act=[(0, 32, AF.Sigmoid)])))
                        else:
                            tasks.append(([(W[:, 3328:3360], 32), (w_mv[l - 1], 32)], 64,
                                          shift_epi(R, "eg2", 64, cvc(l, "mu_g2", 0, 1, 0, 64), dvc(l, "om_g2", 0, 1, 0, 64), [(g2T, db("g2T"))],
                                                    act=[(0, 32, AF.Sigmoid), (32, 64, None)])))
                        for c in range(8):
                            tasks.append(([(W[:, 3360 + c * 128:3360 + (c + 1) * 128], 128)], 128, plain_epi(R, "eq%d" % (c % 2), qT[c * 128:(c + 1) * 128], db("qT", c), 0)))
                        for c in range(8):
                            tasks.append(([(W[:, 4384 + c * 128:4384 + (c + 1) * 128], 128)], 128, plain_epi(R, "ekd%d" % (c % 2), kdT[c * 128:(c + 1) * 128], db("kdT", c), 1)))
                        if 'plainonly' in parts:
                            pe_ = plain_epi(R, 'x', qT[0:128], db('qT', 0), 0)
                            tasks = [(sg_, M_, pe_) for (sg_, M_, e_) in tasks if M_ == 128]
                        if 'ntasks8' in parts:
                            tasks = tasks[:8]
                        proj_fm(es2, hT, hTb, tasks, nper=2, each=True)
                    with scope() as es2:
                        st = Ring(es2, nc, "vst", 2, [128, KC, 128], F32)
                        wbr = Ring(es2, nc, "vwb", 2, [128, KC, 128], BF16)
                        orr = Ring(es2, nc, "vo", 2, [128, 128], BF16)
                        for c in range(8 if 'novd' not in parts else 0):
                            s_, sb_ = st.next()
                            dma("sp", s_[:], w_in[l][:, 5408 + c * 128:5408 + (c + 1) * 128].rearrange("(kc p) n -> p kc n", p=128), writes=[sb_])
                            w_, wb_ = wbr.next()
                            op("dve", lambda h, s_=s_, w_=w_: h.tensor_copy(out=w_[:], in_=s_[:]), reads=[sb_], writes=[wb_])
                            for tt in range(NT):
                                pi = tt % 4
                                for kc in range(KC):
                                    op("pe", lambda h, w_=w_, kc=kc, pi=pi, tt=tt: h.matmul(PS[pi][:, 0:128], lhsT=hT[:, kc, tt * 128:(tt + 1) * 128], rhs=w_[:, kc, :],
                                                                                           start=(kc == 0), stop=(kc == KC - 1)),
                                       reads=[wb_, hTb[tt // 4]], writes=[PSB[pi]], inc=(kc == KC - 1))
                                o_, ob_ = orr.next()
                                op("act" if tt % 2 else "dve", lambda h, o_=o_, pi=pi, tt=tt: (h.copy if tt % 2 else h.tensor_copy)(out=o_[:], in_=PS[pi][:, 0:128]),
                                   reads=[PSB[pi]], writes=[ob_])
                                dma("sp", vd[tt * 128:(tt + 1) * 128, c * 128:(c + 1) * 128], o_[:], reads=[ob_], writes=[db("vd", c)], store=True)

                with scope() as es:
                    if 'norwkv' not in parts:
                        rwkv_phase(es, l)
                with scope() as es:
                    if 'noattn' not in parts:
                        attn_phase(es, l)
                with scope() as es:
                    if 'noO' not in parts:
                        proj_res(es, l, mixT, "mixT", KC, w_out[l], "g_pm")
            with scope() as es:
                hT = sb(es, "hT2", [128, KC, S], BF16)
                hTb = [Buf() for _ in range(NTB)]
                with scope() as es2:
                    norm_to_hT(es2, l, "g_pf", hT, hTb)
                with scope() as es2:
                    tr = Ring(es2, nc, "ft", 2, [128, 512], F32)
                    gr = Ring(es2, nc, "fg", 2, [128, 512], F32)
                    orr = Ring(es2, nc, "fo", 2, [128, 512], BF16)
                    carr = [(sb(es2, "fc%d" % i, [128, 2]), Buf()) for i in range(2)]

                    def ffn_epi(c):
                        car, carb = carr[c % 2]

                        def epi(tb, pss):
                            pg, pu = pss
                            ts = slice(tb * 512, (tb + 1) * 512)
                            t_, tb_ = tr.next()
                            g_, gb_ = gr.next()
                            o_, ob_ = orr.next()
                            op("act", lambda h: h.activation(out=t_[:], in_=PS[pg][:], func=AF.Identity, scale=cvc(l, "cw2", c), bias=cvc(l, "cb", c)),
                               reads=[PSB[pg], cv_b], writes=[tb_])
                            op("dve", lambda h: h.scalar_tensor_tensor(out=t_[:, 1:512], in0=PS[pg][:, 0:511], scalar=cvc(l, "cw1", c), in1=t_[:, 1:512], op0=ALU.mult, op1=ALU.add),
                               reads=[PSB[pg], tb_, cv_b], writes=[tb_])
                            op("dve", lambda h: h.scalar_tensor_tensor(out=t_[:, 2:512], in0=PS[pg][:, 0:510], scalar=cvc(l, "cw0", c), in1=t_[:, 2:512], op0=ALU.mult, op1=ALU.add),
                               reads=[PSB[pg], tb_, cv_b], writes=[tb_])
                            if tb > 0:
                                op("dve", lambda h: h.scalar_tensor_tensor(out=t_[:, 0:2], in0=car[:, 0:2], scalar=cvc(l, "cw0", c), in1=t_[:, 0:2], op0=ALU.mult, op1=ALU.add),
                                   reads=[carb, tb_, cv_b], writes=[tb_])
                                op("dve", lambda h: h.scalar_tensor_tensor(out=t_[:, 0:1], in0=car[:, 1:2], scalar=cvc(l, "cw1", c), in1=t_[:, 0:1], op0=ALU.mult, op1=ALU.add),
                                   reads=[carb, tb_, cv_b], writes=[tb_])
                            op("act", lambda h: h.copy(out=car[:, 0:2], in_=PS[pg][:, 510:512]), reads=[PSB[pg]], writes=[carb])
                            op("act", lambda h: h.activation(out=g_[:], in_=t_[:], func=AF.Gelu_apprx_tanh), reads=[tb_], writes=[gb_])
                            op("dve", lambda h: h.tensor_tensor(out=o_[:], in0=PS[pu][:], in1=g_[:], op=ALU.mult), reads=[PSB[pu], gb_], writes=[ob_])
                            dma("sp", actT[c * 128:(c + 1) * 128, ts], o_[:], reads=[ob_], writes=[db("actT", c)], store=True)
                        return epi
                    tasks = []
                    for c in range(FC):
                        e = ffn_epi(c)
                        tasks.append(([(w_up[l][:, c * 128:(c + 1) * 128], 128)], 128, e))
                        tasks.append(([(w_up[l][:, FF + c * 128:FF + (c + 1) * 128], 128)], 128, e))
                    if 'noproj' not in parts:
                        proj_fm(es2, hT, hTb, tasks, nper=2)
            with scope() as es:
                if 'nores' not in parts:
                    proj_res(es, l, actT, "actT", FC, w_down[l], "g_ff")

        with scope() as es:
            xr = Ring(es, nc, "oxi", 2, [128, KC, 128], F32)
            xo = Ring(es, nc, "oxo", 2, [128, D], F32)
            for tt in range(NT):
                xi, xib = xr.next()
                dma("sp", xi[:], xT[:, tt * 128:(tt + 1) * 128].rearrange("(kc p) t -> p kc t", p=128), reads=[db("xT", kc) for kc in range(KC)], writes=[xib])
                o_, ob_ = xo.next()
                for kc in range(KC):
                    pi = kc % 4
                    op("pe", lambda h, kc=kc, pi=pi: h.transpose(out=PS[pi][:, 0:128], in_=xi[:, kc, :], identity=cn[:, 0:128]), reads=[xib, cn_b], writes=[PSB[pi]])
                    op("act" if kc % 2 else "dve", lambda h, kc=kc, pi=pi: (h.copy if kc % 2 else h.tensor_copy)(out=o_[:, kc * 128:(kc + 1) * 128], in_=PS[pi][:, 0:128]),
                       reads=[PSB[pi]], writes=[ob_])
                dma("sp", y_out[tt * 128:(tt + 1) * 128, :], o_[:], reads=[ob_], writes=[db("yout")], store=True)
        kb.finish()
    return nc


def pack_inputs(inp, S, NL):
    f = np.float32
    cv = np.zeros((128, NL, NCV), f)

    def put(l, name, vec):
        vec = np.asarray(vec, f).reshape(-1)
        n = (vec.size + 127) // 128
        pad = np.zeros(n * 128, f)
        pad[:vec.size] = vec
        cv[:, l, CV[name]:CV[name] + n] = pad.reshape(n, 128).T
    for l in range(NL):
        put(l, "g_pre", inp["pre_mix_norm"][l]); put(l, "g_pm", inp["post_mix_norm"][l])
        put(l, "g_pf", inp["pre_ffn_norm"][l]); put(l, "g_ff", inp["post_ffn_norm"][l])
        mu = np.asarray(inp["shift_mu"][l], f)
        put(l, "mu_r", mu[0:1024]); put(l, "mu_k", mu[1088:2112]); put(l, "mu_v", mu[2112:3136])
        put(l, "mu_wa", np.concatenate([mu[1024:1088], mu[3136:3200]]))
        put(l, "mu_g1", mu[3200:3328])
        g2 = np.zeros(128, f)
        g2[0:32] = mu[3328:3360]
        if l > 0:
            g2[32:64] = np.asarray(inp["shift_mu_mv"][l - 1], f)
        put(l, "mu_g2", g2)
        for nm in ("w0", "a0", "k_k", "k_a", "r_k", "gn_w", "gn_b"):
            put(l, nm, inp[nm][l])
        if l > 0:
            put(l, "v0", inp["v0"][l - 1])
        put(l, "subln", inp["subln_w"][l])
        cw = np.asarray(inp["conv_w"][l], f)
        put(l, "cw0", cw[0]); put(l, "cw1", cw[1]); put(l, "cw2", cw[2]); put(l, "cb", inp["conv_b"][l])
        for nm, k in (("lq1", "lam_q1"), ("lk1", "lam_k1"), ("lq2", "lam_q2"), ("lk2", "lam_k2")):
            cv[:, l, CV[nm]:CV[nm] + 64] = np.asarray(inp[k][l], f)[None, :]
    shared = {
        "cv": np.ascontiguousarray(cv.reshape(128, NL * NCV)),
        "cn": make_consts(),
        "w_in": np.ascontiguousarray(inp["w_in"][:NL], dtype=f),
        "w_mv": np.ascontiguousarray(inp["w_mv_down"][:max(NL - 1, 1)], dtype=f),
        "w2": np.ascontiguousarray(inp["w2"][:NL], dtype=f), "a2": np.ascontiguousarray(inp["a2"][:NL], dtype=f),
        "g2": np.ascontiguousarray(inp["g2"][:NL], dtype=f), "v2": np.ascontiguousarray(inp["v2"][:max(NL - 1, 1)], dtype=f),
        "w_out": np.ascontiguousarray(inp["w_out"][:NL], dtype=f), "w_up": np.ascontiguousarray(inp["w_up"][:NL], dtype=f),
        "w_down": np.ascontiguousarray(inp["w_down"][:NL], dtype=f),
    }
    return shared


def kernel(**inp):
    x = np.asarray(inp["x"], np.float32)
    B, S, _ = x.shape
    NL = 4
    nc = build(S, NL)
    shared = pack_inputs(inp, S, NL)
    pos = np.asarray(inp["positions"], np.int32)
    in_maps = []
    for b in range(B):
        m = dict(shared)
        m["x"] = np.ascontiguousarray(x[b])
        m["pos"] = np.ascontiguousarray(pos[b:b + 1])
        in_maps.append(m)
    res = run_bass_kernel_spmd(nc, in_maps, core_ids=list(range(B)))
    return np.stack([np.asarray(r["y"], np.float32) for r in res.results], 0)
```

```python
import math
from contextlib import ExitStack, contextmanager
import numpy as np
import concourse.bass as bass
import concourse.mybir as mybir
from concourse.bass_utils import run_bass_kernel_spmd

F32 = mybir.dt.float32
BF16 = mybir.dt.bfloat16
I32 = mybir.dt.int32
AF = mybir.ActivationFunctionType
ALU = mybir.AluOpType

D = 2048
KC = 16
FF = 5632
FC = 44
RW = 1024
RCOLS = 3360
INC = 6432
C0 = math.exp(-0.5)
NORM_EPS = 1e-6
GN_EPS = 64e-5
SUBLN_EPS = 1e-5

CV = {}
_o = 0
for _n, _w in (("g_pre", 16), ("g_pm", 16), ("g_pf", 16), ("g_ff", 16), ("mu_r", 8), ("mu_k", 8), ("mu_v", 8),
               ("mu_wa", 1), ("mu_g1", 1), ("mu_g2", 1), ("w0", 8), ("a0", 8), ("k_k", 8), ("k_a", 8), ("r_k", 8),
               ("gn_w", 8), ("gn_b", 8), ("v0", 8), ("subln", 1), ("cw0", 44), ("cw1", 44), ("cw2", 44), ("cb", 44),
               ("lq1", 64), ("lk1", 64), ("lq2", 64), ("lk2", 64)):
    CV[_n] = _o
    _o += _w
NCV = _o
DV = {}
_o = 0
for _n, _w in (("om_r", 8), ("om_k", 8), ("om_v", 8), ("om_wa", 1), ("om_g1", 1), ("om_g2", 1), ("omka", 8), ("lam", 1), ("nlam", 1), ("sub2", 1)):
    DV[_n] = _o
    _o += _w
NDV = _o

CN = {"ident": 0, "bones": 128, "ones": 256, "perm": 384, "m2": 512, "msl": 768, "invf": 896, "sign": 897, "dmask": 898, "scanm": 2946, "hm": 3458}
NCNP = 898
NCN = 3458 + 7 * 256


def make_consts():
    c = np.zeros((128, NCN), np.float32)
    p = np.arange(128)
    c[:, 0:128] = np.eye(128)
    c[:, 128:256] = (p[:, None] // 64 == p[None, :] // 64)
    c[:, 256:384] = 1.0
    part = p.copy()
    for b in (0, 64):
        for i in range(8):
            part[b + i] = b + i + 8
            part[b + 8 + i] = b + i
    perm = np.zeros((128, 128), np.float32)
    for m in range(128):
        if part[m] != m:
            perm[part[m], m] = 1.0
    c[:, 384:512] = perm
    c[:, 512:640] = (p[:, None] < p[None, :])
    c[:, 640:768] = (p[:, None] <= p[None, :])
    c[:, 768:896] = (p[None, :] < p[:, None])
    q = np.arange(512)
    for j in range(4):
        c[:, 898 + j * 512:898 + (j + 1) * 512] = ((j * 128 + p)[:, None] <= q[None, :])
    invf = np.zeros(128, np.float64)
    sign = np.zeros(128, np.float32)
    fr = 500000.0 ** (-np.arange(0, 16, 2, dtype=np.float32) / 16)
    for b in (0, 64):
        for i in range(8):
            invf[b + i] = fr[i]
            invf[b + 8 + i] = fr[i]
            sign[b + i] = -1.0
            sign[b + 8 + i] = 1.0
    c[:, 896] = invf.astype(np.float32)
    c[:, 897] = sign
    sm = np.ones(512, np.float32)
    sm[::128] = 0.0
    c[:, 2946:2946 + 512] = sm[None, :]
    for k in range(7):
        t = p[:, None]
        s_ = p[None, :]
        mk = ((t >> (k + 1)) == (s_ >> (k + 1))) & (((t >> k) & 1) == 1) & (((s_ >> k) & 1) == 0)
        c[:, 3458 + k * 256:3458 + k * 256 + 128] = mk
        c[:, 3458 + k * 256 + 128:3458 + (k + 1) * 256] = mk.T
    return c


class Buf:
    __slots__ = ("w", "r")

    def __init__(self):
        self.w = {}
        self.r = {}


class Eng:
    def __init__(self, name, h, sem):
        self.name, self.h, self.sem, self.cnt, self.seen = name, h, sem, 0, {}


class KB:
    def __init__(self, nc, es, n_epochs=1):
        self.nc = nc
        self.E = {}
        self.sems = {}
        self.keyeng = {}
        self.dead = set()
        self.pool_sems = {}
        hs = (("pe", nc.tensor), ("act", nc.scalar), ("dve", nc.vector), ("pool", nc.gpsimd), ("sp", nc.sync))
        for name, h in hs:
            self.pool_sems[name] = [es.enter_context(nc.semaphore("s_%s%d" % (name, i))) for i in range(n_epochs if name != "sp" else 1)]
            e = Eng(name, h, self.pool_sems[name][0])
            e.key = name + "#0"
            e.epoch = 0
            self.E[name] = e
            self.sems[e.key] = (e.sem, 1)
            self.keyeng[e.key] = e
        self.slots = {}
        for q, n in (("sp", 10),):
            sl = []
            for i in range(n):
                key = "d%s%d" % (q, i)
                sem = es.enter_context(nc.semaphore("s_" + key))
                self.sems[key] = (sem, 16)
                sl.append([key, 0])
            self.slots[q] = [sl, 0]

    def new_epoch(self):
        self.barrier()
        for name in ("pe", "act", "dve", "pool"):
            e = self.E[name]
            if e.epoch + 1 >= len(self.pool_sems[name]):
                continue
            self.dead.add(e.key)
            e.epoch += 1
            e.sem = self.pool_sems[name][e.epoch]
            e.cnt = 0
            e.key = "%s#%d" % (name, e.epoch)
            self.sems[e.key] = (e.sem, 1)
            self.keyeng[e.key] = e

    def _need(self, e, key, n, raw):
        if key in self.dead:
            return
        if key == e.key and (not raw) and e.name == "pe":
            return
        if e.seen.get(key, 0) >= n:
            return
        sem, unit = self.sems[key]
        if key in self.keyeng and key != e.key:
            assert n <= self.keyeng[key].cnt, ("pending ticket", key, n)
        e.h.wait_ge(sem, n * unit)
        e.seen[key] = n

    def _deps(self, e, reads, writes):
        for b in reads:
            for k, n in b.w.items():
                self._need(e, k, n, True)
        for b in writes:
            for k, n in b.w.items():
                self._need(e, k, n, False)
            for k, n in b.r.items():
                self._need(e, k, n, False)

    def op(self, eng, fn, reads=(), writes=(), inc=True):
        e = self.E[eng]
        self._deps(e, reads, writes)
        ins = fn(e.h)
        if inc:
            ins.then_inc(e.sem, 1)
            e.cnt += 1
            t = e.cnt
        else:
            t = e.cnt + 1
        for b in writes:
            b.w = {e.key: t}
            b.r = {}
        for b in reads:
            b.r[e.key] = t
        return ins

    def dma(self, q, out, in_, reads=(), writes=(), store=False):
        e = self.E[q]
        if store:
            for b in reads:
                for k, n in b.w.items():
                    self._need(e, k, n, True)
            for b in writes:
                for k, n in b.r.items():
                    self._need(e, k, n, False)
        else:
            self._deps(e, reads, writes)
        sl, idx = self.slots[q]
        key, cnt = sl[idx]
        self.slots[q][1] = (idx + 1) % len(sl)
        if cnt > 0:
            self._need(e, key, cnt, True)
        sem, unit = self.sems[key]
        e.h.dma_start(out=out, in_=in_).then_inc(sem, 16)
        sl[idx][1] = cnt + 1
        for b in writes:
            if store:
                b.w = {k: v for k, v in b.w.items() if k.startswith("d")}
                b.w[key] = cnt + 1
            else:
                b.w = {key: cnt + 1}
            b.r = {}
        for b in reads:
            b.r[key] = cnt + 1

    def barrier(self):
        for en in ("pe", "act", "dve", "pool", "sp"):
            e = self.E[en]
            for k2 in ("pe", "act", "dve", "pool"):
                o = self.E[k2]
                if k2 != en and o.cnt > 0:
                    self._need(e, o.key, o.cnt, True)
            for q in self.slots:
                for key, cnt in self.slots[q][0]:
                    if cnt > 0:
                        self._need(e, key, cnt, True)

    def finish(self):
        e = self.E["sp"]
        for q in self.slots:
            for key, cnt in self.slots[q][0]:
                if cnt > 0:
                    self._need(e, key, cnt, True)


_UID = [0]


class Ring:
    def __init__(self, es, nc, name, n, shape, dt, psum=False):
        self.t = []
        for i in range(n):
            _UID[0] += 1
            t = es.enter_context((nc.psum_tensor if psum else nc.sbuf_tensor)("rg_%s_%d" % (name, _UID[0]), shape, dt))
            self.t.append((t, Buf()))
        self.i = 0

    def next(self):
        r = self.t[self.i]
        self.i = (self.i + 1) % len(self.t)
        return r


def build(S, NL, dbg=False, parts=('mix', 'ffn')):
    nc = bass.Bass("TRN2", target_bir_lowering=False)
    NTB = S // 512
    NT = S // 128

    def din(name, shape, dt=F32):
        return nc.dram_tensor(name, shape, dt, kind="ExternalInput").ap()

    x_in = din("x", [S, D])
    pos_in = din("pos", [1, S], I32)
    cv_in = din("cv", [128, NL * NCV])
    cn_in = din("cn", [128, NCN])
    w_in = din("w_in", [NL, D, INC])
    w_mv = din("w_mv", [max(NL - 1, 1), D, 32])
    w2_in = din("w2", [NL, 64, RW])
    a2_in = din("a2", [NL, 64, RW])
    g2_in = din("g2", [NL, 160, RW])
    v2_in = din("v2", [max(NL - 1, 1), 32, RW])
    w_out = din("w_out", [NL, D, D])
    w_up = din("w_up", [NL, D, 2 * FF])
    w_down = din("w_down", [NL, FF, D])
    y_out = nc.dram_tensor("y", [S, D], F32, kind="ExternalOutput").ap()

    kindS = "ExternalOutput" if dbg else "Internal"

    def dsc(name, shape, dt):
        return nc.dram_tensor(name, shape, dt, kind=kindS).ap()

    xT = dsc("xT", [D, S], F32)
    yT = dsc("yT", [D, S], F32)
    rT = dsc("rT", [RW, S], BF16)
    kT = dsc("kT", [RW, S], BF16)
    vT = dsc("vT", [RW, S], BF16)
    vfT = dsc("vfT", [RW, S], BF16)
    waT = dsc("waT", [128, S], BF16)
    g1T = dsc("g1T", [128, S], BF16)
    g2T = dsc("g2T", [64, S], BF16)
    qT = dsc("qT", [RW, S], BF16)
    kdT = dsc("kdT", [RW, S], BF16)
    vd = dsc("vd", [S, RW], BF16)
    mixT = dsc("mixT", [D, S], BF16)
    actT = dsc("actT", [FF, S], BF16)
    rotC = dsc("rotC", [128, S], F32)
    rotS = dsc("rotS", [128, S], F32)
    DB = {}

    def db(name, i=0):
        k = (name, i)
        if k not in DB:
            DB[k] = Buf()
        return DB[k]

    with ExitStack() as es0:
        kb = KB(nc, es0, n_epochs=NL + 1)
        op, dma = kb.op, kb.dma

        @contextmanager
        def scope():
            with ExitStack() as e_:
                yield e_
                kb.barrier()

        def sb(es, name, shape, dt=F32):
            _UID[0] += 1
            return es.enter_context(nc.sbuf_tensor("sb_%s_%d" % (name, _UID[0]), shape, dt))

        cn = sb(es0, "cn", [128, NCNP]); cn_b = Buf()
        cv = sb(es0, "cv", [128, NL * NCV]); cv_b = Buf()
        dv = sb(es0, "dv", [128, NL * NDV]); dv_b = Buf()
        identb = sb(es0, "identb", [128, 128], BF16)
        bonesb = sb(es0, "bonesb", [128, 128], BF16)
        onesb = sb(es0, "onesb", [128, 128], BF16)
        permb = sb(es0, "permb", [128, 128], BF16)
        cb_b = Buf()
        PS = [es0.enter_context(nc.psum_tensor("ps%d" % i, [128, 512], F32)) for i in range(7)]
        PSB = [Buf() for _ in range(7)]
        PSH = es0.enter_context(nc.psum_tensor("psh", [128, 1024], BF16)); PSH_b = [Buf()] * 4

        dma("sp", cn[:], cn_in[:, 0:NCNP], writes=[cn_b])
        dma("sp", cv[:], cv_in[:, :], writes=[cv_b])
        for t, o in ((identb, CN["ident"]), (bonesb, CN["bones"]), (onesb, CN["ones"]), (permb, CN["perm"])):
            op("dve", lambda h, t=t, o=o: h.tensor_copy(out=t[:], in_=cn[:, o:o + 128]), reads=[cn_b], writes=[cb_b])

        def cvc(l, name, i=0, n=1, p0=0, p1=128):
            o = l * NCV + CV[name] + i
            return cv[p0:p1, o:o + n]

        def dvc(l, name, i=0, n=1, p0=0, p1=128):
            o = l * NDV + DV[name] + i
            return dv[p0:p1, o:o + n]

        for l in range(NL):
            for a, b_, n in (("om_r", "mu_r", 8), ("om_k", "mu_k", 8), ("om_v", "mu_v", 8), ("om_wa", "mu_wa", 1),
                             ("om_g1", "mu_g1", 1), ("om_g2", "mu_g2", 1), ("omka", "k_a", 8)):
                op("dve", lambda h, l=l, a=a, b_=b_, n=n: h.tensor_scalar(out=dvc(l, a, 0, n), in0=cvc(l, b_, 0, n), scalar1=-1.0, scalar2=1.0,
                                                                        op0=ALU.mult, op1=ALU.add), reads=[cv_b], writes=[dv_b])
        with scope() as es:
            tmp = sb(es, "lamtmp", [128, 64]); tb_ = Buf()
            acc = sb(es, "lamacc", [128, 4]); ab_ = Buf()
            for l in range(NL):
                li = 0.8 - 0.6 * math.exp(-0.3 * l)
                for j, (qa, ka) in enumerate((("lq1", "lk1"), ("lq2", "lk2"))):
                    op("dve", lambda h, l=l, qa=qa, ka=ka: h.tensor_tensor(out=tmp[:], in0=cvc(l, qa, 0, 64), in1=cvc(l, ka, 0, 64), op=ALU.mult),
                       reads=[cv_b], writes=[tb_])
                    op("dve", lambda h, j=j: h.reduce_sum(out=acc[:, j:j + 1], in_=tmp[:], axis=mybir.AxisListType.X), reads=[tb_], writes=[ab_])
                op("act", lambda h: h.activation(out=acc[:, 2:4], in_=acc[:, 0:2], func=AF.Exp), reads=[ab_], writes=[ab_])
                op("dve", lambda h, l=l: h.tensor_tensor(out=dvc(l, "lam"), in0=acc[:, 2:3], in1=acc[:, 3:4], op=ALU.subtract), reads=[ab_, dv_b], writes=[dv_b])
                op("dve", lambda h, l=l, li=li: h.tensor_scalar(out=dvc(l, "nlam"), in0=dvc(l, "lam"), scalar1=float(li), scalar2=-1.0, op0=ALU.add, op1=ALU.mult),
                   reads=[dv_b], writes=[dv_b])
                op("dve", lambda h, l=l, li=li: h.tensor_scalar(out=dvc(l, "sub2"), in0=cvc(l, "subln"), scalar1=float(1.0 - li), scalar2=None, op0=ALU.mult),
                   reads=[cv_b, dv_b], writes=[dv_b])

        with scope() as es:
          if 'norot' not in parts:
              pi_ = sb(es, "posi", [128, 512], I32); pib = Buf()
              pf = sb(es, "posf", [128, 512]); pfb = Buf()
              t1 = sb(es, "rt1", [128, 512]); t1b = Buf()
              t2 = sb(es, "rt2", [128, 512]); t2b = Buf()
              ki = sb(es, "rki", [128, 512], I32); kib = Buf()
              ro = Ring(es, nc, "rto", 2, [128, 512], F32)
              TWO_PI = float(2 * np.pi)
              for tb in range(NTB):
                  ts = slice(tb * 512, (tb + 1) * 512)
                  dma("sp", pi_[:], pos_in[0:1, ts].partition_broadcast(128), writes=[pib])
                  op("dve", lambda h: h.tensor_copy(out=pf[:], in_=pi_[:]), reads=[pib], writes=[pfb])
                  op("dve", lambda h: h.tensor_scalar(out=pf[:], in0=pf[:], scalar1=cn[:, CN["invf"]:CN["invf"] + 1], scalar2=None, op0=ALU.mult),
                     reads=[pfb, cn_b], writes=[pfb])
                  for which, off, dst in (("c", float(np.pi / 2), rotC), ("s", 0.0, rotS)):
                      op("dve", lambda h, off=off: h.tensor_scalar(out=t1[:], in0=pf[:], scalar1=off, scalar2=None, op0=ALU.add), reads=[pfb], writes=[t1b])
                      op("dve", lambda h: h.tensor_scalar(out=t2[:], in0=t1[:], scalar1=float(1 / (2 * np.pi)), scalar2=None, op0=ALU.mult), reads=[t1b], writes=[t2b])
                      op("dve", lambda h: h.tensor_copy(out=ki[:], in_=t2[:]), reads=[t2b], writes=[kib])
                      op("dve", lambda h: h.tensor_copy(out=t2[:], in_=ki[:]), reads=[kib], writes=[t2b])
                      op("dve", lambda h: h.scalar_tensor_tensor(out=t1[:], in0=t2[:], scalar=-TWO_PI, in1=t1[:], op0=ALU.mult, op1=ALU.add), reads=[t2b, t1b], writes=[t1b])
                      op("dve", lambda h: h.tensor_scalar(out=t2[:], in0=t1[:], scalar1=float(np.pi), scalar2=-TWO_PI, op0=ALU.is_gt, op1=ALU.mult), reads=[t1b], writes=[t2b])
                      op("dve", lambda h: h.tensor_tensor(out=t1[:], in0=t1[:], in1=t2[:], op=ALU.add), reads=[t1b, t2b], writes=[t1b])
                      op("dve", lambda h: h.tensor_scalar(out=t2[:], in0=t1[:], scalar1=float(-np.pi), scalar2=TWO_PI, op0=ALU.is_lt, op1=ALU.mult), reads=[t1b], writes=[t2b])
                      op("dve", lambda h: h.tensor_tensor(out=t1[:], in0=t1[:], in1=t2[:], op=ALU.add), reads=[t1b, t2b], writes=[t1b])
                      o_, ob_ = ro.next()
                      op("act", lambda h, o_=o_: h.activation(out=o_[:], in_=t1[:], func=AF.Sin), reads=[t1b], writes=[ob_])
                      if which == "s":
                          op("dve", lambda h, o_=o_: h.tensor_scalar(out=o_[:], in0=o_[:], scalar1=cn[:, CN["sign"]:CN["sign"] + 1], scalar2=None, op0=ALU.mult),
                             reads=[ob_, cn_b], writes=[ob_])
                      dma("sp", dst[:, ts], o_[:], reads=[ob_], writes=[db("rot")], store=True)

        with scope() as es:
            xr = Ring(es, nc, "xin", 2, [128, D], F32)
            xo = Ring(es, nc, "xto", 2, [128, KC, 128], F32)
            for tt in range(NT):
                xi, xib = xr.next()
                dma("sp", xi[:], x_in[tt * 128:(tt + 1) * 128, :], writes=[xib])
                o_, ob_ = xo.next()
                for kc in range(KC):
                    pi = kc % 4
                    op("pe", lambda h, kc=kc, pi=pi: h.transpose(out=PS[pi][:, 0:128], in_=xi[:, kc * 128:(kc + 1) * 128], identity=cn[:, 0:128]),
                       reads=[xib, cn_b], writes=[PSB[pi]])
                    op("act" if kc % 2 else "dve", lambda h, kc=kc, pi=pi: (h.copy if kc % 2 else h.tensor_copy)(out=o_[:, kc, :], in_=PS[pi][:, 0:128]),
                       reads=[PSB[pi]], writes=[ob_])
                dma("sp", xT[:, tt * 128:(tt + 1) * 128].rearrange("(kc p) t -> p kc t", p=128), o_[:], reads=[ob_], writes=[db("xT", kc) for kc in range(KC)], store=True)

        def norm_to_hT(es, l, gname, hT, hTb):
            xs = Ring(es, nc, "nxs", 16, [128, 512], F32)
            sq = Ring(es, nc, "nsq", 2, [128, 512], BF16)
            rs = sb(es, "nrs", [128, 512]); rsb = Buf()
            for tb in range(NTB):
                ts = slice(tb * 512, (tb + 1) * 512)
                tl = []
                for kc in range(KC):
                    x_, xb_ = xs.next()
                    dma("sp", x_[:], xT[kc * 128:(kc + 1) * 128, ts], reads=[db("xT", kc)], writes=[xb_])
                    s_, sb_ = sq.next()
                    op("act", lambda h, x_=x_, s_=s_: h.activation(out=s_[:], in_=x_[:], func=AF.Square), reads=[xb_], writes=[sb_])
                    op("pe", lambda h, s_=s_, kc=kc: h.matmul(PS[6][:], lhsT=onesb[:], rhs=s_[:], start=(kc == 0), stop=(kc == KC - 1)),
                       reads=[sb_, cb_b], writes=[PSB[6]])
                    tl.append((x_, xb_))
                op("dve", lambda h: h.tensor_scalar(out=rs[:], in0=PS[6][:], scalar1=1.0 / D, scalar2=NORM_EPS, op0=ALU.mult, op1=ALU.add), reads=[PSB[6]], writes=[rsb])
                op("act", lambda h: h.activation(out=rs[:], in_=rs[:], func=AF.Sqrt), reads=[rsb], writes=[rsb])
                op("dve", lambda h: h.reciprocal(out=rs[:], in_=rs[:]), reads=[rsb], writes=[rsb])
                for kc in range(KC):
                    x_, xb_ = tl[kc]
                    op("dve", lambda h, x_=x_, kc=kc: h.scalar_tensor_tensor(out=hT[:, kc, ts], in0=x_[:], scalar=cvc(l, gname, kc), in1=rs[:],
                                                                                                   op0=ALU.mult, op1=ALU.mult),
                       reads=[xb_, rsb, cv_b], writes=[hTb[tb]])

        def proj_fm(es, hT, hTb, tasks, nper=1, each=False):
            st = Ring(es, nc, "wst", 2 if nper == 1 else 3, [128, KC, 128], F32)
            wb = Ring(es, nc, "wbf", 3 if nper == 1 else 4, [128, KC, 128], BF16)
            psr = [0]
            cvi = [0]
            groups = [tasks[ti:ti + nper] for ti in range(0, len(tasks), nper)]

            def load(grp):
                wts = []
                for segs, M, epi in grp:
                    s_, sb_ = st.next()
                    o = 0
                    for ap, n in segs:
                        dma("sp", s_[:, :, o:o + n], ap.rearrange("(kc p) n -> p kc n", p=128), writes=[sb_])
                        o += n
                    w_, wb_ = wb.next()
                    cvi[0] += 1
                    if cvi[0] % 2:
                        op("act", lambda h, s_=s_, w_=w_, M=M: h.copy(out=w_[:, :, 0:M], in_=s_[:, :, 0:M]), reads=[sb_], writes=[wb_])
                    else:
                        op("dve", lambda h, s_=s_, w_=w_, M=M: h.tensor_copy(out=w_[:, :, 0:M], in_=s_[:, :, 0:M]), reads=[sb_], writes=[wb_])
                    wts.append((w_, wb_, M))
                return wts
            nxt = load(groups[0])
            for gi, grp in enumerate(groups):
                wts = nxt
                if gi + 1 < len(groups):
                    nxt = load(groups[gi + 1])
                for tb in range(NTB):
                    ts = slice(tb * 512, (tb + 1) * 512)
                    pss = []
                    for w_, wb_, M in wts:
                        pi = psr[0] % 4
                        psr[0] += 1
                        for kc in range(KC):
                            op("pe", lambda h, w_=w_, M=M, kc=kc, pi=pi: h.matmul(PS[pi][0:M, :], lhsT=w_[:, kc, 0:M], rhs=hT[:, kc, ts], start=(kc == 0), stop=(kc == KC - 1)),
                               reads=[wb_, hTb[tb]], writes=[PSB[pi]], inc=(kc == KC - 1))
                        pss.append(pi)
                    if each:
                        for t_, pi_ in zip(grp, pss):
                            t_[2](tb, [pi_])
                    else:
                        grp[0][2](tb, pss)

        def mk_rings(es):
            return {"t": Ring(es, nc, "ept", 2, [128, 512], F32), "of": Ring(es, nc, "epof", 2, [128, 512], F32),
                    "ob": Ring(es, nc, "epob", 3, [128, 512], BF16), "car": sb(es, "epcar", [128, 64]), "ncar": [0]}

        def shift_epi(R, name, M, mu_ap, om_ap, dests, act=None):
            tr = R["t"]
            orr = R["of"] if act else R["ob"]
            obr = R["ob"] if act else None
            ci = R["ncar"][0]
            R["ncar"][0] += 1
            carry = R["car"][:, ci:ci + 1]; cb = Buf()

            def epi(tb, pss):
                pi = pss[0]
                ts = slice(tb * 512, (tb + 1) * 512)
                t_, tb_ = tr.next()
                o_, ob_ = orr.next()
                op("act", lambda h: h.activation(out=t_[0:M, :], in_=PS[pi][0:M, :], func=AF.Identity, scale=om_ap), reads=[PSB[pi], dv_b], writes=[tb_])
                op("dve", lambda h: h.scalar_tensor_tensor(out=o_[0:M, 1:512], in0=PS[pi][0:M, 0:511], scalar=mu_ap, in1=t_[0:M, 1:512], op0=ALU.mult, op1=ALU.add),
                   reads=[PSB[pi], tb_, cv_b], writes=[ob_])
                if tb == 0:
                    op("dve", lambda h: h.tensor_copy(out=o_[0:M, 0:1], in_=t_[0:M, 0:1]), reads=[tb_], writes=[ob_])
                else:
                    op("dve", lambda h: h.scalar_tensor_tensor(out=o_[0:M, 0:1], in0=carry[0:M, :], scalar=mu_ap, in1=t_[0:M, 0:1], op0=ALU.mult, op1=ALU.add),
                       reads=[cb, tb_, cv_b], writes=[ob_])
                op("act", lambda h: h.copy(out=carry[0:M, :], in_=PS[pi][0:M, 511:512]), reads=[PSB[pi]], writes=[cb])
                if act:
                    f_, fb_ = obr.next()
                    for p0, p1, fn in act:
                        if fn is None:
                            op("dve", lambda h, p0=p0, p1=p1: h.tensor_copy(out=f_[p0:p1, :], in_=o_[p0:p1, :]), reads=[ob_], writes=[fb_])
                        else:
                            op("act", lambda h, p0=p0, p1=p1, fn=fn: h.activation(out=f_[p0:p1, :], in_=o_[p0:p1, :], func=fn), reads=[ob_], writes=[fb_])
                    o_, ob_ = f_, fb_
                for dap, dbuf in dests:
                    dma("sp", dap[:, ts], o_[0:M, :], reads=[ob_], writes=[dbuf], store=True)
            return epi

        def plain_epi(R, name, dest, dbuf, flip):
            orr = R["ob"]

            def epi(tb, pss):
                pi = pss[0]
                o_, ob_ = orr.next()
                if (tb + flip) % 2:
                    op("act", lambda h: h.copy(out=o_[:], in_=PS[pi][:]), reads=[PSB[pi]], writes=[ob_])
                else:
                    op("dve", lambda h: h.tensor_copy(out=o_[:], in_=PS[pi][:]), reads=[PSB[pi]], writes=[ob_])
                dma("sp", dest[:, tb * 512:(tb + 1) * 512], o_[:], reads=[ob_], writes=[dbuf], store=True)
            return epi

        def proj_res(es, l, src, srcname, KCn, W, gname):
            TBD = min(1024, S)
            NH = TBD // 512
            srct = sb(es, "prs", [128, KCn, TBD], BF16); srcb = Buf()
            KH = KCn // 4
            st = Ring(es, nc, "prst", 4, [128, KH, 128], F32)
            wbr = Ring(es, nc, "prwb", 3, [128, KCn, 128], BF16)
            yr = Ring(es, nc, "pry", 3, [128, TBD], F32)
            sqr = Ring(es, nc, "prsq", 2, [128, 512], BF16)
            rst = sb(es, "prrs", [128, TBD]); rsb = Buf()
            xr = Ring(es, nc, "prx", 3, [128, TBD], F32)
            for tbd in range(S // TBD):
                ts = slice(tbd * TBD, (tbd + 1) * TBD)
                for kc in range(KCn):
                    dma("sp", srct[:, kc, :], src[kc * 128:(kc + 1) * 128, ts], reads=[db(srcname, kc)], writes=[srcb])
                def loadw(c):
                    w_, wb_ = wbr.next()
                    for hf in range(4):
                        s_, sb_ = st.next()
                        dma("sp", s_[:], W[hf * KH * 128:(hf + 1) * KH * 128, c * 128:(c + 1) * 128].rearrange("(kc p) n -> p kc n", p=128), writes=[sb_])
                        if hf % 2:
                            op("act", lambda h, s_=s_, w_=w_, hf=hf: h.copy(out=w_[:, hf * KH:(hf + 1) * KH, :], in_=s_[:]), reads=[sb_], writes=[wb_])
                        else:
                            op("dve", lambda h, s_=s_, w_=w_, hf=hf: h.tensor_copy(out=w_[:, hf * KH:(hf + 1) * KH, :], in_=s_[:]), reads=[sb_], writes=[wb_])
                    return w_, wb_
                nxtw = loadw(0)
                for c in range(KC):
                    w_, wb_ = nxtw
                    if c + 1 < KC:
                        nxtw = loadw(c + 1)
                    y_, yb_ = yr.next()
                    for hh in range(NH):
                        pi = (c * NH + hh) % 4
                        for kc in range(KCn):
                            op("pe", lambda h, w_=w_, kc=kc, pi=pi, hh=hh: h.matmul(PS[pi][:], lhsT=w_[:, kc, :], rhs=srct[:, kc, hh * 512:(hh + 1) * 512],
                                                                                   start=(kc == 0), stop=(kc == KCn - 1)),
                               reads=[wb_, srcb], writes=[PSB[pi]], inc=(kc == KCn - 1))
                        op("dve", lambda h, y_=y_, pi=pi, hh=hh: h.tensor_copy(out=y_[:, hh * 512:(hh + 1) * 512], in_=PS[pi][:]), reads=[PSB[pi]], writes=[yb_])
                        q_, qb_ = sqr.next()
                        op("act", lambda h, q_=q_, y_=y_, hh=hh: h.activation(out=q_[:], in_=y_[:, hh * 512:(hh + 1) * 512], func=AF.Square), reads=[yb_], writes=[qb_])
                        op("pe", lambda h, q_=q_, hh=hh, c=c: h.matmul(PS[4 + hh][:], lhsT=onesb[:], rhs=q_[:], start=(c == 0), stop=(c == KC - 1)),
                           reads=[qb_, cb_b], writes=[PSB[4 + hh]])
                    dma("sp", yT[c * 128:(c + 1) * 128, ts], y_[:], reads=[yb_], writes=[db("yT", c)], store=True)
                for hh in range(NH):
                    hs = slice(hh * 512, (hh + 1) * 512)
                    op("dve", lambda h, hh=hh, hs=hs: h.tensor_scalar(out=rst[:, hs], in0=PS[4 + hh][:], scalar1=1.0 / D, scalar2=NORM_EPS, op0=ALU.mult, op1=ALU.add),
                       reads=[PSB[4 + hh]], writes=[rsb])
                op("act", lambda h: h.activation(out=rst[:], in_=rst[:], func=AF.Sqrt), reads=[rsb], writes=[rsb])
                op("dve", lambda h: h.reciprocal(out=rst[:], in_=rst[:]), reads=[rsb], writes=[rsb])
                def loadxy(c):
                    y_, yb_ = yr.next()
                    x_, xb_ = xr.next()
                    dma("sp", y_[:], yT[c * 128:(c + 1) * 128, ts], reads=[db("yT", c)], writes=[yb_])
                    dma("sp", x_[:], xT[c * 128:(c + 1) * 128, ts], reads=[db("xT", c)], writes=[xb_])
                    return y_, yb_, x_, xb_
                nxy = loadxy(0)
                for c in range(KC):
                    y_, yb_, x_, xb_ = nxy
                    if c + 1 < KC:
                        nxy = loadxy(c + 1)
                    op("dve", lambda h, y_=y_, c=c: h.scalar_tensor_tensor(out=y_[:], in0=y_[:], scalar=cvc(l, gname, c), in1=rst[:], op0=ALU.mult, op1=ALU.mult),
                       reads=[yb_, rsb, cv_b], writes=[yb_])
                    op("dve", lambda h, y_=y_, x_=x_: h.tensor_tensor(out=x_[:], in0=x_[:], in1=y_[:], op=ALU.add), reads=[yb_, xb_], writes=[xb_])
                    dma("sp", xT[c * 128:(c + 1) * 128, ts], x_[:], reads=[xb_], writes=[db("xT", c)], store=True)


        def attn_phase(es, l):
            C = sb(es, "arC", [128, S]); Cb = Buf()
            Sg = sb(es, "arS", [128, S]); Sgb = Buf()
            dma("sp", C[:], rotC[:, :], reads=[db("rot")], writes=[Cb])
            dma("sp", Sg[:], rotS[:, :], reads=[db("rot")], writes=[Sgb])
            dmf = sb(es, "admf", [128, 2048]); dmfb = Buf()
            dm = sb(es, "adm", [128, 4, 512], BF16); dmb = Buf()
            dma("sp", dmf[:], cn_in[:, CN["dmask"]:CN["dmask"] + 2048], writes=[dmfb])
            op("pool", lambda h: h.tensor_copy(out=dm[:], in_=dmf[:].rearrange("p (a b) -> p a b", a=4)), reads=[dmfb], writes=[dmb])
            raws = Ring(es, nc, "araw", 2, [128, 512], BF16)
            t1r = Ring(es, nc, "at1", 2, [128, 512], F32)
            t2r = Ring(es, nc, "at2", 2, [128, 512], F32)
            qk = Ring(es, nc, "aqk", 4, [128, S], BF16)
            Vr = Ring(es, nc, "aV", 2, [128, NT, 128], BF16)
            Er = Ring(es, nc, "aE", 4, [128, 512], BF16)
            wk = Ring(es, nc, "awk", 6, [128, 512], F32)
            sqr = Ring(es, nc, "asq", 2, [128, 512], BF16)
            outr = Ring(es, nc, "aout", 2, [128, 512], BF16)
            for hd in range(8):
                rot = []
                for src, nm in ((qT, "qT"), (kdT, "kdT")):
                    d_, db_ = qk.next()
                    for tb in range(NTB):
                        ts = slice(tb * 512, (tb + 1) * 512)
                        r_, rb_ = raws.next()
                        dma("sp", r_[:], src[hd * 128:(hd + 1) * 128, ts], reads=[db(nm, hd)], writes=[rb_])
                        op("pe", lambda h, r_=r_: h.matmul(PS[0][:], lhsT=permb[:], rhs=r_[:], start=True, stop=True), reads=[rb_, cb_b], writes=[PSB[0]])
                        a_, ab_ = t1r.next()
                        b_, bb_ = t2r.next()
                        op("dve", lambda h, r_=r_, a_=a_, ts=ts: h.tensor_tensor(out=a_[:], in0=r_[:], in1=C[:, ts], op=ALU.mult), reads=[rb_, Cb], writes=[ab_])
                        op("dve", lambda h, b_=b_, ts=ts: h.tensor_tensor(out=b_[:], in0=PS[0][:], in1=Sg[:, ts], op=ALU.mult), reads=[PSB[0], Sgb], writes=[bb_])
                        op("dve", lambda h, a_=a_, b_=b_, d_=d_, ts=ts: h.tensor_tensor(out=d_[:, ts], in0=a_[:], in1=b_[:], op=ALU.add), reads=[ab_, bb_], writes=[db_])
                    rot.append((d_, db_))
                (q_, qb_), (k_, kb_) = rot
                V_, Vb_ = Vr.next()
                dma("sp", V_[:], vd[:, hd * 128:(hd + 1) * 128].rearrange("(tt p) c -> p tt c", p=128), reads=[db("vd", hd)], writes=[Vb_])
                for qb in range(NTB):
                    qs = slice(qb * 512, (qb + 1) * 512)
                    nk = 4 * qb + 4
                    steps = [(kt, m) for kt in range(nk) for m in range(2)]

                    def emit_s(i):
                        kt, m = steps[i]
                        pi = i % 3
                        op("pe", lambda h: h.matmul(PS[pi][:], lhsT=k_[m * 64:(m + 1) * 64, kt * 128:(kt + 1) * 128], rhs=q_[m * 64:(m + 1) * 64, qs],
                                                    start=True, stop=True), reads=[kb_, qb_], writes=[PSB[pi]])
                    emit_s(0)
                    emit_s(1)
                    for i, (kt, m) in enumerate(steps):
                        pi = i % 3
                        if i + 2 < len(steps):
                            emit_s(i + 2)
                        e_, eb_ = Er.next()
                        op("act", lambda h, e_=e_, pi=pi: h.activation(out=e_[:], in_=PS[pi][:], func=AF.Exp, scale=0.125), reads=[PSB[pi]], writes=[eb_])
                        if kt >= 4 * qb:
                            op("dve", lambda h, e_=e_, j=kt - 4 * qb: h.tensor_tensor(out=e_[:], in0=e_[:], in1=dm[:, j, :], op=ALU.mult), reads=[eb_, dmb], writes=[eb_])
                        op("pe", lambda h, e_=e_, kt=kt, m=m: h.matmul(PS[3 + m][:], lhsT=V_[:, kt, :], rhs=e_[:], start=(kt == 0), stop=(kt == nk - 1)),
                           reads=[Vb_, eb_], writes=[PSB[3 + m]], inc=False)
                        op("pe", lambda h, e_=e_, kt=kt, m=m: h.matmul(PS[5 + m][:], lhsT=onesb[:], rhs=e_[:], start=(kt == 0), stop=(kt == nk - 1)),
                           reads=[cb_b, eb_], writes=[PSB[5 + m]])
                    w = [wk.next() for _ in range(6)]
                    for m in range(2):
                        op("dve", lambda h, m=m: h.reciprocal(out=w[m][0][:], in_=PS[5 + m][:]), reads=[PSB[5 + m]], writes=[w[m][1]])
                        op("dve", lambda h, m=m: h.tensor_tensor(out=w[2 + m][0][:], in0=PS[3 + m][:], in1=w[m][0][:], op=ALU.mult), reads=[PSB[3 + m], w[m][1]], writes=[w[2 + m][1]])
                    op("dve", lambda h: h.scalar_tensor_tensor(out=w[4][0][:], in0=w[3][0][:], scalar=dvc(l, "nlam"), in1=w[2][0][:], op0=ALU.mult, op1=ALU.add),
                       reads=[w[3][1], w[2][1], dv_b], writes=[w[4][1]])
                    s_, sb_ = sqr.next()
                    op("act", lambda h, s_=s_: h.activation(out=s_[:], in_=w[4][0][:], func=AF.Square), reads=[w[4][1]], writes=[sb_])
                    op("pe", lambda h, s_=s_: h.matmul(PS[0][:], lhsT=onesb[:], rhs=s_[:], start=True, stop=True), reads=[sb_, cb_b], writes=[PSB[0]])
                    op("dve", lambda h: h.tensor_scalar(out=w[5][0][:], in0=PS[0][:], scalar1=1.0 / 128, scalar2=SUBLN_EPS, op0=ALU.mult, op1=ALU.add), reads=[PSB[0]], writes=[w[5][1]])
                    op("act", lambda h: h.activation(out=w[5][0][:], in_=w[5][0][:], func=AF.Sqrt), reads=[w[5][1]], writes=[w[5][1]])
                    op("dve", lambda h: h.reciprocal(out=w[5][0][:], in_=w[5][0][:]), reads=[w[5][1]], writes=[w[5][1]])
                    o_, ob_ = outr.next()
                    op("dve", lambda h, o_=o_: h.scalar_tensor_tensor(out=o_[:], in0=w[4][0][:], scalar=dvc(l, "sub2"), in1=w[5][0][:], op0=ALU.mult, op1=ALU.mult),
                       reads=[w[4][1], w[5][1], dv_b], writes=[ob_])
                    dma("sp", mixT[1024 + hd * 128:1024 + (hd + 1) * 128, qs], o_[:], reads=[ob_], writes=[db("mixT", 8 + hd)], store=True)

        def rwkv_phase(es, l):
            f32t = lambda nm, sh=[128, 512]: (sb(es, nm, sh), Buf())
            bft = lambda nm, sh=[128, 512]: (sb(es, nm, sh, BF16), Buf())
            scanm, scb = f32t("scanm")
            dma("sp", scanm[:], cn_in[:, CN["scanm"]:CN["scanm"] + 512], writes=[scb])
            hm, hmb = f32t("hm", [128, 7, 256])
            dma("sp", hm[:], cn_in[:, CN["hm"]:CN["hm"] + 7 * 256].rearrange("p (k c) -> p k c", k=7), writes=[hmb])
            SL = []
            for i in range(8):
                SL.append({"T12": bft("T12_%d" % i, [128, 512]), "T3": bft("T3_%d" % i, [128, 128]), "W1": bft("W1_%d" % i, [128, 128]), "W2": bft("W2_%d" % i, [128, 256]),
                           "p0": bft("p0_%d" % i, [128, 256]), "DG": [bft("DG%d_%d" % (j, i), [128, 256]) for j in range(2)], "ZZ": bft("ZZ_%d" % i, [128, 256]),
                           "tW": bft("tW_%d" % i, [128, 256]), "Xb": bft("Xb_%d" % i, [128, 64]), "Ub": bft("Ub_%d" % i, [128, 64]), "HG": f32t("HG_%d" % i, [128, 64])})
            m2b, m2bb = bft("m2b", [128, 256]); mslb, mslbb = bft("mslb", [128, 128])
            op("pool", lambda h: h.tensor_copy(out=m2b[:], in_=cn[:, CN["m2"]:CN["m2"] + 256]), reads=[cn_b], writes=[m2bb])
            op("pool", lambda h: h.tensor_copy(out=mslb[:], in_=cn[:, CN["msl"]:CN["msl"] + 128]), reads=[cn_b], writes=[mslbb])
            wst, wstb = f32t("lst", [128, 1024])
            wa2b, wa2bb = bft("wa2b", [128, 1024]); g2ab, g2abb = bft("g2ab", [128, 1024]); g2bv, g2bvb = bft("g2bv", [64, 1024])
            dma("sp", wst[0:64, :], w2_in[l], writes=[wstb]); dma("sp", wst[64:128, :], a2_in[l], writes=[wstb])
            op("pool", lambda h: h.tensor_copy(out=wa2b[:], in_=wst[:]), reads=[wstb], writes=[wa2bb])
            dma("sp", wst[:], g2_in[l][0:128, :], writes=[wstb])
            op("pool", lambda h: h.tensor_copy(out=g2ab[:], in_=wst[:]), reads=[wstb], writes=[g2abb])
            dma("sp", wst[0:32, :], g2_in[l][128:160, :], writes=[wstb])
            if l > 0:
                dma("sp", wst[32:64, :], v2_in[l - 1], writes=[wstb])
            op("pool", lambda h: h.tensor_copy(out=g2bv[0:64 if l > 0 else 32, :], in_=wst[0:64 if l > 0 else 32, :]), reads=[wstb], writes=[g2bvb])
            Hf, _ = f32t("Hf", [128, 8, 64]); Hb, _ = bft("Hb", [128, 8, 64])
            Hfb = [Buf(), Buf()]; Hbb = [Buf(), Buf()]
            op("pool", lambda h: h.memset(Hf[:], 0.0), writes=Hfb); op("pool", lambda h: h.memset(Hb[:], 0.0), writes=Hbb)
            wa_t, wab = bft("wa_t"); g1_t, g1b = bft("g1_t"); g2_t, g2b_ = bft("g2_t", [64, 512])
            r_t, rb = bft("r_t"); k_t, kb_ = bft("k_t"); v_t, vb = bft("v_t"); vf_t, vfb = bft("vf_t")
            sg, sgb = f32t("sg"); cs, csb = f32t("cs"); ex, exb = f32t("ex"); eG, eGb = f32t("eG"); eGi, eGib = f32t("eGi"); eGx, eGxb = f32t("eGx")
            a_t, ab = f32t("a_t"); gt, gtb = f32t("gt"); v2t, v2b = f32t("v2t"); kk, kkb = f32t("kk"); sqb_, sqbb = bft("sqb"); rn, rnb = f32t("rn")
            k2, k2b = f32t("k2"); tmp, tmpb = f32t("tmp"); AR, ARb = bft("AR", [128, 2, 512]); BhT, BhTb = bft("BhT"); KhT, KhTb = bft("KhT"); vbf, vbfb = bft("vbf")
            rkb, rkbb = bft("rkb"); bon, bonb = f32t("bon"); OT, _ = f32t("OT"); OTb = [Buf(), Buf()]; ob16, ob16b = bft("ob16"); mean, meanb = f32t("mean"); var, varb = f32t("var")
            Bh = [bft("Bh%d" % i, [128, 128]) for i in range(4)]; Kh = [bft("Kh%d" % i, [128, 128]) for i in range(4)]; Vt = [bft("Vt%d" % i, [128, 128]) for i in range(4)]
            T1, T1b = bft("T1", [128, 256]); T2, T2b = bft("T2", [128, 256]); T3, T3b = bft("T3", [128, 128])
            W1, W1b = bft("W1", [128, 128]); W2, W2b = bft("W2", [128, 256])
            PP = [bft("PP%d" % i, [128, 256]) for i in range(2)]; NTt = [bft("NT%d" % i, [128, 128]) for i in range(2)]
            Xb, Xbb = bft("Xb", [128, 64]); Ub, Ubb = bft("Ub", [128, 64]); mo, mob = bft("mo")
            tt_ = lambda e, o, a, b, opn, rd, wr: op(e, lambda h: h.tensor_tensor(out=o, in0=a, in1=b, op=opn), reads=rd, writes=wr)
            trc = [0]
            G2R = 64 if l > 0 else 32
            for tb in range(NTB):
                ts = slice(tb * 512, (tb + 1) * 512)
                dma("sp", wa_t[:], waT[:, ts], reads=[db("waT")], writes=[wab]); dma("sp", g1_t[:], g1T[:, ts], reads=[db("g1T")], writes=[g1b])
                dma("sp", g2_t[0:G2R, :], g2T[0:G2R, ts], reads=[db("g2T")], writes=[g2b_])
                for c in range(8):
                    cs_ = slice(c * 128, (c + 1) * 128)
                    dma("sp", r_t[:], rT[cs_, ts], reads=[db("rT", c)], writes=[rb]); dma("sp", k_t[:], kT[cs_, ts], reads=[db("kT", c)], writes=[kb_])
                    dma("sp", v_t[:], vT[cs_, ts], reads=[db("vT", c)], writes=[vb])
                    op("pe", lambda h: h.matmul(PS[0][:], lhsT=wa2b[0:64, cs_], rhs=wa_t[0:64, :], start=True, stop=True), reads=[wa2bb, wab], writes=[PSB[0]])
                    op("act", lambda h: h.activation(out=sg[:], in_=PS[0][:], func=AF.Sigmoid, bias=cvc(l, "w0", c)), reads=[PSB[0], cv_b], writes=[sgb])
                    op("dve", lambda h: h.tensor_tensor_scan(out=cs[:], data0=scanm[:], data1=sg[:], initial=0.0, op0=ALU.mult, op1=ALU.add), reads=[scb, sgb], writes=[csb])
                    tt_("dve", ex[:], cs[:], sg[:], ALU.subtract, [csb, sgb], [exb])
                    op("act", lambda h: h.activation(out=eG[:], in_=cs[:], func=AF.Exp, scale=-C0), reads=[csb], writes=[eGb])
                    op("act", lambda h: h.activation(out=eGi[:], in_=cs[:], func=AF.Exp, scale=C0), reads=[csb], writes=[eGib])
                    op("act", lambda h: h.activation(out=eGx[:], in_=ex[:], func=AF.Exp, scale=-C0), reads=[exb], writes=[eGxb])
                    op("pe", lambda h: h.matmul(PS[0][:], lhsT=wa2b[64:128, cs_], rhs=wa_t[64:128, :], start=True, stop=True), reads=[wa2bb, wab], writes=[PSB[0]])
                    op("act", lambda h: h.activation(out=a_t[:], in_=PS[0][:], func=AF.Sigmoid, bias=cvc(l, "a0", c)), reads=[PSB[0], cv_b], writes=[ab])
                    op("pe", lambda h: h.matmul(PS[0][:], lhsT=g2ab[:, cs_], rhs=g1_t[:], start=True, stop=False), reads=[g2abb, g1b], writes=[PSB[0]], inc=False)
                    op("pe", lambda h: h.matmul(PS[0][:], lhsT=g2bv[0:32, cs_], rhs=g2_t[0:32, :], start=False, stop=True), reads=[g2bvb, g2b_], writes=[PSB[0]])
                    op("act", lambda h: h.copy(out=gt[:], in_=PS[0][:]), reads=[PSB[0]], writes=[gtb])
                    if l > 0:
                        dma("sp", vf_t[:], vfT[cs_, ts], reads=[db("vfT", c)], writes=[vfb])
                        op("pe", lambda h: h.matmul(PS[0][:], lhsT=g2bv[32:64, cs_], rhs=g2_t[32:64, :], start=True, stop=True), reads=[g2bvb, g2b_], writes=[PSB[0]])
                        op("act", lambda h: h.activation(out=tmp[:], in_=PS[0][:], func=AF.Sigmoid, bias=cvc(l, "v0", c)), reads=[PSB[0], cv_b], writes=[tmpb])
                        tt_("dve", v2t[:], vf_t[:], v_t[:], ALU.subtract, [vfb, vb], [v2b])
                        tt_("dve", v2t[:], v2t[:], tmp[:], ALU.mult, [v2b, tmpb], [v2b])
                        tt_("dve", v2t[:], v2t[:], v_t[:], ALU.add, [v2b, vb], [v2b])
                    else:
                        op("dve", lambda h: h.tensor_copy(out=v2t[:], in_=v_t[:]), reads=[vb], writes=[v2b])
                    op("act", lambda h: h.copy(out=vbf[:], in_=v2t[:]), reads=[v2b], writes=[vbfb])
                    op("dve", lambda h: h.tensor_scalar(out=kk[:], in0=k_t[:], scalar1=cvc(l, "k_k", c), scalar2=None, op0=ALU.mult), reads=[kb_, cv_b], writes=[kkb])
                    op("act", lambda h: h.activation(out=sqb_[:], in_=kk[:], func=AF.Square), reads=[kkb], writes=[sqbb])
                    op("pe", lambda h: h.matmul(PS[0][:], lhsT=bonesb[:], rhs=sqb_[:], start=True, stop=True), reads=[cb_b, sqbb], writes=[PSB[0]])
                    op("act", lambda h: h.activation(out=rn[:], in_=PS[0][:], func=AF.Sqrt, bias=1e-20), reads=[PSB[0]], writes=[rnb])
                    op("dve", lambda h: h.reciprocal(out=rn[:], in_=rn[:]), reads=[rnb], writes=[rnb])
                    tt_("dve", kk[:], kk[:], rn[:], ALU.mult, [kkb, rnb], [kkb])
                    op("dve", lambda h: h.tensor_scalar(out=tmp[:], in0=a_t[:], scalar1=cvc(l, "k_a", c), scalar2=dvc(l, "omka", c), op0=ALU.mult, op1=ALU.add),
                       reads=[ab, cv_b, dv_b], writes=[tmpb])
                    tt_("dve", k2[:], k_t[:], tmp[:], ALU.mult, [kb_, tmpb], [k2b])
                    op("dve", lambda h: h.scalar_tensor_tensor(out=AR[:, 0, :], in0=kk[:], scalar=-1.0, in1=eGx[:], op0=ALU.mult, op1=ALU.mult), reads=[kkb, eGxb], writes=[ARb])
                    tt_("dve", AR[:, 1, :], r_t[:], eG[:], ALU.mult, [rb, eGb], [ARb])
                    tt_("dve", tmp[:], kk[:], a_t[:], ALU.mult, [kkb, ab], [tmpb])
                    tt_("dve", BhT[:], tmp[:], eGi[:], ALU.mult, [tmpb, eGib], [BhTb])
                    tt_("pool", KhT[:], k2[:], eGi[:], ALU.mult, [k2b, eGib], [KhTb])
                    op("dve", lambda h: h.scalar_tensor_tensor(out=rkb[:], in0=r_t[:], scalar=cvc(l, "r_k", c), in1=k2[:], op0=ALU.mult, op1=ALU.mult), reads=[rb, k2b, cv_b], writes=[rkbb])
                    op("pe", lambda h: h.matmul(PS[0][:], lhsT=bonesb[:], rhs=rkb[:], start=True, stop=True), reads=[cb_b, rkbb], writes=[PSB[0]])
                    tt_("dve", bon[:], PS[0][:], v2t[:], ALU.mult, [PSB[0], v2b], [bonb])
                    for n in range(4):
                        tsl = slice(n * 128, (n + 1) * 128)
                        for src, srcb, dst in ((BhT, BhTb, Bh[n]), (KhT, KhTb, Kh[n]), (vbf, vbfb, Vt[n])):
                            ri = trc[0] % 4
                            trc[0] += 1
                            op("pe", lambda h, src=src, ri=ri: h.transpose(out=PSH[:, ri * 128:(ri + 1) * 128], in_=src[:, tsl], identity=identb[:]), reads=[srcb, cb_b], writes=[PSH_b[ri]])
                            op("act" if ri % 2 else "dve", lambda h, dst=dst, ri=ri: (h.copy if ri % 2 else h.tensor_copy)(out=dst[0][:], in_=PSH[:, ri * 128:(ri + 1) * 128]),
                               reads=[PSH_b[ri]], writes=[dst[1]])
                    def mmq(out, lhsT, rhs, rd, wr, st=True, sp=True, inc=True):
                        op("pe", lambda h: h.matmul(out, lhsT=lhsT, rhs=rhs, start=st, stop=sp), reads=rd, writes=wr, inc=inc)

                    def stage1(n, hh, lane, sl):
                        tsl = slice(n * 128, (n + 1) * 128)
                        P_ = slice(64 * hh, 64 * hh + 64)
                        A, Ab = PS[1 + lane], PSB[1 + lane]
                        e1 = "act" if lane % 2 == 0 else "dve"
                        cp = lambda eng, o, i, rd, wr: op(eng, lambda h: (h.copy if eng == "act" else h.tensor_copy)(out=o, in_=i), reads=rd, writes=wr)
                        T12, T12b = sl["T12"]; T3, T3b = sl["T3"]; W1, W1b = sl["W1"]; W2, W2b = sl["W2"]; p0, p0b = sl["p0"]
                        DGs = sl["DG"]; ZZ, ZZb = sl["ZZ"]; tW, tWb = sl["tW"]
                        mmq(A[:, 0:256], BhT[P_, tsl], AR[P_, :, tsl], [BhTb, ARb], [Ab], inc=False)
                        mmq(A[:, 256:512], KhT[P_, tsl], AR[P_, :, tsl], [KhTb, ARb], [Ab])
                        yield
                        cp(e1, T12[:], A[:, 0:512], [Ab], [T12b, Ab])
                        yield
                        mmq(A[:, 0:128], AR[P_, 0, tsl], BhT[P_, tsl], [ARb, BhTb], [Ab])
                        d0, d0b = DGs[0]
                        tt_("dve", p0[:, 128:256], T12[:, 0:128], m2b[:, 0:128], ALU.mult, [T12b, m2bb], [p0b])
                        tt_("dve", W1[:], T12[:, 128:256], m2b[:, 128:256], ALU.mult, [T12b, m2bb], [W1b])
                        tt_("dve", W2[:], T12[:, 256:512], m2b[:], ALU.mult, [T12b, m2bb], [W2b])
                        tt_("dve", d0[:, 128:256], T12[:, 0:128], hm[:, 0, 128:256], ALU.mult, [T12b, hmb], [d0b])
                        yield
                        cp(e1, T3[:], A[:, 0:128], [Ab], [T3b, Ab])
                        tt_("dve", d0[:, 128:256], d0[:, 128:256], identb[:], ALU.add, [d0b, cb_b], [d0b])
                        yield
                        tt_("dve", p0[:, 0:128], T3[:], mslb[:], ALU.mult, [T3b, mslbb], [p0b])
                        tt_("dve", d0[:, 0:128], T3[:], hm[:, 0, 0:128], ALU.mult, [T3b, hmb], [d0b])
                        tt_("dve", d0[:, 0:128], d0[:, 0:128], identb[:], ALU.add, [d0b, cb_b], [d0b])
                        yield
                        for k in range(1, 7):
                            dc, dcb = DGs[(k - 1) % 2]
                            dn, dnb = DGs[k % 2]
                            mmq(A[:, 0:128], p0[:, 128:256], dc[:, 0:128], [p0b, dcb], [Ab], inc=False)
                            mmq(A[:, 128:256], p0[:, 0:128], dc[:, 128:256], [p0b, dcb], [Ab])
                            yield
                            cp("act" if (k + lane) % 2 else "dve", ZZ[:], A[:, 0:256], [Ab], [ZZb, Ab])
                            yield
                            mmq(A[:, 256:384], dc[:, 128:256], ZZ[:, 0:128], [dcb, ZZb], [Ab], inc=False)
                            mmq(A[:, 384:512], dc[:, 0:128], ZZ[:, 128:256], [dcb, ZZb], [Ab])
                            yield
                            tt_("dve", tW[:], A[:, 256:512], hm[:, k, :], ALU.mult, [Ab, hmb], [tWb, Ab])
                            yield
                            tt_("pool" if (k % 3 == 0 and lane % 2) else "dve", dn[:], dc[:], tW[:], ALU.add, [dcb, tWb], [dnb])
                            yield

                    def stage2(n, hh, lane, sl):
                        tsl = slice(n * 128, (n + 1) * 128)
                        P_ = slice(64 * hh, 64 * hh + 64)
                        B, Bb = PS[1 + lane], PSB[1 + lane]
                        W1, W1b = sl["W1"]; W2, W2b = sl["W2"]; Xb, Xbb = sl["Xb"]; Ub, Ubb = sl["Ub"]; HG, HGb = sl["HG"]
                        ntf, ntfb = sl["DG"][0][0][:, 128:256], sl["DG"][0][1]
                        vt, vtb = Vt[n]
                        mmq(B[:, 0:64], AR[P_, 0, tsl], Hb[P_, c, :], [ARb, Hbb[hh]], [Bb], True, False, False)
                        mmq(B[:, 0:64], W2[:, 0:128], vt[:, P_], [W2b, vtb], [Bb], False, True)
                        yield
                        op("act", lambda h: h.copy(out=Xb[:], in_=B[:, 0:64]), reads=[Bb], writes=[Xbb, Bb])
                        yield
                        mmq(B[:, 64:128], ntf, Xb[:], [ntfb, Xbb], [Bb])
                        yield
                        op("dve", lambda h: h.tensor_copy(out=Ub[:], in_=B[:, 64:128]), reads=[Bb], writes=[Ubb, Bb])
                        gcol = eG[P_, n * 128 + 127:n * 128 + 128]
                        op("dve", lambda h: h.tensor_scalar(out=HG[P_, :], in0=Hf[P_, c, :], scalar1=gcol, scalar2=None, op0=ALU.mult), reads=[Hfb[hh], eGb], writes=[HGb])
                        yield
                        mmq(B[P_, 128:256], Hb[P_, c, :], AR[P_, 1, tsl], [Hbb[hh], ARb], [Bb], True, False, False)
                        mmq(B[P_, 128:256], Ub[:], W1[:], [Ubb, W1b], [Bb], False, False, False)
                        mmq(B[P_, 128:256], vt[:, P_], W2[:, 128:256], [vtb, W2b], [Bb], False, True, False)
                        mmq(B[P_, 256:320], Bh[n][0][:, P_], Ub[:], [Bh[n][1], Ubb], [Bb], True, False, False)
                        mmq(B[P_, 256:320], Kh[n][0][:, P_], vt[:, P_], [Kh[n][1], vtb], [Bb], False, True)
                        yield
                        op("dve", lambda h: h.scalar_tensor_tensor(out=Hf[P_, c, :], in0=B[P_, 256:320], scalar=gcol, in1=HG[P_, :], op0=ALU.mult, op1=ALU.add),
                           reads=[Bb, eGb, HGb], writes=[Hfb[hh], Bb])
                        op("act", lambda h: h.copy(out=OT[P_, tsl], in_=B[P_, 128:256]), reads=[Bb], writes=[OTb[hh], Bb])
                        yield
                        op("act", lambda h: h.copy(out=Hb[P_, c, :], in_=Hf[P_, c, :]), reads=[Hfb[hh]], writes=[Hbb[hh]])
                        yield

                    units = [(n, hh) for n in range(4) for hh in range(2)]
                    for r0 in range(0, 8, 4):
                        lockstep([stage1(n, hh, lane, SL[r0 + lane]) for lane, (n, hh) in enumerate(units[r0:r0 + 4])])
                    for n in range(4):
                        lockstep([stage2(n, hh, hh, SL[2 * n + hh]) for hh in range(2)])
                    op("act", lambda h: h.copy(out=ob16[:], in_=OT[:]), reads=OTb, writes=[ob16b])
                    op("pe", lambda h: h.matmul(PS[0][:], lhsT=bonesb[:], rhs=ob16[:], start=True, stop=True), reads=[cb_b, ob16b], writes=[PSB[0]])
                    op("dve", lambda h: h.tensor_scalar(out=mean[:], in0=PS[0][:], scalar1=1.0 / 64, scalar2=None, op0=ALU.mult), reads=[PSB[0]], writes=[meanb])
                    tt_("dve", OT[:], OT[:], mean[:], ALU.subtract, OTb + [meanb], OTb)
                    op("act", lambda h: h.activation(out=sqb_[:], in_=OT[:], func=AF.Square), reads=OTb, writes=[sqbb])
                    op("pe", lambda h: h.matmul(PS[0][:], lhsT=bonesb[:], rhs=sqb_[:], start=True, stop=True), reads=[cb_b, sqbb], writes=[PSB[0]])
                    op("act", lambda h: h.activation(out=var[:], in_=PS[0][:], func=AF.Sqrt, scale=1.0 / 64, bias=GN_EPS), reads=[PSB[0]], writes=[varb])
                    op("dve", lambda h: h.reciprocal(out=var[:], in_=var[:]), reads=[varb], writes=[varb])
                    tt_("dve", OT[:], OT[:], var[:], ALU.mult, OTb + [varb], OTb)
                    op("act", lambda h: h.activation(out=OT[:], in_=OT[:], func=AF.Identity, scale=cvc(l, "gn_w", c), bias=cvc(l, "gn_b", c)), reads=OTb + [cv_b], writes=OTb)
                    tt_("dve", OT[:], OT[:], bon[:], ALU.add, OTb + [bonb], OTb)
                    tt_("dve", mo[:], OT[:], gt[:], ALU.mult, OTb + [gtb], [mob])
                    dma("sp", mixT[cs_, ts], mo[:], reads=[mob], writes=[db("mixT", c)], store=True)

        def lockstep(gens):
            gens = list(gens)
            while gens:
                nxt = []
                for g in gens:
                    try:
                        next(g)
                        nxt.append(g)
                    except StopIteration:
                        pass
                gens = nxt

        for l in range(NL if 'nolayers' not in parts else 0):
            kb.new_epoch()
            if 'mix' in parts:
                with scope() as es:
                    hT = sb(es, "hT", [128, KC, S], BF16)
                    hTb = [Buf() for _ in range(NTB)]
                    with scope() as es2:
                        if "nonorm1" not in parts:
                            norm_to_hT(es2, l, "g_pre", hT, hTb)
                    with scope() as es2:
                        W = w_in[l]
                        R = mk_rings(es2)
                        tasks = []
                        for c in range(8):
                            tasks.append(([(W[:, c * 128:(c + 1) * 128], 128)], 128,
                                          shift_epi(R, "er%d" % (c % 2), 128, cvc(l, "mu_r", c), dvc(l, "om_r", c), [(rT[c * 128:(c + 1) * 128], db("rT", c))])))
                        for c in range(8):
                            tasks.append(([(W[:, 1088 + c * 128:1088 + (c + 1) * 128], 128)], 128,
                                          shift_epi(R, "ek%d" % (c % 2), 128, cvc(l, "mu_k", c), dvc(l, "om_k", c), [(kT[c * 128:(c + 1) * 128], db("kT", c))])))
                        for c in range(8):
                            dsts = [(vT[c * 128:(c + 1) * 128], db("vT", c))]
                            if l == 0:
                                dsts.append((vfT[c * 128:(c + 1) * 128], db("vfT", c)))
                            tasks.append(([(W[:, 2112 + c * 128:2112 + (c + 1) * 128], 128)], 128,
                                          shift_epi(R, "ev%d" % (c % 2), 128, cvc(l, "mu_v", c), dvc(l, "om_v", c), dsts)))
                        tasks.append(([(W[:, 1024:1088], 64), (W[:, 3136:3200], 64)], 128,
                                      shift_epi(R, "ewa", 128, cvc(l, "mu_wa"), dvc(l, "om_wa"), [(waT, db("waT"))], act=[(0, 64, AF.Tanh), (64, 128, None)])))
                        tasks.append(([(W[:, 3200:3328], 128)], 128,
                                      shift_epi(R, "eg1", 128, cvc(l, "mu_g1"), dvc(l, "om_g1"), [(g1T, db("g1T"))], act=[(0, 128, AF.Sigmoid)])))
                        if l == 0:
                            tasks.append(([(W[:, 3328:3360], 32)], 32,
                                          shift_epi(R, "eg2", 32, cvc(l, "mu_g2", 0, 1, 0, 32), dvc(l, "om_g2", 0, 1, 0, 32), [(g2T[0:32], db("g2T"))], act=[(0, 32, AF.Sigmoid)])))
                        else:
                            tasks.append(([(W[:, 3328:3360], 32), (w_mv[l - 1], 32)], 64,
                                          shift_epi(R, "eg2", 64, cvc(l, "mu_g2", 0, 1, 0, 64), dvc(l, "om_g2", 0, 1, 0, 64), [(g2T, db("g2T"))],
                                                    act=[(0, 32, AF.Sigmoid), (32, 64, None)])))
                        for c in range(8):
                            tasks.append(([(W[:, 3360 + c * 128:3360 + (c + 1) * 128], 128)], 128, plain_epi(R, "eq%d" % (c % 2), qT[c * 128:(c + 1) * 128], db("qT", c), 0)))
                        for c in range(8):
                            tasks.append(([(W[:, 4384 + c * 128:4384 + (c + 1) * 128], 128)], 128, plain_epi(R, "ekd%d" % (c % 2), kdT[c * 128:(c + 1) * 128], db("kdT", c), 1)))
                        if 'plainonly' in parts:
                            pe_ = plain_epi(R, 'x', qT[0:128], db('qT', 0), 0)
                            tasks = [(sg_, M_, pe_) for (sg_, M_, e_) in tasks if M_ == 128]
                        if 'ntasks8' in parts:
                            tasks = tasks[:8]
                        proj_fm(es2, hT, hTb, tasks, nper=2, each=True)
                    with scope() as es2:
                        st = Ring(es2, nc, "vst", 2, [128, KC, 128], F32)
                        wbr = Ring(es2, nc, "vwb", 2, [128, KC, 128], BF16)
                        orr = Ring(es2, nc, "vo", 2, [128, 128], BF16)
                        for c in range(8 if 'novd' not in parts else 0):
                            s_, sb_ = st.next()
                            dma("sp", s_[:], w_in[l][:, 5408 + c * 128:5408 + (c + 1) * 128].rearrange("(kc p) n -> p kc n", p=128), writes=[sb_])
                            w_, wb_ = wbr.next()
                            op("dve", lambda h, s_=s_, w_=w_: h.tensor_copy(out=w_[:], in_=s_[:]), reads=[sb_], writes=[wb_])
                            for tt in range(NT):
                                pi = tt % 4
                                for kc in range(KC):
                                    op("pe", lambda h, w_=w_, kc=kc, pi=pi, tt=tt: h.matmul(PS[pi][:, 0:128], lhsT=hT[:, kc, tt * 128:(tt + 1) * 128], rhs=w_[:, kc, :],
                                                                                           start=(kc == 0), stop=(kc == KC - 1)),
                                       reads=[wb_, hTb[tt // 4]], writes=[PSB[pi]], inc=(kc == KC - 1))
                                o_, ob_ = orr.next()
                                op("act" if tt % 2 else "dve", lambda h, o_=o_, pi=pi, tt=tt: (h.copy if tt % 2 else h.tensor_copy)(out=o_[:], in_=PS[pi][:, 0:128]),
                                   reads=[PSB[pi]], writes=[ob_])
                                dma("sp", vd[tt * 128:(tt + 1) * 128, c * 128:(c + 1) * 128], o_[:], reads=[ob_], writes=[db("vd", c)], store=True)

                with scope() as es:
                    if 'norwkv' not in parts:
                        rwkv_phase(es, l)
                with scope() as es:
                    if 'noattn' not in parts:
                        attn_phase(es, l)
                with scope() as es:
                    if 'noO' not in parts:
                        proj_res(es, l, mixT, "mixT", KC, w_out[l], "g_pm")
            with scope() as es:
                hT = sb(es, "hT2", [128, KC, S], BF16)
                hTb = [Buf() for _ in range(NTB)]
                with scope() as es2:
                    norm_to_hT(es2, l, "g_pf", hT, hTb)
                with scope() as es2:
                    tr = Ring(es2, nc, "ft", 2, [128, 512], F32)
                    gr = Ring(es2, nc, "fg", 2, [128, 512], F32)
                    orr = Ring(es2, nc, "fo", 2, [128, 512], BF16)
                    carr = [(sb(es2, "fc%d" % i, [128, 2]), Buf()) for i in range(2)]

                    def ffn_epi(c):
                        car, carb = carr[c % 2]

                        def epi(tb, pss):
                            pg, pu = pss
                            ts = slice(tb * 512, (tb + 1) * 512)
                            t_, tb_ = tr.next()
                            g_, gb_ = gr.next()
                            o_, ob_ = orr.next()
                            op("act", lambda h: h.activation(out=t_[:], in_=PS[pg][:], func=AF.Identity, scale=cvc(l, "cw2", c), bias=cvc(l, "cb", c)),
                               reads=[PSB[pg], cv_b], writes=[tb_])
                            op("dve", lambda h: h.scalar_tensor_tensor(out=t_[:, 1:512], in0=PS[pg][:, 0:511], scalar=cvc(l, "cw1", c), in1=t_[:, 1:512], op0=ALU.mult, op1=ALU.add),
                               reads=[PSB[pg], tb_, cv_b], writes=[tb_])
                            op("dve", lambda h: h.scalar_tensor_tensor(out=t_[:, 2:512], in0=PS[pg][:, 0:510], scalar=cvc(l, "cw0", c), in1=t_[:, 2:512], op0=ALU.mult, op1=ALU.add),
                               reads=[PSB[pg], tb_, cv_b], writes=[tb_])
                            if tb > 0:
                                op("dve", lambda h: h.scalar_tensor_tensor(out=t_[:, 0:2], in0=car[:, 0:2], scalar=cvc(l, "cw0", c), in1=t_[:, 0:2], op0=ALU.mult, op1=ALU.add),
                                   reads=[carb, tb_, cv_b], writes=[tb_])
                                op("dve", lambda h: h.scalar_tensor_tensor(out=t_[:, 0:1], in0=car[:, 1:2], scalar=cvc(l, "cw1", c), in1=t_[:, 0:1], op0=ALU.mult, op1=ALU.add),
                                   reads=[carb, tb_, cv_b], writes=[tb_])
                            op("act", lambda h: h.copy(out=car[:, 0:2], in_=PS[pg][:, 510:512]), reads=[PSB[pg]], writes=[carb])
                            op("act", lambda h: h.activation(out=g_[:], in_=t_[:], func=AF.Gelu_apprx_tanh), reads=[tb_], writes=[gb_])
                            op("dve", lambda h: h.tensor_tensor(out=o_[:], in0=PS[pu][:], in1=g_[:], op=ALU.mult), reads=[PSB[pu], gb_], writes=[ob_])
                            dma("sp", actT[c * 128:(c + 1) * 128, ts], o_[:], reads=[ob_], writes=[db("actT", c)], store=True)
                        return epi
                    tasks = []
                    for c in range(FC):
                        e = ffn_epi(c)
                        tasks.append(([(w_up[l][:, c * 128:(c + 1) * 128], 128)], 128, e))
                        tasks.append(([(w_up[l][:, FF + c * 128:FF + (c + 1) * 128], 128)], 128, e))
                    if 'noproj' not in parts:
                        proj_fm(es2, hT, hTb, tasks, nper=2)
            with scope() as es:
                if 'nores' not in parts:
                    proj_res(es, l, actT, "actT", FC, w_down[l], "g_ff")

        with scope() as es:
            xr = Ring(es, nc, "oxi", 2, [128, KC, 128], F32)
            xo = Ring(es, nc, "oxo", 2, [128, D], F32)
            for tt in range(NT):
                xi, xib = xr.next()
                dma("sp", xi[:], xT[:, tt * 128:(tt + 1) * 128].rearrange("(kc p) t -> p kc t", p=128), reads=[db("xT", kc) for kc in range(KC)], writes=[xib])
                o_, ob_ = xo.next()
                for kc in range(KC):
                    pi = kc % 4
                    op("pe", lambda h, kc=kc, pi=pi: h.transpose(out=PS[pi][:, 0:128], in_=xi[:, kc, :], identity=cn[:, 0:128]), reads=[xib, cn_b], writes=[PSB[pi]])
                    op("act" if kc % 2 else "dve", lambda h, kc=kc, pi=pi: (h.copy if kc % 2 else h.tensor_copy)(out=o_[:, kc * 128:(kc + 1) * 128], in_=PS[pi][:, 0:128]),
                       reads=[PSB[pi]], writes=[ob_])
                dma("sp", y_out[tt * 128:(tt + 1) * 128, :], o_[:], reads=[ob_], writes=[db("yout")], store=True)
        kb.finish()
    return nc


def pack_inputs(inp, S, NL):
    f = np.float32
    cv = np.zeros((128, NL, NCV), f)

    def put(l, name, vec):
        vec = np.asarray(vec, f).reshape(-1)
        n = (vec.size + 127) // 128
        pad = np.zeros(n * 128, f)
        pad[:vec.size] = vec
        cv[:, l, CV[name]:CV[name] + n] = pad.reshape(n, 128).T
    for l in range(NL):
        put(l, "g_pre", inp["pre_mix_norm"][l]); put(l, "g_pm", inp["post_mix_norm"][l])
        put(l, "g_pf", inp["pre_ffn_norm"][l]); put(l, "g_ff", inp["post_ffn_norm"][l])
        mu = np.asarray(inp["shift_mu"][l], f)
        put(l, "mu_r", mu[0:1024]); put(l, "mu_k", mu[1088:2112]); put(l, "mu_v", mu[2112:3136])
        put(l, "mu_wa", np.concatenate([mu[1024:1088], mu[3136:3200]]))
        put(l, "mu_g1", mu[3200:3328])
        g2 = np.zeros(128, f)
        g2[0:32] = mu[3328:3360]
        if l > 0:
            g2[32:64] = np.asarray(inp["shift_mu_mv"][l - 1], f)
        put(l, "mu_g2", g2)
        for nm in ("w0", "a0", "k_k", "k_a", "r_k", "gn_w", "gn_b"):
            put(l, nm, inp[nm][l])
        if l > 0:
            put(l, "v0", inp["v0"][l - 1])
        put(l, "subln", inp["subln_w"][l])
        cw = np.asarray(inp["conv_w"][l], f)
        put(l, "cw0", cw[0]); put(l, "cw1", cw[1]); put(l, "cw2", cw[2]); put(l, "cb", inp["conv_b"][l])
        for nm, k in (("lq1", "lam_q1"), ("lk1", "lam_k1"), ("lq2", "lam_q2"), ("lk2", "lam_k2")):
            cv[:, l, CV[nm]:CV[nm] + 64] = np.asarray(inp[k][l], f)[None, :]
    shared = {
        "cv": np.ascontiguousarray(cv.reshape(128, NL * NCV)),
        "cn": make_consts(),
        "w_in": np.ascontiguousarray(inp["w_in"][:NL], dtype=f),
        "w_mv": np.ascontiguousarray(inp["w_mv_down"][:max(NL - 1, 1)], dtype=f),
        "w2": np.ascontiguousarray(inp["w2"][:NL], dtype=f), "a2": np.ascontiguousarray(inp["a2"][:NL], dtype=f),
        "g2": np.ascontiguousarray(inp["g2"][:NL], dtype=f), "v2": np.ascontiguousarray(inp["v2"][:max(NL - 1, 1)], dtype=f),
        "w_out": np.ascontiguousarray(inp["w_out"][:NL], dtype=f), "w_up": np.ascontiguousarray(inp["w_up"][:NL], dtype=f),
        "w_down": np.ascontiguousarray(inp["w_down"][:NL], dtype=f),
    }
    return shared


def kernel(**inp):
    x = np.asarray(inp["x"], np.float32)
    B, S, _ = x.shape
    NL = 4
    nc = build(S, NL)
    shared = pack_inputs(inp, S, NL)
    pos = np.asarray(inp["positions"], np.int32)
    in_maps = []
    for b in range(B):
        m = dict(shared)
        m["x"] = np.ascontiguousarray(x[b])
        m["pos"] = np.ascontiguousarray(pos[b:b + 1])
        in_maps.append(m)
    res = run_bass_kernel_spmd(nc, in_maps, core_ids=list(range(B)))
    return np.stack([np.asarray(r["y"], np.float32) for r in res.results], 0)
```

```python
import math
from contextlib import ExitStack, contextmanager
import numpy as np
import concourse.bass as bass
import concourse.mybir as mybir
from concourse.bass_utils import run_bass_kernel_spmd

F32 = mybir.dt.float32
BF16 = mybir.dt.bfloat16
I32 = mybir.dt.int32
AF = mybir.ActivationFunctionType
ALU = mybir.AluOpType

D = 2048
KC = 16
FF = 5632
FC = 44
RW = 1024
RCOLS = 3360
INC = 6432
C0 = math.exp(-0.5)
NORM_EPS = 1e-6
GN_EPS = 64e-5
SUBLN_EPS = 1e-5

CV = {}
_o = 0
for _n, _w in (("g_pre", 16), ("g_pm", 16), ("g_pf", 16), ("g_ff", 16), ("mu_r", 8), ("mu_k", 8), ("mu_v", 8),
               ("mu_wa", 1), ("mu_g1", 1), ("mu_g2", 1), ("w0", 8), ("a0", 8), ("k_k", 8), ("k_a", 8), ("r_k", 8),
               ("gn_w", 8), ("gn_b", 8), ("v0", 8), ("subln", 1), ("cw0", 44), ("cw1", 44), ("cw2", 44), ("cb", 44),
               ("lq1", 64), ("lk1", 64), ("lq2", 64), ("lk2", 64)):
    CV[_n] = _o
    _o += _w
NCV = _o
DV = {}
_o = 0
for _n, _w in (("om_r", 8), ("om_k", 8), ("om_v", 8), ("om_wa", 1), ("om_g1", 1), ("om_g2", 1), ("omka", 8), ("lam", 1), ("nlam", 1), ("sub2", 1)):
    DV[_n] = _o
    _o += _w
NDV = _o

CN = {"ident": 0, "bones": 128, "ones": 256, "perm": 384, "m2": 512, "msl": 768, "invf": 896, "sign": 897, "dmask": 898, "scanm": 2946, "hm": 3458}
NCNP = 898
NCN = 3458 + 7 * 256


def make_consts():
    c = np.zeros((128, NCN), np.float32)
    p = np.arange(128)
    c[:, 0:128] = np.eye(128)
    c[:, 128:256] = (p[:, None] // 64 == p[None, :] // 64)
    c[:, 256:384] = 1.0
    part = p.copy()
    for b in (0, 64):
        for i in range(8):
            part[b + i] = b + i + 8
            part[b + 8 + i] = b + i
    perm = np.zeros((128, 128), np.float32)
    for m in range(128):
        if part[m] != m:
            perm[part[m], m] = 1.0
    c[:, 384:512] = perm
    c[:, 512:640] = (p[:, None] < p[None, :])
    c[:, 640:768] = (p[:, None] <= p[None, :])
    c[:, 768:896] = (p[None, :] < p[:, None])
    q = np.arange(512)
    for j in range(4):
        c[:, 898 + j * 512:898 + (j + 1) * 512] = ((j * 128 + p)[:, None] <= q[None, :])
    invf = np.zeros(128, np.float64)
    sign = np.zeros(128, np.float32)
    fr = 500000.0 ** (-np.arange(0, 16, 2, dtype=np.float32) / 16)
    for b in (0, 64):
        for i in range(8):
            invf[b + i] = fr[i]
            invf[b + 8 + i] = fr[i]
            sign[b + i] = -1.0
            sign[b + 8 + i] = 1.0
    c[:, 896] = invf.astype(np.float32)
    c[:, 897] = sign
    sm = np.ones(512, np.float32)
    sm[::128] = 0.0
    c[:, 2946:2946 + 512] = sm[None, :]
    for k in range(7):
        t = p[:, None]
        s_ = p[None, :]
        mk = ((t >> (k + 1)) == (s_ >> (k + 1))) & (((t >> k) & 1) == 1) & (((s_ >> k) & 1) == 0)
        c[:, 3458 + k * 256:3458 + k * 256 + 128] = mk
        c[:, 3458 + k * 256 + 128:3458 + (k + 1) * 256] = mk.T
    return c


class Buf:
    __slots__ = ("w", "r")

    def __init__(self):
        self.w = {}
        self.r = {}


class Eng:
    def __init__(self, name, h, sem):
        self.name, self.h, self.sem, self.cnt, self.seen = name, h, sem, 0, {}


class KB:
    def __init__(self, nc, es, n_epochs=1):
        self.nc = nc
        self.E = {}
        self.sems = {}
        self.keyeng = {}
        self.dead = set()
        self.pool_sems = {}
        hs = (("pe", nc.tensor), ("act", nc.scalar), ("dve", nc.vector), ("pool", nc.gpsimd), ("sp", nc.sync))
        for name, h in hs:
            self.pool_sems[name] = [es.enter_context(nc.semaphore("s_%s%d" % (name, i))) for i in range(n_epochs if name != "sp" else 1)]
            e = Eng(name, h, self.pool_sems[name][0])
            e.key = name + "#0"
            e.epoch = 0
            self.E[name] = e
            self.sems[e.key] = (e.sem, 1)
            self.keyeng[e.key] = e
        self.slots = {}
        for q, n in (("sp", 10),):
            sl = []
            for i in range(n):
                key = "d%s%d" % (q, i)
                sem = es.enter_context(nc.semaphore("s_" + key))
                self.sems[key] = (sem, 16)
                sl.append([key, 0])
            self.slots[q] = [sl, 0]

    def new_epoch(self):
        self.barrier()
        for name in ("pe", "act", "dve", "pool"):
            e = self.E[name]
            if e.epoch + 1 >= len(self.pool_sems[name]):
                continue
            self.dead.add(e.key)
            e.epoch += 1
            e.sem = self.pool_sems[name][e.epoch]
            e.cnt = 0
            e.key = "%s#%d" % (name, e.epoch)
            self.sems[e.key] = (e.sem, 1)
            self.keyeng[e.key] = e

    def _need(self, e, key, n, raw):
        if key in self.dead:
            return
        if key == e.key and (not raw) and e.name == "pe":
            return
        if e.seen.get(key, 0) >= n:
            return
        sem, unit = self.sems[key]
        if key in self.keyeng and key != e.key:
            assert n <= self.keyeng[key].cnt, ("pending ticket", key, n)
        e.h.wait_ge(sem, n * unit)
        e.seen[key] = n

    def _deps(self, e, reads, writes):
        for b in reads:
            for k, n in b.w.items():
                self._need(e, k, n, True)
        for b in writes:
            for k, n in b.w.items():
                self._need(e, k, n, False)
            for k, n in b.r.items():
                self._need(e, k, n, False)

    def op(self, eng, fn, reads=(), writes=(), inc=True):
        e = self.E[eng]
        self._deps(e, reads, writes)
        ins = fn(e.h)
        if inc:
            ins.then_inc(e.sem, 1)
            e.cnt += 1
            t = e.cnt
        else:
            t = e.cnt + 1
        for b in writes:
            b.w = {e.key: t}
            b.r = {}
        for b in reads:
            b.r[e.key] = t
        return ins

    def dma(self, q, out, in_, reads=(), writes=(), store=False):
        e = self.E[q]
        if store:
            for b in reads:
                for k, n in b.w.items():
                    self._need(e, k, n, True)
            for b in writes:
                for k, n in b.r.items():
                    self._need(e, k, n, False)
        else:
            self._deps(e, reads, writes)
        sl, idx = self.slots[q]
        key, cnt = sl[idx]
        self.slots[q][1] = (idx + 1) % len(sl)
        if cnt > 0:
            self._need(e, key, cnt, True)
        sem, unit = self.sems[key]
        e.h.dma_start(out=out, in_=in_).then_inc(sem, 16)
        sl[idx][1] = cnt + 1
        for b in writes:
            if store:
                b.w = {k: v for k, v in b.w.items() if k.startswith("d")}
                b.w[key] = cnt + 1
            else:
                b.w = {key: cnt + 1}
            b.r = {}
        for b in reads:
            b.r[key] = cnt + 1

    def barrier(self):
        for en in ("pe", "act", "dve", "pool", "sp"):
            e = self.E[en]
            for k2 in ("pe", "act", "dve", "pool"):
                o = self.E[k2]
                if k2 != en and o.cnt > 0:
                    self._need(e, o.key, o.cnt, True)
            for q in self.slots:
                for key, cnt in self.slots[q][0]:
                    if cnt > 0:
                        self._need(e, key, cnt, True)

    def finish(self):
        e = self.E["sp"]
        for q in self.slots:
            for key, cnt in self.slots[q][0]:
                if cnt > 0:
                    self._need(e, key, cnt, True)


_UID = [0]


class Ring:
    def __init__(self, es, nc, name, n, shape, dt, psum=False):
        self.t = []
        for i in range(n):
            _UID[0] += 1
            t = es.enter_context((nc.psum_tensor if psum else nc.sbuf_tensor)("rg_%s_%d" % (name, _UID[0]), shape, dt))
            self.t.append((t, Buf()))
        self.i = 0

    def next(self):
        r = self.t[self.i]
        self.i = (self.i + 1) % len(self.t)
        return r


def build(S, NL, dbg=False, parts=('mix', 'ffn')):
    nc = bass.Bass("TRN2", target_bir_lowering=False)
    NTB = S // 512
    NT = S // 128

    def din(name, shape, dt=F32):
        return nc.dram_tensor(name, shape, dt, kind="ExternalInput").ap()

    x_in = din("x", [S, D])
    pos_in = din("pos", [1, S], I32)
    cv_in = din("cv", [128, NL * NCV])
    cn_in = din("cn", [128, NCN])
    w_in = din("w_in", [NL, D, INC])
    w_mv = din("w_mv", [max(NL - 1, 1), D, 32])
    w2_in = din("w2", [NL, 64, RW])
    a2_in = din("a2", [NL, 64, RW])
    g2_in = din("g2", [NL, 160, RW])
    v2_in = din("v2", [max(NL - 1, 1), 32, RW])
    w_out = din("w_out", [NL, D, D])
    w_up = din("w_up", [NL, D, 2 * FF])
    w_down = din("w_down", [NL, FF, D])
    y_out = nc.dram_tensor("y", [S, D], F32, kind="ExternalOutput").ap()

    kindS = "ExternalOutput" if dbg else "Internal"

    def dsc(name, shape, dt):
        return nc.dram_tensor(name, shape, dt, kind=kindS).ap()

    xT = dsc("xT", [D, S], F32)
    yT = dsc("yT", [D, S], F32)
    rT = dsc("rT", [RW, S], BF16)
    kT = dsc("kT", [RW, S], BF16)
    vT = dsc("vT", [RW, S], BF16)
    vfT = dsc("vfT", [RW, S], BF16)
    waT = dsc("waT", [128, S], BF16)
    g1T = dsc("g1T", [128, S], BF16)
    g2T = dsc("g2T", [64, S], BF16)
    qT = dsc("qT", [RW, S], BF16)
    kdT = dsc("kdT", [RW, S], BF16)
    vd = dsc("vd", [S, RW], BF16)
    mixT = dsc("mixT", [D, S], BF16)
    actT = dsc("actT", [FF, S], BF16)
    rotC = dsc("rotC", [128, S], F32)
    rotS = dsc("rotS", [128, S], F32)
    DB = {}

    def db(name, i=0):
        k = (name, i)
        if k not in DB:
            DB[k] = Buf()
        return DB[k]

    with ExitStack() as es0:
        kb = KB(nc, es0, n_epochs=NL + 1)
        op, dma = kb.op, kb.dma

        @contextmanager
        def scope():
            with ExitStack() as e_:
                yield e_
                kb.barrier()

        def sb(es, name, shape, dt=F32):
            _UID[0] += 1
            return es.enter_context(nc.sbuf_tensor("sb_%s_%d" % (name, _UID[0]), shape, dt))

        cn = sb(es0, "cn", [128, NCNP]); cn_b = Buf()
        cv = sb(es0, "cv", [128, NL * NCV]); cv_b = Buf()
        dv = sb(es0, "dv", [128, NL * NDV]); dv_b = Buf()
        identb = sb(es0, "identb", [128, 128], BF16)
        bonesb = sb(es0, "bonesb", [128, 128], BF16)
        onesb = sb(es0, "onesb", [128, 128], BF16)
        permb = sb(es0, "permb", [128, 128], BF16)
        cb_b = Buf()
        PS = [es0.enter_context(nc.psum_tensor("ps%d" % i, [128, 512], F32)) for i in range(7)]
        PSB = [Buf() for _ in range(7)]
        PSH = es0.enter_context(nc.psum_tensor("psh", [128, 1024], BF16)); PSH_b = [Buf()] * 4

        dma("sp", cn[:], cn_in[:, 0:NCNP], writes=[cn_b])
        dma("sp", cv[:], cv_in[:, :], writes=[cv_b])
        for t, o in ((identb, CN["ident"]), (bonesb, CN["bones"]), (onesb, CN["ones"]), (permb, CN["perm"])):
            op("dve", lambda h, t=t, o=o: h.tensor_copy(out=t[:], in_=cn[:, o:o + 128]), reads=[cn_b], writes=[cb_b])

        def cvc(l, name, i=0, n=1, p0=0, p1=128):
            o = l * NCV + CV[name] + i
            return cv[p0:p1, o:o + n]

        def dvc(l, name, i=0, n=1, p0=0, p1=128):
            o = l * NDV + DV[name] + i
            return dv[p0:p1, o:o + n]

        for l in range(NL):
            for a, b_, n in (("om_r", "mu_r", 8), ("om_k", "mu_k", 8), ("om_v", "mu_v", 8), ("om_wa", "mu_wa", 1),
                             ("om_g1", "mu_g1", 1), ("om_g2", "mu_g2", 1), ("omka", "k_a", 8)):
                op("dve", lambda h, l=l, a=a, b_=b_, n=n: h.tensor_scalar(out=dvc(l, a, 0, n), in0=cvc(l, b_, 0, n), scalar1=-1.0, scalar2=1.0,
                                                                        op0=ALU.mult, op1=ALU.add), reads=[cv_b], writes=[dv_b])
        with scope() as es:
            tmp = sb(es, "lamtmp", [128, 64]); tb_ = Buf()
            acc = sb(es, "lamacc", [128, 4]); ab_ = Buf()
            for l in range(NL):
                li = 0.8 - 0.6 * math.exp(-0.3 * l)
                for j, (qa, ka) in enumerate((("lq1", "lk1"), ("lq2", "lk2"))):
                    op("dve", lambda h, l=l, qa=qa, ka=ka: h.tensor_tensor(out=tmp[:], in0=cvc(l, qa, 0, 64), in1=cvc(l, ka, 0, 64), op=ALU.mult),
                       reads=[cv_b], writes=[tb_])
                    op("dve", lambda h, j=j: h.reduce_sum(out=acc[:, j:j + 1], in_=tmp[:], axis=mybir.AxisListType.X), reads=[tb_], writes=[ab_])
                op("act", lambda h: h.activation(out=acc[:, 2:4], in_=acc[:, 0:2], func=AF.Exp), reads=[ab_], writes=[ab_])
                op("dve", lambda h, l=l: h.tensor_tensor(out=dvc(l, "lam"), in0=acc[:, 2:3], in1=acc[:, 3:4], op=ALU.subtract), reads=[ab_, dv_b], writes=[dv_b])
                op("dve", lambda h, l=l, li=li: h.tensor_scalar(out=dvc(l, "nlam"), in0=dvc(l, "lam"), scalar1=float(li), scalar2=-1.0, op0=ALU.add, op1=ALU.mult),
                   reads=[dv_b], writes=[dv_b])
                op("dve", lambda h, l=l, li=li: h.tensor_scalar(out=dvc(l, "sub2"), in0=cvc(l, "subln"), scalar1=float(1.0 - li), scalar2=None, op0=ALU.mult),
                   reads=[cv_b, dv_b], writes=[dv_b])

        with scope() as es:
          if 'norot' not in parts:
              pi_ = sb(es, "posi", [128, 512], I32); pib = Buf()
              pf = sb(es, "posf", [128, 512]); pfb = Buf()
              t1 = sb(es, "rt1", [128, 512]); t1b = Buf()
              t2 = sb(es, "rt2", [128, 512]); t2b = Buf()
              ki = sb(es, "rki", [128, 512], I32); kib = Buf()
              ro = Ring(es, nc, "rto", 2, [128, 512], F32)
              TWO_PI = float(2 * np.pi)
              for tb in range(NTB):
                  ts = slice(tb * 512, (tb + 1) * 512)
                  dma("sp", pi_[:], pos_in[0:1, ts].partition_broadcast(128), writes=[pib])
                  op("dve", lambda h: h.tensor_copy(out=pf[:], in_=pi_[:]), reads=[pib], writes=[pfb])
                  op("dve", lambda h: h.tensor_scalar(out=pf[:], in0=pf[:], scalar1=cn[:, CN["invf"]:CN["invf"] + 1], scalar2=None, op0=ALU.mult),
                     reads=[pfb, cn_b], writes=[pfb])
                  for which, off, dst in (("c", float(np.pi / 2), rotC), ("s", 0.0, rotS)):
                      op("dve", lambda h, off=off: h.tensor_scalar(out=t1[:], in0=pf[:], scalar1=off, scalar2=None, op0=ALU.add), reads=[pfb], writes=[t1b])
                      op("dve", lambda h: h.tensor_scalar(out=t2[:], in0=t1[:], scalar1=float(1 / (2 * np.pi)), scalar2=None, op0=ALU.mult), reads=[t1b], writes=[t2b])
                      op("dve", lambda h: h.tensor_copy(out=ki[:], in_=t2[:]), reads=[t2b], writes=[kib])
                      op("dve", lambda h: h.tensor_copy(out=t2[:], in_=ki[:]), reads=[kib], writes=[t2b])
                      op("dve", lambda h: h.scalar_tensor_tensor(out=t1[:], in0=t2[:], scalar=-TWO_PI, in1=t1[:], op0=ALU.mult, op1=ALU.add), reads=[t2b, t1b], writes=[t1b])
                      op("dve", lambda h: h.tensor_scalar(out=t2[:], in0=t1[:], scalar1=float(np.pi), scalar2=-TWO_PI, op0=ALU.is_gt, op1=ALU.mult), reads=[t1b], writes=[t2b])
                      op("dve", lambda h: h.tensor_tensor(out=t1[:], in0=t1[:], in1=t2[:], op=ALU.add), reads=[t1b, t2b], writes=[t1b])
                      op("dve", lambda h: h.tensor_scalar(out=t2[:], in0=t1[:], scalar1=float(-np.pi), scalar2=TWO_PI, op0=ALU.is_lt, op1=ALU.mult), reads=[t1b], writes=[t2b])
                      op("dve", lambda h: h.tensor_tensor(out=t1[:], in0=t1[:], in1=t2[:], op=ALU.add), reads=[t1b, t2b], writes=[t1b])
                      o_, ob_ = ro.next()
                      op("act", lambda h, o_=o_: h.activation(out=o_[:], in_=t1[:], func=AF.Sin), reads=[t1b], writes=[ob_])
                      if which == "s":
                          op("dve", lambda h, o_=o_: h.tensor_scalar(out=o_[:], in0=o_[:], scalar1=cn[:, CN["sign"]:CN["sign"] + 1], scalar2=None, op0=ALU.mult),
                             reads=[ob_, cn_b], writes=[ob_])
                      dma("sp", dst[:, ts], o_[:], reads=[ob_], writes=[db("rot")], store=True)

        with scope() as es:
            xr = Ring(es, nc, "xin", 2, [128, D], F32)
            xo = Ring(es, nc, "xto", 2, [128, KC, 128], F32)
            for tt in range(NT):
                xi, xib = xr.next()
                dma("sp", xi[:], x_in[tt * 128:(tt + 1) * 128, :], writes=[xib])
                o_, ob_ = xo.next()
                for kc in range(KC):
                    pi = kc % 4
                    op("pe", lambda h, kc=kc, pi=pi: h.transpose(out=PS[pi][:, 0:128], in_=xi[:, kc * 128:(kc + 1) * 128], identity=cn[:, 0:128]),
                       reads=[xib, cn_b], writes=[PSB[pi]])
                    op("act" if kc % 2 else "dve", lambda h, kc=kc, pi=pi: (h.copy if kc % 2 else h.tensor_copy)(out=o_[:, kc, :], in_=PS[pi][:, 0:128]),
                       reads=[PSB[pi]], writes=[ob_])
                dma("sp", xT[:, tt * 128:(tt + 1) * 128].rearrange("(kc p) t -> p kc t", p=128), o_[:], reads=[ob_], writes=[db("xT", kc) for kc in range(KC)], store=True)

        def norm_to_hT(es, l, gname, hT, hTb):
            xs = Ring(es, nc, "nxs", 16, [128, 512], F32)
            sq = Ring(es, nc, "nsq", 2, [128, 512], BF16)
            rs = sb(es, "nrs", [128, 512]); rsb = Buf()
            for tb in range(NTB):
                ts = slice(tb * 512, (tb + 1) * 512)
                tl = []
                for kc in range(KC):
                    x_, xb_ = xs.next()
                    dma("sp", x_[:], xT[kc * 128:(kc + 1) * 128, ts], reads=[db("xT", kc)], writes=[xb_])
                    s_, sb_ = sq.next()
                    op("act", lambda h, x_=x_, s_=s_: h.activation(out=s_[:], in_=x_[:], func=AF.Square), reads=[xb_], writes=[sb_])
                    op("pe", lambda h, s_=s_, kc=kc: h.matmul(PS[6][:], lhsT=onesb[:], rhs=s_[:], start=(kc == 0), stop=(kc == KC - 1)),
                       reads=[sb_, cb_b], writes=[PSB[6]])
                    tl.append((x_, xb_))
                op("dve", lambda h: h.tensor_scalar(out=rs[:], in0=PS[6][:], scalar1=1.0 / D, scalar2=NORM_EPS, op0=ALU.mult, op1=ALU.add), reads=[PSB[6]], writes=[rsb])
                op("act", lambda h: h.activation(out=rs[:], in_=rs[:], func=AF.Sqrt), reads=[rsb], writes=[rsb])
                op("dve", lambda h: h.reciprocal(out=rs[:], in_=rs[:]), reads=[rsb], writes=[rsb])
                for kc in range(KC):
                    x_, xb_ = tl[kc]
                    op("dve", lambda h, x_=x_, kc=kc: h.scalar_tensor_tensor(out=hT[:, kc, ts], in0=x_[:], scalar=cvc(l, gname, kc), in1=rs[:],
                                                                                                   op0=ALU.mult, op1=ALU.mult),
                       reads=[xb_, rsb, cv_b], writes=[hTb[tb]])

        def proj_fm(es, hT, hTb, tasks, nper=1, each=False):
            st = Ring(es, nc, "wst", 2 if nper == 1 else 3, [128, KC, 128], F32)
            wb = Ring(es, nc, "wbf", 3 if nper == 1 else 4, [128, KC, 128], BF16)
            psr = [0]
            cvi = [0]
            groups = [tasks[ti:ti + nper] for ti in range(0, len(tasks), nper)]

            def load(grp):
                wts = []
                for segs, M, epi in grp:
                    s_, sb_ = st.next()
                    o = 0
                    for ap, n in segs:
                        dma("sp", s_[:, :, o:o + n], ap.rearrange("(kc p) n -> p kc n", p=128), writes=[sb_])
                        o += n
                    w_, wb_ = wb.next()
                    cvi[0] += 1
                    if cvi[0] % 2:
                        op("act", lambda h, s_=s_, w_=w_, M=M: h.copy(out=w_[:, :, 0:M], in_=s_[:, :, 0:M]), reads=[sb_], writes=[wb_])
                    else:
                        op("dve", lambda h, s_=s_, w_=w_, M=M: h.tensor_copy(out=w_[:, :, 0:M], in_=s_[:, :, 0:M]), reads=[sb_], writes=[wb_])
                    wts.append((w_, wb_, M))
                return wts
            nxt = load(groups[0])
            for gi, grp in enumerate(groups):
                wts = nxt
                if gi + 1 < len(groups):
                    nxt = load(groups[gi + 1])
                for tb in range(NTB):
                    ts = slice(tb * 512, (tb + 1) * 512)
                    pss = []
                    for w_, wb_, M in wts:
                        pi = psr[0] % 4
                        psr[0] += 1
                        for kc in range(KC):
                            op("pe", lambda h, w_=w_, M=M, kc=kc, pi=pi: h.matmul(PS[pi][0:M, :], lhsT=w_[:, kc, 0:M], rhs=hT[:, kc, ts], start=(kc == 0), stop=(kc == KC - 1)),
                               reads=[wb_, hTb[tb]], writes=[PSB[pi]], inc=(kc == KC - 1))
                        pss.append(pi)
                    if each:
                        for t_, pi_ in zip(grp, pss):
                            t_[2](tb, [pi_])
                    else:
                        grp[0][2](tb, pss)

        def mk_rings(es):
            return {"t": Ring(es, nc, "ept", 2, [128, 512], F32), "of": Ring(es, nc, "epof", 2, [128, 512], F32),
                    "ob": Ring(es, nc, "epob", 3, [128, 512], BF16), "car": sb(es, "epcar", [128, 64]), "ncar": [0]}

        def shift_epi(R, name, M, mu_ap, om_ap, dests, act=None):
            tr = R["t"]
            orr = R["of"] if act else R["ob"]
            obr = R["ob"] if act else None
            ci = R["ncar"][0]
            R["ncar"][0] += 1
            carry = R["car"][:, ci:ci + 1]; cb = Buf()

            def epi(tb, pss):
                pi = pss[0]
                ts = slice(tb * 512, (tb + 1) * 512)
                t_, tb_ = tr.next()
                o_, ob_ = orr.next()
                op("act", lambda h: h.activation(out=t_[0:M, :], in_=PS[pi][0:M, :], func=AF.Identity, scale=om_ap), reads=[PSB[pi], dv_b], writes=[tb_])
                op("dve", lambda h: h.scalar_tensor_tensor(out=o_[0:M, 1:512], in0=PS[pi][0:M, 0:511], scalar=mu_ap, in1=t_[0:M, 1:512], op0=ALU.mult, op1=ALU.add),
                   reads=[PSB[pi], tb_, cv_b], writes=[ob_])
                if tb == 0:
                    op("dve", lambda h: h.tensor_copy(out=o_[0:M, 0:1], in_=t_[0:M, 0:1]), reads=[tb_], writes=[ob_])
                else:
                    op("dve", lambda h: h.scalar_tensor_tensor(out=o_[0:M, 0:1], in0=carry[0:M, :], scalar=mu_ap, in1=t_[0:M, 0:1], op0=ALU.mult, op1=ALU.add),
                       reads=[cb, tb_, cv_b], writes=[ob_])
                op("act", lambda h: h.copy(out=carry[0:M, :], in_=PS[pi][0:M, 511:512]), reads=[PSB[pi]], writes=[cb])
                if act:
                    f_, fb_ = obr.next()
                    for p0, p1, fn in act:
                        if fn is None:
                            op("dve", lambda h, p0=p0, p1=p1: h.tensor_copy(out=f_[p0:p1, :], in_=o_[p0:p1, :]), reads=[ob_], writes=[fb_])
                        else:
                            op("act", lambda h, p0=p0, p1=p1, fn=fn: h.activation(out=f_[p0:p1, :], in_=o_[p0:p1, :], func=fn), reads=[ob_], writes=[fb_])
                    o_, ob_ = f_, fb_
                for dap, dbuf in dests:
                    dma("sp", dap[:, ts], o_[0:M, :], reads=[ob_], writes=[dbuf], store=True)
            return epi

        def plain_epi(R, name, dest, dbuf, flip):
            orr = R["ob"]

            def epi(tb, pss):
                pi = pss[0]
                o_, ob_ = orr.next()
                if (tb + flip) % 2:
                    op("act", lambda h: h.copy(out=o_[:], in_=PS[pi][:]), reads=[PSB[pi]], writes=[ob_])
                else:
                    op("dve", lambda h: h.tensor_copy(out=o_[:], in_=PS[pi][:]), reads=[PSB[pi]], writes=[ob_])
                dma("sp", dest[:, tb * 512:(tb + 1) * 512], o_[:], reads=[ob_], writes=[dbuf], store=True)
            return epi

        def proj_res(es, l, src, srcname, KCn, W, gname):
            TBD = min(1024, S)
            NH = TBD // 512
            srct = sb(es, "prs", [128, KCn, TBD], BF16); srcb = Buf()
            KH = KCn // 4
            st = Ring(es, nc, "prst", 4, [128, KH, 128], F32)
            wbr = Ring(es, nc, "prwb", 3, [128, KCn, 128], BF16)
            yr = Ring(es, nc, "pry", 3, [128, TBD], F32)
            sqr = Ring(es, nc, "prsq", 2, [128, 512], BF16)
            rst = sb(es, "prrs", [128, TBD]); rsb = Buf()
            xr = Ring(es, nc, "prx", 3, [128, TBD], F32)
            for tbd in range(S // TBD):
                ts = slice(tbd * TBD, (tbd + 1) * TBD)
                for kc in range(KCn):
                    dma("sp", srct[:, kc, :], src[kc * 128:(kc + 1) * 128, ts], reads=[db(srcname, kc)], writes=[srcb])
                def loadw(c):
                    w_, wb_ = wbr.next()
                    for hf in range(4):
                        s_, sb_ = st.next()
                        dma("sp", s_[:], W[hf * KH * 128:(hf + 1) * KH * 128, c * 128:(c + 1) * 128].rearrange("(kc p) n -> p kc n", p=128), writes=[sb_])
                        if hf % 2:
                            op("act", lambda h, s_=s_, w_=w_, hf=hf: h.copy(out=w_[:, hf * KH:(hf + 1) * KH, :], in_=s_[:]), reads=[sb_], writes=[wb_])
                        else:
                            op("dve", lambda h, s_=s_, w_=w_, hf=hf: h.tensor_copy(out=w_[:, hf * KH:(hf + 1) * KH, :], in_=s_[:]), reads=[sb_], writes=[wb_])
                    return w_, wb_
                nxtw = loadw(0)
                for c in range(KC):
                    w_, wb_ = nxtw
                    if c + 1 < KC:
                        nxtw = loadw(c + 1)
                    y_, yb_ = yr.next()
                    for hh in range(NH):
                        pi = (c * NH + hh) % 4
                        for kc in range(KCn):
                            op("pe", lambda h, w_=w_, kc=kc, pi=pi, hh=hh: h.matmul(PS[pi][:], lhsT=w_[:, kc, :], rhs=srct[:, kc, hh * 512:(hh + 1) * 512],
                                                                                   start=(kc == 0), stop=(kc == KCn - 1)),
                               reads=[wb_, srcb], writes=[PSB[pi]], inc=(kc == KCn - 1))
                        op("dve", lambda h, y_=y_, pi=pi, hh=hh: h.tensor_copy(out=y_[:, hh * 512:(hh + 1) * 512], in_=PS[pi][:]), reads=[PSB[pi]], writes=[yb_])
                        q_, qb_ = sqr.next()
                        op("act", lambda h, q_=q_, y_=y_, hh=hh: h.activation(out=q_[:], in_=y_[:, hh * 512:(hh + 1) * 512], func=AF.Square), reads=[yb_], writes=[qb_])
                        op("pe", lambda h, q_=q_, hh=hh, c=c: h.matmul(PS[4 + hh][:], lhsT=onesb[:], rhs=q_[:], start=(c == 0), stop=(c == KC - 1)),
                           reads=[qb_, cb_b], writes=[PSB[4 + hh]])
                    dma("sp", yT[c * 128:(c + 1) * 128, ts], y_[:], reads=[yb_], writes=[db("yT", c)], store=True)
                for hh in range(NH):
                    hs = slice(hh * 512, (hh + 1) * 512)
                    op("dve", lambda h, hh=hh, hs=hs: h.tensor_scalar(out=rst[:, hs], in0=PS[4 + hh][:], scalar1=1.0 / D, scalar2=NORM_EPS, op0=ALU.mult, op1=ALU.add),
                       reads=[PSB[4 + hh]], writes=[rsb])
                op("act", lambda h: h.activation(out=rst[:], in_=rst[:], func=AF.Sqrt), reads=[rsb], writes=[rsb])
                op("dve", lambda h: h.reciprocal(out=rst[:], in_=rst[:]), reads=[rsb], writes=[rsb])
                def loadxy(c):
                    y_, yb_ = yr.next()
                    x_, xb_ = xr.next()
                    dma("sp", y_[:], yT[c * 128:(c + 1) * 128, ts], reads=[db("yT", c)], writes=[yb_])
                    dma("sp", x_[:], xT[c * 128:(c + 1) * 128, ts], reads=[db("xT", c)], writes=[xb_])
                    return y_, yb_, x_, xb_
                nxy = loadxy(0)
                for c in range(KC):
                    y_, yb_, x_, xb_ = nxy
                    if c + 1 < KC:
                        nxy = loadxy(c + 1)
                    op("dve", lambda h, y_=y_, c=c: h.scalar_tensor_tensor(out=y_[:], in0=y_[:], scalar=cvc(l, gname, c), in1=rst[:], op0=ALU.mult, op1=ALU.mult),
                       reads=[yb_, rsb, cv_b], writes=[yb_])
                    op("dve", lambda h, y_=y_, x_=x_: h.tensor_tensor(out=x_[:], in0=x_[:], in1=y_[:], op=ALU.add), reads=[yb_, xb_], writes=[xb_])
                    dma("sp", xT[c * 128:(c + 1) * 128, ts], x_[:], reads=[xb_], writes=[db("xT", c)], store=True)


        def attn_phase(es, l):
            C = sb(es, "arC", [128, S]); Cb = Buf()
            Sg = sb(es, "arS", [128, S]); Sgb = Buf()
            dma("sp", C[:], rotC[:, :], reads=[db("rot")], writes=[Cb])
            dma("sp", Sg[:], rotS[:, :], reads=[db("rot")], writes=[Sgb])
            dmf = sb(es, "admf", [128, 2048]); dmfb = Buf()
            dm = sb(es, "adm", [128, 4, 512], BF16); dmb = Buf()
            dma("sp", dmf[:], cn_in[:, CN["dmask"]:CN["dmask"] + 2048], writes=[dmfb])
            op("pool", lambda h: h.tensor_copy(out=dm[:], in_=dmf[:].rearrange("p (a b) -> p a b", a=4)), reads=[dmfb], writes=[dmb])
            raws = Ring(es, nc, "araw", 2, [128, 512], BF16)
            t1r = Ring(es, nc, "at1", 2, [128, 512], F32)
            t2r = Ring(es, nc, "at2", 2, [128, 512], F32)
            qk = Ring(es, nc, "aqk", 4, [128, S], BF16)
            Vr = Ring(es, nc, "aV", 2, [128, NT, 128], BF16)
            Er = Ring(es, nc, "aE", 4, [128, 512], BF16)
            wk = Ring(es, nc, "awk", 6, [128, 512], F32)
            sqr = Ring(es, nc, "asq", 2, [128, 512], BF16)
            outr = Ring(es, nc, "aout", 2, [128, 512], BF16)
            for hd in range(8):
                rot = []
                for src, nm in ((qT, "qT"), (kdT, "kdT")):
                    d_, db_ = qk.next()
                    for tb in range(NTB):
                        ts = slice(tb * 512, (tb + 1) * 512)
                        r_, rb_ = raws.next()
                        dma("sp", r_[:], src[hd * 128:(hd + 1) * 128, ts], reads=[db(nm, hd)], writes=[rb_])
                        op("pe", lambda h, r_=r_: h.matmul(PS[0][:], lhsT=permb[:], rhs=r_[:], start=True, stop=True), reads=[rb_, cb_b], writes=[PSB[0]])
                        a_, ab_ = t1r.next()
                        b_, bb_ = t2r.next()
                        op("dve", lambda h, r_=r_, a_=a_, ts=ts: h.tensor_tensor(out=a_[:], in0=r_[:], in1=C[:, ts], op=ALU.mult), reads=[rb_, Cb], writes=[ab_])
                        op("dve", lambda h, b_=b_, ts=ts: h.tensor_tensor(out=b_[:], in0=PS[0][:], in1=Sg[:, ts], op=ALU.mult), reads=[PSB[0], Sgb], writes=[bb_])
                        op("dve", lambda h, a_=a_, b_=b_, d_=d_, ts=ts: h.tensor_tensor(out=d_[:, ts], in0=a_[:], in1=b_[:], op=ALU.add), reads=[ab_, bb_], writes=[db_])
                    rot.append((d_, db_))
                (q_, qb_), (k_, kb_) = rot
                V_, Vb_ = Vr.next()
                dma("sp", V_[:], vd[:, hd * 128:(hd + 1) * 128].rearrange("(tt p) c -> p tt c", p=128), reads=[db("vd", hd)], writes=[Vb_])
                for qb in range(NTB):
                    qs = slice(qb * 512, (qb + 1) * 512)
                    nk = 4 * qb + 4
                    steps = [(kt, m) for kt in range(nk) for m in range(2)]

                    def emit_s(i):
                        kt, m = steps[i]
                        pi = i % 3
                        op("pe", lambda h: h.matmul(PS[pi][:], lhsT=k_[m * 64:(m + 1) * 64, kt * 128:(kt + 1) * 128], rhs=q_[m * 64:(m + 1) * 64, qs],
                                                    start=True, stop=True), reads=[kb_, qb_], writes=[PSB[pi]])
                    emit_s(0)
                    emit_s(1)
                    for i, (kt, m) in enumerate(steps):
                        pi = i % 3
                        if i + 2 < len(steps):
                            emit_s(i + 2)
                        e_, eb_ = Er.next()
                        op("act", lambda h, e_=e_, pi=pi: h.activation(out=e_[:], in_=PS[pi][:], func=AF.Exp, scale=0.125), reads=[PSB[pi]], writes=[eb_])
                        if kt >= 4 * qb:
                            op("dve", lambda h, e_=e_, j=kt - 4 * qb: h.tensor_tensor(out=e_[:], in0=e_[:], in1=dm[:, j, :], op=ALU.mult), reads=[eb_, dmb], writes=[eb_])
                        op("pe", lambda h, e_=e_, kt=kt, m=m: h.matmul(PS[3 + m][:], lhsT=V_[:, kt, :], rhs=e_[:], start=(kt == 0), stop=(kt == nk - 1)),
                           reads=[Vb_, eb_], writes=[PSB[3 + m]], inc=False)
                        op("pe", lambda h, e_=e_, kt=kt, m=m: h.matmul(PS[5 + m][:], lhsT=onesb[:], rhs=e_[:], start=(kt == 0), stop=(kt == nk - 1)),
                           reads=[cb_b, eb_], writes=[PSB[5 + m]])
                    w = [wk.next() for _ in range(6)]
                    for m in range(2):
                        op("dve", lambda h, m=m: h.reciprocal(out=w[m][0][:], in_=PS[5 + m][:]), reads=[PSB[5 + m]], writes=[w[m][1]])
                        op("dve", lambda h, m=m: h.tensor_tensor(out=w[2 + m][0][:], in0=PS[3 + m][:], in1=w[m][0][:], op=ALU.mult), reads=[PSB[3 + m], w[m][1]], writes=[w[2 + m][1]])
                    op("dve", lambda h: h.scalar_tensor_tensor(out=w[4][0][:], in0=w[3][0][:], scalar=dvc(l, "nlam"), in1=w[2][0][:], op0=ALU.mult, op1=ALU.add),
                       reads=[w[3][1], w[2][1], dv_b], writes=[w[4][1]])
                    s_, sb_ = sqr.next()
                    op("act", lambda h, s_=s_: h.activation(out=s_[:], in_=w[4][0][:], func=AF.Square), reads=[w[4][1]], writes=[sb_])
                    op("pe", lambda h, s_=s_: h.matmul(PS[0][:], lhsT=onesb[:], rhs=s_[:], start=True, stop=True), reads=[sb_, cb_b], writes=[PSB[0]])
                    op("dve", lambda h: h.tensor_scalar(out=w[5][0][:], in0=PS[0][:], scalar1=1.0 / 128, scalar2=SUBLN_EPS, op0=ALU.mult, op1=ALU.add), reads=[PSB[0]], writes=[w[5][1]])
                    op("act", lambda h: h.activation(out=w[5][0][:], in_=w[5][0][:], func=AF.Sqrt), reads=[w[5][1]], writes=[w[5][1]])
                    op("dve", lambda h: h.reciprocal(out=w[5][0][:], in_=w[5][0][:]), reads=[w[5][1]], writes=[w[5][1]])
                    o_, ob_ = outr.next()
                    op("dve", lambda h, o_=o_: h.scalar_tensor_tensor(out=o_[:], in0=w[4][0][:], scalar=dvc(l, "sub2"), in1=w[5][0][:], op0=ALU.mult, op1=ALU.mult),
                       reads=[w[4][1], w[5][1], dv_b], writes=[ob_])
                    dma("sp", mixT[1024 + hd * 128:1024 + (hd + 1) * 128, qs], o_[:], reads=[ob_], writes=[db("mixT", 8 + hd)], store=True)

        def rwkv_phase(es, l):
            f32t = lambda nm, sh=[128, 512]: (sb(es, nm, sh), Buf())
            bft = lambda nm, sh=[128, 512]: (sb(es, nm, sh, BF16), Buf())
            scanm, scb = f32t("scanm")
            dma("sp", scanm[:], cn_in[:, CN["scanm"]:CN["scanm"] + 512], writes=[scb])
            hm, hmb = f32t("hm", [128, 7, 256])
            dma("sp", hm[:], cn_in[:, CN["hm"]:CN["hm"] + 7 * 256].rearrange("p (k c) -> p k c", k=7), writes=[hmb])
            SL = []
            for i in range(8):
                SL.append({"T12": bft("T12_%d" % i, [128, 512]), "T3": bft("T3_%d" % i, [128, 128]), "W1": bft("W1_%d" % i, [128, 128]), "W2": bft("W2_%d" % i, [128, 256]),
                           "p0": bft("p0_%d" % i, [128, 256]), "DG": [bft("DG%d_%d" % (j, i), [128, 256]) for j in range(2)], "ZZ": bft("ZZ_%d" % i, [128, 256]),
                           "tW": bft("tW_%d" % i, [128, 256]), "Xb": bft("Xb_%d" % i, [128, 64]), "Ub": bft("Ub_%d" % i, [128, 64]), "HG": f32t("HG_%d" % i, [128, 64])})
            m2b, m2bb = bft("m2b", [128, 256]); mslb, mslbb = bft("mslb", [128, 128])
            op("pool", lambda h: h.tensor_copy(out=m2b[:], in_=cn[:, CN["m2"]:CN["m2"] + 256]), reads=[cn_b], writes=[m2bb])
            op("pool", lambda h: h.tensor_copy(out=mslb[:], in_=cn[:, CN["msl"]:CN["msl"] + 128]), reads=[cn_b], writes=[mslbb])
            wst, wstb = f32t("lst", [128, 1024])
            wa2b, wa2bb = bft("wa2b", [128, 1024]); g2ab, g2abb = bft("g2ab", [128, 1024]); g2bv, g2bvb = bft("g2bv", [64, 1024])
            dma("sp", wst[0:64, :], w2_in[l], writes=[wstb]); dma("sp", wst[64:128, :], a2_in[l], writes=[wstb])
            op("pool", lambda h: h.tensor_copy(out=wa2b[:], in_=wst[:]), reads=[wstb], writes=[wa2bb])
            dma("sp", wst[:], g2_in[l][0:128, :], writes=[wstb])
            op("pool", lambda h: h.tensor_copy(out=g2ab[:], in_=wst[:]), reads=[wstb], writes=[g2abb])
            dma("sp", wst[0:32, :], g2_in[l][128:160, :], writes=[wstb])
            if l > 0:
                dma("sp", wst[32:64, :], v2_in[l - 1], writes=[wstb])
            op("pool", lambda h: h.tensor_copy(out=g2bv[0:64 if l > 0 else 32, :], in_=wst[0:64 if l > 0 else 32, :]), reads=[wstb], writes=[g2bvb])
            Hf, _ = f32t("Hf", [128, 8, 64]); Hb, _ = bft("Hb", [128, 8, 64])
            Hfb = [Buf(), Buf()]; Hbb = [Buf(), Buf()]
            op("pool", lambda h: h.memset(Hf[:], 0.0), writes=Hfb); op("pool", lambda h: h.memset(Hb[:], 0.0), writes=Hbb)
            wa_t, wab = bft("wa_t"); g1_t, g1b = bft("g1_t"); g2_t, g2b_ = bft("g2_t", [64, 512])
            r_t, rb = bft("r_t"); k_t, kb_ = bft("k_t"); v_t, vb = bft("v_t"); vf_t, vfb = bft("vf_t")
            sg, sgb = f32t("sg"); cs, csb = f32t("cs"); ex, exb = f32t("ex"); eG, eGb = f32t("eG"); eGi, eGib = f32t("eGi"); eGx, eGxb = f32t("eGx")
            a_t, ab = f32t("a_t"); gt, gtb = f32t("gt"); v2t, v2b = f32t("v2t"); kk, kkb = f32t("kk"); sqb_, sqbb = bft("sqb"); rn, rnb = f32t("rn")
            k2, k2b = f32t("k2"); tmp, tmpb = f32t("tmp"); AR, ARb = bft("AR", [128, 2, 512]); BhT, BhTb = bft("BhT"); KhT, KhTb = bft("KhT"); vbf, vbfb = bft("vbf")
            rkb, rkbb = bft("rkb"); bon, bonb = f32t("bon"); OT, _ = f32t("OT"); OTb = [Buf(), Buf()]; ob16, ob16b = bft("ob16"); mean, meanb = f32t("mean"); var, varb = f32t("var")
            Bh = [bft("Bh%d" % i, [128, 128]) for i in range(4)]; Kh = [bft("Kh%d" % i, [128, 128]) for i in range(4)]; Vt = [bft("Vt%d" % i, [128, 128]) for i in range(4)]
            T1, T1b = bft("T1", [128, 256]); T2, T2b = bft("T2", [128, 256]); T3, T3b = bft("T3", [128, 128])
            W1, W1b = bft("W1", [128, 128]); W2, W2b = bft("W2", [128, 256])
            PP = [bft("PP%d" % i, [128, 256]) for i in range(2)]; NTt = [bft("NT%d" % i, [128, 128]) for i in range(2)]
            Xb, Xbb = bft("Xb", [128, 64]); Ub, Ubb = bft("Ub", [128, 64]); mo, mob = bft("mo")
            tt_ = lambda e, o, a, b, opn, rd, wr: op(e, lambda h: h.tensor_tensor(out=o, in0=a, in1=b, op=opn), reads=rd, writes=wr)
            trc = [0]
            G2R = 64 if l > 0 else 32
            for tb in range(NTB):
                ts = slice(tb * 512, (tb + 1) * 512)
                dma("sp", wa_t[:], waT[:, ts], reads=[db("waT")], writes=[wab]); dma("sp", g1_t[:], g1T[:, ts], reads=[db("g1T")], writes=[g1b])
                dma("sp", g2_t[0:G2R, :], g2T[0:G2R, ts], reads=[db("g2T")], writes=[g2b_])
                for c in range(8):
                    cs_ = slice(c * 128, (c + 1) * 128)
                    dma("sp", r_t[:], rT[cs_, ts], reads=[db("rT", c)], writes=[rb]); dma("sp", k_t[:], kT[cs_, ts], reads=[db("kT", c)], writes=[kb_])
                    dma("sp", v_t[:], vT[cs_, ts], reads=[db("vT", c)], writes=[vb])
                    op("pe", lambda h: h.matmul(PS[0][:], lhsT=wa2b[0:64, cs_], rhs=wa_t[0:64, :], start=True, stop=True), reads=[wa2bb, wab], writes=[PSB[0]])
                    op("act", lambda h: h.activation(out=sg[:], in_=PS[0][:], func=AF.Sigmoid, bias=cvc(l, "w0", c)), reads=[PSB[0], cv_b], writes=[sgb])
                    op("dve", lambda h: h.tensor_tensor_scan(out=cs[:], data0=scanm[:], data1=sg[:], initial=0.0, op0=ALU.mult, op1=ALU.add), reads=[scb, sgb], writes=[csb])
                    tt_("dve", ex[:], cs[:], sg[:], ALU.subtract, [csb, sgb], [exb])
                    op("act", lambda h: h.activation(out=eG[:], in_=cs[:], func=AF.Exp, scale=-C0), reads=[csb], writes=[eGb])
                    op("act", lambda h: h.activation(out=eGi[:], in_=cs[:], func=AF.Exp, scale=C0), reads=[csb], writes=[eGib])
                    op("act", lambda h: h.activation(out=eGx[:], in_=ex[:], func=AF.Exp, scale=-C0), reads=[exb], writes=[eGxb])
                    op("pe", lambda h: h.matmul(PS[0][:], lhsT=wa2b[64:128, cs_], rhs=wa_t[64:128, :], start=True, stop=True), reads=[wa2bb, wab], writes=[PSB[0]])
                    op("act", lambda h: h.activation(out=a_t[:], in_=PS[0][:], func=AF.Sigmoid, bias=cvc(l, "a0", c)), reads=[PSB[0], cv_b], writes=[ab])
                    op("pe", lambda h: h.matmul(PS[0][:], lhsT=g2ab[:, cs_], rhs=g1_t[:], start=True, stop=False), reads=[g2abb, g1b], writes=[PSB[0]], inc=False)
                    op("pe", lambda h: h.matmul(PS[0][:], lhsT=g2bv[0:32, cs_], rhs=g2_t[0:32, :], start=False, stop=True), reads=[g2bvb, g2b_], writes=[PSB[0]])
                    op("act", lambda h: h.copy(out=gt[:], in_=PS[0][:]), reads=[PSB[0]], writes=[gtb])
                    if l > 0:
                        dma("sp", vf_t[:], vfT[cs_, ts], reads=[db("vfT", c)], writes=[vfb])
                        op("pe", lambda h: h.matmul(PS[0][:], lhsT=g2bv[32:64, cs_], rhs=g2_t[32:64, :], start=True, stop=True), reads=[g2bvb, g2b_], writes=[PSB[0]])
                        op("act", lambda h: h.activation(out=tmp[:], in_=PS[0][:], func=AF.Sigmoid, bias=cvc(l, "v0", c)), reads=[PSB[0], cv_b], writes=[tmpb])
                        tt_("dve", v2t[:], vf_t[:], v_t[:], ALU.subtract, [vfb, vb], [v2b])
                        tt_("dve", v2t[:], v2t[:], tmp[:], ALU.mult, [v2b, tmpb], [v2b])
                        tt_("dve", v2t[:], v2t[:], v_t[:], ALU.add, [v2b, vb], [v2b])
                    else:
                        op("dve", lambda h: h.tensor_copy(out=v2t[:], in_=v_t[:]), reads=[vb], writes=[v2b])
                    op("act", lambda h: h.copy(out=vbf[:], in_=v2t[:]), reads=[v2b], writes=[vbfb])
                    op("dve", lambda h: h.tensor_scalar(out=kk[:], in0=k_t[:], scalar1=cvc(l, "k_k", c), scalar2=None, op0=ALU.mult), reads=[kb_, cv_b], writes=[kkb])
                    op("act", lambda h: h.activation(out=sqb_[:], in_=kk[:], func=AF.Square), reads=[kkb], writes=[sqbb])
                    op("pe", lambda h: h.matmul(PS[0][:], lhsT=bonesb[:], rhs=sqb_[:], start=True, stop=True), reads=[cb_b, sqbb], writes=[PSB[0]])
                    op("act", lambda h: h.activation(out=rn[:], in_=PS[0][:], func=AF.Sqrt, bias=1e-20), reads=[PSB[0]], writes=[rnb])
                    op("dve", lambda h: h.reciprocal(out=rn[:], in_=rn[:]), reads=[rnb], writes=[rnb])
                    tt_("dve", kk[:], kk[:], rn[:], ALU.mult, [kkb, rnb], [kkb])
                    op("dve", lambda h: h.tensor_scalar(out=tmp[:], in0=a_t[:], scalar1=cvc(l, "k_a", c), scalar2=dvc(l, "omka", c), op0=ALU.mult, op1=ALU.add),
                       reads=[ab, cv_b, dv_b], writes=[tmpb])
                    tt_("dve", k2[:], k_t[:], tmp[:], ALU.mult, [kb_, tmpb], [k2b])
                    op("dve", lambda h: h.scalar_tensor_tensor(out=AR[:, 0, :], in0=kk[:], scalar=-1.0, in1=eGx[:], op0=ALU.mult, op1=ALU.mult), reads=[kkb, eGxb], writes=[ARb])
                    tt_("dve", AR[:, 1, :], r_t[:], eG[:], ALU.mult, [rb, eGb], [ARb])
                    tt_("dve", tmp[:], kk[:], a_t[:], ALU.mult, [kkb, ab], [tmpb])
                    tt_("dve", BhT[:], tmp[:], eGi[:], ALU.mult, [tmpb, eGib], [BhTb])
                    tt_("pool", KhT[:], k2[:], eGi[:], ALU.mult, [k2b, eGib], [KhTb])
                    op("dve", lambda h: h.scalar_tensor_tensor(out=rkb[:], in0=r_t[:], scalar=cvc(l, "r_k", c), in1=k2[:], op0=ALU.mult, op1=ALU.mult), reads=[rb, k2b, cv_b], writes=[rkbb])
                    op("pe", lambda h: h.matmul(PS[0][:], lhsT=bonesb[:], rhs=rkb[:], start=True, stop=True), reads=[cb_b, rkbb], writes=[PSB[0]])
                    tt_("dve", bon[:], PS[0][:], v2t[:], ALU.mult, [PSB[0], v2b], [bonb])
                    for n in range(4):
                        tsl = slice(n * 128, (n + 1) * 128)
                        for src, srcb, dst in ((BhT, BhTb, Bh[n]), (KhT, KhTb, Kh[n]), (vbf, vbfb, Vt[n])):
                            ri = trc[0] % 4
                            trc[0] += 1
                            op("pe", lambda h, src=src, ri=ri: h.transpose(out=PSH[:, ri * 128:(ri + 1) * 128], in_=src[:, tsl], identity=identb[:]), reads=[srcb, cb_b], writes=[PSH_b[ri]])
                            op("act" if ri % 2 else "dve", lambda h, dst=dst, ri=ri: (h.copy if ri % 2 else h.tensor_copy)(out=dst[0][:], in_=PSH[:, ri * 128:(ri + 1) * 128]),
                               reads=[PSH_b[ri]], writes=[dst[1]])
                    def mmq(out, lhsT, rhs, rd, wr, st=True, sp=True, inc=True):
                        op("pe", lambda h: h.matmul(out, lhsT=lhsT, rhs=rhs, start=st, stop=sp), reads=rd, writes=wr, inc=inc)

                    def stage1(n, hh, lane, sl):
                        tsl = slice(n * 128, (n + 1) * 128)
                        P_ = slice(64 * hh, 64 * hh + 64)
                        A, Ab = PS[1 + lane], PSB[1 + lane]
                        e1 = "act" if lane % 2 == 0 else "dve"
                        cp = lambda eng, o, i, rd, wr: op(eng, lambda h: (h.copy if eng == "act" else h.tensor_copy)(out=o, in_=i), reads=rd, writes=wr)
                        T12, T12b = sl["T12"]; T3, T3b = sl["T3"]; W1, W1b = sl["W1"]; W2, W2b = sl["W2"]; p0, p0b = sl["p0"]
                        DGs = sl["DG"]; ZZ, ZZb = sl["ZZ"]; tW, tWb = sl["tW"]
                        mmq(A[:, 0:256], BhT[P_, tsl], AR[P_, :, tsl], [BhTb, ARb], [Ab], inc=False)
                        mmq(A[:, 256:512], KhT[P_, tsl], AR[P_, :, tsl], [KhTb, ARb], [Ab])
                        yield
                        cp(e1, T12[:], A[:, 0:512], [Ab], [T12b, Ab])
                        yield
                        mmq(A[:, 0:128], AR[P_, 0, tsl], BhT[P_, tsl], [ARb, BhTb], [Ab])
                        d0, d0b = DGs[0]
                        tt_("dve", p0[:, 128:256], T12[:, 0:128], m2b[:, 0:128], ALU.mult, [T12b, m2bb], [p0b])
                        tt_("dve", W1[:], T12[:, 128:256], m2b[:, 128:256], ALU.mult, [T12b, m2bb], [W1b])
                        tt_("dve", W2[:], T12[:, 256:512], m2b[:], ALU.mult, [T12b, m2bb], [W2b])
                        tt_("dve", d0[:, 128:256], T12[:, 0:128], hm[:, 0, 128:256], ALU.mult, [T12b, hmb], [d0b])
                        yield
                        cp(e1, T3[:], A[:, 0:128], [Ab], [T3b, Ab])
                        tt_("dve", d0[:, 128:256], d0[:, 128:256], identb[:], ALU.add, [d0b, cb_b], [d0b])
                        yield
                        tt_("dve", p0[:, 0:128], T3[:], mslb[:], ALU.mult, [T3b, mslbb], [p0b])
                        tt_("dve", d0[:, 0:128], T3[:], hm[:, 0, 0:128], ALU.mult, [T3b, hmb], [d0b])
                        tt_("dve", d0[:, 0:128], d0[:, 0:128], identb[:], ALU.add, [d0b, cb_b], [d0b])
                        yield
                        for k in range(1, 7):
                            dc, dcb = DGs[(k - 1) % 2]
                            dn, dnb = DGs[k % 2]
                            mmq(A[:, 0:128], p0[:, 128:256], dc[:, 0:128], [p0b, dcb], [Ab], inc=False)
                            mmq(A[:, 128:256], p0[:, 0:128], dc[:, 128:256], [p0b, dcb], [Ab])
                            yield
                            cp("act" if (k + lane) % 2 else "dve", ZZ[:], A[:, 0:256], [Ab], [ZZb, Ab])
                            yield
                            mmq(A[:, 256:384], dc[:, 128:256], ZZ[:, 0:128], [dcb, ZZb], [Ab], inc=False)
                            mmq(A[:, 384:512], dc[:, 0:128], ZZ[:, 128:256], [dcb, ZZb], [Ab])
                            yield
                            tt_("dve", tW[:], A[:, 256:512], hm[:, k, :], ALU.mult, [Ab, hmb], [tWb, Ab])
                            yield
                            tt_("pool" if (k % 3 == 0 and lane % 2) else "dve", dn[:], dc[:], tW[:], ALU.add, [dcb, tWb], [dnb])
                            yield

                    def stage2(n, hh, lane, sl):
                        tsl = slice(n * 128, (n + 1) * 128)
                        P_ = slice(64 * hh, 64 * hh + 64)
                        B, Bb = PS[1 + lane], PSB[1 + lane]
                        W1, W1b = sl["W1"]; W2, W2b = sl["W2"]; Xb, Xbb = sl["Xb"]; Ub, Ubb = sl["Ub"]; HG, HGb = sl["HG"]
                        ntf, ntfb = sl["DG"][0][0][:, 128:256], sl["DG"][0][1]
                        vt, vtb = Vt[n]
                        mmq(B[:, 0:64], AR[P_, 0, tsl], Hb[P_, c, :], [ARb, Hbb[hh]], [Bb], True, False, False)
                        mmq(B[:, 0:64], W2[:, 0:128], vt[:, P_], [W2b, vtb], [Bb], False, True)
                        yield
                        op("act", lambda h: h.copy(out=Xb[:], in_=B[:, 0:64]), reads=[Bb], writes=[Xbb, Bb])
                        yield
                        mmq(B[:, 64:128], ntf, Xb[:], [ntfb, Xbb], [Bb])
                        yield
                        op("dve", lambda h: h.tensor_copy(out=Ub[:], in_=B[:, 64:128]), reads=[Bb], writes=[Ubb, Bb])
                        gcol = eG[P_, n * 128 + 127:n * 128 + 128]
                        op("dve", lambda h: h.tensor_scalar(out=HG[P_, :], in0=Hf[P_, c, :], scalar1=gcol, scalar2=None, op0=ALU.mult), reads=[Hfb[hh], eGb], writes=[HGb])
                        yield
                        mmq(B[P_, 128:256], Hb[P_, c, :], AR[P_, 1, tsl], [Hbb[hh], ARb], [Bb], True, False, False)
                        mmq(B[P_, 128:256], Ub[:], W1[:], [Ubb, W1b], [Bb], False, False, False)
                        mmq(B[P_, 128:256], vt[:, P_], W2[:, 128:256], [vtb, W2b], [Bb], False, True, False)
                        mmq(B[P_, 256:320], Bh[n][0][:, P_], Ub[:], [Bh[n][1], Ubb], [Bb], True, False, False)
                        mmq(B[P_, 256:320], Kh[n][0][:, P_], vt[:, P_], [Kh[n][1], vtb], [Bb], False, True)
                        yield
                        op("dve", lambda h: h.scalar_tensor_tensor(out=Hf[P_, c, :], in0=B[P_, 256:320], scalar=gcol, in1=HG[P_, :], op0=ALU.mult, op1=ALU.add),
                           reads=[Bb, eGb, HGb], writes=[Hfb[hh], Bb])
                        op("act", lambda h: h.copy(out=OT[P_, tsl], in_=B[P_, 128:256]), reads=[Bb], writes=[OTb[hh], Bb])
                        yield
                        op("act", lambda h: h.copy(out=Hb[P_, c, :], in_=Hf[P_, c, :]), reads=[Hfb[hh]], writes=[Hbb[hh]])
                        yield

                    units = [(n, hh) for n in range(4) for hh in range(2)]
                    def chain(*gs):
                        for g_ in gs:
                            yield from g_
                    lockstep([stage1(n, hh, lane, SL[lane]) for lane, (n, hh) in enumerate(units[0:4])])
                    lockstep([stage1(n, hh, lane, SL[4 + lane]) for lane, (n, hh) in enumerate(units[4:8])]
                             + [chain(stage2(0, hh, 4 + hh, SL[hh]), stage2(1, hh, 4 + hh, SL[2 + hh])) for hh in range(2)])
                    for n in (2, 3):
                        lockstep([stage2(n, hh, 4 + hh, SL[2 * n + hh]) for hh in range(2)])
                    op("act", lambda h: h.copy(out=ob16[:], in_=OT[:]), reads=OTb, writes=[ob16b])
                    op("pe", lambda h: h.matmul(PS[0][:], lhsT=bonesb[:], rhs=ob16[:], start=True, stop=True), reads=[cb_b, ob16b], writes=[PSB[0]])
                    op("dve", lambda h: h.tensor_scalar(out=mean[:], in0=PS[0][:], scalar1=1.0 / 64, scalar2=None, op0=ALU.mult), reads=[PSB[0]], writes=[meanb])
                    tt_("dve", OT[:], OT[:], mean[:], ALU.subtract, OTb + [meanb], OTb)
                    op("act", lambda h: h.activation(out=sqb_[:], in_=OT[:], func=AF.Square), reads=OTb, writes=[sqbb])
                    op("pe", lambda h: h.matmul(PS[0][:], lhsT=bonesb[:], rhs=sqb_[:], start=True, stop=True), reads=[cb_b, sqbb], writes=[PSB[0]])
                    op("act", lambda h: h.activation(out=var[:], in_=PS[0][:], func=AF.Sqrt, scale=1.0 / 64, bias=GN_EPS), reads=[PSB[0]], writes=[varb])
                    op("dve", lambda h: h.reciprocal(out=var[:], in_=var[:]), reads=[varb], writes=[varb])
                    tt_("dve", OT[:], OT[:], var[:], ALU.mult, OTb + [varb], OTb)
                    op("act", lambda h: h.activation(out=OT[:], in_=OT[:], func=AF.Identity, scale=cvc(l, "gn_w", c), bias=cvc(l, "gn_b", c)), reads=OTb + [cv_b], writes=OTb)
                    tt_("dve", OT[:], OT[:], bon[:], ALU.add, OTb + [bonb], OTb)
                    tt_("dve", mo[:], OT[:], gt[:], ALU.mult, OTb + [gtb], [mob])
                    dma("sp", mixT[cs_, ts], mo[:], reads=[mob], writes=[db("mixT", c)], store=True)

        def lockstep(gens):
            gens = list(gens)
            while gens:
                nxt = []
                for g in gens:
                    try:
                        next(g)
                        nxt.append(g)
                    except StopIteration:
                        pass
                gens = nxt

        for l in range(NL if 'nolayers' not in parts else 0):
            kb.new_epoch()
            if 'mix' in parts:
                with scope() as es:
                    hT = sb(es, "hT", [128, KC, S], BF16)
                    hTb = [Buf() for _ in range(NTB)]
                    with scope() as es2:
                        if "nonorm1" not in parts:
                            norm_to_hT(es2, l, "g_pre", hT, hTb)
                    with scope() as es2:
                        W = w_in[l]
                        R = mk_rings(es2)
                        tasks = []
                        for c in range(8):
                            tasks.append(([(W[:, c * 128:(c + 1) * 128], 128)], 128,
                                          shift_epi(R, "er%d" % (c % 2), 128, cvc(l, "mu_r", c), dvc(l, "om_r", c), [(rT[c * 128:(c + 1) * 128], db("rT", c))])))
                        for c in range(8):
                            tasks.append(([(W[:, 1088 + c * 128:1088 + (c + 1) * 128], 128)], 128,
                                          shift_epi(R, "ek%d" % (c % 2), 128, cvc(l, "mu_k", c), dvc(l, "om_k", c), [(kT[c * 128:(c + 1) * 128], db("kT", c))])))
                        for c in range(8):
                            dsts = [(vT[c * 128:(c + 1) * 128], db("vT", c))]
                            if l == 0:
                                dsts.append((vfT[c * 128:(c + 1) * 128], db("vfT", c)))
                            tasks.append(([(W[:, 2112 + c * 128:2112 + (c + 1) * 128], 128)], 128,
                                          shift_epi(R, "ev%d" % (c % 2), 128, cvc(l, "mu_v", c), dvc(l, "om_v", c), dsts)))
                        tasks.append(([(W[:, 1024:1088], 64), (W[:, 3136:3200], 64)], 128,
                                      shift_epi(R, "ewa", 128, cvc(l, "mu_wa"), dvc(l, "om_wa"), [(waT, db("waT"))], act=[(0, 64, AF.Tanh), (64, 128, None)])))
                        tasks.append(([(W[:, 3200:3328], 128)], 128,
                                      shift_epi(R, "eg1", 128, cvc(l, "mu_g1"), dvc(l, "om_g1"), [(g1T, db("g1T"))], act=[(0, 128, AF.Sigmoid)])))
                        if l == 0:
                            tasks.append(([(W[:, 3328:3360], 32)], 32,
                                          shift_epi(R, "eg2", 32, cvc(l, "mu_g2", 0, 1, 0, 32), dvc(l, "om_g2", 0, 1, 0, 32), [(g2T[0:32], db("g2T"))], act=[(0, 32, AF.Sigmoid)])))
                        else:
                            tasks.append(([(W[:, 3328:3360], 32), (w_mv[l - 1], 32)], 64,
                                          shift_epi(R, "eg2", 64, cvc(l, "mu_g2", 0, 1, 0, 64), dvc(l, "om_g2", 0, 1, 0, 64), [(g2T, db("g2T"))],
                                                    act=[(0, 32, AF.Sigmoid), (32, 64, None)])))
                        for c in range(8):
                            tasks.append(([(W[:, 3360 + c * 128:3360 + (c + 1) * 128], 128)], 128, plain_epi(R, "eq%d" % (c % 2), qT[c * 128:(c + 1) * 128], db("qT", c), 0)))
                        for c in range(8):
                            tasks.append(([(W[:, 4384 + c * 128:4384 + (c + 1) * 128], 128)], 128, plain_epi(R, "ekd%d" % (c % 2), kdT[c * 128:(c + 1) * 128], db("kdT", c), 1)))
                        if 'plainonly' in parts:
                            pe_ = plain_epi(R, 'x', qT[0:128], db('qT', 0), 0)
                            tasks = [(sg_, M_, pe_) for (sg_, M_, e_) in tasks if M_ == 128]
                        if 'ntasks8' in parts:
                            tasks = tasks[:8]
                        proj_fm(es2, hT, hTb, tasks, nper=2, each=True)
                    with scope() as es2:
                        st = Ring(es2, nc, "vst", 2, [128, KC, 128], F32)
                        wbr = Ring(es2, nc, "vwb", 2, [128, KC, 128], BF16)
                        orr = Ring(es2, nc, "vo", 2, [128, 128], BF16)
                        for c in range(8 if 'novd' not in parts else 0):
                            s_, sb_ = st.next()
                            dma("sp", s_[:], w_in[l][:, 5408 + c * 128:5408 + (c + 1) * 128].rearrange("(kc p) n -> p kc n", p=128), writes=[sb_])
                            w_, wb_ = wbr.next()
                            op("dve", lambda h, s_=s_, w_=w_: h.tensor_copy(out=w_[:], in_=s_[:]), reads=[sb_], writes=[wb_])
                            for tt in range(NT):
                                pi = tt % 4
                                for kc in range(KC):
                                    op("pe", lambda h, w_=w_, kc=kc, pi=pi, tt=tt: h.matmul(PS[pi][:, 0:128], lhsT=hT[:, kc, tt * 128:(tt + 1) * 128], rhs=w_[:, kc, :],
                                                                                           start=(kc == 0), stop=(kc == KC - 1)),
                                       reads=[wb_, hTb[tt // 4]], writes=[PSB[pi]], inc=(kc == KC - 1))
                                o_, ob_ = orr.next()
                                op("act" if tt % 2 else "dve", lambda h, o_=o_, pi=pi, tt=tt: (h.copy if tt % 2 else h.tensor_copy)(out=o_[:], in_=PS[pi][:, 0:128]),
                                   reads=[PSB[pi]], writes=[ob_])
                                dma("sp", vd[tt * 128:(tt + 1) * 128, c * 128:(c + 1) * 128], o_[:], reads=[ob_], writes=[db("vd", c)], store=True)

                with scope() as es:
                    if 'norwkv' not in parts:
                        rwkv_phase(es, l)
                with scope() as es:
                    if 'noattn' not in parts:
                        attn_phase(es, l)
                with scope() as es:
                    if 'noO' not in parts:
                        proj_res(es, l, mixT, "mixT", KC, w_out[l], "g_pm")
            with scope() as es:
                hT = sb(es, "hT2", [128, KC, S], BF16)
                hTb = [Buf() for _ in range(NTB)]
                with scope() as es2:
                    norm_to_hT(es2, l, "g_pf", hT, hTb)
                with scope() as es2:
                    tr = Ring(es2, nc, "ft", 2, [128, 512], F32)
                    gr = Ring(es2, nc, "fg", 2, [128, 512], F32)
                    orr = Ring(es2, nc, "fo", 2, [128, 512], BF16)
                    carr = [(sb(es2, "fc%d" % i, [128, 2]), Buf()) for i in range(2)]

                    def ffn_epi(c):
                        car, carb = carr[c % 2]

                        def epi(tb, pss):
                            pg, pu = pss
                            ts = slice(tb * 512, (tb + 1) * 512)
                            t_, tb_ = tr.next()
                            g_, gb_ = gr.next()
                            o_, ob_ = orr.next()
                            op("act", lambda h: h.activation(out=t_[:], in_=PS[pg][:], func=AF.Identity, scale=cvc(l, "cw2", c), bias=cvc(l, "cb", c)),
                               reads=[PSB[pg], cv_b], writes=[tb_])
                            op("dve", lambda h: h.scalar_tensor_tensor(out=t_[:, 1:512], in0=PS[pg][:, 0:511], scalar=cvc(l, "cw1", c), in1=t_[:, 1:512], op0=ALU.mult, op1=ALU.add),
                               reads=[PSB[pg], tb_, cv_b], writes=[tb_])
                            op("dve", lambda h: h.scalar_tensor_tensor(out=t_[:, 2:512], in0=PS[pg][:, 0:510], scalar=cvc(l, "cw0", c), in1=t_[:, 2:512], op0=ALU.mult, op1=ALU.add),
                               reads=[PSB[pg], tb_, cv_b], writes=[tb_])
                            if tb > 0:
                                op("dve", lambda h: h.scalar_tensor_tensor(out=t_[:, 0:2], in0=car[:, 0:2], scalar=cvc(l, "cw0", c), in1=t_[:, 0:2], op0=ALU.mult, op1=ALU.add),
                                   reads=[carb, tb_, cv_b], writes=[tb_])
                                op("dve", lambda h: h.scalar_tensor_tensor(out=t_[:, 0:1], in0=car[:, 1:2], scalar=cvc(l, "cw1", c), in1=t_[:, 0:1], op0=ALU.mult, op1=ALU.add),
                                   reads=[carb, tb_, cv_b], writes=[tb_])
                            op("act", lambda h: h.copy(out=car[:, 0:2], in_=PS[pg][:, 510:512]), reads=[PSB[pg]], writes=[carb])
                            op("act", lambda h: h.activation(out=g_[:], in_=t_[:], func=AF.Gelu_apprx_tanh), reads=[tb_], writes=[gb_])
                            op("dve", lambda h: h.tensor_tensor(out=o_[:], in0=PS[pu][:], in1=g_[:], op=ALU.mult), reads=[PSB[pu], gb_], writes=[ob_])
                            dma("sp", actT[c * 128:(c + 1) * 128, ts], o_[:], reads=[ob_], writes=[db("actT", c)], store=True)
                        return epi
                    tasks = []
                    for c in range(FC):
                        e = ffn_epi(c)
                        tasks.append(([(w_up[l][:, c * 128:(c + 1) * 128], 128)], 128, e))
                        tasks.append(([(w_up[l][:, FF + c * 128:FF + (c + 1) * 128], 128)], 128, e))
                    if 'noproj' not in parts:
                        proj_fm(es2, hT, hTb, tasks, nper=2)
            with scope() as es:
                if 'nores' not in parts:
                    proj_res(es, l, actT, "actT", FC, w_down[l], "g_ff")

        with scope() as es:
            xr = Ring(es, nc, "oxi", 2, [128, KC, 128], F32)
            xo = Ring(es, nc, "oxo", 2, [128, D], F32)
            for tt in range(NT):
                xi, xib = xr.next()
                dma("sp", xi[:], xT[:, tt * 128:(tt + 1) * 128].rearrange("(kc p) t -> p kc t", p=128), reads=[db("xT", kc) for kc in range(KC)], writes=[xib])
                o_, ob_ = xo.next()
                for kc in range(KC):
                    pi = kc % 4
                    op("pe", lambda h, kc=kc, pi=pi: h.transpose(out=PS[pi][:, 0:128], in_=xi[:, kc, :], identity=cn[:, 0:128]), reads=[xib, cn_b], writes=[PSB[pi]])
                    op("act" if kc % 2 else "dve", lambda h, kc=kc, pi=pi: (h.copy if kc % 2 else h.tensor_copy)(out=o_[:, kc * 128:(kc + 1) * 128], in_=PS[pi][:, 0:128]),
                       reads=[PSB[pi]], writes=[ob_])
                dma("sp", y_out[tt * 128:(tt + 1) * 128, :], o_[:], reads=[ob_], writes=[db("yout")], store=True)
        kb.finish()
    return nc


def pack_inputs(inp, S, NL):
    f = np.float32
    cv = np.zeros((128, NL, NCV), f)

    def put(l, name, vec):
        vec = np.asarray(vec, f).reshape(-1)
        n = (vec.size + 127) // 128
        pad = np.zeros(n * 128, f)
        pad[:vec.size] = vec
        cv[:, l, CV[name]:CV[name] + n] = pad.reshape(n, 128).T
    for l in range(NL):
        put(l, "g_pre", inp["pre_mix_norm"][l]); put(l, "g_pm", inp["post_mix_norm"][l])
        put(l, "g_pf", inp["pre_ffn_norm"][l]); put(l, "g_ff", inp["post_ffn_norm"][l])
        mu = np.asarray(inp["shift_mu"][l], f)
        put(l, "mu_r", mu[0:1024]); put(l, "mu_k", mu[1088:2112]); put(l, "mu_v", mu[2112:3136])
        put(l, "mu_wa", np.concatenate([mu[1024:1088], mu[3136:3200]]))
        put(l, "mu_g1", mu[3200:3328])
        g2 = np.zeros(128, f)
        g2[0:32] = mu[3328:3360]
        if l > 0:
            g2[32:64] = np.asarray(inp["shift_mu_mv"][l - 1], f)
        put(l, "mu_g2", g2)
        for nm in ("w0", "a0", "k_k", "k_a", "r_k", "gn_w", "gn_b"):
            put(l, nm, inp[nm][l])
        if l > 0:
            put(l, "v0", inp["v0"][l - 1])
        put(l, "subln", inp["subln_w"][l])
        cw = np.asarray(inp["conv_w"][l], f)
        put(l, "cw0", cw[0]); put(l, "cw1", cw[1]); put(l, "cw2", cw[2]); put(l, "cb", inp["conv_b"][l])
        for nm, k in (("lq1", "lam_q1"), ("lk1", "lam_k1"), ("lq2", "lam_q2"), ("lk2", "lam_k2")):
            cv[:, l, CV[nm]:CV[nm] + 64] = np.asarray(inp[k][l], f)[None, :]
    shared = {
        "cv": np.ascontiguousarray(cv.reshape(128, NL * NCV)),
        "cn": make_consts(),
        "w_in": np.ascontiguousarray(inp["w_in"][:NL], dtype=f),
        "w_mv": np.ascontiguousarray(inp["w_mv_down"][:max(NL - 1, 1)], dtype=f),
        "w2": np.ascontiguousarray(inp["w2"][:NL], dtype=f), "a2": np.ascontiguousarray(inp["a2"][:NL], dtype=f),
        "g2": np.ascontiguousarray(inp["g2"][:NL], dtype=f), "v2": np.ascontiguousarray(inp["v2"][:max(NL - 1, 1)], dtype=f),
        "w_out": np.ascontiguousarray(inp["w_out"][:NL], dtype=f), "w_up": np.ascontiguousarray(inp["w_up"][:NL], dtype=f),
        "w_down": np.ascontiguousarray(inp["w_down"][:NL], dtype=f),
    }
    return shared


def kernel(**inp):
    x = np.asarray(inp["x"], np.float32)
    B, S, _ = x.shape
    NL = 4
    nc = build(S, NL)
    shared = pack_inputs(inp, S, NL)
    pos = np.asarray(inp["positions"], np.int32)
    in_maps = []
    for b in range(B):
        m = dict(shared)
        m["x"] = np.ascontiguousarray(x[b])
        m["pos"] = np.ascontiguousarray(pos[b:b + 1])
        in_maps.append(m)
    res = run_bass_kernel_spmd(nc, in_maps, core_ids=list(range(B)))
    return np.stack([np.asarray(r["y"], np.float32) for r in res.results], 0)
```
